# Optimizing a Trainium2 kernel written in Bass

```python
import jax, jax.numpy as jnp
from jax import lax
import numpy as np

D_MODEL = 1024
BATCH = 4
SEQ = 4096
DEPTH = 2

N_MIXERS = 2

A_HEADS = 8
A_HEAD_DIM = 128
A_WIDTH = A_HEADS * A_HEAD_DIM
A_KV_LATENT = 256
IDX_HEADS = 8
IDX_DIM = 64
TOPK_MAX = 256
Q_BLOCK = 128
A_IN_COLS = A_WIDTH + A_KV_LATENT + IDX_HEADS * IDX_DIM + IDX_DIM + IDX_HEADS + A_WIDTH

B_HEADS = 4
B_WIDTH = 2 * D_MODEL
B_V_DIM = B_WIDTH // B_HEADS
B_QK_DIM = B_V_DIM // 2
B_QK_COLS = 2 * B_HEADS * B_QK_DIM
CONV_WIDTH = 4
CHUNK = 64
B_IN_COLS = B_QK_COLS + B_WIDTH + 2 * B_HEADS + B_WIDTH + B_WIDTH

N_A_LAYERS = (DEPTH + 1) // 2
N_B_LAYERS = DEPTH // 2
DEEPNORM_ALPHA = (2 * DEPTH) ** 0.25
DEEPNORM_BETA = (8 * DEPTH) ** -0.25
NORM_EPS = 1e-5

kernel_name = "dsa_mlstm_interleaved_deepnorm"


def _split_points(sizes):
    return [int(s) for s in np.cumsum(sizes)]


def _layernorm(x, g, b):
    xf = x.astype(jnp.float32)
    mu = jnp.mean(xf, axis=-1, keepdims=True)
    var = jnp.mean(jnp.square(xf - mu), axis=-1, keepdims=True)
    return ((xf - mu) * lax.rsqrt(var + NORM_EPS) * g + b).astype(x.dtype)


def _rmsnorm(x, g):
    xf = x.astype(jnp.float32)
    return xf * lax.rsqrt(jnp.mean(xf * xf, axis=-1, keepdims=True) + NORM_EPS) * g


def _dsa_mixer(x, w_in, kv_norm_g, w_uk, w_uv, w_out):
    f32 = jnp.float32
    B, T, _ = x.shape
    topk = min(TOPK_MAX, T // 4)
    nb = T // Q_BLOCK
    proj = x @ w_in
    q, c_kv, q_idx, k_idx, w_idx, z = jnp.split(
        proj, _split_points([A_WIDTH, A_KV_LATENT, IDX_HEADS * IDX_DIM, IDX_DIM, IDX_HEADS]), axis=-1)
    q = q.reshape(B, T, A_HEADS, A_HEAD_DIM)
    c_kv = _rmsnorm(c_kv, kv_norm_g)
    q_lat = jnp.einsum('bthd,hdc->bthc', q, w_uk).astype(f32) * (A_HEAD_DIM ** -0.5)
    q_idx = q_idx.reshape(B, T, IDX_HEADS, IDX_DIM).astype(f32)
    k_idx = k_idx.astype(f32)
    w_idx = w_idx.astype(f32)
    slopes = 2.0 ** (-8.0 * jnp.arange(1, A_HEADS + 1, dtype=f32) / A_HEADS)
    s_pos = jnp.arange(T)

    def to_blocks(a):
        return a.reshape(B, nb, Q_BLOCK, *a.shape[2:]).swapaxes(0, 1)

    def block(args):
        blk, qb, qib, wb = args
        t_pos = blk * Q_BLOCK + jnp.arange(Q_BLOCK)
        rel = jax.nn.relu(jnp.einsum('bqhd,bsd->bqhs', qib, k_idx))
        idx_score = jnp.einsum('bqh,bqhs->bqs', wb, rel)
        causal = s_pos[None, :] <= t_pos[:, None]
        idx_score = jnp.where(causal[None], idx_score, -jnp.inf)
        _, sel = lax.top_k(idx_score, topk)
        c_sel = jax.vmap(lambda c, i: c[i])(c_kv, sel)
        logits = jnp.einsum('bqhc,bqkc->bqhk', qb, c_sel)
        dist = t_pos[None, :, None] - sel
        logits = logits - slopes[None, None, :, None] * dist[:, :, None, :].astype(f32)
        logits = jnp.where((dist >= 0)[:, :, None, :], logits, -jnp.inf)
        p = jax.nn.softmax(logits, axis=-1)
        return jnp.einsum('bqhk,bqkc->bqhc', p, c_sel)

    o_lat = lax.map(block, (jnp.arange(nb), to_blocks(q_lat), to_blocks(q_idx), to_blocks(w_idx)))
    o_lat = o_lat.swapaxes(0, 1).reshape(B, T, A_HEADS, A_KV_LATENT)
    o = jnp.einsum('bthc,hcd->bthd', o_lat, w_uv.astype(f32)).reshape(B, T, A_WIDTH).astype(x.dtype)
    return (o * jax.nn.silu(z)) @ w_out


def _causal_conv(u, w, b):
    T = u.shape[1]
    up = jnp.pad(u, ((0, 0), (CONV_WIDTH - 1, 0), (0, 0)))
    return sum(up[:, j:j + T] * w[j] for j in range(CONV_WIDTH)) + b


def _mlstm_mixer(x, w_in, i_bias, f_bias, conv_w, conv_b, head_norm_g, w_out):
    f32 = jnp.float32
    B, T, _ = x.shape
    nc = T // CHUNK
    proj = x @ w_in
    qk, v, ig, fg, o, z = jnp.split(
        proj, _split_points([B_QK_COLS, B_WIDTH, B_HEADS, B_HEADS, B_WIDTH]), axis=-1)
    qk = jax.nn.silu(_causal_conv(qk, conv_w, conv_b))
    q, k = jnp.split(qk, 2, axis=-1)

    def heads(a, d):
        return a.reshape(B, nc, CHUNK, B_HEADS, d).transpose(1, 0, 3, 2, 4).astype(f32)

    def gates(a):
        return a.astype(f32).reshape(B, nc, CHUNK, B_HEADS).transpose(1, 0, 3, 2)

    q = heads(q, B_QK_DIM) * (B_QK_DIM ** -0.5)
    k = heads(k, B_QK_DIM)
    v = heads(v, B_V_DIM)
    log_i = gates(ig + i_bias)
    log_f = jax.nn.log_sigmoid(gates(fg + f_bias))
    causal = jnp.tril(jnp.ones((CHUNK, CHUNK), dtype=bool))

    def step(carry, inp):
        C, n, m = carry
        qc, kc, vc, ic, fc = inp
        b = jnp.cumsum(fc, axis=-1)
        d_log = jnp.where(causal, b[..., :, None] - b[..., None, :] + ic[..., None, :], -jnp.inf)
        m_inter = b + m[..., None]
        m_t = jnp.maximum(m_inter, jnp.max(d_log, axis=-1))
        s = jnp.einsum('bhtd,bhsd->bhts', qc, kc) * jnp.exp(d_log - m_t[..., None])
        g_inter = jnp.exp(m_inter - m_t)
        num = jnp.einsum('bhts,bhsv->bhtv', s, vc) + g_inter[..., None] * jnp.einsum('bhtd,bhdv->bhtv', qc, C)
        den = jnp.sum(s, axis=-1) + g_inter * jnp.einsum('bhtd,bhd->bht', qc, n)
        h = num / jnp.maximum(jnp.abs(den), jnp.exp(-m_t))[..., None]
        b_end = b[..., -1]
        w_log = b_end[..., None] - b + ic
        m_new = jnp.maximum(b_end + m, jnp.max(w_log, axis=-1))
        decay = jnp.exp(b_end + m - m_new)
        w_s = jnp.exp(w_log - m_new[..., None])
        C_new = decay[..., None, None] * C + jnp.einsum('bhs,bhsd,bhsv->bhdv', w_s, kc, vc)
        n_new = decay[..., None] * n + jnp.einsum('bhs,bhsd->bhd', w_s, kc)
        return (C_new, n_new, m_new), h

    init = (jnp.zeros((B, B_HEADS, B_QK_DIM, B_V_DIM), f32),
            jnp.zeros((B, B_HEADS, B_QK_DIM), f32),
            jnp.zeros((B, B_HEADS), f32))
    _, h = lax.scan(step, init, (q, k, v, log_i, log_f))
    h = h.transpose(1, 0, 3, 2, 4).reshape(B, T, B_HEADS, B_V_DIM)
    h = _rmsnorm(h, head_norm_g)
    h = jax.nn.sigmoid(o.astype(f32)).reshape(B, T, B_HEADS, B_V_DIM) * h
    h = h.reshape(B, T, B_WIDTH).astype(x.dtype)
    return (h * jax.nn.silu(z)) @ w_out


def setup_inputs(seed: int = 0) -> dict:
    key = jax.random.key(seed)
    ks = jax.random.split(key, 20)
    f32 = jnp.float32

    def nrm(k, shape, scale):
        return jax.random.normal(k, shape, f32) * scale

    nA, nB = N_A_LAYERS, N_B_LAYERS
    return {
        "x": nrm(ks[0], (BATCH, SEQ, D_MODEL), 1.0),
        "a_w_in": nrm(ks[1], (nA, D_MODEL, A_IN_COLS), D_MODEL ** -0.5),
        "a_kv_norm_g": 1.0 + nrm(ks[2], (nA, A_KV_LATENT), 0.02),
        "a_w_uk": nrm(ks[3], (nA, A_HEADS, A_HEAD_DIM, A_KV_LATENT), A_KV_LATENT ** -0.5),
        "a_w_uv": nrm(ks[4], (nA, A_HEADS, A_KV_LATENT, A_HEAD_DIM), A_KV_LATENT ** -0.5),
        "a_w_out": nrm(ks[5], (nA, A_WIDTH, D_MODEL), A_WIDTH ** -0.5 * DEEPNORM_BETA),
        "a_ln_g": 1.0 + nrm(ks[6], (nA, D_MODEL), 0.02),
        "a_ln_b": nrm(ks[7], (nA, D_MODEL), 0.02),
        "b_w_in": nrm(ks[8], (nB, D_MODEL, B_IN_COLS), D_MODEL ** -0.5),
        "b_i_bias": nrm(ks[9], (nB, B_HEADS), 0.1),
        "b_f_bias": jnp.linspace(3.0, 6.0, B_HEADS, dtype=f32)[None, :] + nrm(ks[10], (nB, B_HEADS), 0.1),
        "b_conv_w": nrm(ks[11], (nB, CONV_WIDTH, B_QK_COLS), CONV_WIDTH ** -0.5),
        "b_conv_b": nrm(ks[12], (nB, B_QK_COLS), 0.02),
        "b_head_norm_g": 1.0 + nrm(ks[13], (nB, B_HEADS, B_V_DIM), 0.02),
        "b_w_out": nrm(ks[14], (nB, B_WIDTH, D_MODEL), B_WIDTH ** -0.5 * DEEPNORM_BETA),
        "b_ln_g": 1.0 + nrm(ks[15], (nB, D_MODEL), 0.02),
        "b_ln_b": nrm(ks[16], (nB, D_MODEL), 0.02),
    }


def reference(x, a_w_in, a_kv_norm_g, a_w_uk, a_w_uv, a_w_out, a_ln_g, a_ln_b,
              b_w_in, b_i_bias, b_f_bias, b_conv_w, b_conv_b, b_head_norm_g, b_w_out,
              b_ln_g, b_ln_b):
    for layer in range(DEPTH):
        j = layer // N_MIXERS
        if layer % N_MIXERS == 0:
            y = _dsa_mixer(x, a_w_in[j], a_kv_norm_g[j], a_w_uk[j], a_w_uv[j], a_w_out[j])
            x = _layernorm(DEEPNORM_ALPHA * x + y, a_ln_g[j], a_ln_b[j])
        else:
            y = _mlstm_mixer(x, b_w_in[j], b_i_bias[j], b_f_bias[j], b_conv_w[j], b_conv_b[j],
                             b_head_norm_g[j], b_w_out[j])
            x = _layernorm(DEEPNORM_ALPHA * x + y, b_ln_g[j], b_ln_b[j])
    return x
```

```python
import numpy as np
from contextlib import ExitStack
import concourse.bass as bass
import concourse.mybir as mybir
from concourse.bass_utils import run_bass_kernel_spmd

F32 = mybir.dt.float32
BF16 = mybir.dt.bfloat16
AF = mybir.ActivationFunctionType
ALU = mybir.AluOpType
AX = mybir.AxisListType

T = 4096
D = 1024
NB = T // 128
A_IN = 2888
B_IN = 8200
ALPHA = float(4 ** 0.25)
EPS = 1e-5
NDMA = 12
BIS_STEPS = 28
BIS_LO = -2048.0
LAYERS = (0, 1)


class Buf:
    __slots__ = ("name", "w", "r")

    def __init__(self, name):
        self.name = name
        self.w = None
        self.r = {}


class KB:
    def __init__(self, nc, es):
        self.nc = nc
        self.es = es
        self.eng = {"pe": nc.tensor, "act": nc.scalar, "dve": nc.vector, "pool": nc.gpsimd, "sp": nc.sync}
        self.sem = {e: es.enter_context(nc.semaphore("s_" + e)) for e in ("pe", "act", "dve", "pool")}
        self.cnt = {e: 0 for e in self.sem}
        self.known = {e: {} for e in self.eng}
        self.dsem = [es.enter_context(nc.semaphore("d%d" % k)) for k in range(NDMA)]
        self.dtot = [0] * NDMA
        self.dnext = 0
        self.nbuf = 0
        self.root_es = es
        self.xsem = {}

    def buf(self, name=None):
        self.nbuf += 1
        return Buf(name or ("b%d" % self.nbuf))

    def sb(self, name, shape, dt):
        t = self.es.enter_context(self.nc.sbuf_tensor(name, list(shape), dt))
        return t, Buf(name)

    def ps(self, name, shape, dt):
        return self.es.enter_context(self.nc.psum_tensor(name, list(shape), dt))

    def _deps(self, reads, writes):
        toks = []
        for b in reads:
            if b.w is not None:
                toks.append(b.w)
        for b in writes:
            if b.w is not None:
                toks.append(b.w)
            toks.extend(b.r.items())
        return toks

    def _wait(self, e, toks):
        need = {}
        for k, v in toks:
            if k == e and e == "pe":
                continue
            if self.known[e].get(k, 0) >= v:
                continue
            if need.get(k, 0) < v:
                need[k] = v
        for k, v in need.items():
            sem = self.sem[k] if isinstance(k, str) else (self.dsem[k[1]] if k[0] == "d" else self.xsem[k])
            self.eng[e].wait_ge(sem, v)
            self.known[e][k] = v

    def _mark(self, tok, reads, writes):
        k, v = tok
        for b in reads:
            if b.r.get(k, 0) < v:
                b.r[k] = v
        for b in writes:
            b.w = tok
            b.r = {}

    def op(self, e, fn, reads=(), writes=(), inc=True):
        self._wait(e, self._deps(reads, writes))
        ins = fn(self.eng[e])
        if inc:
            self.cnt[e] += 1
            ins.then_inc(self.sem[e], 1)
            tok = (e, self.cnt[e])
        else:
            assert e == "pe"
            tok = (e, self.cnt[e] + 1)
        self._mark(tok, reads, writes)
        return tok

    def dma(self, out, in_, reads=(), writes=(), q="sp"):
        toks = self._deps(reads, writes)
        if q == "pool":
            key = ("x", len(self.xsem))
            self.xsem[key] = self.root_es.enter_context(self.nc.semaphore("x%d" % len(self.xsem)))
            self._wait(q, toks)
            self.eng[q].dma_start(out=out, in_=in_).then_inc(self.xsem[key], 16)
            tok = (key, 16)
            self._mark(tok, reads, writes)
            return tok
        k = self.dnext
        self.dnext = (k + 1) % NDMA
        if self.dtot[k] > 0:
            toks.append((("d", k), self.dtot[k]))
        self._wait(q, toks)
        self.dtot[k] += 16
        self.eng[q].dma_start(out=out, in_=in_).then_inc(self.dsem[k], 16)
        tok = (("d", k), self.dtot[k])
        self._mark(tok, reads, writes)
        return tok

    def barrier(self):
        toks = [(("d", k), self.dtot[k]) for k in range(NDMA) if self.dtot[k] > 0]
        toks += [(k, 16) for k in self.xsem]
        toks += [(e, c) for e, c in self.cnt.items() if c > 0]
        for e in self.eng:
            self._wait(e, toks)

    def finish(self):
        toks = [(("d", k), self.dtot[k]) for k in range(NDMA) if self.dtot[k] > 0]
        toks += [(k, 16) for k in self.xsem]
        toks += [(e, c) for e, c in self.cnt.items() if c > 0]
        self._wait("sp", toks)


def mm(kb, out, lhsT, rhs, reads, writes, start=True, stop=True, inc=None):
    if inc is None:
        inc = stop
    return kb.op("pe", lambda e: e.matmul(out, lhsT, rhs, start=start, stop=stop), reads, writes, inc=inc)


def tr(kb, out, in_, ident, reads, writes, inc=True):
    return kb.op("pe", lambda e: e.transpose(out, in_, ident), reads, writes, inc=inc)


def layernorm_store(kb, st, r_ap, rB, out_dram_ap, outB, G, Bt, GB):
    s1, s1B = st["s1"]
    kb.op("dve", lambda e: e.reduce_sum(s1[:], r_ap, AX.X), [rB], [s1B])
    kb.op("dve", lambda e: e.tensor_scalar(s1[:], s1[:], -1.0 / D, None, ALU.mult), [s1B], [s1B])
    kb.op("dve", lambda e: e.tensor_scalar(r_ap, r_ap, s1[:], None, ALU.add), [rB, s1B], [rB])
    junk, junkB = st["lnjunk"]
    ss, ssB = st["ss"]
    kb.op("act", lambda e: e.activation(junk[:], r_ap, AF.Square, accum_out=ss[:]), [rB], [junkB, ssB])
    kb.op("dve", lambda e: e.tensor_scalar(ss[:], ss[:], 1.0 / D, EPS, ALU.mult, ALU.add), [ssB], [ssB])
    kb.op("act", lambda e: e.activation(ss[:], ss[:], AF.Sqrt), [ssB], [ssB])
    kb.op("dve", lambda e: e.reciprocal(ss[:], ss[:]), [ssB], [ssB])
    kb.op("dve", lambda e: e.scalar_tensor_tensor(r_ap, r_ap, ss[:], G[:], ALU.mult, ALU.mult), [rB, ssB] + list(GB), [rB])
    kb.op("dve", lambda e: e.tensor_tensor(r_ap, r_ap, Bt[:], ALU.add), [rB] + list(GB), [rB])
    kb.dma(out_dram_ap, r_ap, [rB], [outB])


def emit_layer0(nc, kb, dr, x_in, xinB, x_out, xoutB, cst, nblk=NB):
    es = ExitStack()
    kb_es = kb.es
    kb.es = es
    sb = kb.sb
    identB, identF, CM, BIAS, ones, cB = cst["identB"], cst["identF"], cst["CM"], cst["BIAS"], cst["ones"], cst["buf"]

    W0, W0B = sb("W0", [128, 8, A_IN], BF16)
    Wk2, Wk2B = sb("Wk2", [128, 8, 128], BF16)
    Wuk, WukB = sb("Wuk", [128, 8, 256], BF16)
    Wuv, WuvB = sb("Wuv", [128, 8, 2, 128], BF16)
    Wout, WoutB = sb("Wout", [128, 8, 1024], BF16)
    Gkv, GkvB = sb("Gkv", [128, 256], F32)
    LG, LGB = sb("LG", [128, 1024], F32)
    LB, LBB = sb("LB", [128, 1024], F32)
    win = dr["a_w_in"].rearrange("(k p) n -> p k n", p=128)
    for k in range(0, 8, 4):
        kb.dma(W0[:, k:k + 4, :], win[:, k:k + 4, :], [], [W0B], q="pool")
    kb.dma(Wk2[:, :, 0:64], win[:, :, 1792:1856], [], [Wk2B], q="pool")
    kb.dma(Wk2[:, :, 64:128], win[:, :, 1792:1856], [], [Wk2B], q="pool")
    kb.dma(Wuk[:], dr["a_w_uk"].rearrange("h d c -> d h c"), [], [WukB], q="pool")
    kb.dma(Wuv[:], dr["a_w_uv"].rearrange("h (cc p) d -> p h cc d", p=128), [], [WuvB], q="pool")
    kb.dma(Wout[:], dr["a_w_out"].rearrange("(k p) n -> p k n", p=128), [], [WoutB], q="pool")
    kb.dma(Gkv[:], dr["a_kv_norm_g"].partition_broadcast(128), [], [GkvB])
    kb.dma(LG[:], dr["a_ln_g"].partition_broadcast(128), [], [LGB])
    kb.dma(LB[:], dr["a_ln_b"].partition_broadcast(128), [], [LBB])

    kT2, kT2B = sb("kT2", [128, T], BF16)
    cN, cNB = sb("cN", [128, NB, 256], BF16)
    cT, cTB = sb("cT", [128, 2, T], BF16)
    SC, SCB = sb("SC", [128, T], F32)
    M, MB = sb("M", [128, T], BF16)
    MT, MTB = sb("MT", [128, NB, 128], BF16)
    XF = [sb("XF%d" % s, [128, 1024], F32) for s in range(1)]
    xT, xTB = sb("xT", [128, 8, 128], BF16)
    qT, qTB = sb("qT", [128, 8, 128], BF16)
    qiT, qiTB = sb("qiT", [128, 4, 128], BF16)
    qlT, qlTB = sb("qlT", [128, 2, 1024], BF16)
    sz, szB = sb("sz", [128, 1024], BF16)
    cf, cfB = sb("cf", [128, 256], F32)
    wsb, wsbB = sb("wsb", [128, 8], F32)
    RR = [sb("R%d" % s, [128, 512], F32) for s in range(3)]
    PT = [sb("PT%d" % s, [128, 8, 128], BF16) for s in range(2)]
    olT, olTB = sb("olT", [128, 2, 1024], BF16)
    og, ogB = sb("og", [128, 1024], BF16)
    ogT, ogTB = sb("ogT", [128, 8, 128], BF16)
    rr, rrB = sb("rr", [128, 1024], F32)
    rden, rdenB = sb("rden", [128, 8], F32)
    dens, densB = sb("dens", [128, 8], F32)
    bh, bhB = sb("bh", [128, 32], F32)
    jm, jmB = sb("jm", [128, 1], F32)
    VH, VHB = sb("VH", [128, 8, 1], BF16)
    VHD, VHDB = sb("VHD", [128, 8, 8], BF16)
    SH, SHB = sb("SH", [8, 1024], BF16)
    CMISC = cst["CMISC"]
    OLs, OLsB = sb("OLs", [128, 2, 1024], F32)
    lo, loB = sb("lo", [128, 1], F32)
    mid, midB = sb("mid", [128, 1], F32)
    cnt, cntB = sb("cnt", [128, 1], F32)
    cntA, cntAB = sb("cntA", [128, 1], F32)
    MBa = kb.buf("MBa")
    ge, geB = sb("ge", [128, 1], F32)
    st = {"s1": sb("s1", [128, 1], F32), "ss": sb("ss", [128, 1], F32), "lnjunk": (og, ogB)}
    ssc, sscB = sb("ssc", [128, 1], F32)
    cjunk, cjunkB = sb("cjunk", [128, 256], BF16)

    P0 = kb.ps("P0", [128, 1024], F32)
    P1 = kb.ps("P1", [128, 1024], F32)
    P2 = kb.ps("P2", [128, 1024], F32)
    PX = kb.ps("PX", [128, 512], F32)
    P16 = kb.ps("P16", [128, 1024], BF16)
    B0a, B0b, B1a, B1b, B2a, B2b, BX, B16 = [kb.buf("ps%d" % i) for i in range(8)]

    scale_q = float(128 ** -0.5)

    for i in range(nblk):
        n = 128 * (i + 1)
        xf, xfB = XF[0]
        kb.dma(xf[:], x_in[i * 128:(i + 1) * 128, :], [xinB], [xfB])
        import os
        stage = float(os.environ.get("K_STAGE", "99")) if i == 0 else float(os.environ.get("K_STAGE1", os.environ.get("K_STAGE", "99")))
        if stage <= 0:
            kb.dma(x_out[i * 128:(i + 1) * 128, :], xf[:], [xfB], [xoutB])
            continue
        for k in range(8):
            tr(kb, P0[:, k * 128:(k + 1) * 128], xf[:, k * 128:(k + 1) * 128], identF[:], [xfB, cB], [B0a if k < 4 else B0b], inc=(k == 7))
        kb.op("act", lambda e: e.copy(xT[:].rearrange("p k t -> p (k t)"), P0[:]), [B0a, B0b], [xTB])
        if stage <= 0.1:
            kb.op("dve", lambda e: e.memset(rr[:], 0.0), [], [rrB])
            kb.op("dve", lambda e: e.tensor_copy(rr[:, 0:128], xT[:, 0, :]), [xTB], [rrB])
            kb.dma(x_out[i * 128:(i + 1) * 128, :], rr[:], [rrB], [xoutB])
            continue
        for h in range(8):
            for k in range(8):
                mm(kb, P1[:, h * 128:(h + 1) * 128], W0[:, k, h * 128:(h + 1) * 128], xT[:, k, :], [W0B, xTB],
                   [B1a if h < 4 else B1b], start=(k == 0), stop=(k == 7), inc=(k == 7 and h == 7))
        kb.op("dve", lambda e: e.tensor_copy(qT[:].rearrange("p h t -> p (h t)"), P1[:]), [B1a, B1b], [qTB])
        if stage <= 0.2:
            kb.op("dve", lambda e: e.memset(rr[:], 0.0), [], [rrB])
            kb.op("dve", lambda e: e.tensor_copy(rr[:, 0:128], xT[:, 0, :]), [xTB], [rrB])
            kb.dma(x_out[i * 128:(i + 1) * 128, :], rr[:], [rrB], [xoutB])
            continue
        for c in range(4):
            for k in range(8):
                mm(kb, P2[:, c * 128:(c + 1) * 128], W0[:, k, 1280 + c * 128:1280 + (c + 1) * 128], xT[:, k, :],
                   [W0B, xTB], [B2a], start=(k == 0), stop=(k == 7), inc=(k == 7 and c == 3))
        kb.op("act", lambda e: e.copy(qiT[:].rearrange("p c t -> p (c t)"), P2[:, 0:512]), [B2a], [qiTB])
        if stage <= 0.25:
            kb.op("dve", lambda e: e.memset(rr[:], 0.0), [], [rrB])
            kb.op("dve", lambda e: e.tensor_copy(rr[:, 0:128], xT[:, 0, :]), [xTB], [rrB])
            kb.dma(x_out[i * 128:(i + 1) * 128, :], rr[:], [rrB], [xoutB])
            continue
        for k in range(8):
            mm(kb, P2[:, 512:640], Wk2[:, k, :], xT[:, k, :], [Wk2B, xTB], [B2b], start=(k == 0), stop=(k == 7), inc=False)
        for k in range(8):
            mm(kb, P2[:, 640:896], xT[:, k, :], W0[:, k, 1024:1280], [W0B, xTB], [B2b], start=(k == 0), stop=(k == 7), inc=False)
        for k in range(8):
            mm(kb, P2[:, 896:960], xT[:, k, :], W0[:, k, 1856:1920], [W0B, xTB], [B2b], start=(k == 0), stop=(k == 7), inc=(k == 7))
        if stage <= 0.27:
            kb.op("dve", lambda e: e.memset(rr[:], 0.0), [], [rrB])
            kb.op("dve", lambda e: e.tensor_copy(rr[:, 0:128], xT[:, 0, :]), [xTB], [rrB])
            kb.dma(x_out[i * 128:(i + 1) * 128, :], rr[:], [rrB], [xoutB])
            continue
        kb.op("dve", lambda e: e.tensor_copy(kT2[:, i * 128:(i + 1) * 128], P2[:, 512:640]), [B2b], [kT2B])
        if stage <= 0.28:
            kb.op("dve", lambda e: e.memset(rr[:], 0.0), [], [rrB])
            kb.op("dve", lambda e: e.tensor_copy(rr[:, 0:128], xT[:, 0, :]), [xTB], [rrB])
            kb.dma(x_out[i * 128:(i + 1) * 128, :], rr[:], [rrB], [xoutB])
            continue
        kb.op("dve", lambda e: e.tensor_copy(wsb[:], P2[:, 896:904]), [B2b], [wsbB])
        if stage <= 0.29:
            kb.op("dve", lambda e: e.memset(rr[:], 0.0), [], [rrB])
            kb.op("dve", lambda e: e.tensor_copy(rr[:, 0:128], xT[:, 0, :]), [xTB], [rrB])
            kb.dma(x_out[i * 128:(i + 1) * 128, :], rr[:], [rrB], [xoutB])
            continue
        kb.op("dve", lambda e: e.tensor_copy(cf[:], P2[:, 640:896]), [B2b], [cfB])
        if stage <= 0.3:
            kb.op("dve", lambda e: e.memset(rr[:], 0.0), [], [rrB])
            kb.op("dve", lambda e: e.tensor_copy(rr[:, 0:128], xT[:, 0, :]), [xTB], [rrB])
            kb.dma(x_out[i * 128:(i + 1) * 128, :], rr[:], [rrB], [xoutB])
            continue
        for nn in range(2):
            for k in range(8):
                mm(kb, P0[:, nn * 512:(nn + 1) * 512], xT[:, k, :], W0[:, k, 1864 + nn * 512:1864 + (nn + 1) * 512],
                   [W0B, xTB], [B0a if nn == 0 else B0b], start=(k == 0), stop=(k == 7), inc=(k == 7))
        kb.op("act", lambda e: e.activation(sz[:], P0[:], AF.Silu), [B0a, B0b], [szB])
        if stage <= 0.4:
            kb.op("dve", lambda e: e.memset(rr[:], 0.0), [], [rrB])
            kb.op("dve", lambda e: e.tensor_copy(rr[:, 0:128], xT[:, 0, :]), [xTB], [rrB])
            kb.dma(x_out[i * 128:(i + 1) * 128, :], rr[:], [rrB], [xoutB])
            continue
        kb.op("act", lambda e: e.activation(cjunk[:], cf[:], AF.Square, accum_out=ssc[:]), [cfB], [cjunkB, sscB])
        kb.op("dve", lambda e: e.tensor_scalar(ssc[:], ssc[:], 1.0 / 256, EPS, ALU.mult, ALU.add), [sscB], [sscB])
        kb.op("act", lambda e: e.activation(ssc[:], ssc[:], AF.Sqrt), [sscB], [sscB])
        kb.op("dve", lambda e: e.reciprocal(ssc[:], ssc[:]), [sscB], [sscB])
        kb.op("dve", lambda e: e.scalar_tensor_tensor(cN[:, i, :], cf[:], ssc[:], Gkv[:], ALU.mult, ALU.mult),
              [cfB, sscB, GkvB], [cNB])
        for cc in range(2):
            tr(kb, P16[:, cc * 128:(cc + 1) * 128], cN[:, i, cc * 128:(cc + 1) * 128], identB[:], [cNB, cB], [B16], inc=(cc == 1))
        kb.op("dve", lambda e: e.tensor_copy(cT[:, :, i * 128:(i + 1) * 128], P16[:, 0:256].rearrange("p (c t) -> p c t", c=2)),
              [B16], [cTB])
        if stage <= 0.6:
            kb.op("dve", lambda e: e.memset(rr[:], 0.0), [], [rrB])
            kb.op("dve", lambda e: e.tensor_copy(rr[:, 0:128], xT[:, 0, :]), [xTB], [rrB])
            kb.dma(x_out[i * 128:(i + 1) * 128, :], rr[:], [rrB], [xoutB])
            continue
        for cc in range(2):
            Pq = P0 if cc == 0 else P1
            for h in range(8):
                bq = (B0a, B0b, B1a, B1b)[cc * 2 + (h // 4)]
                mm(kb, Pq[:, h * 128:(h + 1) * 128], Wuk[:, h, cc * 128:(cc + 1) * 128], qT[:, h, :], [WukB, qTB], [bq],
                   inc=(h == 7))
        kb.op("act", lambda e: e.activation(qlT[:, 0, :], P0[:], AF.Copy, scale=scale_q), [B0a, B0b], [qlTB])
        kb.op("dve", lambda e: e.tensor_scalar(qlT[:, 1, :], P1[:], scale_q, None, ALU.mult), [B1a, B1b], [qlTB])

        if stage <= 1:
            kb.op("dve", lambda e: e.tensor_copy(rr[:, 0:256], cN[:, i, :]), [cNB], [rrB])
            kb.op("dve", lambda e: e.tensor_copy(rr[:, 256:1024], qlT[:, 0, 0:768]), [qlTB], [rrB])
            kb.dma(x_out[i * 128:(i + 1) * 128, :], rr[:], [rrB], [xoutB])
            continue
        nkc = (n + 511) // 512
        slots = [(PX, BX, slice(0, 512)), (P2, B2a, slice(0, 512)), (P2, B2b, slice(512, 1024))]
        u = 0
        for kc in range(nkc):
            wd = min(512, n - 512 * kc)
            c0 = kc * 512
            for h in range(8):
                Pt, Bt, sl = slots[u % 3]
                R, RB = RR[u % 3]
                u += 1
                pr = slice((h % 2) * 64, (h % 2) * 64 + 64)
                mm(kb, Pt[:, sl.start:sl.start + wd], qiT[pr, h // 2, :], kT2[pr, c0:c0 + wd], [qiTB, kT2B], [Bt])
                kb.op("act", lambda e: e.activation(R[:, 0:wd], Pt[:, sl.start:sl.start + wd], AF.Relu), [Bt], [RB])
                if h == 0:
                    kb.op("dve", lambda e: e.tensor_scalar(SC[:, c0:c0 + wd], R[:, 0:wd], wsb[:, 0:1], None, ALU.mult),
                          [RB, wsbB], [SCB])
                else:
                    kb.op("dve", lambda e: e.scalar_tensor_tensor(SC[:, c0:c0 + wd], R[:, 0:wd], wsb[:, h:h + 1], SC[:, c0:c0 + wd],
                                                                   ALU.mult, ALU.add), [RB, wsbB, SCB], [SCB])
        kb.op("dve", lambda e: e.tensor_tensor(SC[:, i * 128:n], SC[:, i * 128:n], CM[:], ALU.add), [SCB, cB], [SCB])

        MBall = [MB, MBa]
        if i >= 2:
            nd = 128 * max(1, int(round(0.45 * (i + 1))))
            na = n - nd
            kb.op("dve", lambda e: e.memset(lo[:], BIS_LO), [], [loB])
            for s in range(BIS_STEPS):
                wk = float(-BIS_LO / (2 ** s))
                kb.op("dve", lambda e: e.tensor_scalar(mid[:], lo[:], wk, None, ALU.add), [loB], [midB])
                kb.op("dve", lambda e: e.tensor_scalar(M[:, 0:nd], SC[:, 0:nd], mid[:], None, ALU.is_ge, ALU.add, accum_out=cnt[:]),
                      [SCB, midB], [MB, cntB])
                kb.op("act", lambda e: e.activation(M[:, nd:n], SC[:, nd:n], AF.Sign, bias=mid[:], scale=-1.0, accum_out=cntA[:]),
                      [SCB, midB], [MBa, cntAB])
                kb.op("dve", lambda e: e.scalar_tensor_tensor(ge[:], cnt[:], 2.0, cntA[:], ALU.mult, ALU.subtract), [cntB, cntAB], [geB])
                kb.op("dve", lambda e: e.tensor_scalar(ge[:], ge[:], float(511 - na), wk, ALU.is_ge, ALU.mult), [geB], [geB])
                kb.op("dve", lambda e: e.tensor_tensor(lo[:], lo[:], ge[:], ALU.add), [loB, geB], [loB])
            kb.op("dve", lambda e: e.tensor_scalar(M[:, 0:n], SC[:, 0:n], lo[:], -30000.0, ALU.is_lt, ALU.mult), [SCB, loB], MBall)
        else:
            kb.op("dve", lambda e: e.tensor_scalar(M[:, 0:n], SC[:, 0:n], -1e29, -30000.0, ALU.is_lt, ALU.mult), [SCB], MBall)
        nbk = i + 1
        kb.op("dve", lambda e: e.tensor_reduce(bh[:, 0:nbk], M[:, 0:n].rearrange("p (j s) -> p j s", s=128), AX.X, ALU.max), MBall, [bhB])
        kb.op("dve", lambda e: e.tensor_tensor(bh[:, 0:nbk], bh[:, 0:nbk], CMISC[:, 0:nbk], ALU.add), [bhB, cB], [bhB])
        kb.op("dve", lambda e: e.reduce_max(jm[:], bh[:, 0:nbk], AX.X), [bhB], [jmB])
        kb.op("dve", lambda e: e.tensor_scalar(jm[:], jm[:], -1.0, float(i + 1), ALU.mult, ALU.add), [jmB], [jmB])
        kb.op("dve", lambda e: e.tensor_scalar(VH[:].rearrange("p h o -> p (h o)"), CMISC[:, 32:40], jm[:], None, ALU.mult), [jmB, cB], [VHB])
        kb.op("dve", lambda e: e.tensor_tensor(VHD[:], CMISC[:, 40:104].rearrange("p (h g) -> p h g", g=8), VH[:].to_broadcast([128, 8, 8]), ALU.mult),
              [VHB, cB], [VHDB])
        for h in range(8):
            tr(kb, P16[0:8, h * 128:(h + 1) * 128], VHD[:, h, :], identB[:], [VHDB, cB], [B16], inc=(h == 7))
        kb.op("act", lambda e: e.copy(SH[:], P16[0:8, :]), [B16], [SHB])
        if stage <= 2:
            kb.op("dve", lambda e: e.tensor_copy(rr[:, 0:128], M[:, 0:128]), [MB], [rrB])
            kb.op("dve", lambda e: e.tensor_copy(rr[:, 128:256], SC[:, 0:128]), [SCB], [rrB])
            kb.dma(x_out[i * 128:(i + 1) * 128, :], rr[:], [rrB], [xoutB])
            continue
        for j0 in range(0, i + 1, 8):
            nj = min(8, i + 1 - j0)
            for jj in range(nj):
                j = j0 + jj
                tr(kb, P16[:, jj * 128:(jj + 1) * 128], M[:, j * 128:(j + 1) * 128], identB[:], [MB, MBa, cB], [B16], inc=(jj == nj - 1))
            kb.op("act", lambda e: e.copy(MT[:, j0:j0 + nj, :].rearrange("p j t -> p (j t)"), P16[:, 0:nj * 128]), [B16], [MTB])

        jlist = list(range(i + 1))
        if os.environ.get("K_JLAST"):
            jlist = [i]
        for j in jlist:
            d = i - j
            Pt_, PtB = PT[j % 2]
            for hb in range(2):
                bS = B0a if hb == 0 else B0b
                for cc in range(2):
                    mm(kb, P0[:, hb * 512:(hb + 1) * 512], cT[:, cc, j * 128:(j + 1) * 128], qlT[:, cc, hb * 512:(hb + 1) * 512],
                       [cTB, qlTB], [bS], start=(cc == 0), stop=False, inc=False)
                mm(kb, P0[:, hb * 512:(hb + 1) * 512], ones[0:8, :], SH[:, hb * 512:(hb + 1) * 512], [SHB, cB], [bS],
                   start=False, stop=False, inc=False)
                mm(kb, P0[:, hb * 512:(hb + 1) * 512], identB[:], MT[:, j:j + 1, :].to_broadcast([128, 4, 128]), [MTB, cB], [bS],
                   start=False, stop=True, inc=True)
            for h in range(8):
                kb.op("act", lambda e: e.activation(Pt_[:, h, :], P0[:, h * 128:(h + 1) * 128], AF.Exp,
                                                     bias=BIAS[:, h * 32 + d:h * 32 + d + 1], scale=1.0),
                      [B0a if h < 4 else B0b, cB], [PtB])
            first = (j == jlist[0])
            for cc in range(2):
                Po = P1 if cc == 0 else P2
                for hb in range(2):
                    bo = (B1a, B1b, B2a, B2b)[cc * 2 + hb]
                    mm(kb, Po[:, hb * 512:(hb + 1) * 512], cN[:, j, cc * 128:(cc + 1) * 128],
                       Pt_[:, 4 * hb:4 * hb + 4, :].rearrange("p h t -> p (h t)"), [cNB, PtB], [bo],
                       start=True, stop=True, inc=False)
            for h in range(8):
                mm(kb, PX[:, h:h + 1], Pt_[:, h, :], ones[:, 0:1], [PtB, cB], [BX], start=(h == 0), stop=(h == 7), inc=(h == 7))
            if first:
                kb.op("dve", lambda e: e.tensor_copy(OLs[:, 0, :], P1[:]), [B1a, B1b], [OLsB])
                kb.op("dve", lambda e: e.tensor_copy(OLs[:, 1, :], P2[:]), [B2a, B2b], [OLsB])
                kb.op("dve", lambda e: e.tensor_copy(dens[:], PX[:, 0:8]), [BX], [densB])
            else:
                kb.op("dve", lambda e: e.tensor_tensor(OLs[:, 0, :], OLs[:, 0, :], P1[:], ALU.add), [B1a, B1b, OLsB], [OLsB])
                kb.op("dve", lambda e: e.tensor_tensor(OLs[:, 1, :], OLs[:, 1, :], P2[:], ALU.add), [B2a, B2b, OLsB], [OLsB])
                kb.op("dve", lambda e: e.tensor_tensor(dens[:], dens[:], PX[:, 0:8], ALU.add), [BX, densB], [densB])

        kb.op("dve", lambda e: e.reciprocal(rden[:], dens[:]), [densB], [rdenB])
        kb.op("act", lambda e: e.copy(olT[:, 0, :], OLs[:, 0, :]), [OLsB], [olTB])
        kb.op("dve", lambda e: e.tensor_copy(olT[:, 1, :], OLs[:, 1, :]), [OLsB], [olTB])
        for h in range(8):
            for cc in range(2):
                mm(kb, P0[:, h * 128:(h + 1) * 128], olT[:, cc, h * 128:(h + 1) * 128], Wuv[:, h, cc, :], [olTB, WuvB],
                   [B0a if h < 4 else B0b], start=(cc == 0), stop=(cc == 1), inc=(cc == 1 and h == 7))
        for h in range(8):
            kb.op("dve", lambda e: e.scalar_tensor_tensor(og[:, h * 128:(h + 1) * 128], P0[:, h * 128:(h + 1) * 128], rden[:, h:h + 1],
                                                           sz[:, h * 128:(h + 1) * 128], ALU.mult, ALU.mult),
                  [B0a if h < 4 else B0b, rdenB, szB], [ogB])
        for k in range(8):
            tr(kb, P16[:, k * 128:(k + 1) * 128], og[:, k * 128:(k + 1) * 128], identB[:], [ogB, cB], [B16], inc=(k == 7))
        kb.op("act", lambda e: e.copy(ogT[:].rearrange("p k t -> p (k t)"), P16[:]), [B16], [ogTB])
        for nn in range(2):
            for k in range(8):
                mm(kb, P1[:, nn * 512:(nn + 1) * 512], ogT[:, k, :], Wout[:, k, nn * 512:(nn + 1) * 512], [ogTB, WoutB],
                   [B1a if nn == 0 else B1b], start=(k == 0), stop=(k == 7), inc=(k == 7))
        kb.op("dve", lambda e: e.scalar_tensor_tensor(rr[:], xf[:], ALPHA, P1[:], ALU.mult, ALU.add), [xfB, B1a, B1b], [rrB])
        layernorm_store(kb, st, rr[:], rrB, x_out[i * 128:(i + 1) * 128, :], xoutB, LG, LB, [LGB, LBB])

    kb.finish_layer = True
    kb.es = kb_es
    return es


def load_xT(kb, x_in, xinB, i, xf, xfB, xT, xTB, Ptr, Bs, identF, cB):
    kb.dma(xf[:], x_in[i * 128:(i + 1) * 128, :], [xinB], [xfB])
    for k in range(8):
        tr(kb, Ptr[:, k * 128:(k + 1) * 128], xf[:, k * 128:(k + 1) * 128], identF[:], [xfB, cB], Bs, inc=(k == 7))
    kb.op("act", lambda e: e.copy(xT[:].rearrange("p k t -> p (k t)"), Ptr[:, 0:1024]), Bs, [xTB])


def emit_layer1a(nc, kb, dr, x_in, xinB, hn, hnB, cst, nblk=NB):
    es = ExitStack()
    kb_es = kb.es
    kb.es = es
    sb = kb.sb
    identB, identF, ones, cB, TRIf = cst["identB"], cst["identF"], cst["ones"], cst["buf"], cst["TRIf"]
    win = dr["b_w_in"].rearrange("(k p) n -> p k n", p=128)
    Wqk, WqkB = sb("Wqk", [128, 8, 2048], BF16)
    Wv, WvB = sb("Wv", [128, 8, 2048], BF16)
    Wg, WgB = sb("Wg", [128, 8, 8], BF16)
    for k in range(0, 8, 4):
        kb.dma(Wqk[:, k:k + 4, :], win[:, k:k + 4, 0:2048], [], [WqkB], q="pool")
        kb.dma(Wv[:, k:k + 4, :], win[:, k:k + 4, 2048:4096], [], [WvB], q="pool")
    kb.dma(Wg[:], win[:, :, 4096:4104], [], [WgB], q="pool")
    CWr, CWrB = sb("CWr", [80, 128], F32)
    CWT, CWTB = sb("CWT", [128, 80], F32)
    kb.dma(CWr[0:64, :], dr["b_conv_w"].rearrange("j (c p) -> (j c) p", p=128), [], [CWrB])
    kb.dma(CWr[64:80, :], dr["b_conv_b"].rearrange("o (c p) -> (o c) p", p=128), [], [CWrB])
    GBt, GBB = sb("GBt", [128, 8], F32)
    kb.dma(GBt[:, 0:4], dr["b_i_bias"].partition_broadcast(128), [], [GBB])
    kb.dma(GBt[:, 4:8], dr["b_f_bias"].partition_broadcast(128), [], [GBB])
    HG, HGB = sb("HG", [128, 2048], F32)
    kb.dma(HG[:], dr["b_head_norm_g"].rearrange("o h v -> o (h v)").partition_broadcast(128), [], [HGB])

    PA = kb.ps("L1PA", [128, 2048], F32)
    PB = kb.ps("L1PB", [128, 1024], F32)
    PC = kb.ps("L1PC", [128, 512], F32)
    P16 = kb.ps("L1P16", [128, 1024], BF16)
    BA = [kb.buf("pa%d" % i) for i in range(4)]
    BBa, BBb, BC, B16 = kb.buf("pba"), kb.buf("pbb"), kb.buf("pc"), kb.buf("p16")
    BRW = [kb.buf("brw%d" % s_) for s_ in range(2)]; BST = [kb.buf("bst%d" % s_) for s_ in range(2)]
    BDN = [kb.buf("bdn%d" % s_) for s_ in range(2)]; BNM = [kb.buf("bnm%d" % s_) for s_ in range(2)]; BNU = kb.buf("bnu")
    REG = {0: BRW, 1: BST, 2: BDN + [BNU], 3: [BNM[1]]}

    tr(kb, PC[:, 0:80], CWr[:], identF[0:80, 0:80], [CWrB, cB], [BC])
    kb.op("dve", lambda e: e.tensor_copy(CWT[:], PC[:, 0:80]), [BC], [CWTB])

    xf, xfB = sb("xf1", [128, 1024], F32)
    xT, xTB = sb("xT1", [128, 8, 128], BF16)
    QKP, QKPB = sb("QKP", [128, 16, 131], F32)
    tmpc, tmpcB = sb("tmpc", [128, 128], F32)
    qkT, qkTB = sb("qkT", [128, 16, 128], BF16)
    vt, vtB = sb("vt", [128, 2048], BF16)
    gt, gtB = sb("gt", [128, 8], F32)
    lf, lfB = sb("lf", [128, 4], F32)
    ibm, ibmB = sb("ibm", [128, 4], F32)
    bcol, bcolB = sb("bcol", [128, 4], F32)
    ONESf, ONESfB = sb("ONESf", [128, 128], F32)
    LFB_2 = [sb("LFB%d" % s_, [128, 128], F32) for s_ in range(2)]
    EB_2 = [sb("EB%d" % s_, [128, 128], F32) for s_ in range(2)]
    DT_2 = [sb("DT%d" % s_, [128, 128], F32) for s_ in range(2)]
    sT_2 = [sb("sT%d" % s_, [128, 128], BF16) for s_ in range(2)]
    qh_2 = [sb("qh%d" % s_, [128, 2, 128], BF16) for s_ in range(2)]
    kTok_2 = [sb("kTok%d" % s_, [128, 256], BF16) for s_ in range(2)]
    vs_2 = [sb("vs%d" % s_, [128, 512], BF16) for s_ in range(2)]
    wcol_2 = [sb("wcol%d" % s_, [128, 1], BF16) for s_ in range(2)]
    hh_2 = [sb("hh%d" % s_, [128, 512], F32) for s_ in range(2)]
    hjunk_2 = [sb("hjunk%d" % s_, [128, 512], BF16) for s_ in range(2)]
    HN, HNB = sb("HN", [128, 2048], BF16)
    rec_2 = [sb("rec%d" % s_, [128, 1], F32) for s_ in range(2)]
    ssh_2 = [sb("ssh%d" % s_, [128, 1], F32) for s_ in range(2)]
    Cs = [sb("C%d" % h, [128, 2, 512], F32) for h in range(4)]
    Cb = [sb("Cb%d" % h, [128, 2, 512], BF16) for h in range(4)]
    ns = [sb("n%d" % h, [128, 2], F32) for h in range(4)]
    nb = [sb("nb%d" % h, [128, 2], BF16) for h in range(4)]
    kb.op("dve", lambda e: e.memset(ONESf[:], 1.0), [], [ONESfB])
    kb.op("dve", lambda e: e.memset(QKP[:], 0.0), [], [QKPB])
    for h in range(4):
        kb.op("dve", lambda e: e.memset(Cs[h][0][:], 0.0), [], [Cs[h][1]])
        kb.op("dve", lambda e: e.memset(Cb[h][0][:], 0.0), [], [Cb[h][1]])
        kb.op("dve", lambda e: e.memset(ns[h][0][:], 0.0), [], [ns[h][1]])
        kb.op("dve", lambda e: e.memset(nb[h][0][:], 0.0), [], [nb[h][1]])

    for i in range(nblk):
        load_xT(kb, x_in, xinB, i, xf, xfB, xT, xTB, PB, [BBa, BBb, BNM[0]], identF, cB)
        for c in range(16):
            for k in range(8):
                mm(kb, PA[:, c * 128:(c + 1) * 128], Wqk[:, k, c * 128:(c + 1) * 128], xT[:, k, :], [WqkB, xTB], [BA[c // 4]] + REG[c // 4],
                   start=(k == 0), stop=(k == 7), inc=(k == 7 and c % 4 == 3))
        kb.op("dve", lambda e: e.tensor_copy(QKP[:, 0:8, 3:131], PA[:, 0:1024].rearrange("p (c t) -> p c t", c=8)), [BA[0], BA[1]], [QKPB])
        kb.op("dve", lambda e: e.tensor_copy(QKP[:, 8:16, 3:131], PA[:, 1024:2048].rearrange("p (c t) -> p c t", c=8)), [BA[2], BA[3]], [QKPB])
        for c in range(16):
            kb.op("dve", lambda e: e.tensor_scalar(tmpc[:], QKP[:, c, 0:128], CWT[:, c:c + 1], CWT[:, 64 + c:65 + c], ALU.mult, ALU.add),
                  [QKPB, CWTB], [tmpcB])
            for j in range(1, 4):
                kb.op("dve", lambda e: e.scalar_tensor_tensor(tmpc[:], QKP[:, c, j:j + 128], CWT[:, j * 16 + c:j * 16 + c + 1], tmpc[:],
                                                               ALU.mult, ALU.add), [QKPB, CWTB, tmpcB], [tmpcB])
            kb.op("act", lambda e: e.activation(qkT[:, c, :], tmpc[:], AF.Silu), [tmpcB], [qkTB])
        kb.op("dve", lambda e: e.tensor_copy(tmpc[:, 0:48].rearrange("p (c t) -> p c t", c=16), QKP[:, :, 128:131]), [QKPB], [tmpcB])
        kb.op("dve", lambda e: e.tensor_copy(QKP[:, :, 0:3], tmpc[:, 0:48].rearrange("p (c t) -> p c t", c=16)), [tmpcB], [QKPB])
        for nn in range(4):
            for k in range(8):
                mm(kb, PA[:, nn * 512:(nn + 1) * 512], xT[:, k, :], Wv[:, k, nn * 512:(nn + 1) * 512], [WvB, xTB], [BA[nn]],
                   start=(k == 0), stop=(k == 7), inc=(k == 7))
        kb.op("act", lambda e: e.copy(vt[:, 0:1024], PA[:, 0:1024]), [BA[0], BA[1]], [vtB])
        kb.op("dve", lambda e: e.tensor_copy(vt[:, 1024:2048], PA[:, 1024:2048]), [BA[2], BA[3]], [vtB])
        for k in range(8):
            mm(kb, PC[:, 0:8], xT[:, k, :], Wg[:, k, :], [WgB, xTB], [BC], start=(k == 0), stop=(k == 7), inc=(k == 7))
        kb.op("dve", lambda e: e.tensor_tensor(gt[:], PC[:, 0:8], GBt[:], ALU.add), [BC, GBB], [gtB])
        kb.op("act", lambda e: e.activation(lf[:], gt[:, 4:8], AF.Exp, scale=-1.0), [gtB], [lfB])
        kb.op("act", lambda e: e.activation(lf[:], lf[:], AF.Ln, bias=1.0), [lfB], [lfB])
        kb.op("dve", lambda e: e.tensor_scalar(lf[:], lf[:], -1.0, None, ALU.mult), [lfB], [lfB])
        mm(kb, PC[:, 8:12], TRIf[:], lf[:], [lfB, cB], [BC])
        kb.op("dve", lambda e: e.tensor_copy(bcol[:], PC[:, 8:12]), [BC], [bcolB])
        kb.op("dve", lambda e: e.tensor_tensor(ibm[:], gt[:, 0:4], bcol[:], ALU.subtract), [gtB, bcolB], [ibmB])
        for h in range(4):
            par = h % 2
            LFB, LFBB = LFB_2[par]; EB, EBB = EB_2[par]; DT, DTB = DT_2[par]; sT, sTB = sT_2[par]; qh, qhB = qh_2[par]
            kTok, kTokB = kTok_2[par]; vs, vsB = vs_2[par]; wcol, wcolB = wcol_2[par]; hh, hhB = hh_2[par]
            hjunk, hjunkB = hjunk_2[par]; rec, recB = rec_2[par]; ssh, sshB = ssh_2[par]
            pBRW = PA[:, par * 128:par * 128 + 128]; bBRW = BRW[par]
            pBST = PA[:, 512 + par * 128:512 + par * 128 + 128]; bBST = BST[par]
            pDN = PA[:, 1024 + par * 8:1024 + par * 8 + 1]; bDN = BDN[par]
            pNU = PA[:, 1088:1089]
            pNM = PB[:, 0:512] if par == 0 else PA[:, 1536:2048]; bNM = BNM[par]; aNM = BBa if par == 0 else BA[3]
            C, CB_ = Cs[h]
            Cbh, CbB = Cb[h]
            nh, nhB = ns[h]
            nbh, nbB = nb[h]
            kb.op("dve", lambda e: e.tensor_scalar(LFB[:], ONESf[:], lf[:, h:h + 1], None, ALU.mult), [ONESfB, lfB], [LFBB])
            mm(kb, pBRW, LFB[:], TRIf[:], [LFBB, cB, BA[0]], [bBRW])
            kb.op("act", lambda e: e.activation(EB[:], pBRW, AF.Exp), [bBRW], [EBB])
            kb.op("act", lambda e: e.activation(DT[:], pBRW, AF.Exp, bias=ibm[:, h:h + 1]), [bBRW, ibmB], [DTB])
            kb.op("dve", lambda e: e.tensor_copy(wcol[:], DT[:, 127:128]), [DTB], [wcolB])
            kb.op("dve", lambda e: e.tensor_scalar(vs[:], vt[:, h * 512:(h + 1) * 512], DT[:, 127:128], None, ALU.mult), [vtB, DTB], [vsB])
            kb.op("dve", lambda e: e.tensor_tensor(DT[:], DT[:], TRIf[:], ALU.mult), [DTB, cB], [DTB])
            for cc in range(2):
                kb.op("dve", lambda e: e.scalar_tensor_tensor(qh[:, cc, :], qkT[:, 2 * h + cc, :], 0.0625, EB[:], ALU.mult, ALU.mult),
                      [qkTB, EBB], [qhB])
            for cc in range(2):
                mm(kb, pBST, qkT[:, 8 + 2 * h + cc, :], qkT[:, 2 * h + cc, :], [qkTB, BA[1]], [bBST], start=(cc == 0), stop=(cc == 1), inc=(cc == 1))
            kb.op("dve", lambda e: e.scalar_tensor_tensor(sT[:], pBST, 0.0625, DT[:], ALU.mult, ALU.mult), [bBST, DTB], [sTB])
            mm(kb, pNM, sT[:], vt[:, h * 512:(h + 1) * 512], [sTB, vtB, aNM], [bNM], start=True, stop=False, inc=False)
            for cc in range(2):
                mm(kb, pNM, qh[:, cc, :], Cbh[:, cc, :], [qhB, CbB, aNM], [bNM], start=False, stop=(cc == 1), inc=(cc == 1))
            mm(kb, pDN, sT[:], ones[:, 0:1], [sTB, cB, BA[2]], [bDN], start=True, stop=False, inc=False)
            for cc in range(2):
                mm(kb, pDN, qh[:, cc, :], nbh[:, cc:cc + 1], [qhB, nbB, BA[2]], [bDN], start=False, stop=(cc == 1), inc=(cc == 1))
            kb.op("dve", lambda e: e.tensor_scalar(rec[:], pDN, -1.0, None, ALU.mult), [bDN], [recB])
            kb.op("dve", lambda e: e.tensor_tensor(rec[:], rec[:], pDN, ALU.max), [bDN, recB], [recB])
            kb.op("dve", lambda e: e.tensor_scalar(rec[:], rec[:], 1.0, None, ALU.max), [recB], [recB])
            kb.op("dve", lambda e: e.reciprocal(rec[:], rec[:]), [recB], [recB])
            kb.op("dve", lambda e: e.tensor_scalar(hh[:], pNM, rec[:], None, ALU.mult), [bNM, recB], [hhB])
            kb.op("act", lambda e: e.activation(hjunk[:], hh[:], AF.Square, accum_out=ssh[:]), [hhB], [hjunkB, sshB])
            kb.op("dve", lambda e: e.tensor_scalar(ssh[:], ssh[:], 1.0 / 512, EPS, ALU.mult, ALU.add), [sshB], [sshB])
            kb.op("act", lambda e: e.activation(ssh[:], ssh[:], AF.Sqrt), [sshB], [sshB])
            kb.op("dve", lambda e: e.reciprocal(ssh[:], ssh[:]), [sshB], [sshB])
            kb.op("dve", lambda e: e.scalar_tensor_tensor(HN[:, h * 512:(h + 1) * 512], hh[:], ssh[:], HG[:, h * 512:(h + 1) * 512],
                                                           ALU.mult, ALU.mult), [hhB, sshB, HGB], [HNB])
            for cc in range(2):
                tr(kb, P16[:, cc * 128:(cc + 1) * 128], qkT[:, 8 + 2 * h + cc, :], identB[:], [qkTB, cB], [B16], inc=(cc == 1))
            kb.op("act", lambda e: e.copy(kTok[:], P16[:, 0:256]), [B16], [kTokB])
            for cc in range(2):
                mm(kb, PB[:, 512:1024], kTok[:, cc * 128:(cc + 1) * 128], vs[:], [kTokB, vsB], [BBb])
                kb.op("dve", lambda e: e.scalar_tensor_tensor(C[:, cc, :], C[:, cc, :], EB[:, 127:128], PB[:, 512:1024], ALU.mult, ALU.add),
                      [CB_, EBB, BBb], [CB_])
                mm(kb, pNU, kTok[:, cc * 128:(cc + 1) * 128], wcol[:], [kTokB, wcolB, BA[2]], [BNU])
                kb.op("dve", lambda e: e.scalar_tensor_tensor(nh[:, cc:cc + 1], nh[:, cc:cc + 1], EB[:, 127:128], pNU, ALU.mult, ALU.add),
                      [nhB, EBB, BNU], [nhB])
            kb.op("act", lambda e: e.copy(Cbh[:].rearrange("p c v -> p (c v)"), C[:].rearrange("p c v -> p (c v)")), [CB_], [CbB])
            kb.op("dve", lambda e: e.tensor_copy(nbh[:], nh[:]), [nhB], [nbB])
        kb.dma(hn[i * 128:(i + 1) * 128, :], HN[:], [HNB], [hnB])
    kb.es = kb_es
    return es


def emit_layer1b(nc, kb, dr, x_in, xinB, hn, hnB, x_out, xoutB, cst, nblk=NB):
    es = ExitStack()
    kb_es = kb.es
    kb.es = es
    sb = kb.sb
    identB, identF, cB = cst["identB"], cst["identF"], cst["buf"]
    win = dr["b_w_in"].rearrange("(k p) n -> p k n", p=128)
    Wo, WoB = sb("Wo", [128, 8, 2048], BF16)
    Wz, WzB = sb("Wz", [128, 8, 2048], BF16)
    Wout, WoutB = sb("Wout1", [128, 16, 1024], BF16)
    wout = dr["b_w_out"].rearrange("(k p) n -> p k n", p=128)
    for k in range(0, 8, 4):
        kb.dma(Wo[:, k:k + 4, :], win[:, k:k + 4, 4104:6152], [], [WoB], q="pool")
        kb.dma(Wz[:, k:k + 4, :], win[:, k:k + 4, 6152:8200], [], [WzB], q="pool")
    for k in range(0, 16, 8):
        kb.dma(Wout[:, k:k + 8, :], wout[:, k:k + 8, :], [], [WoutB], q="pool")
    LG, LGB = sb("LG1", [128, 1024], F32)
    LB, LBB = sb("LB1", [128, 1024], F32)
    kb.dma(LG[:], dr["b_ln_g"].partition_broadcast(128), [], [LGB])
    kb.dma(LB[:], dr["b_ln_b"].partition_broadcast(128), [], [LBB])
    PA = kb.ps("L2PA", [128, 2048], F32)
    PB = kb.ps("L2PB", [128, 1024], F32)
    P16 = kb.ps("L2P16", [128, 2048], BF16)
    BA = [kb.buf("qa%d" % i) for i in range(4)]
    BBa, BBb, B16 = kb.buf("qba"), kb.buf("qbb"), kb.buf("q16")
    xf, xfB = sb("xf2", [128, 1024], F32)
    xT, xTB = sb("xT2", [128, 8, 128], BF16)
    so, soB = sb("so", [128, 2048], BF16)
    hnb, hnbB = sb("hnb", [128, 2048], BF16)
    hg, hgB = sb("hg", [128, 2048], BF16)
    hgT, hgTB = sb("hgT", [128, 16, 128], BF16)
    rr, rrB = sb("rr2", [128, 1024], F32)
    st = {"s1": sb("s1b", [128, 1], F32), "ss": sb("ssb", [128, 1], F32), "lnjunk": sb("lnjunkb", [128, 1024], BF16)}
    for i in range(nblk):
        load_xT(kb, x_in, xinB, i, xf, xfB, xT, xTB, PB, [BBa, BBb], identF, cB)
        kb.dma(hnb[:], hn[i * 128:(i + 1) * 128, :], [hnB], [hnbB])
        for nn in range(4):
            for k in range(8):
                mm(kb, PA[:, nn * 512:(nn + 1) * 512], xT[:, k, :], Wo[:, k, nn * 512:(nn + 1) * 512], [WoB, xTB], [BA[nn]],
                   start=(k == 0), stop=(k == 7), inc=(k == 7))
        kb.op("act", lambda e: e.activation(so[:], PA[:], AF.Sigmoid), BA, [soB])
        kb.op("dve", lambda e: e.tensor_tensor(hg[:], hnb[:], so[:], ALU.mult), [hnbB, soB], [hgB])
        for nn in range(4):
            for k in range(8):
                mm(kb, PA[:, nn * 512:(nn + 1) * 512], xT[:, k, :], Wz[:, k, nn * 512:(nn + 1) * 512], [WzB, xTB], [BA[nn]],
                   start=(k == 0), stop=(k == 7), inc=(k == 7))
        kb.op("act", lambda e: e.activation(so[:], PA[:], AF.Silu), BA, [soB])
        kb.op("dve", lambda e: e.tensor_tensor(hg[:], hg[:], so[:], ALU.mult), [hgB, soB], [hgB])
        for k in range(16):
            tr(kb, P16[:, k * 128:(k + 1) * 128], hg[:, k * 128:(k + 1) * 128], identB[:], [hgB, cB], [B16], inc=(k == 15))
        kb.op("act", lambda e: e.copy(hgT[:].rearrange("p k t -> p (k t)"), P16[:]), [B16], [hgTB])
        for nn in range(2):
            for k in range(16):
                mm(kb, PB[:, nn * 512:(nn + 1) * 512], hgT[:, k, :], Wout[:, k, nn * 512:(nn + 1) * 512], [hgTB, WoutB],
                   [BBa if nn == 0 else BBb], start=(k == 0), stop=(k == 15), inc=(k == 15))
        kb.op("dve", lambda e: e.scalar_tensor_tensor(rr[:], xf[:], ALPHA, PB[:], ALU.mult, ALU.add), [xfB, BBa, BBb], [rrB])
        layernorm_store(kb, st, rr[:], rrB, x_out[i * 128:(i + 1) * 128, :], xoutB, LG, LB, [LGB, LBB])
    kb.es = kb_es
    return es


def make_consts():
    identF = np.eye(128, dtype=np.float32)
    q = np.arange(128)[:, None]
    s = np.arange(128)[None, :]
    CM = np.where(s <= q, 0.0, -1e30).astype(np.float32)
    slopes = 2.0 ** (-(np.arange(1, 9, dtype=np.float64)))
    sp = np.arange(128, dtype=np.float64)[:, None, None]
    dd = np.arange(32, dtype=np.float64)[None, None, :]
    BIAS = (slopes[None, :, None] * (sp - 127.0 - 128.0 * dd)).reshape(128, 256).astype(np.float32)
    misc = np.zeros((128, 104), np.float32)
    misc[:, 0:32] = np.arange(1, 33, dtype=np.float32)[None, :]
    misc[:, 32:40] = (128.0 * slopes)[None, :]
    misc[:, 40:104] = np.eye(8, dtype=np.float32).reshape(1, 64)
    tri = (np.arange(128)[:, None] <= np.arange(128)[None, :]).astype(np.float32)
    return {"c_ident": identF, "c_cm": CM, "c_bias": BIAS, "c_misc": misc, "c_tri": tri}


def build(layers=LAYERS, nblk=NB):
    nc = bass.Bass("TRN2", target_bir_lowering=False)
    dr = {}

    def din(name, shape):
        dr[name] = nc.dram_tensor(name, list(shape), F32, kind="ExternalInput").ap()

    din("x", [T, D])
    din("a_w_in", [D, A_IN]); din("a_kv_norm_g", [1, 256]); din("a_w_uk", [8, 128, 256]); din("a_w_uv", [8, 256, 128])
    din("a_w_out", [D, D]); din("a_ln_g", [1, D]); din("a_ln_b", [1, D])
    din("c_ident", [128, 128]); din("c_cm", [128, 128]); din("c_bias", [128, 256]); din("c_misc", [128, 104]); din("c_tri", [128, 128])
    din("b_w_in", [D, B_IN]); din("b_i_bias", [1, 4]); din("b_f_bias", [1, 4]); din("b_conv_w", [4, 2048]); din("b_conv_b", [1, 2048])
    din("b_head_norm_g", [1, 4, 512]); din("b_w_out", [2048, D]); din("b_ln_g", [1, D]); din("b_ln_b", [1, D])
    out = nc.dram_tensor("out", [T, D], F32, kind="ExternalOutput").ap()
    with ExitStack() as es:
        kb = KB(nc, es)
        identF, idB = kb.sb("identF", [128, 128], F32)
        identB, _ = kb.sb("identB", [128, 128], BF16)
        CM, _ = kb.sb("CM", [128, 128], F32)
        BIAS, _ = kb.sb("BIAS", [128, 256], F32)
        ones, _ = kb.sb("ones", [128, 128], BF16)
        CMISC, _ = kb.sb("CMISC", [128, 104], F32)
        TRIf, _ = kb.sb("TRIf", [128, 128], F32)
        cB = idB
        kb.dma(identF[:], dr["c_ident"], [], [cB])
        kb.dma(identB[:], dr["c_ident"], [], [cB], q="pool")
        kb.dma(CM[:], dr["c_cm"], [], [cB])
        kb.dma(BIAS[:], dr["c_bias"], [], [cB])
        kb.dma(CMISC[:], dr["c_misc"], [], [cB])
        kb.dma(TRIf[:], dr["c_tri"], [], [cB])
        kb.op("dve", lambda e: e.memset(ones[:], 1.0), [], [cB])
        cst = {"identB": identB, "identF": identF, "CM": CM, "BIAS": BIAS, "ones": ones, "buf": cB, "CMISC": CMISC, "TRIf": TRIf}
        xinB = kb.buf("xin")
        outB = kb.buf("out")
        x1d = nc.dram_tensor("x1d", [T, D], F32).ap()
        hnd = nc.dram_tensor("hnd", [T, 2048], BF16).ap()
        x1B = kb.buf("x1d")
        hnB = kb.buf("hnd")
        if layers == (0,):
            l0 = emit_layer0(nc, kb, dr, dr["x"], xinB, out, outB, cst, nblk=nblk)
            kb.finish(); l0.close()
        elif layers == (1,):
            la = emit_layer1a(nc, kb, dr, dr["x"], xinB, hnd, hnB, cst, nblk=nblk)
            kb.barrier(); la.close()
            lb = emit_layer1b(nc, kb, dr, dr["x"], xinB, hnd, hnB, out, outB, cst, nblk=nblk)
            kb.finish(); lb.close()
        else:
            l0 = emit_layer0(nc, kb, dr, dr["x"], xinB, x1d, x1B, cst, nblk=nblk)
            kb.barrier(); l0.close()
            la = emit_layer1a(nc, kb, dr, x1d, x1B, hnd, hnB, cst, nblk=nblk)
            kb.barrier(); la.close()
            lb = emit_layer1b(nc, kb, dr, x1d, x1B, hnd, hnB, out, outB, cst, nblk=nblk)
            kb.finish(); lb.close()
    return nc


_CACHE = {}


def make_inmaps(inputs, xs):
    cst = make_consts()
    shared = {k: np.ascontiguousarray(inputs[k][0], dtype=np.float32) for k in
              ("a_w_in", "a_w_uk", "a_w_uv", "a_w_out", "b_w_in", "b_conv_w", "b_w_out")}
    for k in ("a_kv_norm_g", "a_ln_g", "a_ln_b", "b_i_bias", "b_f_bias", "b_conv_b", "b_ln_g", "b_ln_b"):
        shared[k] = np.ascontiguousarray(inputs[k], dtype=np.float32).reshape(1, -1)
    shared["b_head_norm_g"] = np.ascontiguousarray(inputs["b_head_norm_g"], dtype=np.float32).reshape(1, 4, 512)
    shared.update(cst)
    maps = []
    for xx in xs:
        m = dict(shared)
        m["x"] = np.ascontiguousarray(xx, dtype=np.float32)
        maps.append(m)
    return maps


def kernel(**inputs):
    x = np.ascontiguousarray(inputs["x"], dtype=np.float32)
    if "nc" not in _CACHE:
        import os
        lay = os.environ.get("K_LAYERS")
        _CACHE["nc"] = build(layers=tuple(int(c) for c in lay)) if lay else build()
    nc = _CACHE["nc"]
    in_maps = make_inmaps(inputs, [x[c % 4] for c in range(8)])
    res = run_bass_kernel_spmd(nc, in_maps, core_ids=list(range(8)))
    return np.stack([res.results[c]["out"] for c in range(4)], axis=0)
```

```python
import numpy as np
from contextlib import ExitStack
import concourse.bass as bass
import concourse.mybir as mybir
from concourse.bass_utils import run_bass_kernel_spmd

F32 = mybir.dt.float32
BF16 = mybir.dt.bfloat16
AF = mybir.ActivationFunctionType
ALU = mybir.AluOpType
AX = mybir.AxisListType

T = 4096
D = 1024
NB = T // 128
A_IN = 2888
B_IN = 8200
ALPHA = float(4 ** 0.25)
EPS = 1e-5
NDMA = 12
BIS_STEPS = 28
BIS_LO = -2048.0
LAYERS = (0, 1)


class Buf:
    __slots__ = ("name", "w", "r")

    def __init__(self, name):
        self.name = name
        self.w = None
        self.r = {}


class KB:
    def __init__(self, nc, es):
        self.nc = nc
        self.es = es
        self.eng = {"pe": nc.tensor, "act": nc.scalar, "dve": nc.vector, "pool": nc.gpsimd, "sp": nc.sync}
        self.sem = {e: es.enter_context(nc.semaphore("s_" + e)) for e in ("pe", "act", "dve", "pool")}
        self.cnt = {e: 0 for e in self.sem}
        self.known = {e: {} for e in self.eng}
        self.dsem = [es.enter_context(nc.semaphore("d%d" % k)) for k in range(NDMA)]
        self.dtot = [0] * NDMA
        self.dnext = 0
        self.nbuf = 0
        self.root_es = es
        self.xsem = {}

    def buf(self, name=None):
        self.nbuf += 1
        return Buf(name or ("b%d" % self.nbuf))

    def sb(self, name, shape, dt):
        t = self.es.enter_context(self.nc.sbuf_tensor(name, list(shape), dt))
        return t, Buf(name)

    def ps(self, name, shape, dt):
        return self.es.enter_context(self.nc.psum_tensor(name, list(shape), dt))

    def _deps(self, reads, writes):
        toks = []
        for b in reads:
            if b.w is not None:
                toks.append(b.w)
        for b in writes:
            if b.w is not None:
                toks.append(b.w)
            toks.extend(b.r.items())
        return toks

    def _wait(self, e, toks):
        need = {}
        for k, v in toks:
            if k == e and e == "pe":
                continue
            if self.known[e].get(k, 0) >= v:
                continue
            if need.get(k, 0) < v:
                need[k] = v
        for k, v in need.items():
            sem = self.sem[k] if isinstance(k, str) else (self.dsem[k[1]] if k[0] == "d" else self.xsem[k])
            self.eng[e].wait_ge(sem, v)
            self.known[e][k] = v

    def _mark(self, tok, reads, writes):
        k, v = tok
        for b in reads:
            if b.r.get(k, 0) < v:
                b.r[k] = v
        for b in writes:
            b.w = tok
            b.r = {}

    def op(self, e, fn, reads=(), writes=(), inc=True):
        self._wait(e, self._deps(reads, writes))
        ins = fn(self.eng[e])
        if inc:
            self.cnt[e] += 1
            ins.then_inc(self.sem[e], 1)
            tok = (e, self.cnt[e])
        else:
            assert e == "pe"
            tok = (e, self.cnt[e] + 1)
        self._mark(tok, reads, writes)
        return tok

    def dma(self, out, in_, reads=(), writes=(), q="sp"):
        toks = self._deps(reads, writes)
        if q == "pool":
            key = ("x", len(self.xsem))
            self.xsem[key] = self.root_es.enter_context(self.nc.semaphore("x%d" % len(self.xsem)))
            self._wait(q, toks)
            self.eng[q].dma_start(out=out, in_=in_).then_inc(self.xsem[key], 16)
            tok = (key, 16)
            self._mark(tok, reads, writes)
            return tok
        k = self.dnext
        self.dnext = (k + 1) % NDMA
        if self.dtot[k] > 0:
            toks.append((("d", k), self.dtot[k]))
        self._wait(q, toks)
        self.dtot[k] += 16
        self.eng[q].dma_start(out=out, in_=in_).then_inc(self.dsem[k], 16)
        tok = (("d", k), self.dtot[k])
        self._mark(tok, reads, writes)
        return tok

    def barrier(self):
        toks = [(("d", k), self.dtot[k]) for k in range(NDMA) if self.dtot[k] > 0]
        toks += [(k, 16) for k in self.xsem]
        toks += [(e, c) for e, c in self.cnt.items() if c > 0]
        for e in self.eng:
            self._wait(e, toks)

    def finish(self):
        toks = [(("d", k), self.dtot[k]) for k in range(NDMA) if self.dtot[k] > 0]
        toks += [(k, 16) for k in self.xsem]
        toks += [(e, c) for e, c in self.cnt.items() if c > 0]
        self._wait("sp", toks)


def mm(kb, out, lhsT, rhs, reads, writes, start=True, stop=True, inc=None):
    if inc is None:
        inc = stop
    return kb.op("pe", lambda e: e.matmul(out, lhsT, rhs, start=start, stop=stop), reads, writes, inc=inc)


def tr(kb, out, in_, ident, reads, writes, inc=True):
    return kb.op("pe", lambda e: e.transpose(out, in_, ident), reads, writes, inc=inc)


def layernorm_store(kb, st, r_ap, rB, out_dram_ap, outB, G, Bt, GB):
    s1, s1B = st["s1"]
    kb.op("dve", lambda e: e.reduce_sum(s1[:], r_ap, AX.X), [rB], [s1B])
    kb.op("dve", lambda e: e.tensor_scalar(s1[:], s1[:], -1.0 / D, None, ALU.mult), [s1B], [s1B])
    kb.op("dve", lambda e: e.tensor_scalar(r_ap, r_ap, s1[:], None, ALU.add), [rB, s1B], [rB])
    junk, junkB = st["lnjunk"]
    ss, ssB = st["ss"]
    kb.op("act", lambda e: e.activation(junk[:], r_ap, AF.Square, accum_out=ss[:]), [rB], [junkB, ssB])
    kb.op("dve", lambda e: e.tensor_scalar(ss[:], ss[:], 1.0 / D, EPS, ALU.mult, ALU.add), [ssB], [ssB])
    kb.op("act", lambda e: e.activation(ss[:], ss[:], AF.Sqrt), [ssB], [ssB])
    kb.op("dve", lambda e: e.reciprocal(ss[:], ss[:]), [ssB], [ssB])
    kb.op("dve", lambda e: e.scalar_tensor_tensor(r_ap, r_ap, ss[:], G[:], ALU.mult, ALU.mult), [rB, ssB] + list(GB), [rB])
    kb.op("dve", lambda e: e.tensor_tensor(r_ap, r_ap, Bt[:], ALU.add), [rB] + list(GB), [rB])
    kb.dma(out_dram_ap, r_ap, [rB], [outB])


def emit_layer0(nc, kb, dr, x_in, xinB, x_out, xoutB, cst, nblk=NB):
    es = ExitStack()
    kb_es = kb.es
    kb.es = es
    sb = kb.sb
    identB, identF, CM, BIAS, ones, cB = cst["identB"], cst["identF"], cst["CM"], cst["BIAS"], cst["ones"], cst["buf"]

    W0, W0B = sb("W0", [128, 8, A_IN], BF16)
    Wk2, Wk2B = sb("Wk2", [128, 8, 128], BF16)
    Wuk, WukB = sb("Wuk", [128, 8, 256], BF16)
    Wuv, WuvB = sb("Wuv", [128, 8, 2, 128], BF16)
    Wout, WoutB = sb("Wout", [128, 8, 1024], BF16)
    Gkv, GkvB = sb("Gkv", [128, 256], F32)
    LG, LGB = sb("LG", [128, 1024], F32)
    LB, LBB = sb("LB", [128, 1024], F32)
    win = dr["a_w_in"].rearrange("(k p) n -> p k n", p=128)
    for k in range(0, 8, 4):
        kb.dma(W0[:, k:k + 4, :], win[:, k:k + 4, :], [], [W0B], q="pool")
    kb.dma(Wk2[:, :, 0:64], win[:, :, 1792:1856], [], [Wk2B], q="pool")
    kb.dma(Wk2[:, :, 64:128], win[:, :, 1792:1856], [], [Wk2B], q="pool")
    kb.dma(Wuk[:], dr["a_w_uk"].rearrange("h d c -> d h c"), [], [WukB], q="pool")
    kb.dma(Wuv[:], dr["a_w_uv"].rearrange("h (cc p) d -> p h cc d", p=128), [], [WuvB], q="pool")
    kb.dma(Wout[:], dr["a_w_out"].rearrange("(k p) n -> p k n", p=128), [], [WoutB], q="pool")
    kb.dma(Gkv[:], dr["a_kv_norm_g"].partition_broadcast(128), [], [GkvB])
    kb.dma(LG[:], dr["a_ln_g"].partition_broadcast(128), [], [LGB])
    kb.dma(LB[:], dr["a_ln_b"].partition_broadcast(128), [], [LBB])

    kT2, kT2B = sb("kT2", [128, T], BF16)
    cN, cNB = sb("cN", [128, NB, 256], BF16)
    cT, cTB = sb("cT", [128, 2, T], BF16)
    SC, SCB = sb("SC", [128, T], F32)
    M, MB = sb("M", [128, T], BF16)
    MT, MTB = sb("MT", [128, NB, 128], BF16)
    XF = [sb("XF%d" % s, [128, 1024], F32) for s in range(1)]
    xT, xTB = sb("xT", [128, 8, 128], BF16)
    qT, qTB = sb("qT", [128, 8, 128], BF16)
    qiT, qiTB = sb("qiT", [128, 4, 128], BF16)
    qlT, qlTB = sb("qlT", [128, 2, 1024], BF16)
    sz, szB = sb("sz", [128, 1024], BF16)
    cf, cfB = sb("cf", [128, 256], F32)
    wsb, wsbB = sb("wsb", [128, 8], F32)
    RR = [sb("R%d" % s, [128, 512], F32) for s in range(3)]
    PT = [sb("PT%d" % s, [128, 8, 128], BF16) for s in range(2)]
    olT, olTB = sb("olT", [128, 2, 1024], BF16)
    og, ogB = sb("og", [128, 1024], BF16)
    ogT, ogTB = sb("ogT", [128, 8, 128], BF16)
    rr, rrB = sb("rr", [128, 1024], F32)
    rden, rdenB = sb("rden", [128, 8], F32)
    dens, densB = sb("dens", [128, 8], F32)
    bh, bhB = sb("bh", [128, 32], F32)
    jm, jmB = sb("jm", [128, 1], F32)
    VH, VHB = sb("VH", [128, 8, 1], BF16)
    VHD, VHDB = sb("VHD", [128, 8, 8], BF16)
    SH, SHB = sb("SH", [8, 1024], BF16)
    CMISC = cst["CMISC"]
    OLs, OLsB = sb("OLs", [128, 2, 1024], F32)
    lo, loB = sb("lo", [128, 1], F32)
    mid, midB = sb("mid", [128, 1], F32)
    cnt, cntB = sb("cnt", [128, 1], F32)
    cntA, cntAB = sb("cntA", [128, 1], F32)
    MBa = kb.buf("MBa")
    ge, geB = sb("ge", [128, 1], F32)
    st = {"s1": sb("s1", [128, 1], F32), "ss": sb("ss", [128, 1], F32), "lnjunk": (og, ogB)}
    ssc, sscB = sb("ssc", [128, 1], F32)
    cjunk, cjunkB = sb("cjunk", [128, 256], BF16)

    P0 = kb.ps("P0", [128, 1024], F32)
    P1 = kb.ps("P1", [128, 1024], F32)
    P2 = kb.ps("P2", [128, 1024], F32)
    PX = kb.ps("PX", [128, 512], F32)
    P16 = kb.ps("P16", [128, 1024], BF16)
    B0a, B0b, B1a, B1b, B2a, B2b, BX, B16 = [kb.buf("ps%d" % i) for i in range(8)]

    scale_q = float(128 ** -0.5)

    for i in range(nblk):
        n = 128 * (i + 1)
        xf, xfB = XF[0]
        kb.dma(xf[:], x_in[i * 128:(i + 1) * 128, :], [xinB], [xfB])
        import os
        stage = float(os.environ.get("K_STAGE", "99")) if i == 0 else float(os.environ.get("K_STAGE1", os.environ.get("K_STAGE", "99")))
        if stage <= 0:
            kb.dma(x_out[i * 128:(i + 1) * 128, :], xf[:], [xfB], [xoutB])
            continue
        for k in range(8):
            tr(kb, P0[:, k * 128:(k + 1) * 128], xf[:, k * 128:(k + 1) * 128], identF[:], [xfB, cB], [B0a if k < 4 else B0b], inc=(k == 7))
        kb.op("act", lambda e: e.copy(xT[:].rearrange("p k t -> p (k t)"), P0[:]), [B0a, B0b], [xTB])
        if stage <= 0.1:
            kb.op("dve", lambda e: e.memset(rr[:], 0.0), [], [rrB])
            kb.op("dve", lambda e: e.tensor_copy(rr[:, 0:128], xT[:, 0, :]), [xTB], [rrB])
            kb.dma(x_out[i * 128:(i + 1) * 128, :], rr[:], [rrB], [xoutB])
            continue
        for h in range(8):
            for k in range(8):
                mm(kb, P1[:, h * 128:(h + 1) * 128], W0[:, k, h * 128:(h + 1) * 128], xT[:, k, :], [W0B, xTB],
                   [B1a if h < 4 else B1b], start=(k == 0), stop=(k == 7), inc=(k == 7 and h == 7))
        kb.op("dve", lambda e: e.tensor_copy(qT[:].rearrange("p h t -> p (h t)"), P1[:]), [B1a, B1b], [qTB])
        if stage <= 0.2:
            kb.op("dve", lambda e: e.memset(rr[:], 0.0), [], [rrB])
            kb.op("dve", lambda e: e.tensor_copy(rr[:, 0:128], xT[:, 0, :]), [xTB], [rrB])
            kb.dma(x_out[i * 128:(i + 1) * 128, :], rr[:], [rrB], [xoutB])
            continue
        for c in range(4):
            for k in range(8):
                mm(kb, P2[:, c * 128:(c + 1) * 128], W0[:, k, 1280 + c * 128:1280 + (c + 1) * 128], xT[:, k, :],
                   [W0B, xTB], [B2a], start=(k == 0), stop=(k == 7), inc=(k == 7 and c == 3))
        kb.op("act", lambda e: e.copy(qiT[:].rearrange("p c t -> p (c t)"), P2[:, 0:512]), [B2a], [qiTB])
        if stage <= 0.25:
            kb.op("dve", lambda e: e.memset(rr[:], 0.0), [], [rrB])
            kb.op("dve", lambda e: e.tensor_copy(rr[:, 0:128], xT[:, 0, :]), [xTB], [rrB])
            kb.dma(x_out[i * 128:(i + 1) * 128, :], rr[:], [rrB], [xoutB])
            continue
        for k in range(8):
            mm(kb, P2[:, 512:640], Wk2[:, k, :], xT[:, k, :], [Wk2B, xTB], [B2b], start=(k == 0), stop=(k == 7), inc=False)
        for k in range(8):
            mm(kb, P2[:, 640:896], xT[:, k, :], W0[:, k, 1024:1280], [W0B, xTB], [B2b], start=(k == 0), stop=(k == 7), inc=False)
        for k in range(8):
            mm(kb, P2[:, 896:960], xT[:, k, :], W0[:, k, 1856:1920], [W0B, xTB], [B2b], start=(k == 0), stop=(k == 7), inc=(k == 7))
        if stage <= 0.27:
            kb.op("dve", lambda e: e.memset(rr[:], 0.0), [], [rrB])
            kb.op("dve", lambda e: e.tensor_copy(rr[:, 0:128], xT[:, 0, :]), [xTB], [rrB])
            kb.dma(x_out[i * 128:(i + 1) * 128, :], rr[:], [rrB], [xoutB])
            continue
        kb.op("dve", lambda e: e.tensor_copy(kT2[:, i * 128:(i + 1) * 128], P2[:, 512:640]), [B2b], [kT2B])
        if stage <= 0.28:
            kb.op("dve", lambda e: e.memset(rr[:], 0.0), [], [rrB])
            kb.op("dve", lambda e: e.tensor_copy(rr[:, 0:128], xT[:, 0, :]), [xTB], [rrB])
            kb.dma(x_out[i * 128:(i + 1) * 128, :], rr[:], [rrB], [xoutB])
            continue
        kb.op("dve", lambda e: e.tensor_copy(wsb[:], P2[:, 896:904]), [B2b], [wsbB])
        if stage <= 0.29:
            kb.op("dve", lambda e: e.memset(rr[:], 0.0), [], [rrB])
            kb.op("dve", lambda e: e.tensor_copy(rr[:, 0:128], xT[:, 0, :]), [xTB], [rrB])
            kb.dma(x_out[i * 128:(i + 1) * 128, :], rr[:], [rrB], [xoutB])
            continue
        kb.op("dve", lambda e: e.tensor_copy(cf[:], P2[:, 640:896]), [B2b], [cfB])
        if stage <= 0.3:
            kb.op("dve", lambda e: e.memset(rr[:], 0.0), [], [rrB])
            kb.op("dve", lambda e: e.tensor_copy(rr[:, 0:128], xT[:, 0, :]), [xTB], [rrB])
            kb.dma(x_out[i * 128:(i + 1) * 128, :], rr[:], [rrB], [xoutB])
            continue
        for nn in range(2):
            for k in range(8):
                mm(kb, P0[:, nn * 512:(nn + 1) * 512], xT[:, k, :], W0[:, k, 1864 + nn * 512:1864 + (nn + 1) * 512],
                   [W0B, xTB], [B0a if nn == 0 else B0b], start=(k == 0), stop=(k == 7), inc=(k == 7))
        kb.op("act", lambda e: e.activation(sz[:], P0[:], AF.Silu), [B0a, B0b], [szB])
        if stage <= 0.4:
            kb.op("dve", lambda e: e.memset(rr[:], 0.0), [], [rrB])
            kb.op("dve", lambda e: e.tensor_copy(rr[:, 0:128], xT[:, 0, :]), [xTB], [rrB])
            kb.dma(x_out[i * 128:(i + 1) * 128, :], rr[:], [rrB], [xoutB])
            continue
        kb.op("act", lambda e: e.activation(cjunk[:], cf[:], AF.Square, accum_out=ssc[:]), [cfB], [cjunkB, sscB])
        kb.op("dve", lambda e: e.tensor_scalar(ssc[:], ssc[:], 1.0 / 256, EPS, ALU.mult, ALU.add), [sscB], [sscB])
        kb.op("act", lambda e: e.activation(ssc[:], ssc[:], AF.Sqrt), [sscB], [sscB])
        kb.op("dve", lambda e: e.reciprocal(ssc[:], ssc[:]), [sscB], [sscB])
        kb.op("dve", lambda e: e.scalar_tensor_tensor(cN[:, i, :], cf[:], ssc[:], Gkv[:], ALU.mult, ALU.mult),
              [cfB, sscB, GkvB], [cNB])
        for cc in range(2):
            tr(kb, P16[:, cc * 128:(cc + 1) * 128], cN[:, i, cc * 128:(cc + 1) * 128], identB[:], [cNB, cB], [B16], inc=(cc == 1))
        kb.op("dve", lambda e: e.tensor_copy(cT[:, :, i * 128:(i + 1) * 128], P16[:, 0:256].rearrange("p (c t) -> p c t", c=2)),
              [B16], [cTB])
        if stage <= 0.6:
            kb.op("dve", lambda e: e.memset(rr[:], 0.0), [], [rrB])
            kb.op("dve", lambda e: e.tensor_copy(rr[:, 0:128], xT[:, 0, :]), [xTB], [rrB])
            kb.dma(x_out[i * 128:(i + 1) * 128, :], rr[:], [rrB], [xoutB])
            continue
        for cc in range(2):
            Pq = P0 if cc == 0 else P1
            for h in range(8):
                bq = (B0a, B0b, B1a, B1b)[cc * 2 + (h // 4)]
                mm(kb, Pq[:, h * 128:(h + 1) * 128], Wuk[:, h, cc * 128:(cc + 1) * 128], qT[:, h, :], [WukB, qTB], [bq],
                   inc=(h == 7))
        kb.op("act", lambda e: e.activation(qlT[:, 0, :], P0[:], AF.Copy, scale=scale_q), [B0a, B0b], [qlTB])
        kb.op("dve", lambda e: e.tensor_scalar(qlT[:, 1, :], P1[:], scale_q, None, ALU.mult), [B1a, B1b], [qlTB])

        if stage <= 1:
            kb.op("dve", lambda e: e.tensor_copy(rr[:, 0:256], cN[:, i, :]), [cNB], [rrB])
            kb.op("dve", lambda e: e.tensor_copy(rr[:, 256:1024], qlT[:, 0, 0:768]), [qlTB], [rrB])
            kb.dma(x_out[i * 128:(i + 1) * 128, :], rr[:], [rrB], [xoutB])
            continue
        nkc = (n + 511) // 512
        slots = [(PX, BX, slice(0, 512)), (P2, B2a, slice(0, 512)), (P2, B2b, slice(512, 1024))]
        u = 0
        for kc in range(nkc):
            wd = min(512, n - 512 * kc)
            c0 = kc * 512
            for h in range(8):
                Pt, Bt, sl = slots[u % 3]
                R, RB = RR[u % 3]
                u += 1
                pr = slice((h % 2) * 64, (h % 2) * 64 + 64)
                mm(kb, Pt[:, sl.start:sl.start + wd], qiT[pr, h // 2, :], kT2[pr, c0:c0 + wd], [qiTB, kT2B], [Bt])
                kb.op("act", lambda e: e.activation(R[:, 0:wd], Pt[:, sl.start:sl.start + wd], AF.Relu), [Bt], [RB])
                if h == 0:
                    kb.op("dve", lambda e: e.tensor_scalar(SC[:, c0:c0 + wd], R[:, 0:wd], wsb[:, 0:1], None, ALU.mult),
                          [RB, wsbB], [SCB])
                else:
                    kb.op("dve", lambda e: e.scalar_tensor_tensor(SC[:, c0:c0 + wd], R[:, 0:wd], wsb[:, h:h + 1], SC[:, c0:c0 + wd],
                                                                   ALU.mult, ALU.add), [RB, wsbB, SCB], [SCB])
        kb.op("dve", lambda e: e.tensor_tensor(SC[:, i * 128:n], SC[:, i * 128:n], CM[:], ALU.add), [SCB, cB], [SCB])

        MBall = [MB, MBa]
        if i >= 2:
            nd = 128 * max(1, int(round(0.45 * (i + 1))))
            na = n - nd
            kb.op("dve", lambda e: e.memset(mid[:], 0.0), [], [midB])
            for s in range(BIS_STEPS):
                wk = float(-BIS_LO / (2 ** s))
                last = (s == BIS_STEPS - 1)
                kb.op("dve", lambda e: e.tensor_scalar(M[:, 0:nd], SC[:, 0:nd], mid[:], None, ALU.is_ge, ALU.add, accum_out=cnt[:]),
                      [SCB, midB], [MB, cntB])
                kb.op("act", lambda e: e.activation(M[:, nd:n], SC[:, nd:n], AF.Sign, bias=mid[:], scale=-1.0, accum_out=cntA[:]),
                      [SCB, midB], [MBa, cntAB])
                kb.op("dve", lambda e: e.scalar_tensor_tensor(ge[:], cnt[:], 2.0, cntA[:], ALU.mult, ALU.subtract), [cntB, cntAB], [geB])
                kb.op("dve", lambda e: e.tensor_scalar(ge[:], ge[:], float(511 - na), wk, ALU.is_ge, ALU.mult), [geB], [geB])
                if not last:
                    kb.op("dve", lambda e: e.scalar_tensor_tensor(mid[:], ge[:], -0.5 * wk, mid[:], ALU.add, ALU.add), [geB, midB], [midB])
                else:
                    kb.op("dve", lambda e: e.scalar_tensor_tensor(lo[:], ge[:], -wk, mid[:], ALU.add, ALU.add), [geB, midB], [loB])
            kb.op("dve", lambda e: e.tensor_scalar(M[:, 0:n], SC[:, 0:n], lo[:], -30000.0, ALU.is_lt, ALU.mult), [SCB, loB], MBall)
        else:
            kb.op("dve", lambda e: e.tensor_scalar(M[:, 0:n], SC[:, 0:n], -1e29, -30000.0, ALU.is_lt, ALU.mult), [SCB], MBall)
        nbk = i + 1
        kb.op("dve", lambda e: e.tensor_reduce(bh[:, 0:nbk], M[:, 0:n].rearrange("p (j s) -> p j s", s=128), AX.X, ALU.max), MBall, [bhB])
        kb.op("dve", lambda e: e.tensor_tensor(bh[:, 0:nbk], bh[:, 0:nbk], CMISC[:, 0:nbk], ALU.add), [bhB, cB], [bhB])
        kb.op("dve", lambda e: e.reduce_max(jm[:], bh[:, 0:nbk], AX.X), [bhB], [jmB])
        kb.op("dve", lambda e: e.tensor_scalar(jm[:], jm[:], -1.0, float(i + 1), ALU.mult, ALU.add), [jmB], [jmB])
        kb.op("dve", lambda e: e.tensor_scalar(VH[:].rearrange("p h o -> p (h o)"), CMISC[:, 32:40], jm[:], None, ALU.mult), [jmB, cB], [VHB])
        kb.op("dve", lambda e: e.tensor_tensor(VHD[:], CMISC[:, 40:104].rearrange("p (h g) -> p h g", g=8), VH[:].to_broadcast([128, 8, 8]), ALU.mult),
              [VHB, cB], [VHDB])
        for h in range(8):
            tr(kb, P16[0:8, h * 128:(h + 1) * 128], VHD[:, h, :], identB[:], [VHDB, cB], [B16], inc=(h == 7))
        kb.op("act", lambda e: e.copy(SH[:], P16[0:8, :]), [B16], [SHB])
        if stage <= 2:
            kb.op("dve", lambda e: e.tensor_copy(rr[:, 0:128], M[:, 0:128]), [MB], [rrB])
            kb.op("dve", lambda e: e.tensor_copy(rr[:, 128:256], SC[:, 0:128]), [SCB], [rrB])
            kb.dma(x_out[i * 128:(i + 1) * 128, :], rr[:], [rrB], [xoutB])
            continue
        for j0 in range(0, i + 1, 8):
            nj = min(8, i + 1 - j0)
            for jj in range(nj):
                j = j0 + jj
                tr(kb, P16[:, jj * 128:(jj + 1) * 128], M[:, j * 128:(j + 1) * 128], identB[:], [MB, MBa, cB], [B16], inc=(jj == nj - 1))
            kb.op("act", lambda e: e.copy(MT[:, j0:j0 + nj, :].rearrange("p j t -> p (j t)"), P16[:, 0:nj * 128]), [B16], [MTB])

        jlist = list(range(i + 1))
        if os.environ.get("K_JLAST"):
            jlist = [i]
        for j in jlist:
            d = i - j
            Pt_, PtB = PT[j % 2]
            for hb in range(2):
                bS = B0a if hb == 0 else B0b
                for cc in range(2):
                    mm(kb, P0[:, hb * 512:(hb + 1) * 512], cT[:, cc, j * 128:(j + 1) * 128], qlT[:, cc, hb * 512:(hb + 1) * 512],
                       [cTB, qlTB], [bS], start=(cc == 0), stop=False, inc=False)
                mm(kb, P0[:, hb * 512:(hb + 1) * 512], ones[0:8, :], SH[:, hb * 512:(hb + 1) * 512], [SHB, cB], [bS],
                   start=False, stop=False, inc=False)
                mm(kb, P0[:, hb * 512:(hb + 1) * 512], identB[:], MT[:, j:j + 1, :].to_broadcast([128, 4, 128]), [MTB, cB], [bS],
                   start=False, stop=True, inc=True)
            for h in range(8):
                kb.op("act", lambda e: e.activation(Pt_[:, h, :], P0[:, h * 128:(h + 1) * 128], AF.Exp,
                                                     bias=BIAS[:, h * 32 + d:h * 32 + d + 1], scale=1.0),
                      [B0a if h < 4 else B0b, cB], [PtB])
            first = (j == jlist[0])
            for cc in range(2):
                Po = P1 if cc == 0 else P2
                for hb in range(2):
                    bo = (B1a, B1b, B2a, B2b)[cc * 2 + hb]
                    mm(kb, Po[:, hb * 512:(hb + 1) * 512], cN[:, j, cc * 128:(cc + 1) * 128],
                       Pt_[:, 4 * hb:4 * hb + 4, :].rearrange("p h t -> p (h t)"), [cNB, PtB], [bo],
                       start=True, stop=True, inc=False)
            for h in range(8):
                mm(kb, PX[:, h:h + 1], Pt_[:, h, :], ones[:, 0:1], [PtB, cB], [BX], start=(h == 0), stop=(h == 7), inc=(h == 7))
            if first:
                kb.op("dve", lambda e: e.tensor_copy(OLs[:, 0, :], P1[:]), [B1a, B1b], [OLsB])
                kb.op("dve", lambda e: e.tensor_copy(OLs[:, 1, :], P2[:]), [B2a, B2b], [OLsB])
                kb.op("dve", lambda e: e.tensor_copy(dens[:], PX[:, 0:8]), [BX], [densB])
            else:
                kb.op("dve", lambda e: e.tensor_tensor(OLs[:, 0, :], OLs[:, 0, :], P1[:], ALU.add), [B1a, B1b, OLsB], [OLsB])
                kb.op("dve", lambda e: e.tensor_tensor(OLs[:, 1, :], OLs[:, 1, :], P2[:], ALU.add), [B2a, B2b, OLsB], [OLsB])
                kb.op("dve", lambda e: e.tensor_tensor(dens[:], dens[:], PX[:, 0:8], ALU.add), [BX, densB], [densB])

        kb.op("dve", lambda e: e.reciprocal(rden[:], dens[:]), [densB], [rdenB])
        kb.op("act", lambda e: e.copy(olT[:, 0, :], OLs[:, 0, :]), [OLsB], [olTB])
        kb.op("dve", lambda e: e.tensor_copy(olT[:, 1, :], OLs[:, 1, :]), [OLsB], [olTB])
        for h in range(8):
            for cc in range(2):
                mm(kb, P0[:, h * 128:(h + 1) * 128], olT[:, cc, h * 128:(h + 1) * 128], Wuv[:, h, cc, :], [olTB, WuvB],
                   [B0a if h < 4 else B0b], start=(cc == 0), stop=(cc == 1), inc=(cc == 1 and h == 7))
        for h in range(8):
            kb.op("dve", lambda e: e.scalar_tensor_tensor(og[:, h * 128:(h + 1) * 128], P0[:, h * 128:(h + 1) * 128], rden[:, h:h + 1],
                                                           sz[:, h * 128:(h + 1) * 128], ALU.mult, ALU.mult),
                  [B0a if h < 4 else B0b, rdenB, szB], [ogB])
        for k in range(8):
            tr(kb, P16[:, k * 128:(k + 1) * 128], og[:, k * 128:(k + 1) * 128], identB[:], [ogB, cB], [B16], inc=(k == 7))
        kb.op("act", lambda e: e.copy(ogT[:].rearrange("p k t -> p (k t)"), P16[:]), [B16], [ogTB])
        for nn in range(2):
            for k in range(8):
                mm(kb, P1[:, nn * 512:(nn + 1) * 512], ogT[:, k, :], Wout[:, k, nn * 512:(nn + 1) * 512], [ogTB, WoutB],
                   [B1a if nn == 0 else B1b], start=(k == 0), stop=(k == 7), inc=(k == 7))
        kb.op("dve", lambda e: e.scalar_tensor_tensor(rr[:], xf[:], ALPHA, P1[:], ALU.mult, ALU.add), [xfB, B1a, B1b], [rrB])
        layernorm_store(kb, st, rr[:], rrB, x_out[i * 128:(i + 1) * 128, :], xoutB, LG, LB, [LGB, LBB])

    kb.finish_layer = True
    kb.es = kb_es
    return es


def load_xT(kb, x_in, xinB, i, xf, xfB, xT, xTB, Ptr, Bs, identF, cB):
    kb.dma(xf[:], x_in[i * 128:(i + 1) * 128, :], [xinB], [xfB])
    for k in range(8):
        tr(kb, Ptr[:, k * 128:(k + 1) * 128], xf[:, k * 128:(k + 1) * 128], identF[:], [xfB, cB], Bs, inc=(k == 7))
    kb.op("act", lambda e: e.copy(xT[:].rearrange("p k t -> p (k t)"), Ptr[:, 0:1024]), Bs, [xTB])


def emit_layer1a(nc, kb, dr, x_in, xinB, hn, hnB, cst, nblk=NB):
    es = ExitStack()
    kb_es = kb.es
    kb.es = es
    sb = kb.sb
    identB, identF, ones, cB, TRIf = cst["identB"], cst["identF"], cst["ones"], cst["buf"], cst["TRIf"]
    win = dr["b_w_in"].rearrange("(k p) n -> p k n", p=128)
    Wqk, WqkB = sb("Wqk", [128, 8, 2048], BF16)
    Wv, WvB = sb("Wv", [128, 8, 2048], BF16)
    Wg, WgB = sb("Wg", [128, 8, 8], BF16)
    for k in range(0, 8, 4):
        kb.dma(Wqk[:, k:k + 4, :], win[:, k:k + 4, 0:2048], [], [WqkB], q="pool")
        kb.dma(Wv[:, k:k + 4, :], win[:, k:k + 4, 2048:4096], [], [WvB], q="pool")
    kb.dma(Wg[:], win[:, :, 4096:4104], [], [WgB], q="pool")
    CWr, CWrB = sb("CWr", [80, 128], F32)
    CWT, CWTB = sb("CWT", [128, 80], F32)
    kb.dma(CWr[0:64, :], dr["b_conv_w"].rearrange("j (c p) -> (j c) p", p=128), [], [CWrB])
    kb.dma(CWr[64:80, :], dr["b_conv_b"].rearrange("o (c p) -> (o c) p", p=128), [], [CWrB])
    GBt, GBB = sb("GBt", [128, 8], F32)
    kb.dma(GBt[:, 0:4], dr["b_i_bias"].partition_broadcast(128), [], [GBB])
    kb.dma(GBt[:, 4:8], dr["b_f_bias"].partition_broadcast(128), [], [GBB])
    HG, HGB = sb("HG", [128, 2048], F32)
    kb.dma(HG[:], dr["b_head_norm_g"].rearrange("o h v -> o (h v)").partition_broadcast(128), [], [HGB])

    PA = kb.ps("L1PA", [128, 2048], F32)
    PB = kb.ps("L1PB", [128, 1024], F32)
    PC = kb.ps("L1PC", [128, 512], F32)
    P16 = kb.ps("L1P16", [128, 1024], BF16)
    BA = [kb.buf("pa%d" % i) for i in range(4)]
    BBa, BBb, BC, B16 = kb.buf("pba"), kb.buf("pbb"), kb.buf("pc"), kb.buf("p16")
    BRW = [kb.buf("brw%d" % s_) for s_ in range(2)]; BST = [kb.buf("bst%d" % s_) for s_ in range(2)]
    BDN = [kb.buf("bdn%d" % s_) for s_ in range(2)]; BNM = [kb.buf("bnm%d" % s_) for s_ in range(2)]; BNU = kb.buf("bnu")
    REG = {0: BRW, 1: BST, 2: BDN + [BNU], 3: [BNM[1]]}

    tr(kb, PC[:, 0:80], CWr[:], identF[0:80, 0:80], [CWrB, cB], [BC])
    kb.op("dve", lambda e: e.tensor_copy(CWT[:], PC[:, 0:80]), [BC], [CWTB])

    xf, xfB = sb("xf1", [128, 1024], F32)
    xT, xTB = sb("xT1", [128, 8, 128], BF16)
    QKP, QKPB = sb("QKP", [128, 16, 131], F32)
    tmpc, tmpcB = sb("tmpc", [128, 128], F32)
    qkT, qkTB = sb("qkT", [128, 16, 128], BF16)
    vt, vtB = sb("vt", [128, 2048], BF16)
    gt, gtB = sb("gt", [128, 8], F32)
    lf, lfB = sb("lf", [128, 4], F32)
    ibm, ibmB = sb("ibm", [128, 4], F32)
    bcol, bcolB = sb("bcol", [128, 4], F32)
    ONESf, ONESfB = sb("ONESf", [128, 128], F32)
    LFB_2 = [sb("LFB%d" % s_, [128, 128], F32) for s_ in range(2)]
    EB_2 = [sb("EB%d" % s_, [128, 128], F32) for s_ in range(2)]
    DT_2 = [sb("DT%d" % s_, [128, 128], F32) for s_ in range(2)]
    sT_2 = [sb("sT%d" % s_, [128, 128], BF16) for s_ in range(2)]
    qh_2 = [sb("qh%d" % s_, [128, 2, 128], BF16) for s_ in range(2)]
    kTok_2 = [sb("kTok%d" % s_, [128, 256], BF16) for s_ in range(2)]
    vs_2 = [sb("vs%d" % s_, [128, 512], BF16) for s_ in range(2)]
    wcol_2 = [sb("wcol%d" % s_, [128, 1], BF16) for s_ in range(2)]
    hh_2 = [sb("hh%d" % s_, [128, 512], F32) for s_ in range(2)]
    hjunk_2 = [sb("hjunk%d" % s_, [128, 512], BF16) for s_ in range(2)]
    HN, HNB = sb("HN", [128, 2048], BF16)
    rec_2 = [sb("rec%d" % s_, [128, 1], F32) for s_ in range(2)]
    ssh_2 = [sb("ssh%d" % s_, [128, 1], F32) for s_ in range(2)]
    Cs = [sb("C%d" % h, [128, 2, 512], F32) for h in range(4)]
    Cb = [sb("Cb%d" % h, [128, 2, 512], BF16) for h in range(4)]
    ns = [sb("n%d" % h, [128, 2], F32) for h in range(4)]
    nb = [sb("nb%d" % h, [128, 2], BF16) for h in range(4)]
    kb.op("dve", lambda e: e.memset(ONESf[:], 1.0), [], [ONESfB])
    kb.op("dve", lambda e: e.memset(QKP[:], 0.0), [], [QKPB])
    for h in range(4):
        kb.op("dve", lambda e: e.memset(Cs[h][0][:], 0.0), [], [Cs[h][1]])
        kb.op("dve", lambda e: e.memset(Cb[h][0][:], 0.0), [], [Cb[h][1]])
        kb.op("dve", lambda e: e.memset(ns[h][0][:], 0.0), [], [ns[h][1]])
        kb.op("dve", lambda e: e.memset(nb[h][0][:], 0.0), [], [nb[h][1]])

    for i in range(nblk):
        load_xT(kb, x_in, xinB, i, xf, xfB, xT, xTB, PB, [BBa, BBb, BNM[0]], identF, cB)
        for c in range(16):
            for k in range(8):
                mm(kb, PA[:, c * 128:(c + 1) * 128], Wqk[:, k, c * 128:(c + 1) * 128], xT[:, k, :], [WqkB, xTB], [BA[c // 4]] + REG[c // 4],
                   start=(k == 0), stop=(k == 7), inc=(k == 7 and c % 4 == 3))
        kb.op("dve", lambda e: e.tensor_copy(QKP[:, 0:8, 3:131], PA[:, 0:1024].rearrange("p (c t) -> p c t", c=8)), [BA[0], BA[1]], [QKPB])
        kb.op("dve", lambda e: e.tensor_copy(QKP[:, 8:16, 3:131], PA[:, 1024:2048].rearrange("p (c t) -> p c t", c=8)), [BA[2], BA[3]], [QKPB])
        for c in range(16):
            kb.op("dve", lambda e: e.tensor_scalar(tmpc[:], QKP[:, c, 0:128], CWT[:, c:c + 1], CWT[:, 64 + c:65 + c], ALU.mult, ALU.add),
                  [QKPB, CWTB], [tmpcB])
            for j in range(1, 4):
                kb.op("dve", lambda e: e.scalar_tensor_tensor(tmpc[:], QKP[:, c, j:j + 128], CWT[:, j * 16 + c:j * 16 + c + 1], tmpc[:],
                                                               ALU.mult, ALU.add), [QKPB, CWTB, tmpcB], [tmpcB])
            kb.op("act", lambda e: e.activation(qkT[:, c, :], tmpc[:], AF.Silu), [tmpcB], [qkTB])
        kb.op("dve", lambda e: e.tensor_copy(tmpc[:, 0:48].rearrange("p (c t) -> p c t", c=16), QKP[:, :, 128:131]), [QKPB], [tmpcB])
        kb.op("dve", lambda e: e.tensor_copy(QKP[:, :, 0:3], tmpc[:, 0:48].rearrange("p (c t) -> p c t", c=16)), [tmpcB], [QKPB])
        for nn in range(4):
            for k in range(8):
                mm(kb, PA[:, nn * 512:(nn + 1) * 512], xT[:, k, :], Wv[:, k, nn * 512:(nn + 1) * 512], [WvB, xTB], [BA[nn]],
                   start=(k == 0), stop=(k == 7), inc=(k == 7))
        kb.op("act", lambda e: e.copy(vt[:, 0:1024], PA[:, 0:1024]), [BA[0], BA[1]], [vtB])
        kb.op("dve", lambda e: e.tensor_copy(vt[:, 1024:2048], PA[:, 1024:2048]), [BA[2], BA[3]], [vtB])
        for k in range(8):
            mm(kb, PC[:, 0:8], xT[:, k, :], Wg[:, k, :], [WgB, xTB], [BC], start=(k == 0), stop=(k == 7), inc=(k == 7))
        kb.op("dve", lambda e: e.tensor_tensor(gt[:], PC[:, 0:8], GBt[:], ALU.add), [BC, GBB], [gtB])
        kb.op("act", lambda e: e.activation(lf[:], gt[:, 4:8], AF.Exp, scale=-1.0), [gtB], [lfB])
        kb.op("act", lambda e: e.activation(lf[:], lf[:], AF.Ln, bias=1.0), [lfB], [lfB])
        kb.op("dve", lambda e: e.tensor_scalar(lf[:], lf[:], -1.0, None, ALU.mult), [lfB], [lfB])
        mm(kb, PC[:, 8:12], TRIf[:], lf[:], [lfB, cB], [BC])
        kb.op("dve", lambda e: e.tensor_copy(bcol[:], PC[:, 8:12]), [BC], [bcolB])
        kb.op("dve", lambda e: e.tensor_tensor(ibm[:], gt[:, 0:4], bcol[:], ALU.subtract), [gtB, bcolB], [ibmB])
        for h in range(4):
            par = h % 2
            LFB, LFBB = LFB_2[par]; EB, EBB = EB_2[par]; DT, DTB = DT_2[par]; sT, sTB = sT_2[par]; qh, qhB = qh_2[par]
            kTok, kTokB = kTok_2[par]; vs, vsB = vs_2[par]; wcol, wcolB = wcol_2[par]; hh, hhB = hh_2[par]
            hjunk, hjunkB = hjunk_2[par]; rec, recB = rec_2[par]; ssh, sshB = ssh_2[par]
            pBRW = PA[:, par * 128:par * 128 + 128]; bBRW = BRW[par]
            pBST = PA[:, 512 + par * 128:512 + par * 128 + 128]; bBST = BST[par]
            pDN = PA[:, 1024 + par * 8:1024 + par * 8 + 1]; bDN = BDN[par]
            pNU = PA[:, 1088:1089]
            pNM = PB[:, 0:512] if par == 0 else PA[:, 1536:2048]; bNM = BNM[par]; aNM = BBa if par == 0 else BA[3]
            C, CB_ = Cs[h]
            Cbh, CbB = Cb[h]
            nh, nhB = ns[h]
            nbh, nbB = nb[h]
            kb.op("dve", lambda e: e.tensor_scalar(LFB[:], ONESf[:], lf[:, h:h + 1], None, ALU.mult), [ONESfB, lfB], [LFBB])
            mm(kb, pBRW, LFB[:], TRIf[:], [LFBB, cB, BA[0]], [bBRW])
            kb.op("act", lambda e: e.activation(EB[:], pBRW, AF.Exp), [bBRW], [EBB])
            kb.op("act", lambda e: e.activation(DT[:], pBRW, AF.Exp, bias=ibm[:, h:h + 1]), [bBRW, ibmB], [DTB])
            kb.op("dve", lambda e: e.tensor_copy(wcol[:], DT[:, 127:128]), [DTB], [wcolB])
            kb.op("dve", lambda e: e.tensor_scalar(vs[:], vt[:, h * 512:(h + 1) * 512], DT[:, 127:128], None, ALU.mult), [vtB, DTB], [vsB])
            kb.op("dve", lambda e: e.tensor_tensor(DT[:], DT[:], TRIf[:], ALU.mult), [DTB, cB], [DTB])
            for cc in range(2):
                kb.op("dve", lambda e: e.scalar_tensor_tensor(qh[:, cc, :], qkT[:, 2 * h + cc, :], 0.0625, EB[:], ALU.mult, ALU.mult),
                      [qkTB, EBB], [qhB])
            for cc in range(2):
                mm(kb, pBST, qkT[:, 8 + 2 * h + cc, :], qkT[:, 2 * h + cc, :], [qkTB, BA[1]], [bBST], start=(cc == 0), stop=(cc == 1), inc=(cc == 1))
            kb.op("dve", lambda e: e.scalar_tensor_tensor(sT[:], pBST, 0.0625, DT[:], ALU.mult, ALU.mult), [bBST, DTB], [sTB])
            mm(kb, pNM, sT[:], vt[:, h * 512:(h + 1) * 512], [sTB, vtB, aNM], [bNM], start=True, stop=False, inc=False)
            for cc in range(2):
                mm(kb, pNM, qh[:, cc, :], Cbh[:, cc, :], [qhB, CbB, aNM], [bNM], start=False, stop=(cc == 1), inc=(cc == 1))
            mm(kb, pDN, sT[:], ones[:, 0:1], [sTB, cB, BA[2]], [bDN], start=True, stop=False, inc=False)
            for cc in range(2):
                mm(kb, pDN, qh[:, cc, :], nbh[:, cc:cc + 1], [qhB, nbB, BA[2]], [bDN], start=False, stop=(cc == 1), inc=(cc == 1))
            kb.op("dve", lambda e: e.tensor_scalar(rec[:], pDN, -1.0, None, ALU.mult), [bDN], [recB])
            kb.op("dve", lambda e: e.tensor_tensor(rec[:], rec[:], pDN, ALU.max), [bDN, recB], [recB])
            kb.op("dve", lambda e: e.tensor_scalar(rec[:], rec[:], 1.0, None, ALU.max), [recB], [recB])
            kb.op("dve", lambda e: e.reciprocal(rec[:], rec[:]), [recB], [recB])
            kb.op("dve", lambda e: e.tensor_scalar(hh[:], pNM, rec[:], None, ALU.mult), [bNM, recB], [hhB])
            kb.op("act", lambda e: e.activation(hjunk[:], hh[:], AF.Square, accum_out=ssh[:]), [hhB], [hjunkB, sshB])
            kb.op("dve", lambda e: e.tensor_scalar(ssh[:], ssh[:], 1.0 / 512, EPS, ALU.mult, ALU.add), [sshB], [sshB])
            kb.op("act", lambda e: e.activation(ssh[:], ssh[:], AF.Sqrt), [sshB], [sshB])
            kb.op("dve", lambda e: e.reciprocal(ssh[:], ssh[:]), [sshB], [sshB])
            kb.op("dve", lambda e: e.scalar_tensor_tensor(HN[:, h * 512:(h + 1) * 512], hh[:], ssh[:], HG[:, h * 512:(h + 1) * 512],
                                                           ALU.mult, ALU.mult), [hhB, sshB, HGB], [HNB])
            for cc in range(2):
                tr(kb, P16[:, cc * 128:(cc + 1) * 128], qkT[:, 8 + 2 * h + cc, :], identB[:], [qkTB, cB], [B16], inc=(cc == 1))
            kb.op("act", lambda e: e.copy(kTok[:], P16[:, 0:256]), [B16], [kTokB])
            for cc in range(2):
                mm(kb, PB[:, 512:1024], kTok[:, cc * 128:(cc + 1) * 128], vs[:], [kTokB, vsB], [BBb])
                kb.op("dve", lambda e: e.scalar_tensor_tensor(C[:, cc, :], C[:, cc, :], EB[:, 127:128], PB[:, 512:1024], ALU.mult, ALU.add),
                      [CB_, EBB, BBb], [CB_])
                mm(kb, pNU, kTok[:, cc * 128:(cc + 1) * 128], wcol[:], [kTokB, wcolB, BA[2]], [BNU])
                kb.op("dve", lambda e: e.scalar_tensor_tensor(nh[:, cc:cc + 1], nh[:, cc:cc + 1], EB[:, 127:128], pNU, ALU.mult, ALU.add),
                      [nhB, EBB, BNU], [nhB])
            kb.op("act", lambda e: e.copy(Cbh[:].rearrange("p c v -> p (c v)"), C[:].rearrange("p c v -> p (c v)")), [CB_], [CbB])
            kb.op("dve", lambda e: e.tensor_copy(nbh[:], nh[:]), [nhB], [nbB])
        kb.dma(hn[i * 128:(i + 1) * 128, :], HN[:], [HNB], [hnB])
    kb.es = kb_es
    return es


def emit_layer1b(nc, kb, dr, x_in, xinB, hn, hnB, x_out, xoutB, cst, nblk=NB):
    es = ExitStack()
    kb_es = kb.es
    kb.es = es
    sb = kb.sb
    identB, identF, cB = cst["identB"], cst["identF"], cst["buf"]
    win = dr["b_w_in"].rearrange("(k p) n -> p k n", p=128)
    Wo, WoB = sb("Wo", [128, 8, 2048], BF16)
    Wz, WzB = sb("Wz", [128, 8, 2048], BF16)
    Wout, WoutB = sb("Wout1", [128, 16, 1024], BF16)
    wout = dr["b_w_out"].rearrange("(k p) n -> p k n", p=128)
    for k in range(0, 8, 4):
        kb.dma(Wo[:, k:k + 4, :], win[:, k:k + 4, 4104:6152], [], [WoB], q="pool")
        kb.dma(Wz[:, k:k + 4, :], win[:, k:k + 4, 6152:8200], [], [WzB], q="pool")
    for k in range(0, 16, 8):
        kb.dma(Wout[:, k:k + 8, :], wout[:, k:k + 8, :], [], [WoutB], q="pool")
    LG, LGB = sb("LG1", [128, 1024], F32)
    LB, LBB = sb("LB1", [128, 1024], F32)
    kb.dma(LG[:], dr["b_ln_g"].partition_broadcast(128), [], [LGB])
    kb.dma(LB[:], dr["b_ln_b"].partition_broadcast(128), [], [LBB])
    PA = kb.ps("L2PA", [128, 2048], F32)
    PB = kb.ps("L2PB", [128, 1024], F32)
    P16 = kb.ps("L2P16", [128, 2048], BF16)
    BA = [kb.buf("qa%d" % i) for i in range(4)]
    BBa, BBb, B16 = kb.buf("qba"), kb.buf("qbb"), kb.buf("q16")
    xf, xfB = sb("xf2", [128, 1024], F32)
    xT, xTB = sb("xT2", [128, 8, 128], BF16)
    so, soB = sb("so", [128, 2048], BF16)
    hnb, hnbB = sb("hnb", [128, 2048], BF16)
    hg, hgB = sb("hg", [128, 2048], BF16)
    hgT, hgTB = sb("hgT", [128, 16, 128], BF16)
    rr, rrB = sb("rr2", [128, 1024], F32)
    st = {"s1": sb("s1b", [128, 1], F32), "ss": sb("ssb", [128, 1], F32), "lnjunk": sb("lnjunkb", [128, 1024], BF16)}
    for i in range(nblk):
        load_xT(kb, x_in, xinB, i, xf, xfB, xT, xTB, PB, [BBa, BBb], identF, cB)
        kb.dma(hnb[:], hn[i * 128:(i + 1) * 128, :], [hnB], [hnbB])
        for nn in range(4):
            for k in range(8):
                mm(kb, PA[:, nn * 512:(nn + 1) * 512], xT[:, k, :], Wo[:, k, nn * 512:(nn + 1) * 512], [WoB, xTB], [BA[nn]],
                   start=(k == 0), stop=(k == 7), inc=(k == 7))
        kb.op("act", lambda e: e.activation(so[:], PA[:], AF.Sigmoid), BA, [soB])
        kb.op("dve", lambda e: e.tensor_tensor(hg[:], hnb[:], so[:], ALU.mult), [hnbB, soB], [hgB])
        for nn in range(4):
            for k in range(8):
                mm(kb, PA[:, nn * 512:(nn + 1) * 512], xT[:, k, :], Wz[:, k, nn * 512:(nn + 1) * 512], [WzB, xTB], [BA[nn]],
                   start=(k == 0), stop=(k == 7), inc=(k == 7))
        kb.op("act", lambda e: e.activation(so[:], PA[:], AF.Silu), BA, [soB])
        kb.op("dve", lambda e: e.tensor_tensor(hg[:], hg[:], so[:], ALU.mult), [hgB, soB], [hgB])
        for k in range(16):
            tr(kb, P16[:, k * 128:(k + 1) * 128], hg[:, k * 128:(k + 1) * 128], identB[:], [hgB, cB], [B16], inc=(k == 15))
        kb.op("act", lambda e: e.copy(hgT[:].rearrange("p k t -> p (k t)"), P16[:]), [B16], [hgTB])
        for nn in range(2):
            for k in range(16):
                mm(kb, PB[:, nn * 512:(nn + 1) * 512], hgT[:, k, :], Wout[:, k, nn * 512:(nn + 1) * 512], [hgTB, WoutB],
                   [BBa if nn == 0 else BBb], start=(k == 0), stop=(k == 15), inc=(k == 15))
        kb.op("dve", lambda e: e.scalar_tensor_tensor(rr[:], xf[:], ALPHA, PB[:], ALU.mult, ALU.add), [xfB, BBa, BBb], [rrB])
        layernorm_store(kb, st, rr[:], rrB, x_out[i * 128:(i + 1) * 128, :], xoutB, LG, LB, [LGB, LBB])
    kb.es = kb_es
    return es


def make_consts():
    identF = np.eye(128, dtype=np.float32)
    q = np.arange(128)[:, None]
    s = np.arange(128)[None, :]
    CM = np.where(s <= q, 0.0, -1e30).astype(np.float32)
    slopes = 2.0 ** (-(np.arange(1, 9, dtype=np.float64)))
    sp = np.arange(128, dtype=np.float64)[:, None, None]
    dd = np.arange(32, dtype=np.float64)[None, None, :]
    BIAS = (slopes[None, :, None] * (sp - 127.0 - 128.0 * dd)).reshape(128, 256).astype(np.float32)
    misc = np.zeros((128, 104), np.float32)
    misc[:, 0:32] = np.arange(1, 33, dtype=np.float32)[None, :]
    misc[:, 32:40] = (128.0 * slopes)[None, :]
    misc[:, 40:104] = np.eye(8, dtype=np.float32).reshape(1, 64)
    tri = (np.arange(128)[:, None] <= np.arange(128)[None, :]).astype(np.float32)
    return {"c_ident": identF, "c_cm": CM, "c_bias": BIAS, "c_misc": misc, "c_tri": tri}


def build(layers=LAYERS, nblk=NB):
    nc = bass.Bass("TRN2", target_bir_lowering=False)
    dr = {}

    def din(name, shape):
        dr[name] = nc.dram_tensor(name, list(shape), F32, kind="ExternalInput").ap()

    din("x", [T, D])
    din("a_w_in", [D, A_IN]); din("a_kv_norm_g", [1, 256]); din("a_w_uk", [8, 128, 256]); din("a_w_uv", [8, 256, 128])
    din("a_w_out", [D, D]); din("a_ln_g", [1, D]); din("a_ln_b", [1, D])
    din("c_ident", [128, 128]); din("c_cm", [128, 128]); din("c_bias", [128, 256]); din("c_misc", [128, 104]); din("c_tri", [128, 128])
    din("b_w_in", [D, B_IN]); din("b_i_bias", [1, 4]); din("b_f_bias", [1, 4]); din("b_conv_w", [4, 2048]); din("b_conv_b", [1, 2048])
    din("b_head_norm_g", [1, 4, 512]); din("b_w_out", [2048, D]); din("b_ln_g", [1, D]); din("b_ln_b", [1, D])
    out = nc.dram_tensor("out", [T, D], F32, kind="ExternalOutput").ap()
    with ExitStack() as es:
        kb = KB(nc, es)
        identF, idB = kb.sb("identF", [128, 128], F32)
        identB, _ = kb.sb("identB", [128, 128], BF16)
        CM, _ = kb.sb("CM", [128, 128], F32)
        BIAS, _ = kb.sb("BIAS", [128, 256], F32)
        ones, _ = kb.sb("ones", [128, 128], BF16)
        CMISC, _ = kb.sb("CMISC", [128, 104], F32)
        TRIf, _ = kb.sb("TRIf", [128, 128], F32)
        cB = idB
        kb.dma(identF[:], dr["c_ident"], [], [cB])
        kb.dma(identB[:], dr["c_ident"], [], [cB], q="pool")
        kb.dma(CM[:], dr["c_cm"], [], [cB])
        kb.dma(BIAS[:], dr["c_bias"], [], [cB])
        kb.dma(CMISC[:], dr["c_misc"], [], [cB])
        kb.dma(TRIf[:], dr["c_tri"], [], [cB])
        kb.op("dve", lambda e: e.memset(ones[:], 1.0), [], [cB])
        cst = {"identB": identB, "identF": identF, "CM": CM, "BIAS": BIAS, "ones": ones, "buf": cB, "CMISC": CMISC, "TRIf": TRIf}
        xinB = kb.buf("xin")
        outB = kb.buf("out")
        x1d = nc.dram_tensor("x1d", [T, D], F32).ap()
        hnd = nc.dram_tensor("hnd", [T, 2048], BF16).ap()
        x1B = kb.buf("x1d")
        hnB = kb.buf("hnd")
        if layers == (0,):
            l0 = emit_layer0(nc, kb, dr, dr["x"], xinB, out, outB, cst, nblk=nblk)
            kb.finish(); l0.close()
        elif layers == (1,):
            la = emit_layer1a(nc, kb, dr, dr["x"], xinB, hnd, hnB, cst, nblk=nblk)
            kb.barrier(); la.close()
            lb = emit_layer1b(nc, kb, dr, dr["x"], xinB, hnd, hnB, out, outB, cst, nblk=nblk)
            kb.finish(); lb.close()
        else:
            l0 = emit_layer0(nc, kb, dr, dr["x"], xinB, x1d, x1B, cst, nblk=nblk)
            kb.barrier(); l0.close()
            la = emit_layer1a(nc, kb, dr, x1d, x1B, hnd, hnB, cst, nblk=nblk)
            kb.barrier(); la.close()
            lb = emit_layer1b(nc, kb, dr, x1d, x1B, hnd, hnB, out, outB, cst, nblk=nblk)
            kb.finish(); lb.close()
    return nc


_CACHE = {}


def make_inmaps(inputs, xs):
    cst = make_consts()
    shared = {k: np.ascontiguousarray(inputs[k][0], dtype=np.float32) for k in
              ("a_w_in", "a_w_uk", "a_w_uv", "a_w_out", "b_w_in", "b_conv_w", "b_w_out")}
    for k in ("a_kv_norm_g", "a_ln_g", "a_ln_b", "b_i_bias", "b_f_bias", "b_conv_b", "b_ln_g", "b_ln_b"):
        shared[k] = np.ascontiguousarray(inputs[k], dtype=np.float32).reshape(1, -1)
    shared["b_head_norm_g"] = np.ascontiguousarray(inputs["b_head_norm_g"], dtype=np.float32).reshape(1, 4, 512)
    shared.update(cst)
    maps = []
    for xx in xs:
        m = dict(shared)
        m["x"] = np.ascontiguousarray(xx, dtype=np.float32)
        maps.append(m)
    return maps


def kernel(**inputs):
    x = np.ascontiguousarray(inputs["x"], dtype=np.float32)
    if "nc" not in _CACHE:
        import os
        lay = os.environ.get("K_LAYERS")
        _CACHE["nc"] = build(layers=tuple(int(c) for c in lay)) if lay else build()
    nc = _CACHE["nc"]
    in_maps = make_inmaps(inputs, [x[c % 4] for c in range(8)])
    res = run_bass_kernel_spmd(nc, in_maps, core_ids=list(range(8)))
    return np.stack([res.results[c]["out"] for c in range(4)], axis=0)
```

```python
import numpy as np
from contextlib import ExitStack
import concourse.bass as bass
import concourse.mybir as mybir
from concourse.bass_utils import run_bass_kernel_spmd

F32 = mybir.dt.float32
BF16 = mybir.dt.bfloat16
AF = mybir.ActivationFunctionType
ALU = mybir.AluOpType
AX = mybir.AxisListType

T = 4096
D = 1024
NB = T // 128
A_IN = 2888
B_IN = 8200
ALPHA = float(4 ** 0.25)
EPS = 1e-5
NDMA = 12
BIS_STEPS = 28
BIS_LO = -2048.0
LAYERS = (0, 1)


class Buf:
    __slots__ = ("name", "w", "r")

    def __init__(self, name):
        self.name = name
        self.w = None
        self.r = {}


class KB:
    def __init__(self, nc, es):
        self.nc = nc
        self.es = es
        self.eng = {"pe": nc.tensor, "act": nc.scalar, "dve": nc.vector, "pool": nc.gpsimd, "sp": nc.sync}
        self.sem = {e: es.enter_context(nc.semaphore("s_" + e)) for e in ("pe", "act", "dve", "pool")}
        self.cnt = {e: 0 for e in self.sem}
        self.known = {e: {} for e in self.eng}
        self.dsem = [es.enter_context(nc.semaphore("d%d" % k)) for k in range(NDMA)]
        self.dtot = [0] * NDMA
        self.dnext = 0
        self.nbuf = 0
        self.root_es = es
        self.xsem = {}

    def buf(self, name=None):
        self.nbuf += 1
        return Buf(name or ("b%d" % self.nbuf))

    def sb(self, name, shape, dt):
        t = self.es.enter_context(self.nc.sbuf_tensor(name, list(shape), dt))
        return t, Buf(name)

    def ps(self, name, shape, dt):
        return self.es.enter_context(self.nc.psum_tensor(name, list(shape), dt))

    def _deps(self, reads, writes):
        toks = []
        for b in reads:
            if b.w is not None:
                toks.append(b.w)
        for b in writes:
            if b.w is not None:
                toks.append(b.w)
            toks.extend(b.r.items())
        return toks

    def _wait(self, e, toks):
        need = {}
        for k, v in toks:
            if k == e and e == "pe":
                continue
            if self.known[e].get(k, 0) >= v:
                continue
            if need.get(k, 0) < v:
                need[k] = v
        for k, v in need.items():
            sem = self.sem[k] if isinstance(k, str) else (self.dsem[k[1]] if k[0] == "d" else self.xsem[k])
            self.eng[e].wait_ge(sem, v)
            self.known[e][k] = v

    def _mark(self, tok, reads, writes):
        k, v = tok
        for b in reads:
            if b.r.get(k, 0) < v:
                b.r[k] = v
        for b in writes:
            b.w = tok
            b.r = {}

    def op(self, e, fn, reads=(), writes=(), inc=True):
        self._wait(e, self._deps(reads, writes))
        ins = fn(self.eng[e])
        if inc:
            self.cnt[e] += 1
            ins.then_inc(self.sem[e], 1)
            tok = (e, self.cnt[e])
        else:
            assert e == "pe"
            tok = (e, self.cnt[e] + 1)
        self._mark(tok, reads, writes)
        return tok

    def dma(self, out, in_, reads=(), writes=(), q="sp"):
        toks = self._deps(reads, writes)
        if q == "pool":
            key = ("x", len(self.xsem))
            self.xsem[key] = self.root_es.enter_context(self.nc.semaphore("x%d" % len(self.xsem)))
            self._wait(q, toks)
            self.eng[q].dma_start(out=out, in_=in_).then_inc(self.xsem[key], 16)
            tok = (key, 16)
            self._mark(tok, reads, writes)
            return tok
        k = self.dnext
        self.dnext = (k + 1) % NDMA
        if self.dtot[k] > 0:
            toks.append((("d", k), self.dtot[k]))
        self._wait(q, toks)
        self.dtot[k] += 16
        self.eng[q].dma_start(out=out, in_=in_).then_inc(self.dsem[k], 16)
        tok = (("d", k), self.dtot[k])
        self._mark(tok, reads, writes)
        return tok

    def barrier(self):
        toks = [(("d", k), self.dtot[k]) for k in range(NDMA) if self.dtot[k] > 0]
        toks += [(k, 16) for k in self.xsem]
        toks += [(e, c) for e, c in self.cnt.items() if c > 0]
        for e in self.eng:
            self._wait(e, toks)

    def finish(self):
        toks = [(("d", k), self.dtot[k]) for k in range(NDMA) if self.dtot[k] > 0]
        toks += [(k, 16) for k in self.xsem]
        toks += [(e, c) for e, c in self.cnt.items() if c > 0]
        self._wait("sp", toks)


def mm(kb, out, lhsT, rhs, reads, writes, start=True, stop=True, inc=None):
    if inc is None:
        inc = stop
    return kb.op("pe", lambda e: e.matmul(out, lhsT, rhs, start=start, stop=stop), reads, writes, inc=inc)


def tr(kb, out, in_, ident, reads, writes, inc=True):
    return kb.op("pe", lambda e: e.transpose(out, in_, ident), reads, writes, inc=inc)


def layernorm_store(kb, st, r_ap, rB, out_dram_ap, outB, G, Bt, GB):
    s1, s1B = st["s1"]
    kb.op("dve", lambda e: e.reduce_sum(s1[:], r_ap, AX.X), [rB], [s1B])
    kb.op("dve", lambda e: e.tensor_scalar(s1[:], s1[:], -1.0 / D, None, ALU.mult), [s1B], [s1B])
    kb.op("dve", lambda e: e.tensor_scalar(r_ap, r_ap, s1[:], None, ALU.add), [rB, s1B], [rB])
    junk, junkB = st["lnjunk"]
    ss, ssB = st["ss"]
    kb.op("act", lambda e: e.activation(junk[:], r_ap, AF.Square, accum_out=ss[:]), [rB], [junkB, ssB])
    kb.op("dve", lambda e: e.tensor_scalar(ss[:], ss[:], 1.0 / D, EPS, ALU.mult, ALU.add), [ssB], [ssB])
    kb.op("act", lambda e: e.activation(ss[:], ss[:], AF.Sqrt), [ssB], [ssB])
    kb.op("dve", lambda e: e.reciprocal(ss[:], ss[:]), [ssB], [ssB])
    kb.op("dve", lambda e: e.scalar_tensor_tensor(r_ap, r_ap, ss[:], G[:], ALU.mult, ALU.mult), [rB, ssB] + list(GB), [rB])
    kb.op("dve", lambda e: e.tensor_tensor(r_ap, r_ap, Bt[:], ALU.add), [rB] + list(GB), [rB])
    kb.dma(out_dram_ap, r_ap, [rB], [outB])


def emit_layer0(nc, kb, dr, x_in, xinB, x_out, xoutB, cst, nblk=NB):
    es = ExitStack()
    kb_es = kb.es
    kb.es = es
    sb = kb.sb
    identB, identF, CM, BIAS, ones, cB = cst["identB"], cst["identF"], cst["CM"], cst["BIAS"], cst["ones"], cst["buf"]

    W0, W0B = sb("W0", [128, 8, A_IN], BF16)
    Wk2, Wk2B = sb("Wk2", [128, 8, 128], BF16)
    Wuk, WukB = sb("Wuk", [128, 8, 256], BF16)
    Wuv, WuvB = sb("Wuv", [128, 8, 2, 128], BF16)
    Wout, WoutB = sb("Wout", [128, 8, 1024], BF16)
    Gkv, GkvB = sb("Gkv", [128, 256], F32)
    LG, LGB = sb("LG", [128, 1024], F32)
    LB, LBB = sb("LB", [128, 1024], F32)
    win = dr["a_w_in"].rearrange("(k p) n -> p k n", p=128)
    for k in range(0, 8, 4):
        kb.dma(W0[:, k:k + 4, :], win[:, k:k + 4, :], [], [W0B], q="pool")
    kb.dma(Wk2[:, :, 0:64], win[:, :, 1792:1856], [], [Wk2B], q="pool")
    kb.dma(Wk2[:, :, 64:128], win[:, :, 1792:1856], [], [Wk2B], q="pool")
    kb.dma(Wuk[:], dr["a_w_uk"].rearrange("h d c -> d h c"), [], [WukB], q="pool")
    kb.dma(Wuv[:], dr["a_w_uv"].rearrange("h (cc p) d -> p h cc d", p=128), [], [WuvB], q="pool")
    kb.dma(Wout[:], dr["a_w_out"].rearrange("(k p) n -> p k n", p=128), [], [WoutB], q="pool")
    kb.dma(Gkv[:], dr["a_kv_norm_g"].partition_broadcast(128), [], [GkvB])
    kb.dma(LG[:], dr["a_ln_g"].partition_broadcast(128), [], [LGB])
    kb.dma(LB[:], dr["a_ln_b"].partition_broadcast(128), [], [LBB])

    kT2, kT2B = sb("kT2", [128, T], BF16)
    cN, cNB = sb("cN", [128, NB, 256], BF16)
    cT, cTB = sb("cT", [128, 2, T], BF16)
    SC, SCB = sb("SC", [128, T], F32)
    M, MB = sb("M", [128, T], BF16)
    MT, MTB = sb("MT", [128, NB, 128], BF16)
    XF = [sb("XF%d" % s, [128, 1024], F32) for s in range(1)]
    xT, xTB = sb("xT", [128, 8, 128], BF16)
    qT, qTB = sb("qT", [128, 8, 128], BF16)
    qiT, qiTB = sb("qiT", [128, 4, 128], BF16)
    qlT, qlTB = sb("qlT", [128, 2, 1024], BF16)
    sz, szB = sb("sz", [128, 1024], BF16)
    cf, cfB = sb("cf", [128, 256], F32)
    wsb, wsbB = sb("wsb", [128, 8], F32)
    RR = [sb("R%d" % s, [128, 512], F32) for s in range(3)]
    PT = [sb("PT%d" % s, [128, 8, 128], BF16) for s in range(2)]
    olT, olTB = sb("olT", [128, 2, 1024], BF16)
    og, ogB = sb("og", [128, 1024], BF16)
    ogT, ogTB = sb("ogT", [128, 8, 128], BF16)
    rr, rrB = sb("rr", [128, 1024], F32)
    rden, rdenB = sb("rden", [128, 8], F32)
    dens, densB = sb("dens", [128, 8], F32)
    bh, bhB = sb("bh", [128, 32], F32)
    jm, jmB = sb("jm", [128, 1], F32)
    VH, VHB = sb("VH", [128, 8, 1], BF16)
    VHD, VHDB = sb("VHD", [128, 8, 8], BF16)
    SH, SHB = sb("SH", [8, 1024], BF16)
    CMISC = cst["CMISC"]
    OLs, OLsB = sb("OLs", [128, 2, 1024], F32)
    lo, loB = sb("lo", [128, 1], F32)
    mid, midB = sb("mid", [128, 1], F32)
    cnt, cntB = sb("cnt", [128, 1], F32)
    cntA, cntAB = sb("cntA", [128, 1], F32)
    MBa = kb.buf("MBa")
    ge, geB = sb("ge", [128, 1], F32)
    st = {"s1": sb("s1", [128, 1], F32), "ss": sb("ss", [128, 1], F32), "lnjunk": (og, ogB)}
    ssc, sscB = sb("ssc", [128, 1], F32)
    cjunk, cjunkB = sb("cjunk", [128, 256], BF16)

    P0 = kb.ps("P0", [128, 1024], F32)
    P1 = kb.ps("P1", [128, 1024], F32)
    P2 = kb.ps("P2", [128, 1024], F32)
    PX = kb.ps("PX", [128, 512], F32)
    P16 = kb.ps("P16", [128, 1024], BF16)
    B0a, B0b, B1a, B1b, B2a, B2b, BX, B16 = [kb.buf("ps%d" % i) for i in range(8)]
    OPB = [[B1a, B1b], [B2a, B2b]]
    BXh = [BX, BX]
    PTB = [[kb.buf("pt%d%d" % (s_, h_)) for h_ in range(2)] for s_ in range(2)]
    OLB = [[kb.buf("ol%d%d" % (c_, h_)) for h_ in range(2)] for c_ in range(2)]
    DNB = [kb.buf("dn0"), kb.buf("dn1")]

    scale_q = float(128 ** -0.5)

    for i in range(nblk):
        n = 128 * (i + 1)
        xf, xfB = XF[0]
        kb.dma(xf[:], x_in[i * 128:(i + 1) * 128, :], [xinB], [xfB])
        import os
        stage = float(os.environ.get("K_STAGE", "99")) if i == 0 else float(os.environ.get("K_STAGE1", os.environ.get("K_STAGE", "99")))
        if stage <= 0:
            kb.dma(x_out[i * 128:(i + 1) * 128, :], xf[:], [xfB], [xoutB])
            continue
        for k in range(8):
            tr(kb, P0[:, k * 128:(k + 1) * 128], xf[:, k * 128:(k + 1) * 128], identF[:], [xfB, cB], [B0a if k < 4 else B0b], inc=(k == 7))
        kb.op("act", lambda e: e.copy(xT[:].rearrange("p k t -> p (k t)"), P0[:]), [B0a, B0b], [xTB])
        if stage <= 0.1:
            kb.op("dve", lambda e: e.memset(rr[:], 0.0), [], [rrB])
            kb.op("dve", lambda e: e.tensor_copy(rr[:, 0:128], xT[:, 0, :]), [xTB], [rrB])
            kb.dma(x_out[i * 128:(i + 1) * 128, :], rr[:], [rrB], [xoutB])
            continue
        for h in range(8):
            for k in range(8):
                mm(kb, P1[:, h * 128:(h + 1) * 128], W0[:, k, h * 128:(h + 1) * 128], xT[:, k, :], [W0B, xTB],
                   [B1a if h < 4 else B1b], start=(k == 0), stop=(k == 7), inc=(k == 7 and h == 7))
        kb.op("dve", lambda e: e.tensor_copy(qT[:].rearrange("p h t -> p (h t)"), P1[:]), [B1a, B1b], [qTB])
        if stage <= 0.2:
            kb.op("dve", lambda e: e.memset(rr[:], 0.0), [], [rrB])
            kb.op("dve", lambda e: e.tensor_copy(rr[:, 0:128], xT[:, 0, :]), [xTB], [rrB])
            kb.dma(x_out[i * 128:(i + 1) * 128, :], rr[:], [rrB], [xoutB])
            continue
        for c in range(4):
            for k in range(8):
                mm(kb, P2[:, c * 128:(c + 1) * 128], W0[:, k, 1280 + c * 128:1280 + (c + 1) * 128], xT[:, k, :],
                   [W0B, xTB], [B2a], start=(k == 0), stop=(k == 7), inc=(k == 7 and c == 3))
        kb.op("act", lambda e: e.copy(qiT[:].rearrange("p c t -> p (c t)"), P2[:, 0:512]), [B2a], [qiTB])
        if stage <= 0.25:
            kb.op("dve", lambda e: e.memset(rr[:], 0.0), [], [rrB])
            kb.op("dve", lambda e: e.tensor_copy(rr[:, 0:128], xT[:, 0, :]), [xTB], [rrB])
            kb.dma(x_out[i * 128:(i + 1) * 128, :], rr[:], [rrB], [xoutB])
            continue
        for k in range(8):
            mm(kb, P2[:, 512:640], Wk2[:, k, :], xT[:, k, :], [Wk2B, xTB], [B2b], start=(k == 0), stop=(k == 7), inc=False)
        for k in range(8):
            mm(kb, P2[:, 640:896], xT[:, k, :], W0[:, k, 1024:1280], [W0B, xTB], [B2b], start=(k == 0), stop=(k == 7), inc=False)
        for k in range(8):
            mm(kb, P2[:, 896:960], xT[:, k, :], W0[:, k, 1856:1920], [W0B, xTB], [B2b], start=(k == 0), stop=(k == 7), inc=(k == 7))
        if stage <= 0.27:
            kb.op("dve", lambda e: e.memset(rr[:], 0.0), [], [rrB])
            kb.op("dve", lambda e: e.tensor_copy(rr[:, 0:128], xT[:, 0, :]), [xTB], [rrB])
            kb.dma(x_out[i * 128:(i + 1) * 128, :], rr[:], [rrB], [xoutB])
            continue
        kb.op("dve", lambda e: e.tensor_copy(kT2[:, i * 128:(i + 1) * 128], P2[:, 512:640]), [B2b], [kT2B])
        if stage <= 0.28:
            kb.op("dve", lambda e: e.memset(rr[:], 0.0), [], [rrB])
            kb.op("dve", lambda e: e.tensor_copy(rr[:, 0:128], xT[:, 0, :]), [xTB], [rrB])
            kb.dma(x_out[i * 128:(i + 1) * 128, :], rr[:], [rrB], [xoutB])
            continue
        kb.op("dve", lambda e: e.tensor_copy(wsb[:], P2[:, 896:904]), [B2b], [wsbB])
        if stage <= 0.29:
            kb.op("dve", lambda e: e.memset(rr[:], 0.0), [], [rrB])
            kb.op("dve", lambda e: e.tensor_copy(rr[:, 0:128], xT[:, 0, :]), [xTB], [rrB])
            kb.dma(x_out[i * 128:(i + 1) * 128, :], rr[:], [rrB], [xoutB])
            continue
        kb.op("dve", lambda e: e.tensor_copy(cf[:], P2[:, 640:896]), [B2b], [cfB])
        if stage <= 0.3:
            kb.op("dve", lambda e: e.memset(rr[:], 0.0), [], [rrB])
            kb.op("dve", lambda e: e.tensor_copy(rr[:, 0:128], xT[:, 0, :]), [xTB], [rrB])
            kb.dma(x_out[i * 128:(i + 1) * 128, :], rr[:], [rrB], [xoutB])
            continue
        for nn in range(2):
            for k in range(8):
                mm(kb, P0[:, nn * 512:(nn + 1) * 512], xT[:, k, :], W0[:, k, 1864 + nn * 512:1864 + (nn + 1) * 512],
                   [W0B, xTB], [B0a if nn == 0 else B0b], start=(k == 0), stop=(k == 7), inc=(k == 7))
        kb.op("act", lambda e: e.activation(sz[:], P0[:], AF.Silu), [B0a, B0b], [szB])
        if stage <= 0.4:
            kb.op("dve", lambda e: e.memset(rr[:], 0.0), [], [rrB])
            kb.op("dve", lambda e: e.tensor_copy(rr[:, 0:128], xT[:, 0, :]), [xTB], [rrB])
            kb.dma(x_out[i * 128:(i + 1) * 128, :], rr[:], [rrB], [xoutB])
            continue
        kb.op("act", lambda e: e.activation(cjunk[:], cf[:], AF.Square, accum_out=ssc[:]), [cfB], [cjunkB, sscB])
        kb.op("dve", lambda e: e.tensor_scalar(ssc[:], ssc[:], 1.0 / 256, EPS, ALU.mult, ALU.add), [sscB], [sscB])
        kb.op("act", lambda e: e.activation(ssc[:], ssc[:], AF.Sqrt), [sscB], [sscB])
        kb.op("dve", lambda e: e.reciprocal(ssc[:], ssc[:]), [sscB], [sscB])
        kb.op("dve", lambda e: e.scalar_tensor_tensor(cN[:, i, :], cf[:], ssc[:], Gkv[:], ALU.mult, ALU.mult),
              [cfB, sscB, GkvB], [cNB])
        for cc in range(2):
            tr(kb, P16[:, cc * 128:(cc + 1) * 128], cN[:, i, cc * 128:(cc + 1) * 128], identB[:], [cNB, cB], [B16], inc=(cc == 1))
        kb.op("dve", lambda e: e.tensor_copy(cT[:, :, i * 128:(i + 1) * 128], P16[:, 0:256].rearrange("p (c t) -> p c t", c=2)),
              [B16], [cTB])
        if stage <= 0.6:
            kb.op("dve", lambda e: e.memset(rr[:], 0.0), [], [rrB])
            kb.op("dve", lambda e: e.tensor_copy(rr[:, 0:128], xT[:, 0, :]), [xTB], [rrB])
            kb.dma(x_out[i * 128:(i + 1) * 128, :], rr[:], [rrB], [xoutB])
            continue
        for cc in range(2):
            Pq = P0 if cc == 0 else P1
            for h in range(8):
                bq = (B0a, B0b, B1a, B1b)[cc * 2 + (h // 4)]
                mm(kb, Pq[:, h * 128:(h + 1) * 128], Wuk[:, h, cc * 128:(cc + 1) * 128], qT[:, h, :], [WukB, qTB], [bq],
                   inc=(h == 7))
        kb.op("act", lambda e: e.activation(qlT[:, 0, :], P0[:], AF.Copy, scale=scale_q), [B0a, B0b], [qlTB])
        kb.op("dve", lambda e: e.tensor_scalar(qlT[:, 1, :], P1[:], scale_q, None, ALU.mult), [B1a, B1b], [qlTB])

        if stage <= 1:
            kb.op("dve", lambda e: e.tensor_copy(rr[:, 0:256], cN[:, i, :]), [cNB], [rrB])
            kb.op("dve", lambda e: e.tensor_copy(rr[:, 256:1024], qlT[:, 0, 0:768]), [qlTB], [rrB])
            kb.dma(x_out[i * 128:(i + 1) * 128, :], rr[:], [rrB], [xoutB])
            continue
        nkc = (n + 511) // 512
        slots = [(PX, BX, slice(0, 512)), (P2, B2a, slice(0, 512)), (P2, B2b, slice(512, 1024))]
        u = 0
        for kc in range(nkc):
            wd = min(512, n - 512 * kc)
            c0 = kc * 512
            for h in range(8):
                Pt, Bt, sl = slots[u % 3]
                R, RB = RR[u % 3]
                u += 1
                pr = slice((h % 2) * 64, (h % 2) * 64 + 64)
                mm(kb, Pt[:, sl.start:sl.start + wd], qiT[pr, h // 2, :], kT2[pr, c0:c0 + wd], [qiTB, kT2B], [Bt])
                kb.op("act", lambda e: e.activation(R[:, 0:wd], Pt[:, sl.start:sl.start + wd], AF.Relu), [Bt], [RB])
                if h == 0:
                    kb.op("dve", lambda e: e.tensor_scalar(SC[:, c0:c0 + wd], R[:, 0:wd], wsb[:, 0:1], None, ALU.mult),
                          [RB, wsbB], [SCB])
                else:
                    kb.op("dve", lambda e: e.scalar_tensor_tensor(SC[:, c0:c0 + wd], R[:, 0:wd], wsb[:, h:h + 1], SC[:, c0:c0 + wd],
                                                                   ALU.mult, ALU.add), [RB, wsbB, SCB], [SCB])
        kb.op("dve", lambda e: e.tensor_tensor(SC[:, i * 128:n], SC[:, i * 128:n], CM[:], ALU.add), [SCB, cB], [SCB])

        MBall = [MB, MBa]
        if i >= 2:
            nd = 128 * max(1, int(round(0.45 * (i + 1))))
            na = n - nd
            kb.op("dve", lambda e: e.memset(mid[:], 0.0), [], [midB])
            for s in range(BIS_STEPS):
                wk = float(-BIS_LO / (2 ** s))
                last = (s == BIS_STEPS - 1)
                kb.op("dve", lambda e: e.tensor_scalar(M[:, 0:nd], SC[:, 0:nd], mid[:], None, ALU.is_ge, ALU.add, accum_out=cnt[:]),
                      [SCB, midB], [MB, cntB])
                kb.op("act", lambda e: e.activation(M[:, nd:n], SC[:, nd:n], AF.Sign, bias=mid[:], scale=-1.0, accum_out=cntA[:]),
                      [SCB, midB], [MBa, cntAB])
                kb.op("dve", lambda e: e.scalar_tensor_tensor(ge[:], cnt[:], 2.0, cntA[:], ALU.mult, ALU.subtract), [cntB, cntAB], [geB])
                kb.op("dve", lambda e: e.tensor_scalar(ge[:], ge[:], float(511 - na), wk, ALU.is_ge, ALU.mult), [geB], [geB])
                if not last:
                    kb.op("dve", lambda e: e.scalar_tensor_tensor(mid[:], ge[:], -0.5 * wk, mid[:], ALU.add, ALU.add), [geB, midB], [midB])
                else:
                    kb.op("dve", lambda e: e.scalar_tensor_tensor(lo[:], ge[:], -wk, mid[:], ALU.add, ALU.add), [geB, midB], [loB])
            kb.op("dve", lambda e: e.tensor_scalar(M[:, 0:n], SC[:, 0:n], lo[:], -30000.0, ALU.is_lt, ALU.mult), [SCB, loB], MBall)
        else:
            kb.op("dve", lambda e: e.tensor_scalar(M[:, 0:n], SC[:, 0:n], -1e29, -30000.0, ALU.is_lt, ALU.mult), [SCB], MBall)
        nbk = i + 1
        kb.op("dve", lambda e: e.tensor_reduce(bh[:, 0:nbk], M[:, 0:n].rearrange("p (j s) -> p j s", s=128), AX.X, ALU.max), MBall, [bhB])
        kb.op("dve", lambda e: e.tensor_tensor(bh[:, 0:nbk], bh[:, 0:nbk], CMISC[:, 0:nbk], ALU.add), [bhB, cB], [bhB])
        kb.op("dve", lambda e: e.reduce_max(jm[:], bh[:, 0:nbk], AX.X), [bhB], [jmB])
        kb.op("dve", lambda e: e.tensor_scalar(jm[:], jm[:], -1.0, float(i + 1), ALU.mult, ALU.add), [jmB], [jmB])
        kb.op("dve", lambda e: e.tensor_scalar(VH[:].rearrange("p h o -> p (h o)"), CMISC[:, 32:40], jm[:], None, ALU.mult), [jmB, cB], [VHB])
        kb.op("dve", lambda e: e.tensor_tensor(VHD[:], CMISC[:, 40:104].rearrange("p (h g) -> p h g", g=8), VH[:].to_broadcast([128, 8, 8]), ALU.mult),
              [VHB, cB], [VHDB])
        for h in range(8):
            tr(kb, P16[0:8, h * 128:(h + 1) * 128], VHD[:, h, :], identB[:], [VHDB, cB], [B16], inc=(h == 7))
        kb.op("act", lambda e: e.copy(SH[:], P16[0:8, :]), [B16], [SHB])
        if stage <= 2:
            kb.op("dve", lambda e: e.tensor_copy(rr[:, 0:128], M[:, 0:128]), [MB], [rrB])
            kb.op("dve", lambda e: e.tensor_copy(rr[:, 128:256], SC[:, 0:128]), [SCB], [rrB])
            kb.dma(x_out[i * 128:(i + 1) * 128, :], rr[:], [rrB], [xoutB])
            continue
        for j0 in range(0, i + 1, 8):
            nj = min(8, i + 1 - j0)
            for jj in range(nj):
                j = j0 + jj
                tr(kb, P16[:, jj * 128:(jj + 1) * 128], M[:, j * 128:(j + 1) * 128], identB[:], [MB, MBa, cB], [B16], inc=(jj == nj - 1))
            kb.op("act", lambda e: e.copy(MT[:, j0:j0 + nj, :].rearrange("p j t -> p (j t)"), P16[:, 0:nj * 128]), [B16], [MTB])

        jlist = list(range(i + 1))
        if os.environ.get("K_JLAST"):
            jlist = [i]
        units = [(j, hb) for j in jlist for hb in range(2)]

        def emit_st(j, hb):
            bS = B0a if hb == 0 else B0b
            for cc in range(2):
                mm(kb, P0[:, hb * 512:(hb + 1) * 512], cT[:, cc, j * 128:(j + 1) * 128], qlT[:, cc, hb * 512:(hb + 1) * 512],
                   [cTB, qlTB], [bS], start=(cc == 0), stop=False, inc=False)
            mm(kb, P0[:, hb * 512:(hb + 1) * 512], ones[0:8, :], SH[:, hb * 512:(hb + 1) * 512], [SHB, cB], [bS],
               start=False, stop=False, inc=False)
            mm(kb, P0[:, hb * 512:(hb + 1) * 512], identB[:], MT[:, j:j + 1, :].to_broadcast([128, 4, 128]), [MTB, cB], [bS],
               start=False, stop=True, inc=True)

        def emit_rest(j, hb):
            d = i - j
            bS = B0a if hb == 0 else B0b
            Pt_ = PT[j % 2][0]
            PtB = PTB[j % 2][hb]
            for h in range(4 * hb, 4 * hb + 4):
                kb.op("act", lambda e: e.activation(Pt_[:, h, :], P0[:, h * 128:(h + 1) * 128], AF.Exp,
                                                     bias=BIAS[:, h * 32 + d:h * 32 + d + 1], scale=1.0), [bS, cB], [PtB])
            for cc in range(2):
                Po = P1 if cc == 0 else P2
                mm(kb, Po[:, hb * 512:(hb + 1) * 512], cN[:, j, cc * 128:(cc + 1) * 128],
                   Pt_[:, 4 * hb:4 * hb + 4, :].rearrange("p h t -> p (h t)"), [cNB, PtB], [OPB[cc][hb]], start=True, stop=True, inc=False)
            for h in range(4 * hb, 4 * hb + 4):
                mm(kb, PX[:, h:h + 1], Pt_[:, h, :], ones[:, 0:1], [PtB, cB], [BXh[hb]], start=(h % 4 == 0), stop=(h % 4 == 3), inc=(h % 4 == 3))
            first = (j == jlist[0])
            hs = slice(hb * 512, (hb + 1) * 512)
            ds = slice(4 * hb, 4 * hb + 4)
            if first:
                kb.op("dve", lambda e: e.tensor_copy(dens[:, ds], PX[:, ds]), [BXh[hb]], [DNB[hb]])
            else:
                kb.op("dve", lambda e: e.tensor_tensor(dens[:, ds], dens[:, ds], PX[:, ds], ALU.add), [BXh[hb], DNB[hb]], [DNB[hb]])
            for cc in range(2):
                Po = P1 if cc == 0 else P2
                if first:
                    kb.op("dve", lambda e: e.tensor_copy(OLs[:, cc, hs], Po[:, hs]), [OPB[cc][hb]], [OLB[cc][hb]])
                else:
                    kb.op("dve", lambda e: e.tensor_tensor(OLs[:, cc, hs], OLs[:, cc, hs], Po[:, hs], ALU.add), [OPB[cc][hb], OLB[cc][hb]], [OLB[cc][hb]])

        emit_st(*units[0])
        for ui, (j, hb) in enumerate(units):
            if ui + 1 < len(units):
                emit_st(*units[ui + 1])
            emit_rest(j, hb)

        kb.op("dve", lambda e: e.reciprocal(rden[:], dens[:]), DNB, [rdenB])
        kb.op("act", lambda e: e.copy(olT[:, 0, :], OLs[:, 0, :]), OLB[0], [olTB])
        kb.op("dve", lambda e: e.tensor_copy(olT[:, 1, :], OLs[:, 1, :]), OLB[1], [olTB])
        for h in range(8):
            for cc in range(2):
                mm(kb, P0[:, h * 128:(h + 1) * 128], olT[:, cc, h * 128:(h + 1) * 128], Wuv[:, h, cc, :], [olTB, WuvB],
                   [B0a if h < 4 else B0b], start=(cc == 0), stop=(cc == 1), inc=(cc == 1 and h == 7))
        for h in range(8):
            kb.op("dve", lambda e: e.scalar_tensor_tensor(og[:, h * 128:(h + 1) * 128], P0[:, h * 128:(h + 1) * 128], rden[:, h:h + 1],
                                                           sz[:, h * 128:(h + 1) * 128], ALU.mult, ALU.mult),
                  [B0a if h < 4 else B0b, rdenB, szB], [ogB])
        for k in range(8):
            tr(kb, P16[:, k * 128:(k + 1) * 128], og[:, k * 128:(k + 1) * 128], identB[:], [ogB, cB], [B16], inc=(k == 7))
        kb.op("act", lambda e: e.copy(ogT[:].rearrange("p k t -> p (k t)"), P16[:]), [B16], [ogTB])
        for nn in range(2):
            for k in range(8):
                mm(kb, P1[:, nn * 512:(nn + 1) * 512], ogT[:, k, :], Wout[:, k, nn * 512:(nn + 1) * 512], [ogTB, WoutB],
                   [B1a if nn == 0 else B1b], start=(k == 0), stop=(k == 7), inc=(k == 7))
        kb.op("dve", lambda e: e.scalar_tensor_tensor(rr[:], xf[:], ALPHA, P1[:], ALU.mult, ALU.add), [xfB, B1a, B1b], [rrB])
        layernorm_store(kb, st, rr[:], rrB, x_out[i * 128:(i + 1) * 128, :], xoutB, LG, LB, [LGB, LBB])

    kb.finish_layer = True
    kb.es = kb_es
    return es


def load_xT(kb, x_in, xinB, i, xf, xfB, xT, xTB, Ptr, Bs, identF, cB):
    kb.dma(xf[:], x_in[i * 128:(i + 1) * 128, :], [xinB], [xfB])
    for k in range(8):
        tr(kb, Ptr[:, k * 128:(k + 1) * 128], xf[:, k * 128:(k + 1) * 128], identF[:], [xfB, cB], Bs, inc=(k == 7))
    kb.op("act", lambda e: e.copy(xT[:].rearrange("p k t -> p (k t)"), Ptr[:, 0:1024]), Bs, [xTB])


def emit_layer1a(nc, kb, dr, x_in, xinB, hn, hnB, cst, nblk=NB):
    es = ExitStack()
    kb_es = kb.es
    kb.es = es
    sb = kb.sb
    identB, identF, ones, cB, TRIf = cst["identB"], cst["identF"], cst["ones"], cst["buf"], cst["TRIf"]
    win = dr["b_w_in"].rearrange("(k p) n -> p k n", p=128)
    Wqk, WqkB = sb("Wqk", [128, 8, 2048], BF16)
    Wv, WvB = sb("Wv", [128, 8, 2048], BF16)
    Wg, WgB = sb("Wg", [128, 8, 8], BF16)
    for k in range(0, 8, 4):
        kb.dma(Wqk[:, k:k + 4, :], win[:, k:k + 4, 0:2048], [], [WqkB], q="pool")
        kb.dma(Wv[:, k:k + 4, :], win[:, k:k + 4, 2048:4096], [], [WvB], q="pool")
    kb.dma(Wg[:], win[:, :, 4096:4104], [], [WgB], q="pool")
    CWr, CWrB = sb("CWr", [80, 128], F32)
    CWT, CWTB = sb("CWT", [128, 80], F32)
    kb.dma(CWr[0:64, :], dr["b_conv_w"].rearrange("j (c p) -> (j c) p", p=128), [], [CWrB])
    kb.dma(CWr[64:80, :], dr["b_conv_b"].rearrange("o (c p) -> (o c) p", p=128), [], [CWrB])
    GBt, GBB = sb("GBt", [128, 8], F32)
    kb.dma(GBt[:, 0:4], dr["b_i_bias"].partition_broadcast(128), [], [GBB])
    kb.dma(GBt[:, 4:8], dr["b_f_bias"].partition_broadcast(128), [], [GBB])
    HG, HGB = sb("HG", [128, 2048], F32)
    kb.dma(HG[:], dr["b_head_norm_g"].rearrange("o h v -> o (h v)").partition_broadcast(128), [], [HGB])

    PA = kb.ps("L1PA", [128, 2048], F32)
    PB = kb.ps("L1PB", [128, 1024], F32)
    PC = kb.ps("L1PC", [128, 512], F32)
    P16 = kb.ps("L1P16", [128, 1024], BF16)
    BA = [kb.buf("pa%d" % i) for i in range(4)]
    BBa, BBb, BC, B16 = kb.buf("pba"), kb.buf("pbb"), kb.buf("pc"), kb.buf("p16")
    BRW = [kb.buf("brw%d" % s_) for s_ in range(2)]; BST = [kb.buf("bst%d" % s_) for s_ in range(2)]
    BDN = [kb.buf("bdn%d" % s_) for s_ in range(2)]; BNM = [kb.buf("bnm%d" % s_) for s_ in range(2)]; BNU = kb.buf("bnu")
    REG = {0: BRW, 1: BST, 2: BDN + [BNU], 3: [BNM[1]]}

    tr(kb, PC[:, 0:80], CWr[:], identF[0:80, 0:80], [CWrB, cB], [BC])
    kb.op("dve", lambda e: e.tensor_copy(CWT[:], PC[:, 0:80]), [BC], [CWTB])

    xf, xfB = sb("xf1", [128, 1024], F32)
    xT, xTB = sb("xT1", [128, 8, 128], BF16)
    QKP, QKPB = sb("QKP", [128, 16, 131], F32)
    tmpc, tmpcB = sb("tmpc", [128, 128], F32)
    qkT, qkTB = sb("qkT", [128, 16, 128], BF16)
    vt, vtB = sb("vt", [128, 2048], BF16)
    gt, gtB = sb("gt", [128, 8], F32)
    lf, lfB = sb("lf", [128, 4], F32)
    ibm, ibmB = sb("ibm", [128, 4], F32)
    bcol, bcolB = sb("bcol", [128, 4], F32)
    ONESf, ONESfB = sb("ONESf", [128, 128], F32)
    LFB_2 = [sb("LFB%d" % s_, [128, 128], F32) for s_ in range(2)]
    EB_2 = [sb("EB%d" % s_, [128, 128], F32) for s_ in range(2)]
    DT_2 = [sb("DT%d" % s_, [128, 128], F32) for s_ in range(2)]
    sT_2 = [sb("sT%d" % s_, [128, 128], BF16) for s_ in range(2)]
    qh_2 = [sb("qh%d" % s_, [128, 2, 128], BF16) for s_ in range(2)]
    kTok_2 = [sb("kTok%d" % s_, [128, 256], BF16) for s_ in range(2)]
    vs_2 = [sb("vs%d" % s_, [128, 512], BF16) for s_ in range(2)]
    wcol_2 = [sb("wcol%d" % s_, [128, 1], BF16) for s_ in range(2)]
    hh_2 = [sb("hh%d" % s_, [128, 512], F32) for s_ in range(2)]
    hjunk_2 = [sb("hjunk%d" % s_, [128, 512], BF16) for s_ in range(2)]
    HN, HNB = sb("HN", [128, 2048], BF16)
    rec_2 = [sb("rec%d" % s_, [128, 1], F32) for s_ in range(2)]
    ssh_2 = [sb("ssh%d" % s_, [128, 1], F32) for s_ in range(2)]
    Cs = [sb("C%d" % h, [128, 2, 512], F32) for h in range(4)]
    Cb = [sb("Cb%d" % h, [128, 2, 512], BF16) for h in range(4)]
    ns = [sb("n%d" % h, [128, 2], F32) for h in range(4)]
    nb = [sb("nb%d" % h, [128, 2], BF16) for h in range(4)]
    kb.op("dve", lambda e: e.memset(ONESf[:], 1.0), [], [ONESfB])
    kb.op("dve", lambda e: e.memset(QKP[:], 0.0), [], [QKPB])
    for h in range(4):
        kb.op("dve", lambda e: e.memset(Cs[h][0][:], 0.0), [], [Cs[h][1]])
        kb.op("dve", lambda e: e.memset(Cb[h][0][:], 0.0), [], [Cb[h][1]])
        kb.op("dve", lambda e: e.memset(ns[h][0][:], 0.0), [], [ns[h][1]])
        kb.op("dve", lambda e: e.memset(nb[h][0][:], 0.0), [], [nb[h][1]])

    for i in range(nblk):
        load_xT(kb, x_in, xinB, i, xf, xfB, xT, xTB, PB, [BBa, BBb, BNM[0]], identF, cB)
        for c in range(16):
            for k in range(8):
                mm(kb, PA[:, c * 128:(c + 1) * 128], Wqk[:, k, c * 128:(c + 1) * 128], xT[:, k, :], [WqkB, xTB], [BA[c // 4]] + REG[c // 4],
                   start=(k == 0), stop=(k == 7), inc=(k == 7 and c % 4 == 3))
        kb.op("dve", lambda e: e.tensor_copy(QKP[:, 0:8, 3:131], PA[:, 0:1024].rearrange("p (c t) -> p c t", c=8)), [BA[0], BA[1]], [QKPB])
        kb.op("dve", lambda e: e.tensor_copy(QKP[:, 8:16, 3:131], PA[:, 1024:2048].rearrange("p (c t) -> p c t", c=8)), [BA[2], BA[3]], [QKPB])
        for c in range(16):
            kb.op("dve", lambda e: e.tensor_scalar(tmpc[:], QKP[:, c, 0:128], CWT[:, c:c + 1], CWT[:, 64 + c:65 + c], ALU.mult, ALU.add),
                  [QKPB, CWTB], [tmpcB])
            for j in range(1, 4):
                kb.op("dve", lambda e: e.scalar_tensor_tensor(tmpc[:], QKP[:, c, j:j + 128], CWT[:, j * 16 + c:j * 16 + c + 1], tmpc[:],
                                                               ALU.mult, ALU.add), [QKPB, CWTB, tmpcB], [tmpcB])
            kb.op("act", lambda e: e.activation(qkT[:, c, :], tmpc[:], AF.Silu), [tmpcB], [qkTB])
        kb.op("dve", lambda e: e.tensor_copy(tmpc[:, 0:48].rearrange("p (c t) -> p c t", c=16), QKP[:, :, 128:131]), [QKPB], [tmpcB])
        kb.op("dve", lambda e: e.tensor_copy(QKP[:, :, 0:3], tmpc[:, 0:48].rearrange("p (c t) -> p c t", c=16)), [tmpcB], [QKPB])
        for nn in range(4):
            for k in range(8):
                mm(kb, PA[:, nn * 512:(nn + 1) * 512], xT[:, k, :], Wv[:, k, nn * 512:(nn + 1) * 512], [WvB, xTB], [BA[nn]],
                   start=(k == 0), stop=(k == 7), inc=(k == 7))
        kb.op("act", lambda e: e.copy(vt[:, 0:1024], PA[:, 0:1024]), [BA[0], BA[1]], [vtB])
        kb.op("dve", lambda e: e.tensor_copy(vt[:, 1024:2048], PA[:, 1024:2048]), [BA[2], BA[3]], [vtB])
        for k in range(8):
            mm(kb, PC[:, 0:8], xT[:, k, :], Wg[:, k, :], [WgB, xTB], [BC], start=(k == 0), stop=(k == 7), inc=(k == 7))
        kb.op("dve", lambda e: e.tensor_tensor(gt[:], PC[:, 0:8], GBt[:], ALU.add), [BC, GBB], [gtB])
        kb.op("act", lambda e: e.activation(lf[:], gt[:, 4:8], AF.Exp, scale=-1.0), [gtB], [lfB])
        kb.op("act", lambda e: e.activation(lf[:], lf[:], AF.Ln, bias=1.0), [lfB], [lfB])
        kb.op("dve", lambda e: e.tensor_scalar(lf[:], lf[:], -1.0, None, ALU.mult), [lfB], [lfB])
        mm(kb, PC[:, 8:12], TRIf[:], lf[:], [lfB, cB], [BC])
        kb.op("dve", lambda e: e.tensor_copy(bcol[:], PC[:, 8:12]), [BC], [bcolB])
        kb.op("dve", lambda e: e.tensor_tensor(ibm[:], gt[:, 0:4], bcol[:], ALU.subtract), [gtB, bcolB], [ibmB])
        for h in range(4):
            par = h % 2
            LFB, LFBB = LFB_2[par]; EB, EBB = EB_2[par]; DT, DTB = DT_2[par]; sT, sTB = sT_2[par]; qh, qhB = qh_2[par]
            kTok, kTokB = kTok_2[par]; vs, vsB = vs_2[par]; wcol, wcolB = wcol_2[par]; hh, hhB = hh_2[par]
            hjunk, hjunkB = hjunk_2[par]; rec, recB = rec_2[par]; ssh, sshB = ssh_2[par]
            pBRW = PA[:, par * 128:par * 128 + 128]; bBRW = BRW[par]
            pBST = PA[:, 512 + par * 128:512 + par * 128 + 128]; bBST = BST[par]
            pDN = PA[:, 1024 + par * 8:1024 + par * 8 + 1]; bDN = BDN[par]
            pNU = PA[:, 1088:1089]
            pNM = PB[:, 0:512] if par == 0 else PA[:, 1536:2048]; bNM = BNM[par]; aNM = BBa if par == 0 else BA[3]
            C, CB_ = Cs[h]
            Cbh, CbB = Cb[h]
            nh, nhB = ns[h]
            nbh, nbB = nb[h]
            kb.op("dve", lambda e: e.tensor_scalar(LFB[:], ONESf[:], lf[:, h:h + 1], None, ALU.mult), [ONESfB, lfB], [LFBB])
            mm(kb, pBRW, LFB[:], TRIf[:], [LFBB, cB, BA[0]], [bBRW])
            kb.op("act", lambda e: e.activation(EB[:], pBRW, AF.Exp), [bBRW], [EBB])
            kb.op("act", lambda e: e.activation(DT[:], pBRW, AF.Exp, bias=ibm[:, h:h + 1]), [bBRW, ibmB], [DTB])
            kb.op("dve", lambda e: e.tensor_copy(wcol[:], DT[:, 127:128]), [DTB], [wcolB])
            kb.op("dve", lambda e: e.tensor_scalar(vs[:], vt[:, h * 512:(h + 1) * 512], DT[:, 127:128], None, ALU.mult), [vtB, DTB], [vsB])
            kb.op("dve", lambda e: e.tensor_tensor(DT[:], DT[:], TRIf[:], ALU.mult), [DTB, cB], [DTB])
            for cc in range(2):
                kb.op("dve", lambda e: e.scalar_tensor_tensor(qh[:, cc, :], qkT[:, 2 * h + cc, :], 0.0625, EB[:], ALU.mult, ALU.mult),
                      [qkTB, EBB], [qhB])
            for cc in range(2):
                mm(kb, pBST, qkT[:, 8 + 2 * h + cc, :], qkT[:, 2 * h + cc, :], [qkTB, BA[1]], [bBST], start=(cc == 0), stop=(cc == 1), inc=(cc == 1))
            kb.op("dve", lambda e: e.scalar_tensor_tensor(sT[:], pBST, 0.0625, DT[:], ALU.mult, ALU.mult), [bBST, DTB], [sTB])
            mm(kb, pNM, sT[:], vt[:, h * 512:(h + 1) * 512], [sTB, vtB, aNM], [bNM], start=True, stop=False, inc=False)
            for cc in range(2):
                mm(kb, pNM, qh[:, cc, :], Cbh[:, cc, :], [qhB, CbB, aNM], [bNM], start=False, stop=(cc == 1), inc=(cc == 1))
            mm(kb, pDN, sT[:], ones[:, 0:1], [sTB, cB, BA[2]], [bDN], start=True, stop=False, inc=False)
            for cc in range(2):
                mm(kb, pDN, qh[:, cc, :], nbh[:, cc:cc + 1], [qhB, nbB, BA[2]], [bDN], start=False, stop=(cc == 1), inc=(cc == 1))
            kb.op("dve", lambda e: e.tensor_scalar(rec[:], pDN, -1.0, None, ALU.mult), [bDN], [recB])
            kb.op("dve", lambda e: e.tensor_tensor(rec[:], rec[:], pDN, ALU.max), [bDN, recB], [recB])
            kb.op("dve", lambda e: e.tensor_scalar(rec[:], rec[:], 1.0, None, ALU.max), [recB], [recB])
            kb.op("dve", lambda e: e.reciprocal(rec[:], rec[:]), [recB], [recB])
            kb.op("dve", lambda e: e.tensor_scalar(hh[:], pNM, rec[:], None, ALU.mult), [bNM, recB], [hhB])
            kb.op("act", lambda e: e.activation(hjunk[:], hh[:], AF.Square, accum_out=ssh[:]), [hhB], [hjunkB, sshB])
            kb.op("dve", lambda e: e.tensor_scalar(ssh[:], ssh[:], 1.0 / 512, EPS, ALU.mult, ALU.add), [sshB], [sshB])
            kb.op("act", lambda e: e.activation(ssh[:], ssh[:], AF.Sqrt), [sshB], [sshB])
            kb.op("dve", lambda e: e.reciprocal(ssh[:], ssh[:]), [sshB], [sshB])
            kb.op("dve", lambda e: e.scalar_tensor_tensor(HN[:, h * 512:(h + 1) * 512], hh[:], ssh[:], HG[:, h * 512:(h + 1) * 512],
                                                           ALU.mult, ALU.mult), [hhB, sshB, HGB], [HNB])
            for cc in range(2):
                tr(kb, P16[:, cc * 128:(cc + 1) * 128], qkT[:, 8 + 2 * h + cc, :], identB[:], [qkTB, cB], [B16], inc=(cc == 1))
            kb.op("act", lambda e: e.copy(kTok[:], P16[:, 0:256]), [B16], [kTokB])
            for cc in range(2):
                mm(kb, PB[:, 512:1024], kTok[:, cc * 128:(cc + 1) * 128], vs[:], [kTokB, vsB], [BBb])
                kb.op("dve", lambda e: e.scalar_tensor_tensor(C[:, cc, :], C[:, cc, :], EB[:, 127:128], PB[:, 512:1024], ALU.mult, ALU.add),
                      [CB_, EBB, BBb], [CB_])
                mm(kb, pNU, kTok[:, cc * 128:(cc + 1) * 128], wcol[:], [kTokB, wcolB, BA[2]], [BNU])
                kb.op("dve", lambda e: e.scalar_tensor_tensor(nh[:, cc:cc + 1], nh[:, cc:cc + 1], EB[:, 127:128], pNU, ALU.mult, ALU.add),
                      [nhB, EBB, BNU], [nhB])
            kb.op("act", lambda e: e.copy(Cbh[:].rearrange("p c v -> p (c v)"), C[:].rearrange("p c v -> p (c v)")), [CB_], [CbB])
            kb.op("dve", lambda e: e.tensor_copy(nbh[:], nh[:]), [nhB], [nbB])
        kb.dma(hn[i * 128:(i + 1) * 128, :], HN[:], [HNB], [hnB])
    kb.es = kb_es
    return es


def emit_layer1b(nc, kb, dr, x_in, xinB, hn, hnB, x_out, xoutB, cst, nblk=NB):
    es = ExitStack()
    kb_es = kb.es
    kb.es = es
    sb = kb.sb
    identB, identF, cB = cst["identB"], cst["identF"], cst["buf"]
    win = dr["b_w_in"].rearrange("(k p) n -> p k n", p=128)
    Wo, WoB = sb("Wo", [128, 8, 2048], BF16)
    Wz, WzB = sb("Wz", [128, 8, 2048], BF16)
    Wout, WoutB = sb("Wout1", [128, 16, 1024], BF16)
    wout = dr["b_w_out"].rearrange("(k p) n -> p k n", p=128)
    for k in range(0, 8, 4):
        kb.dma(Wo[:, k:k + 4, :], win[:, k:k + 4, 4104:6152], [], [WoB], q="pool")
        kb.dma(Wz[:, k:k + 4, :], win[:, k:k + 4, 6152:8200], [], [WzB], q="pool")
    for k in range(0, 16, 8):
        kb.dma(Wout[:, k:k + 8, :], wout[:, k:k + 8, :], [], [WoutB], q="pool")
    LG, LGB = sb("LG1", [128, 1024], F32)
    LB, LBB = sb("LB1", [128, 1024], F32)
    kb.dma(LG[:], dr["b_ln_g"].partition_broadcast(128), [], [LGB])
    kb.dma(LB[:], dr["b_ln_b"].partition_broadcast(128), [], [LBB])
    PA = kb.ps("L2PA", [128, 2048], F32)
    PB = kb.ps("L2PB", [128, 1024], F32)
    P16 = kb.ps("L2P16", [128, 2048], BF16)
    BA = [kb.buf("qa%d" % i) for i in range(4)]
    BBa, BBb, B16 = kb.buf("qba"), kb.buf("qbb"), kb.buf("q16")
    xf, xfB = sb("xf2", [128, 1024], F32)
    xT, xTB = sb("xT2", [128, 8, 128], BF16)
    so, soB = sb("so", [128, 2048], BF16)
    hnb, hnbB = sb("hnb", [128, 2048], BF16)
    hg, hgB = sb("hg", [128, 2048], BF16)
    hgT, hgTB = sb("hgT", [128, 16, 128], BF16)
    rr, rrB = sb("rr2", [128, 1024], F32)
    st = {"s1": sb("s1b", [128, 1], F32), "ss": sb("ssb", [128, 1], F32), "lnjunk": sb("lnjunkb", [128, 1024], BF16)}
    for i in range(nblk):
        load_xT(kb, x_in, xinB, i, xf, xfB, xT, xTB, PB, [BBa, BBb], identF, cB)
        kb.dma(hnb[:], hn[i * 128:(i + 1) * 128, :], [hnB], [hnbB])
        for nn in range(4):
            for k in range(8):
                mm(kb, PA[:, nn * 512:(nn + 1) * 512], xT[:, k, :], Wo[:, k, nn * 512:(nn + 1) * 512], [WoB, xTB], [BA[nn]],
                   start=(k == 0), stop=(k == 7), inc=(k == 7))
        kb.op("act", lambda e: e.activation(so[:], PA[:], AF.Sigmoid), BA, [soB])
        kb.op("dve", lambda e: e.tensor_tensor(hg[:], hnb[:], so[:], ALU.mult), [hnbB, soB], [hgB])
        for nn in range(4):
            for k in range(8):
                mm(kb, PA[:, nn * 512:(nn + 1) * 512], xT[:, k, :], Wz[:, k, nn * 512:(nn + 1) * 512], [WzB, xTB], [BA[nn]],
                   start=(k == 0), stop=(k == 7), inc=(k == 7))
        kb.op("act", lambda e: e.activation(so[:], PA[:], AF.Silu), BA, [soB])
        kb.op("dve", lambda e: e.tensor_tensor(hg[:], hg[:], so[:], ALU.mult), [hgB, soB], [hgB])
        for k in range(16):
            tr(kb, P16[:, k * 128:(k + 1) * 128], hg[:, k * 128:(k + 1) * 128], identB[:], [hgB, cB], [B16], inc=(k == 15))
        kb.op("act", lambda e: e.copy(hgT[:].rearrange("p k t -> p (k t)"), P16[:]), [B16], [hgTB])
        for nn in range(2):
            for k in range(16):
                mm(kb, PB[:, nn * 512:(nn + 1) * 512], hgT[:, k, :], Wout[:, k, nn * 512:(nn + 1) * 512], [hgTB, WoutB],
                   [BBa if nn == 0 else BBb], start=(k == 0), stop=(k == 15), inc=(k == 15))
        kb.op("dve", lambda e: e.scalar_tensor_tensor(rr[:], xf[:], ALPHA, PB[:], ALU.mult, ALU.add), [xfB, BBa, BBb], [rrB])
        layernorm_store(kb, st, rr[:], rrB, x_out[i * 128:(i + 1) * 128, :], xoutB, LG, LB, [LGB, LBB])
    kb.es = kb_es
    return es


def make_consts():
    identF = np.eye(128, dtype=np.float32)
    q = np.arange(128)[:, None]
    s = np.arange(128)[None, :]
    CM = np.where(s <= q, 0.0, -1e30).astype(np.float32)
    slopes = 2.0 ** (-(np.arange(1, 9, dtype=np.float64)))
    sp = np.arange(128, dtype=np.float64)[:, None, None]
    dd = np.arange(32, dtype=np.float64)[None, None, :]
    BIAS = (slopes[None, :, None] * (sp - 127.0 - 128.0 * dd)).reshape(128, 256).astype(np.float32)
    misc = np.zeros((128, 104), np.float32)
    misc[:, 0:32] = np.arange(1, 33, dtype=np.float32)[None, :]
    misc[:, 32:40] = (128.0 * slopes)[None, :]
    misc[:, 40:104] = np.eye(8, dtype=np.float32).reshape(1, 64)
    tri = (np.arange(128)[:, None] <= np.arange(128)[None, :]).astype(np.float32)
    return {"c_ident": identF, "c_cm": CM, "c_bias": BIAS, "c_misc": misc, "c_tri": tri}


def build(layers=LAYERS, nblk=NB):
    nc = bass.Bass("TRN2", target_bir_lowering=False)
    dr = {}

    def din(name, shape):
        dr[name] = nc.dram_tensor(name, list(shape), F32, kind="ExternalInput").ap()

    din("x", [T, D])
    din("a_w_in", [D, A_IN]); din("a_kv_norm_g", [1, 256]); din("a_w_uk", [8, 128, 256]); din("a_w_uv", [8, 256, 128])
    din("a_w_out", [D, D]); din("a_ln_g", [1, D]); din("a_ln_b", [1, D])
    din("c_ident", [128, 128]); din("c_cm", [128, 128]); din("c_bias", [128, 256]); din("c_misc", [128, 104]); din("c_tri", [128, 128])
    din("b_w_in", [D, B_IN]); din("b_i_bias", [1, 4]); din("b_f_bias", [1, 4]); din("b_conv_w", [4, 2048]); din("b_conv_b", [1, 2048])
    din("b_head_norm_g", [1, 4, 512]); din("b_w_out", [2048, D]); din("b_ln_g", [1, D]); din("b_ln_b", [1, D])
    out = nc.dram_tensor("out", [T, D], F32, kind="ExternalOutput").ap()
    with ExitStack() as es:
        kb = KB(nc, es)
        identF, idB = kb.sb("identF", [128, 128], F32)
        identB, _ = kb.sb("identB", [128, 128], BF16)
        CM, _ = kb.sb("CM", [128, 128], F32)
        BIAS, _ = kb.sb("BIAS", [128, 256], F32)
        ones, _ = kb.sb("ones", [128, 128], BF16)
        CMISC, _ = kb.sb("CMISC", [128, 104], F32)
        TRIf, _ = kb.sb("TRIf", [128, 128], F32)
        cB = idB
        kb.dma(identF[:], dr["c_ident"], [], [cB])
        kb.dma(identB[:], dr["c_ident"], [], [cB], q="pool")
        kb.dma(CM[:], dr["c_cm"], [], [cB])
        kb.dma(BIAS[:], dr["c_bias"], [], [cB])
        kb.dma(CMISC[:], dr["c_misc"], [], [cB])
        kb.dma(TRIf[:], dr["c_tri"], [], [cB])
        kb.op("dve", lambda e: e.memset(ones[:], 1.0), [], [cB])
        cst = {"identB": identB, "identF": identF, "CM": CM, "BIAS": BIAS, "ones": ones, "buf": cB, "CMISC": CMISC, "TRIf": TRIf}
        xinB = kb.buf("xin")
        outB = kb.buf("out")
        x1d = nc.dram_tensor("x1d", [T, D], F32).ap()
        hnd = nc.dram_tensor("hnd", [T, 2048], BF16).ap()
        x1B = kb.buf("x1d")
        hnB = kb.buf("hnd")
        if layers == (0,):
            l0 = emit_layer0(nc, kb, dr, dr["x"], xinB, out, outB, cst, nblk=nblk)
            kb.finish(); l0.close()
        elif layers == (1,):
            la = emit_layer1a(nc, kb, dr, dr["x"], xinB, hnd, hnB, cst, nblk=nblk)
            kb.barrier(); la.close()
            lb = emit_layer1b(nc, kb, dr, dr["x"], xinB, hnd, hnB, out, outB, cst, nblk=nblk)
            kb.finish(); lb.close()
        else:
            l0 = emit_layer0(nc, kb, dr, dr["x"], xinB, x1d, x1B, cst, nblk=nblk)
            kb.barrier(); l0.close()
            la = emit_layer1a(nc, kb, dr, x1d, x1B, hnd, hnB, cst, nblk=nblk)
            kb.barrier(); la.close()
            lb = emit_layer1b(nc, kb, dr, x1d, x1B, hnd, hnB, out, outB, cst, nblk=nblk)
            kb.finish(); lb.close()
    return nc


_CACHE = {}


def make_inmaps(inputs, xs):
    cst = make_consts()
    shared = {k: np.ascontiguousarray(inputs[k][0], dtype=np.float32) for k in
              ("a_w_in", "a_w_uk", "a_w_uv", "a_w_out", "b_w_in", "b_conv_w", "b_w_out")}
    for k in ("a_kv_norm_g", "a_ln_g", "a_ln_b", "b_i_bias", "b_f_bias", "b_conv_b", "b_ln_g", "b_ln_b"):
        shared[k] = np.ascontiguousarray(inputs[k], dtype=np.float32).reshape(1, -1)
    shared["b_head_norm_g"] = np.ascontiguousarray(inputs["b_head_norm_g"], dtype=np.float32).reshape(1, 4, 512)
    shared.update(cst)
    maps = []
    for xx in xs:
        m = dict(shared)
        m["x"] = np.ascontiguousarray(xx, dtype=np.float32)
        maps.append(m)
    return maps


def kernel(**inputs):
    x = np.ascontiguousarray(inputs["x"], dtype=np.float32)
    if "nc" not in _CACHE:
        import os
        lay = os.environ.get("K_LAYERS")
        _CACHE["nc"] = build(layers=tuple(int(c) for c in lay)) if lay else build()
    nc = _CACHE["nc"]
    in_maps = make_inmaps(inputs, [x[c % 4] for c in range(8)])
    res = run_bass_kernel_spmd(nc, in_maps, core_ids=list(range(8)))
    return np.stack([res.results[c]["out"] for c in range(4)], axis=0)
```

```python
import numpy as np
from contextlib import ExitStack
import concourse.bass as bass
import concourse.mybir as mybir
from concourse.bass_utils import run_bass_kernel_spmd

F32 = mybir.dt.float32
BF16 = mybir.dt.bfloat16
AF = mybir.ActivationFunctionType
ALU = mybir.AluOpType
AX = mybir.AxisListType

T = 4096
D = 1024
NB = T // 128
A_IN = 2888
B_IN = 8200
ALPHA = float(4 ** 0.25)
EPS = 1e-5
NDMA = 12
BIS_STEPS = 28
BIS_LO = -2048.0
LAYERS = (0, 1)


class Buf:
    __slots__ = ("name", "w", "r")

    def __init__(self, name):
        self.name = name
        self.w = None
        self.r = {}


class KB:
    def __init__(self, nc, es):
        self.nc = nc
        self.es = es
        self.eng = {"pe": nc.tensor, "act": nc.scalar, "dve": nc.vector, "pool": nc.gpsimd, "sp": nc.sync}
        self.sem = {e: es.enter_context(nc.semaphore("s_" + e)) for e in ("pe", "act", "dve", "pool")}
        self.cnt = {e: 0 for e in self.sem}
        self.known = {e: {} for e in self.eng}
        self.dsem = [es.enter_context(nc.semaphore("d%d" % k)) for k in range(NDMA)]
        self.dtot = [0] * NDMA
        self.dnext = 0
        self.nbuf = 0
        self.root_es = es
        self.xsem = {}

    def buf(self, name=None):
        self.nbuf += 1
        return Buf(name or ("b%d" % self.nbuf))

    def sb(self, name, shape, dt):
        t = self.es.enter_context(self.nc.sbuf_tensor(name, list(shape), dt))
        return t, Buf(name)

    def ps(self, name, shape, dt):
        return self.es.enter_context(self.nc.psum_tensor(name, list(shape), dt))

    def _deps(self, reads, writes):
        toks = []
        for b in reads:
            if b.w is not None:
                toks.append(b.w)
        for b in writes:
            if b.w is not None:
                toks.append(b.w)
            toks.extend(b.r.items())
        return toks

    def _wait(self, e, toks):
        need = {}
        for k, v in toks:
            if k == e and e == "pe":
                continue
            if self.known[e].get(k, 0) >= v:
                continue
            if need.get(k, 0) < v:
                need[k] = v
        for k, v in need.items():
            sem = self.sem[k] if isinstance(k, str) else (self.dsem[k[1]] if k[0] == "d" else self.xsem[k])
            self.eng[e].wait_ge(sem, v)
            self.known[e][k] = v

    def _mark(self, tok, reads, writes):
        k, v = tok
        for b in reads:
            if b.r.get(k, 0) < v:
                b.r[k] = v
        for b in writes:
            b.w = tok
            b.r = {}

    def op(self, e, fn, reads=(), writes=(), inc=True):
        self._wait(e, self._deps(reads, writes))
        ins = fn(self.eng[e])
        if inc:
            self.cnt[e] += 1
            ins.then_inc(self.sem[e], 1)
            tok = (e, self.cnt[e])
        else:
            assert e == "pe"
            tok = (e, self.cnt[e] + 1)
        self._mark(tok, reads, writes)
        return tok

    def dma(self, out, in_, reads=(), writes=(), q="sp"):
        toks = self._deps(reads, writes)
        if q == "pool":
            key = ("x", len(self.xsem))
            self.xsem[key] = self.root_es.enter_context(self.nc.semaphore("x%d" % len(self.xsem)))
            self._wait(q, toks)
            self.eng[q].dma_start(out=out, in_=in_).then_inc(self.xsem[key], 16)
            tok = (key, 16)
            self._mark(tok, reads, writes)
            return tok
        k = self.dnext
        self.dnext = (k + 1) % NDMA
        if self.dtot[k] > 0:
            toks.append((("d", k), self.dtot[k]))
        self._wait(q, toks)
        self.dtot[k] += 16
        self.eng[q].dma_start(out=out, in_=in_).then_inc(self.dsem[k], 16)
        tok = (("d", k), self.dtot[k])
        self._mark(tok, reads, writes)
        return tok

    def barrier(self):
        toks = [(("d", k), self.dtot[k]) for k in range(NDMA) if self.dtot[k] > 0]
        toks += [(k, 16) for k in self.xsem]
        toks += [(e, c) for e, c in self.cnt.items() if c > 0]
        for e in self.eng:
            self._wait(e, toks)

    def finish(self):
        toks = [(("d", k), self.dtot[k]) for k in range(NDMA) if self.dtot[k] > 0]
        toks += [(k, 16) for k in self.xsem]
        toks += [(e, c) for e, c in self.cnt.items() if c > 0]
        self._wait("sp", toks)


def mm(kb, out, lhsT, rhs, reads, writes, start=True, stop=True, inc=None):
    if inc is None:
        inc = stop
    return kb.op("pe", lambda e: e.matmul(out, lhsT, rhs, start=start, stop=stop), reads, writes, inc=inc)


def tr(kb, out, in_, ident, reads, writes, inc=True):
    return kb.op("pe", lambda e: e.transpose(out, in_, ident), reads, writes, inc=inc)


def layernorm_store(kb, st, r_ap, rB, out_dram_ap, outB, G, Bt, GB):
    s1, s1B = st["s1"]
    kb.op("dve", lambda e: e.reduce_sum(s1[:], r_ap, AX.X), [rB], [s1B])
    kb.op("dve", lambda e: e.tensor_scalar(s1[:], s1[:], -1.0 / D, None, ALU.mult), [s1B], [s1B])
    kb.op("dve", lambda e: e.tensor_scalar(r_ap, r_ap, s1[:], None, ALU.add), [rB, s1B], [rB])
    junk, junkB = st["lnjunk"]
    ss, ssB = st["ss"]
    kb.op("act", lambda e: e.activation(junk[:], r_ap, AF.Square, accum_out=ss[:]), [rB], [junkB, ssB])
    kb.op("dve", lambda e: e.tensor_scalar(ss[:], ss[:], 1.0 / D, EPS, ALU.mult, ALU.add), [ssB], [ssB])
    kb.op("act", lambda e: e.activation(ss[:], ss[:], AF.Sqrt), [ssB], [ssB])
    kb.op("dve", lambda e: e.reciprocal(ss[:], ss[:]), [ssB], [ssB])
    kb.op("dve", lambda e: e.scalar_tensor_tensor(r_ap, r_ap, ss[:], G[:], ALU.mult, ALU.mult), [rB, ssB] + list(GB), [rB])
    kb.op("dve", lambda e: e.tensor_tensor(r_ap, r_ap, Bt[:], ALU.add), [rB] + list(GB), [rB])
    kb.dma(out_dram_ap, r_ap, [rB], [outB])


def emit_layer0(nc, kb, dr, x_in, xinB, x_out, xoutB, cst, nblk=NB):
    es = ExitStack()
    kb_es = kb.es
    kb.es = es
    sb = kb.sb
    identB, identF, CM, BIAS, ones, cB = cst["identB"], cst["identF"], cst["CM"], cst["BIAS"], cst["ones"], cst["buf"]

    W0, W0B = sb("W0", [128, 8, A_IN], BF16)
    Wk2, Wk2B = sb("Wk2", [128, 8, 128], BF16)
    Wuk, WukB = sb("Wuk", [128, 8, 256], BF16)
    Wuv, WuvB = sb("Wuv", [128, 8, 2, 128], BF16)
    Wout, WoutB = sb("Wout", [128, 8, 1024], BF16)
    Gkv, GkvB = sb("Gkv", [128, 256], F32)
    LG, LGB = sb("LG", [128, 1024], F32)
    LB, LBB = sb("LB", [128, 1024], F32)
    win = dr["a_w_in"].rearrange("(k p) n -> p k n", p=128)
    for k in range(0, 8, 4):
        kb.dma(W0[:, k:k + 4, :], win[:, k:k + 4, :], [], [W0B], q="pool")
    kb.dma(Wk2[:, :, 0:64], win[:, :, 1792:1856], [], [Wk2B], q="pool")
    kb.dma(Wk2[:, :, 64:128], win[:, :, 1792:1856], [], [Wk2B], q="pool")
    kb.dma(Wuk[:], dr["a_w_uk"].rearrange("h d c -> d h c"), [], [WukB], q="pool")
    kb.dma(Wuv[:], dr["a_w_uv"].rearrange("h (cc p) d -> p h cc d", p=128), [], [WuvB], q="pool")
    kb.dma(Wout[:], dr["a_w_out"].rearrange("(k p) n -> p k n", p=128), [], [WoutB], q="pool")
    kb.dma(Gkv[:], dr["a_kv_norm_g"].partition_broadcast(128), [], [GkvB])
    kb.dma(LG[:], dr["a_ln_g"].partition_broadcast(128), [], [LGB])
    kb.dma(LB[:], dr["a_ln_b"].partition_broadcast(128), [], [LBB])

    kT2, kT2B = sb("kT2", [128, T], BF16)
    cN, cNB = sb("cN", [128, NB, 256], BF16)
    cT, cTB = sb("cT", [128, 2, T], BF16)
    SC, SCB = sb("SC", [128, T], F32)
    M, MB = sb("M", [128, T], BF16)
    MT, MTB = sb("MT", [128, NB, 128], BF16)
    XF = [sb("XF%d" % s, [128, 1024], F32) for s in range(1)]
    xT, xTB = sb("xT", [128, 8, 128], BF16)
    qT, qTB = sb("qT", [128, 8, 128], BF16)
    qiT, qiTB = sb("qiT", [128, 4, 128], BF16)
    qlT, qlTB = sb("qlT", [128, 2, 1024], BF16)
    sz, szB = sb("sz", [128, 1024], BF16)
    cf, cfB = sb("cf", [128, 256], F32)
    wsb, wsbB = sb("wsb", [128, 8], F32)
    RR = [sb("R%d" % s, [128, 512], F32) for s in range(3)]
    PT = [sb("PT%d" % s, [128, 8, 128], BF16) for s in range(2)]
    olT, olTB = sb("olT", [128, 2, 1024], BF16)
    og, ogB = sb("og", [128, 1024], BF16)
    ogT, ogTB = sb("ogT", [128, 8, 128], BF16)
    rr, rrB = sb("rr", [128, 1024], F32)
    rden, rdenB = sb("rden", [128, 8], F32)
    dens, densB = sb("dens", [128, 8], F32)
    bh, bhB = sb("bh", [128, 32], F32)
    jm, jmB = sb("jm", [128, 1], F32)
    VH, VHB = sb("VH", [128, 8, 1], BF16)
    VHD, VHDB = sb("VHD", [128, 8, 8], BF16)
    SH, SHB = sb("SH", [8, 1024], BF16)
    CMISC = cst["CMISC"]
    OLs, OLsB = sb("OLs", [128, 2, 1024], F32)
    lo, loB = sb("lo", [128, 1], F32)
    mid, midB = sb("mid", [128, 1], F32)
    cnt, cntB = sb("cnt", [128, 1], F32)
    cntA, cntAB = sb("cntA", [128, 1], F32)
    MBa = kb.buf("MBa")
    ge, geB = sb("ge", [128, 1], F32)
    st = {"s1": sb("s1", [128, 1], F32), "ss": sb("ss", [128, 1], F32), "lnjunk": (og, ogB)}
    ssc, sscB = sb("ssc", [128, 1], F32)
    cjunk, cjunkB = sb("cjunk", [128, 256], BF16)

    P0 = kb.ps("P0", [128, 1024], F32)
    P1 = kb.ps("P1", [128, 1024], F32)
    P2 = kb.ps("P2", [128, 1024], F32)
    PX = kb.ps("PX", [128, 512], F32)
    P16 = kb.ps("P16", [128, 1024], BF16)
    B0a, B0b, B1a, B1b, B2a, B2b, BX, B16 = [kb.buf("ps%d" % i) for i in range(8)]
    OPB = [[B1a, B1b], [B2a, B2b]]
    BXh = [BX, BX]
    PTB = [[kb.buf("pt%d%d" % (s_, h_)) for h_ in range(2)] for s_ in range(2)]
    OLB = [[kb.buf("ol%d%d" % (c_, h_)) for h_ in range(2)] for c_ in range(2)]
    DNB = [kb.buf("dn0"), kb.buf("dn1")]

    scale_q = float(128 ** -0.5)

    for i in range(nblk):
        n = 128 * (i + 1)
        xf, xfB = XF[0]
        kb.dma(xf[:], x_in[i * 128:(i + 1) * 128, :], [xinB], [xfB])
        import os
        stage = float(os.environ.get("K_STAGE", "99")) if i == 0 else float(os.environ.get("K_STAGE1", os.environ.get("K_STAGE", "99")))
        if stage <= 0:
            kb.dma(x_out[i * 128:(i + 1) * 128, :], xf[:], [xfB], [xoutB])
            continue
        for k in range(8):
            tr(kb, P0[:, k * 128:(k + 1) * 128], xf[:, k * 128:(k + 1) * 128], identF[:], [xfB, cB], [B0a if k < 4 else B0b], inc=(k == 7))
        kb.op("act", lambda e: e.copy(xT[:].rearrange("p k t -> p (k t)"), P0[:]), [B0a, B0b], [xTB])
        if stage <= 0.1:
            kb.op("dve", lambda e: e.memset(rr[:], 0.0), [], [rrB])
            kb.op("dve", lambda e: e.tensor_copy(rr[:, 0:128], xT[:, 0, :]), [xTB], [rrB])
            kb.dma(x_out[i * 128:(i + 1) * 128, :], rr[:], [rrB], [xoutB])
            continue
        for h in range(8):
            for k in range(8):
                mm(kb, P1[:, h * 128:(h + 1) * 128], W0[:, k, h * 128:(h + 1) * 128], xT[:, k, :], [W0B, xTB],
                   [B1a if h < 4 else B1b], start=(k == 0), stop=(k == 7), inc=(k == 7 and h == 7))
        kb.op("dve", lambda e: e.tensor_copy(qT[:].rearrange("p h t -> p (h t)"), P1[:]), [B1a, B1b], [qTB])
        if stage <= 0.2:
            kb.op("dve", lambda e: e.memset(rr[:], 0.0), [], [rrB])
            kb.op("dve", lambda e: e.tensor_copy(rr[:, 0:128], xT[:, 0, :]), [xTB], [rrB])
            kb.dma(x_out[i * 128:(i + 1) * 128, :], rr[:], [rrB], [xoutB])
            continue
        for c in range(4):
            for k in range(8):
                mm(kb, P2[:, c * 128:(c + 1) * 128], W0[:, k, 1280 + c * 128:1280 + (c + 1) * 128], xT[:, k, :],
                   [W0B, xTB], [B2a], start=(k == 0), stop=(k == 7), inc=(k == 7 and c == 3))
        kb.op("act", lambda e: e.copy(qiT[:].rearrange("p c t -> p (c t)"), P2[:, 0:512]), [B2a], [qiTB])
        if stage <= 0.25:
            kb.op("dve", lambda e: e.memset(rr[:], 0.0), [], [rrB])
            kb.op("dve", lambda e: e.tensor_copy(rr[:, 0:128], xT[:, 0, :]), [xTB], [rrB])
            kb.dma(x_out[i * 128:(i + 1) * 128, :], rr[:], [rrB], [xoutB])
            continue
        for k in range(8):
            mm(kb, P2[:, 512:640], Wk2[:, k, :], xT[:, k, :], [Wk2B, xTB], [B2b], start=(k == 0), stop=(k == 7), inc=False)
        for k in range(8):
            mm(kb, P2[:, 640:896], xT[:, k, :], W0[:, k, 1024:1280], [W0B, xTB], [B2b], start=(k == 0), stop=(k == 7), inc=False)
        for k in range(8):
            mm(kb, P2[:, 896:960], xT[:, k, :], W0[:, k, 1856:1920], [W0B, xTB], [B2b], start=(k == 0), stop=(k == 7), inc=(k == 7))
        if stage <= 0.27:
            kb.op("dve", lambda e: e.memset(rr[:], 0.0), [], [rrB])
            kb.op("dve", lambda e: e.tensor_copy(rr[:, 0:128], xT[:, 0, :]), [xTB], [rrB])
            kb.dma(x_out[i * 128:(i + 1) * 128, :], rr[:], [rrB], [xoutB])
            continue
        kb.op("dve", lambda e: e.tensor_copy(kT2[:, i * 128:(i + 1) * 128], P2[:, 512:640]), [B2b], [kT2B])
        if stage <= 0.28:
            kb.op("dve", lambda e: e.memset(rr[:], 0.0), [], [rrB])
            kb.op("dve", lambda e: e.tensor_copy(rr[:, 0:128], xT[:, 0, :]), [xTB], [rrB])
            kb.dma(x_out[i * 128:(i + 1) * 128, :], rr[:], [rrB], [xoutB])
            continue
        kb.op("dve", lambda e: e.tensor_copy(wsb[:], P2[:, 896:904]), [B2b], [wsbB])
        if stage <= 0.29:
            kb.op("dve", lambda e: e.memset(rr[:], 0.0), [], [rrB])
            kb.op("dve", lambda e: e.tensor_copy(rr[:, 0:128], xT[:, 0, :]), [xTB], [rrB])
            kb.dma(x_out[i * 128:(i + 1) * 128, :], rr[:], [rrB], [xoutB])
            continue
        kb.op("dve", lambda e: e.tensor_copy(cf[:], P2[:, 640:896]), [B2b], [cfB])
        if stage <= 0.3:
            kb.op("dve", lambda e: e.memset(rr[:], 0.0), [], [rrB])
            kb.op("dve", lambda e: e.tensor_copy(rr[:, 0:128], xT[:, 0, :]), [xTB], [rrB])
            kb.dma(x_out[i * 128:(i + 1) * 128, :], rr[:], [rrB], [xoutB])
            continue
        for nn in range(2):
            for k in range(8):
                mm(kb, P0[:, nn * 512:(nn + 1) * 512], xT[:, k, :], W0[:, k, 1864 + nn * 512:1864 + (nn + 1) * 512],
                   [W0B, xTB], [B0a if nn == 0 else B0b], start=(k == 0), stop=(k == 7), inc=(k == 7))
        kb.op("act", lambda e: e.activation(sz[:], P0[:], AF.Silu), [B0a, B0b], [szB])
        if stage <= 0.4:
            kb.op("dve", lambda e: e.memset(rr[:], 0.0), [], [rrB])
            kb.op("dve", lambda e: e.tensor_copy(rr[:, 0:128], xT[:, 0, :]), [xTB], [rrB])
            kb.dma(x_out[i * 128:(i + 1) * 128, :], rr[:], [rrB], [xoutB])
            continue
        kb.op("act", lambda e: e.activation(cjunk[:], cf[:], AF.Square, accum_out=ssc[:]), [cfB], [cjunkB, sscB])
        kb.op("dve", lambda e: e.tensor_scalar(ssc[:], ssc[:], 1.0 / 256, EPS, ALU.mult, ALU.add), [sscB], [sscB])
        kb.op("act", lambda e: e.activation(ssc[:], ssc[:], AF.Sqrt), [sscB], [sscB])
        kb.op("dve", lambda e: e.reciprocal(ssc[:], ssc[:]), [sscB], [sscB])
        kb.op("dve", lambda e: e.scalar_tensor_tensor(cN[:, i, :], cf[:], ssc[:], Gkv[:], ALU.mult, ALU.mult),
              [cfB, sscB, GkvB], [cNB])
        for cc in range(2):
            tr(kb, P16[:, cc * 128:(cc + 1) * 128], cN[:, i, cc * 128:(cc + 1) * 128], identB[:], [cNB, cB], [B16], inc=(cc == 1))
        kb.op("dve", lambda e: e.tensor_copy(cT[:, :, i * 128:(i + 1) * 128], P16[:, 0:256].rearrange("p (c t) -> p c t", c=2)),
              [B16], [cTB])
        if stage <= 0.6:
            kb.op("dve", lambda e: e.memset(rr[:], 0.0), [], [rrB])
            kb.op("dve", lambda e: e.tensor_copy(rr[:, 0:128], xT[:, 0, :]), [xTB], [rrB])
            kb.dma(x_out[i * 128:(i + 1) * 128, :], rr[:], [rrB], [xoutB])
            continue
        for cc in range(2):
            Pq = P0 if cc == 0 else P1
            for h in range(8):
                bq = (B0a, B0b, B1a, B1b)[cc * 2 + (h // 4)]
                mm(kb, Pq[:, h * 128:(h + 1) * 128], Wuk[:, h, cc * 128:(cc + 1) * 128], qT[:, h, :], [WukB, qTB], [bq],
                   inc=(h == 7))
        kb.op("act", lambda e: e.activation(qlT[:, 0, :], P0[:], AF.Copy, scale=scale_q), [B0a, B0b], [qlTB])
        kb.op("dve", lambda e: e.tensor_scalar(qlT[:, 1, :], P1[:], scale_q, None, ALU.mult), [B1a, B1b], [qlTB])

        if stage <= 1:
            kb.op("dve", lambda e: e.tensor_copy(rr[:, 0:256], cN[:, i, :]), [cNB], [rrB])
            kb.op("dve", lambda e: e.tensor_copy(rr[:, 256:1024], qlT[:, 0, 0:768]), [qlTB], [rrB])
            kb.dma(x_out[i * 128:(i + 1) * 128, :], rr[:], [rrB], [xoutB])
            continue
        nkc = (n + 511) // 512
        slots = [(PX, BX, slice(0, 512)), (P2, B2a, slice(0, 512)), (P2, B2b, slice(512, 1024))]
        u = 0
        for kc in range(nkc):
            wd = min(512, n - 512 * kc)
            c0 = kc * 512
            for h in range(8):
                Pt, Bt, sl = slots[u % 3]
                R, RB = RR[u % 3]
                u += 1
                pr = slice((h % 2) * 64, (h % 2) * 64 + 64)
                mm(kb, Pt[:, sl.start:sl.start + wd], qiT[pr, h // 2, :], kT2[pr, c0:c0 + wd], [qiTB, kT2B], [Bt])
                kb.op("act", lambda e: e.activation(R[:, 0:wd], Pt[:, sl.start:sl.start + wd], AF.Relu), [Bt], [RB])
                if h == 0:
                    kb.op("dve", lambda e: e.tensor_scalar(SC[:, c0:c0 + wd], R[:, 0:wd], wsb[:, 0:1], None, ALU.mult),
                          [RB, wsbB], [SCB])
                else:
                    kb.op("dve", lambda e: e.scalar_tensor_tensor(SC[:, c0:c0 + wd], R[:, 0:wd], wsb[:, h:h + 1], SC[:, c0:c0 + wd],
                                                                   ALU.mult, ALU.add), [RB, wsbB, SCB], [SCB])
        kb.op("dve", lambda e: e.tensor_tensor(SC[:, i * 128:n], SC[:, i * 128:n], CM[:], ALU.add), [SCB, cB], [SCB])

        MBall = [MB, MBa]
        if i >= 2:
            nd = 128 * max(1, int(round(0.45 * (i + 1))))
            na = n - nd
            kb.op("dve", lambda e: e.memset(mid[:], 0.0), [], [midB])
            for s in range(BIS_STEPS):
                wk = float(-BIS_LO / (2 ** s))
                last = (s == BIS_STEPS - 1)
                kb.op("dve", lambda e: e.tensor_scalar(M[:, 0:nd], SC[:, 0:nd], mid[:], None, ALU.is_ge, ALU.add, accum_out=cnt[:]),
                      [SCB, midB], [MB, cntB])
                kb.op("act", lambda e: e.activation(M[:, nd:n], SC[:, nd:n], AF.Sign, bias=mid[:], scale=-1.0, accum_out=cntA[:]),
                      [SCB, midB], [MBa, cntAB])
                kb.op("dve", lambda e: e.scalar_tensor_tensor(ge[:], cnt[:], 2.0, cntA[:], ALU.mult, ALU.subtract), [cntB, cntAB], [geB])
                kb.op("dve", lambda e: e.tensor_scalar(ge[:], ge[:], float(511 - na), wk, ALU.is_ge, ALU.mult), [geB], [geB])
                if not last:
                    kb.op("dve", lambda e: e.scalar_tensor_tensor(mid[:], ge[:], -0.5 * wk, mid[:], ALU.add, ALU.add), [geB, midB], [midB])
                else:
                    kb.op("dve", lambda e: e.scalar_tensor_tensor(lo[:], ge[:], -wk, mid[:], ALU.add, ALU.add), [geB, midB], [loB])
            kb.op("dve", lambda e: e.tensor_scalar(M[:, 0:n], SC[:, 0:n], lo[:], -30000.0, ALU.is_lt, ALU.mult), [SCB, loB], MBall)
        else:
            kb.op("dve", lambda e: e.tensor_scalar(M[:, 0:n], SC[:, 0:n], -1e29, -30000.0, ALU.is_lt, ALU.mult), [SCB], MBall)
        nbk = i + 1
        kb.op("dve", lambda e: e.tensor_reduce(bh[:, 0:nbk], M[:, 0:n].rearrange("p (j s) -> p j s", s=128), AX.X, ALU.max), MBall, [bhB])
        kb.op("dve", lambda e: e.tensor_tensor(bh[:, 0:nbk], bh[:, 0:nbk], CMISC[:, 0:nbk], ALU.add), [bhB, cB], [bhB])
        kb.op("dve", lambda e: e.reduce_max(jm[:], bh[:, 0:nbk], AX.X), [bhB], [jmB])
        kb.op("dve", lambda e: e.tensor_scalar(jm[:], jm[:], -1.0, float(i + 1), ALU.mult, ALU.add), [jmB], [jmB])
        kb.op("dve", lambda e: e.tensor_scalar(VH[:].rearrange("p h o -> p (h o)"), CMISC[:, 32:40], jm[:], None, ALU.mult), [jmB, cB], [VHB])
        kb.op("dve", lambda e: e.tensor_tensor(VHD[:], CMISC[:, 40:104].rearrange("p (h g) -> p h g", g=8), VH[:].to_broadcast([128, 8, 8]), ALU.mult),
              [VHB, cB], [VHDB])
        for h in range(8):
            tr(kb, P16[0:8, h * 128:(h + 1) * 128], VHD[:, h, :], identB[:], [VHDB, cB], [B16], inc=(h == 7))
        kb.op("act", lambda e: e.copy(SH[:], P16[0:8, :]), [B16], [SHB])
        if stage <= 2:
            kb.op("dve", lambda e: e.tensor_copy(rr[:, 0:128], M[:, 0:128]), [MB], [rrB])
            kb.op("dve", lambda e: e.tensor_copy(rr[:, 128:256], SC[:, 0:128]), [SCB], [rrB])
            kb.dma(x_out[i * 128:(i + 1) * 128, :], rr[:], [rrB], [xoutB])
            continue
        for j0 in range(0, i + 1, 8):
            nj = min(8, i + 1 - j0)
            for jj in range(nj):
                j = j0 + jj
                tr(kb, P16[:, jj * 128:(jj + 1) * 128], M[:, j * 128:(j + 1) * 128], identB[:], [MB, MBa, cB], [B16], inc=(jj == nj - 1))
            kb.op("act", lambda e: e.copy(MT[:, j0:j0 + nj, :].rearrange("p j t -> p (j t)"), P16[:, 0:nj * 128]), [B16], [MTB])

        jlist = list(range(i + 1))
        if os.environ.get("K_JLAST"):
            jlist = [i]
        units = [(j, hb) for j in jlist for hb in range(2)]

        def emit_st(j, hb):
            bS = B0a if hb == 0 else B0b
            for cc in range(2):
                mm(kb, P0[:, hb * 512:(hb + 1) * 512], cT[:, cc, j * 128:(j + 1) * 128], qlT[:, cc, hb * 512:(hb + 1) * 512],
                   [cTB, qlTB], [bS], start=(cc == 0), stop=False, inc=False)
            mm(kb, P0[:, hb * 512:(hb + 1) * 512], ones[0:8, :], SH[:, hb * 512:(hb + 1) * 512], [SHB, cB], [bS],
               start=False, stop=False, inc=False)
            mm(kb, P0[:, hb * 512:(hb + 1) * 512], identB[:], MT[:, j:j + 1, :].to_broadcast([128, 4, 128]), [MTB, cB], [bS],
               start=False, stop=True, inc=True)

        def emit_rest(j, hb):
            d = i - j
            bS = B0a if hb == 0 else B0b
            Pt_ = PT[j % 2][0]
            PtB = PTB[j % 2][hb]
            for h in range(4 * hb, 4 * hb + 4):
                kb.op("act", lambda e: e.activation(Pt_[:, h, :], P0[:, h * 128:(h + 1) * 128], AF.Exp,
                                                     bias=BIAS[:, h * 32 + d:h * 32 + d + 1], scale=1.0), [bS, cB], [PtB])
            for cc in range(2):
                Po = P1 if cc == 0 else P2
                mm(kb, Po[:, hb * 512:(hb + 1) * 512], cN[:, j, cc * 128:(cc + 1) * 128],
                   Pt_[:, 4 * hb:4 * hb + 4, :].rearrange("p h t -> p (h t)"), [cNB, PtB], [OPB[cc][hb]], start=True, stop=True, inc=False)
            for h in range(4 * hb, 4 * hb + 4):
                mm(kb, PX[:, h:h + 1], Pt_[:, h, :], ones[:, 0:1], [PtB, cB], [BXh[hb]], start=(h % 4 == 0), stop=(h % 4 == 3), inc=(h % 4 == 3))
            first = (j == jlist[0])
            hs = slice(hb * 512, (hb + 1) * 512)
            ds = slice(4 * hb, 4 * hb + 4)
            if first:
                kb.op("dve", lambda e: e.tensor_copy(dens[:, ds], PX[:, ds]), [BXh[hb]], [DNB[hb]])
            else:
                kb.op("dve", lambda e: e.tensor_tensor(dens[:, ds], dens[:, ds], PX[:, ds], ALU.add), [BXh[hb], DNB[hb]], [DNB[hb]])
            for cc in range(2):
                Po = P1 if cc == 0 else P2
                if first:
                    kb.op("dve", lambda e: e.tensor_copy(OLs[:, cc, hs], Po[:, hs]), [OPB[cc][hb]], [OLB[cc][hb]])
                else:
                    kb.op("dve", lambda e: e.tensor_tensor(OLs[:, cc, hs], OLs[:, cc, hs], Po[:, hs], ALU.add), [OPB[cc][hb], OLB[cc][hb]], [OLB[cc][hb]])

        emit_st(*units[0])
        for ui, (j, hb) in enumerate(units):
            if ui + 1 < len(units):
                emit_st(*units[ui + 1])
            emit_rest(j, hb)

        kb.op("dve", lambda e: e.reciprocal(rden[:], dens[:]), DNB, [rdenB])
        kb.op("act", lambda e: e.copy(olT[:, 0, :], OLs[:, 0, :]), OLB[0], [olTB])
        kb.op("dve", lambda e: e.tensor_copy(olT[:, 1, :], OLs[:, 1, :]), OLB[1], [olTB])
        for h in range(8):
            for cc in range(2):
                mm(kb, P0[:, h * 128:(h + 1) * 128], olT[:, cc, h * 128:(h + 1) * 128], Wuv[:, h, cc, :], [olTB, WuvB],
                   [B0a if h < 4 else B0b], start=(cc == 0), stop=(cc == 1), inc=(cc == 1 and h == 7))
        for h in range(8):
            kb.op("dve", lambda e: e.scalar_tensor_tensor(og[:, h * 128:(h + 1) * 128], P0[:, h * 128:(h + 1) * 128], rden[:, h:h + 1],
                                                           sz[:, h * 128:(h + 1) * 128], ALU.mult, ALU.mult),
                  [B0a if h < 4 else B0b, rdenB, szB], [ogB])
        for k in range(8):
            tr(kb, P16[:, k * 128:(k + 1) * 128], og[:, k * 128:(k + 1) * 128], identB[:], [ogB, cB], [B16], inc=(k == 7))
        kb.op("act", lambda e: e.copy(ogT[:].rearrange("p k t -> p (k t)"), P16[:]), [B16], [ogTB])
        for nn in range(2):
            for k in range(8):
                mm(kb, P1[:, nn * 512:(nn + 1) * 512], ogT[:, k, :], Wout[:, k, nn * 512:(nn + 1) * 512], [ogTB, WoutB],
                   [B1a if nn == 0 else B1b], start=(k == 0), stop=(k == 7), inc=(k == 7))
        kb.op("dve", lambda e: e.scalar_tensor_tensor(rr[:], xf[:], ALPHA, P1[:], ALU.mult, ALU.add), [xfB, B1a, B1b], [rrB])
        layernorm_store(kb, st, rr[:], rrB, x_out[i * 128:(i + 1) * 128, :], xoutB, LG, LB, [LGB, LBB])

    kb.finish_layer = True
    kb.es = kb_es
    return es


def load_xT(kb, x_in, xinB, i, xf, xfB, xT, xTB, Ptr, Bs, identF, cB):
    kb.dma(xf[:], x_in[i * 128:(i + 1) * 128, :], [xinB], [xfB])
    for k in range(8):
        tr(kb, Ptr[:, k * 128:(k + 1) * 128], xf[:, k * 128:(k + 1) * 128], identF[:], [xfB, cB], Bs, inc=(k == 7))
    kb.op("act", lambda e: e.copy(xT[:].rearrange("p k t -> p (k t)"), Ptr[:, 0:1024]), Bs, [xTB])


def emit_layer1a(nc, kb, dr, x_in, xinB, hn, hnB, cst, nblk=NB):
    es = ExitStack()
    kb_es = kb.es
    kb.es = es
    sb = kb.sb
    identB, identF, ones, cB, TRIf = cst["identB"], cst["identF"], cst["ones"], cst["buf"], cst["TRIf"]
    win = dr["b_w_in"].rearrange("(k p) n -> p k n", p=128)
    Wqk, WqkB = sb("Wqk", [128, 8, 2048], BF16)
    Wv, WvB = sb("Wv", [128, 8, 2048], BF16)
    Wg, WgB = sb("Wg", [128, 8, 8], BF16)
    for k in range(0, 8, 4):
        kb.dma(Wqk[:, k:k + 4, :], win[:, k:k + 4, 0:2048], [], [WqkB], q="pool")
        kb.dma(Wv[:, k:k + 4, :], win[:, k:k + 4, 2048:4096], [], [WvB], q="pool")
    kb.dma(Wg[:], win[:, :, 4096:4104], [], [WgB], q="pool")
    CWr, CWrB = sb("CWr", [80, 128], F32)
    CWT, CWTB = sb("CWT", [128, 80], F32)
    kb.dma(CWr[0:64, :], dr["b_conv_w"].rearrange("j (c p) -> (j c) p", p=128), [], [CWrB])
    kb.dma(CWr[64:80, :], dr["b_conv_b"].rearrange("o (c p) -> (o c) p", p=128), [], [CWrB])
    GBt, GBB = sb("GBt", [128, 8], F32)
    kb.dma(GBt[:, 0:4], dr["b_i_bias"].partition_broadcast(128), [], [GBB])
    kb.dma(GBt[:, 4:8], dr["b_f_bias"].partition_broadcast(128), [], [GBB])
    HG, HGB = sb("HG", [128, 2048], F32)
    kb.dma(HG[:], dr["b_head_norm_g"].rearrange("o h v -> o (h v)").partition_broadcast(128), [], [HGB])

    PA = kb.ps("L1PA", [128, 2048], F32)
    PB = kb.ps("L1PB", [128, 1024], F32)
    PC = kb.ps("L1PC", [128, 512], F32)
    P16 = kb.ps("L1P16", [128, 1024], BF16)
    BA = [kb.buf("pa%d" % i) for i in range(4)]
    BBa, BBb, BC, B16 = kb.buf("pba"), kb.buf("pbb"), kb.buf("pc"), kb.buf("p16")

    tr(kb, PC[:, 0:80], CWr[:], identF[0:80, 0:80], [CWrB, cB], [BC])
    kb.op("dve", lambda e: e.tensor_copy(CWT[:], PC[:, 0:80]), [BC], [CWTB])

    XF2 = [sb("xf1_%d" % s_, [128, 1024], F32) for s_ in range(2)]
    XT2 = [sb("xT1_%d" % s_, [128, 8, 128], BF16) for s_ in range(2)]
    QKP, QKPB = sb("QKP", [128, 16, 131], F32)
    tmpc, tmpcB = sb("tmpc", [128, 128], F32)
    qkT, qkTB = sb("qkT", [128, 16, 128], BF16)
    VT2 = [sb("vt_%d" % s_, [128, 2048], BF16) for s_ in range(2)]
    gt, gtB = sb("gt", [128, 8], F32)
    lf, lfB = sb("lf", [128, 4], F32)
    ibm, ibmB = sb("ibm", [128, 4], F32)
    bcol, bcolB = sb("bcol", [128, 4], F32)
    ONESf, ONESfB = sb("ONESf", [128, 128], F32)
    LFB, LFBB = sb("LFB", [128, 128], F32)
    EB, EBB = sb("EB", [128, 128], F32)
    DT, DTB = sb("DT", [128, 128], F32)
    sT, sTB = sb("sT", [128, 128], BF16)
    qh, qhB = sb("qh", [128, 2, 128], BF16)
    kTok, kTokB = sb("kTok", [128, 256], BF16)
    vs, vsB = sb("vs", [128, 512], BF16)
    wcol, wcolB = sb("wcol", [128, 1], BF16)
    hh, hhB = sb("hh", [128, 512], F32)
    hjunk, hjunkB = sb("hjunk", [128, 512], BF16)
    HN, HNB = sb("HN", [128, 2048], BF16)
    rec, recB = sb("rec", [128, 1], F32)
    ssh, sshB = sb("ssh", [128, 1], F32)
    Cs = [sb("C%d" % h, [128, 2, 512], F32) for h in range(4)]
    Cb = [sb("Cb%d" % h, [128, 2, 512], BF16) for h in range(4)]
    ns = [sb("n%d" % h, [128, 2], F32) for h in range(4)]
    nb = [sb("nb%d" % h, [128, 2], BF16) for h in range(4)]
    kb.op("dve", lambda e: e.memset(ONESf[:], 1.0), [], [ONESfB])
    kb.op("dve", lambda e: e.memset(QKP[:], 0.0), [], [QKPB])
    for h in range(4):
        kb.op("dve", lambda e: e.memset(Cs[h][0][:], 0.0), [], [Cs[h][1]])
        kb.op("dve", lambda e: e.memset(Cb[h][0][:], 0.0), [], [Cb[h][1]])
        kb.op("dve", lambda e: e.memset(ns[h][0][:], 0.0), [], [ns[h][1]])
        kb.op("dve", lambda e: e.memset(nb[h][0][:], 0.0), [], [nb[h][1]])

    def seg_A(i):
        xf, xfB = XF2[i % 2]; xT, xTB = XT2[i % 2]; vt, vtB = VT2[i % 2]
        load_xT(kb, x_in, xinB, i, xf, xfB, xT, xTB, PB, [BBa, BBb], identF, cB)

    def seg_Bq(i):
        xf, xfB = XF2[i % 2]; xT, xTB = XT2[i % 2]; vt, vtB = VT2[i % 2]
        for c in range(16):
            for k in range(8):
                mm(kb, PA[:, c * 128:(c + 1) * 128], Wqk[:, k, c * 128:(c + 1) * 128], xT[:, k, :], [WqkB, xTB], [BA[c // 4]],
                   start=(k == 0), stop=(k == 7), inc=(k == 7 and c % 4 == 3))

    def seg_Be(i):
        xf, xfB = XF2[i % 2]; xT, xTB = XT2[i % 2]; vt, vtB = VT2[i % 2]
        kb.op("dve", lambda e: e.tensor_copy(QKP[:, 0:8, 3:131], PA[:, 0:1024].rearrange("p (c t) -> p c t", c=8)), [BA[0], BA[1]], [QKPB])
        kb.op("dve", lambda e: e.tensor_copy(QKP[:, 8:16, 3:131], PA[:, 1024:2048].rearrange("p (c t) -> p c t", c=8)), [BA[2], BA[3]], [QKPB])

    def seg_mid(i):
        xf, xfB = XF2[i % 2]; xT, xTB = XT2[i % 2]; vt, vtB = VT2[i % 2]
        for c in range(16):
            kb.op("dve", lambda e: e.tensor_scalar(tmpc[:], QKP[:, c, 0:128], CWT[:, c:c + 1], CWT[:, 64 + c:65 + c], ALU.mult, ALU.add),
                  [QKPB, CWTB], [tmpcB])
            for j in range(1, 4):
                kb.op("dve", lambda e: e.scalar_tensor_tensor(tmpc[:], QKP[:, c, j:j + 128], CWT[:, j * 16 + c:j * 16 + c + 1], tmpc[:],
                                                               ALU.mult, ALU.add), [QKPB, CWTB, tmpcB], [tmpcB])
            kb.op("act", lambda e: e.activation(qkT[:, c, :], tmpc[:], AF.Silu), [tmpcB], [qkTB])
        kb.op("dve", lambda e: e.tensor_copy(tmpc[:, 0:48].rearrange("p (c t) -> p c t", c=16), QKP[:, :, 128:131]), [QKPB], [tmpcB])
        kb.op("dve", lambda e: e.tensor_copy(QKP[:, :, 0:3], tmpc[:, 0:48].rearrange("p (c t) -> p c t", c=16)), [tmpcB], [QKPB])

    def seg_V(i):
        xf, xfB = XF2[i % 2]; xT, xTB = XT2[i % 2]; vt, vtB = VT2[i % 2]
        for nn in range(4):
            for k in range(8):
                mm(kb, PA[:, nn * 512:(nn + 1) * 512], xT[:, k, :], Wv[:, k, nn * 512:(nn + 1) * 512], [WvB, xTB], [BA[nn]],
                   start=(k == 0), stop=(k == 7), inc=(k == 7))
        kb.op("act", lambda e: e.copy(vt[:, 0:1024], PA[:, 0:1024]), [BA[0], BA[1]], [vtB])
        kb.op("dve", lambda e: e.tensor_copy(vt[:, 1024:2048], PA[:, 1024:2048]), [BA[2], BA[3]], [vtB])
        for k in range(8):
            mm(kb, PC[:, 0:8], xT[:, k, :], Wg[:, k, :], [WgB, xTB], [BC], start=(k == 0), stop=(k == 7), inc=(k == 7))
        kb.op("dve", lambda e: e.tensor_tensor(gt[:], PC[:, 0:8], GBt[:], ALU.add), [BC, GBB], [gtB])
        kb.op("act", lambda e: e.activation(lf[:], gt[:, 4:8], AF.Exp, scale=-1.0), [gtB], [lfB])
        kb.op("act", lambda e: e.activation(lf[:], lf[:], AF.Ln, bias=1.0), [lfB], [lfB])
        kb.op("dve", lambda e: e.tensor_scalar(lf[:], lf[:], -1.0, None, ALU.mult), [lfB], [lfB])
        mm(kb, PC[:, 8:12], TRIf[:], lf[:], [lfB, cB], [BC])
        kb.op("dve", lambda e: e.tensor_copy(bcol[:], PC[:, 8:12]), [BC], [bcolB])
        kb.op("dve", lambda e: e.tensor_tensor(ibm[:], gt[:, 0:4], bcol[:], ALU.subtract), [gtB, bcolB], [ibmB])

    def seg_H(i):
        xf, xfB = XF2[i % 2]; xT, xTB = XT2[i % 2]; vt, vtB = VT2[i % 2]
        for h in range(4):
            C, CB_ = Cs[h]
            Cbh, CbB = Cb[h]
            nh, nhB = ns[h]
            nbh, nbB = nb[h]
            kb.op("dve", lambda e: e.tensor_scalar(LFB[:], ONESf[:], lf[:, h:h + 1], None, ALU.mult), [ONESfB, lfB], [LFBB])
            mm(kb, PC[:, 128:256], LFB[:], TRIf[:], [LFBB, cB], [BC])
            kb.op("act", lambda e: e.activation(EB[:], PC[:, 128:256], AF.Exp), [BC], [EBB])
            kb.op("act", lambda e: e.activation(DT[:], PC[:, 128:256], AF.Exp, bias=ibm[:, h:h + 1]), [BC, ibmB], [DTB])
            kb.op("dve", lambda e: e.tensor_copy(wcol[:], DT[:, 127:128]), [DTB], [wcolB])
            kb.op("dve", lambda e: e.tensor_scalar(vs[:], vt[:, h * 512:(h + 1) * 512], DT[:, 127:128], None, ALU.mult), [vtB, DTB], [vsB])
            kb.op("dve", lambda e: e.tensor_tensor(DT[:], DT[:], TRIf[:], ALU.mult), [DTB, cB], [DTB])
            for cc in range(2):
                kb.op("dve", lambda e: e.scalar_tensor_tensor(qh[:, cc, :], qkT[:, 2 * h + cc, :], 0.0625, EB[:], ALU.mult, ALU.mult),
                      [qkTB, EBB], [qhB])
            for cc in range(2):
                mm(kb, PC[:, 256:384], qkT[:, 8 + 2 * h + cc, :], qkT[:, 2 * h + cc, :], [qkTB], [BC], start=(cc == 0), stop=(cc == 1), inc=(cc == 1))
            kb.op("dve", lambda e: e.scalar_tensor_tensor(sT[:], PC[:, 256:384], 0.0625, DT[:], ALU.mult, ALU.mult), [BC, DTB], [sTB])
            mm(kb, PB[:, 0:512], sT[:], vt[:, h * 512:(h + 1) * 512], [sTB, vtB], [BBa], start=True, stop=False, inc=False)
            for cc in range(2):
                mm(kb, PB[:, 0:512], qh[:, cc, :], Cbh[:, cc, :], [qhB, CbB], [BBa], start=False, stop=(cc == 1), inc=(cc == 1))
            mm(kb, PC[:, 384:385], sT[:], ones[:, 0:1], [sTB, cB], [BC], start=True, stop=False, inc=False)
            for cc in range(2):
                mm(kb, PC[:, 384:385], qh[:, cc, :], nbh[:, cc:cc + 1], [qhB, nbB], [BC], start=False, stop=(cc == 1), inc=(cc == 1))
            kb.op("dve", lambda e: e.tensor_scalar(rec[:], PC[:, 384:385], -1.0, None, ALU.mult), [BC], [recB])
            kb.op("dve", lambda e: e.tensor_tensor(rec[:], rec[:], PC[:, 384:385], ALU.max), [BC, recB], [recB])
            kb.op("dve", lambda e: e.tensor_scalar(rec[:], rec[:], 1.0, None, ALU.max), [recB], [recB])
            kb.op("dve", lambda e: e.reciprocal(rec[:], rec[:]), [recB], [recB])
            kb.op("dve", lambda e: e.tensor_scalar(hh[:], PB[:, 0:512], rec[:], None, ALU.mult), [BBa, recB], [hhB])
            kb.op("act", lambda e: e.activation(hjunk[:], hh[:], AF.Square, accum_out=ssh[:]), [hhB], [hjunkB, sshB])
            kb.op("dve", lambda e: e.tensor_scalar(ssh[:], ssh[:], 1.0 / 512, EPS, ALU.mult, ALU.add), [sshB], [sshB])
            kb.op("act", lambda e: e.activation(ssh[:], ssh[:], AF.Sqrt), [sshB], [sshB])
            kb.op("dve", lambda e: e.reciprocal(ssh[:], ssh[:]), [sshB], [sshB])
            kb.op("dve", lambda e: e.scalar_tensor_tensor(HN[:, h * 512:(h + 1) * 512], hh[:], ssh[:], HG[:, h * 512:(h + 1) * 512],
                                                           ALU.mult, ALU.mult), [hhB, sshB, HGB], [HNB])
            for cc in range(2):
                tr(kb, P16[:, cc * 128:(cc + 1) * 128], qkT[:, 8 + 2 * h + cc, :], identB[:], [qkTB, cB], [B16], inc=(cc == 1))
            kb.op("act", lambda e: e.copy(kTok[:], P16[:, 0:256]), [B16], [kTokB])
            for cc in range(2):
                mm(kb, PB[:, 512:1024], kTok[:, cc * 128:(cc + 1) * 128], vs[:], [kTokB, vsB], [BBb])
                kb.op("dve", lambda e: e.scalar_tensor_tensor(C[:, cc, :], C[:, cc, :], EB[:, 127:128], PB[:, 512:1024], ALU.mult, ALU.add),
                      [CB_, EBB, BBb], [CB_])
                mm(kb, PC[:, 400:401], kTok[:, cc * 128:(cc + 1) * 128], wcol[:], [kTokB, wcolB], [BC])
                kb.op("dve", lambda e: e.scalar_tensor_tensor(nh[:, cc:cc + 1], nh[:, cc:cc + 1], EB[:, 127:128], PC[:, 400:401], ALU.mult, ALU.add),
                      [nhB, EBB, BC], [nhB])
            kb.op("act", lambda e: e.copy(Cbh[:].rearrange("p c v -> p (c v)"), C[:].rearrange("p c v -> p (c v)")), [CB_], [CbB])
            kb.op("dve", lambda e: e.tensor_copy(nbh[:], nh[:]), [nhB], [nbB])
        kb.dma(hn[i * 128:(i + 1) * 128, :], HN[:], [HNB], [hnB])

    seg_A(0)
    seg_Bq(0)
    seg_Be(0)
    for i in range(nblk):
        seg_V(i)
        if i + 1 < nblk:
            seg_A(i + 1)
        seg_mid(i)
        if i + 1 < nblk:
            seg_Bq(i + 1)
            seg_Be(i + 1)
        seg_H(i)
    kb.es = kb_es
    return es


def emit_layer1b(nc, kb, dr, x_in, xinB, hn, hnB, x_out, xoutB, cst, nblk=NB):
    es = ExitStack()
    kb_es = kb.es
    kb.es = es
    sb = kb.sb
    identB, identF, cB = cst["identB"], cst["identF"], cst["buf"]
    win = dr["b_w_in"].rearrange("(k p) n -> p k n", p=128)
    Wo, WoB = sb("Wo", [128, 8, 2048], BF16)
    Wz, WzB = sb("Wz", [128, 8, 2048], BF16)
    Wout, WoutB = sb("Wout1", [128, 16, 1024], BF16)
    wout = dr["b_w_out"].rearrange("(k p) n -> p k n", p=128)
    for k in range(0, 8, 4):
        kb.dma(Wo[:, k:k + 4, :], win[:, k:k + 4, 4104:6152], [], [WoB], q="pool")
        kb.dma(Wz[:, k:k + 4, :], win[:, k:k + 4, 6152:8200], [], [WzB], q="pool")
    for k in range(0, 16, 8):
        kb.dma(Wout[:, k:k + 8, :], wout[:, k:k + 8, :], [], [WoutB], q="pool")
    LG, LGB = sb("LG1", [128, 1024], F32)
    LB, LBB = sb("LB1", [128, 1024], F32)
    kb.dma(LG[:], dr["b_ln_g"].partition_broadcast(128), [], [LGB])
    kb.dma(LB[:], dr["b_ln_b"].partition_broadcast(128), [], [LBB])
    PA = kb.ps("L2PA", [128, 2048], F32)
    PB = kb.ps("L2PB", [128, 1024], F32)
    P16 = kb.ps("L2P16", [128, 2048], BF16)
    BA = [kb.buf("qa%d" % i) for i in range(4)]
    BBa, BBb, B16 = kb.buf("qba"), kb.buf("qbb"), kb.buf("q16")
    xf, xfB = sb("xf2", [128, 1024], F32)
    xT, xTB = sb("xT2", [128, 8, 128], BF16)
    so, soB = sb("so", [128, 2048], BF16)
    hnb, hnbB = sb("hnb", [128, 2048], BF16)
    hg, hgB = sb("hg", [128, 2048], BF16)
    hgT, hgTB = sb("hgT", [128, 16, 128], BF16)
    rr, rrB = sb("rr2", [128, 1024], F32)
    st = {"s1": sb("s1b", [128, 1], F32), "ss": sb("ssb", [128, 1], F32), "lnjunk": sb("lnjunkb", [128, 1024], BF16)}
    for i in range(nblk):
        load_xT(kb, x_in, xinB, i, xf, xfB, xT, xTB, PB, [BBa, BBb], identF, cB)
        kb.dma(hnb[:], hn[i * 128:(i + 1) * 128, :], [hnB], [hnbB])
        for nn in range(4):
            for k in range(8):
                mm(kb, PA[:, nn * 512:(nn + 1) * 512], xT[:, k, :], Wo[:, k, nn * 512:(nn + 1) * 512], [WoB, xTB], [BA[nn]],
                   start=(k == 0), stop=(k == 7), inc=(k == 7))
        kb.op("act", lambda e: e.activation(so[:], PA[:], AF.Sigmoid), BA, [soB])
        kb.op("dve", lambda e: e.tensor_tensor(hg[:], hnb[:], so[:], ALU.mult), [hnbB, soB], [hgB])
        for nn in range(4):
            for k in range(8):
                mm(kb, PA[:, nn * 512:(nn + 1) * 512], xT[:, k, :], Wz[:, k, nn * 512:(nn + 1) * 512], [WzB, xTB], [BA[nn]],
                   start=(k == 0), stop=(k == 7), inc=(k == 7))
        kb.op("act", lambda e: e.activation(so[:], PA[:], AF.Silu), BA, [soB])
        kb.op("dve", lambda e: e.tensor_tensor(hg[:], hg[:], so[:], ALU.mult), [hgB, soB], [hgB])
        for k in range(16):
            tr(kb, P16[:, k * 128:(k + 1) * 128], hg[:, k * 128:(k + 1) * 128], identB[:], [hgB, cB], [B16], inc=(k == 15))
        kb.op("act", lambda e: e.copy(hgT[:].rearrange("p k t -> p (k t)"), P16[:]), [B16], [hgTB])
        for nn in range(2):
            for k in range(16):
                mm(kb, PB[:, nn * 512:(nn + 1) * 512], hgT[:, k, :], Wout[:, k, nn * 512:(nn + 1) * 512], [hgTB, WoutB],
                   [BBa if nn == 0 else BBb], start=(k == 0), stop=(k == 15), inc=(k == 15))
        kb.op("dve", lambda e: e.scalar_tensor_tensor(rr[:], xf[:], ALPHA, PB[:], ALU.mult, ALU.add), [xfB, BBa, BBb], [rrB])
        layernorm_store(kb, st, rr[:], rrB, x_out[i * 128:(i + 1) * 128, :], xoutB, LG, LB, [LGB, LBB])
    kb.es = kb_es
    return es


def make_consts():
    identF = np.eye(128, dtype=np.float32)
    q = np.arange(128)[:, None]
    s = np.arange(128)[None, :]
    CM = np.where(s <= q, 0.0, -1e30).astype(np.float32)
    slopes = 2.0 ** (-(np.arange(1, 9, dtype=np.float64)))
    sp = np.arange(128, dtype=np.float64)[:, None, None]
    dd = np.arange(32, dtype=np.float64)[None, None, :]
    BIAS = (slopes[None, :, None] * (sp - 127.0 - 128.0 * dd)).reshape(128, 256).astype(np.float32)
    misc = np.zeros((128, 104), np.float32)
    misc[:, 0:32] = np.arange(1, 33, dtype=np.float32)[None, :]
    misc[:, 32:40] = (128.0 * slopes)[None, :]
    misc[:, 40:104] = np.eye(8, dtype=np.float32).reshape(1, 64)
    tri = (np.arange(128)[:, None] <= np.arange(128)[None, :]).astype(np.float32)
    return {"c_ident": identF, "c_cm": CM, "c_bias": BIAS, "c_misc": misc, "c_tri": tri}


def build(layers=LAYERS, nblk=NB):
    nc = bass.Bass("TRN2", target_bir_lowering=False)
    dr = {}

    def din(name, shape):
        dr[name] = nc.dram_tensor(name, list(shape), F32, kind="ExternalInput").ap()

    din("x", [T, D])
    din("a_w_in", [D, A_IN]); din("a_kv_norm_g", [1, 256]); din("a_w_uk", [8, 128, 256]); din("a_w_uv", [8, 256, 128])
    din("a_w_out", [D, D]); din("a_ln_g", [1, D]); din("a_ln_b", [1, D])
    din("c_ident", [128, 128]); din("c_cm", [128, 128]); din("c_bias", [128, 256]); din("c_misc", [128, 104]); din("c_tri", [128, 128])
    din("b_w_in", [D, B_IN]); din("b_i_bias", [1, 4]); din("b_f_bias", [1, 4]); din("b_conv_w", [4, 2048]); din("b_conv_b", [1, 2048])
    din("b_head_norm_g", [1, 4, 512]); din("b_w_out", [2048, D]); din("b_ln_g", [1, D]); din("b_ln_b", [1, D])
    out = nc.dram_tensor("out", [T, D], F32, kind="ExternalOutput").ap()
    with ExitStack() as es:
        kb = KB(nc, es)
        identF, idB = kb.sb("identF", [128, 128], F32)
        identB, _ = kb.sb("identB", [128, 128], BF16)
        CM, _ = kb.sb("CM", [128, 128], F32)
        BIAS, _ = kb.sb("BIAS", [128, 256], F32)
        ones, _ = kb.sb("ones", [128, 128], BF16)
        CMISC, _ = kb.sb("CMISC", [128, 104], F32)
        TRIf, _ = kb.sb("TRIf", [128, 128], F32)
        cB = idB
        kb.dma(identF[:], dr["c_ident"], [], [cB])
        kb.dma(identB[:], dr["c_ident"], [], [cB], q="pool")
        kb.dma(CM[:], dr["c_cm"], [], [cB])
        kb.dma(BIAS[:], dr["c_bias"], [], [cB])
        kb.dma(CMISC[:], dr["c_misc"], [], [cB])
        kb.dma(TRIf[:], dr["c_tri"], [], [cB])
        kb.op("dve", lambda e: e.memset(ones[:], 1.0), [], [cB])
        cst = {"identB": identB, "identF": identF, "CM": CM, "BIAS": BIAS, "ones": ones, "buf": cB, "CMISC": CMISC, "TRIf": TRIf}
        xinB = kb.buf("xin")
        outB = kb.buf("out")
        x1d = nc.dram_tensor("x1d", [T, D], F32).ap()
        hnd = nc.dram_tensor("hnd", [T, 2048], BF16).ap()
        x1B = kb.buf("x1d")
        hnB = kb.buf("hnd")
        if layers == (0,):
            l0 = emit_layer0(nc, kb, dr, dr["x"], xinB, out, outB, cst, nblk=nblk)
            kb.finish(); l0.close()
        elif layers == (1,):
            la = emit_layer1a(nc, kb, dr, dr["x"], xinB, hnd, hnB, cst, nblk=nblk)
            kb.barrier(); la.close()
            lb = emit_layer1b(nc, kb, dr, dr["x"], xinB, hnd, hnB, out, outB, cst, nblk=nblk)
            kb.finish(); lb.close()
        else:
            l0 = emit_layer0(nc, kb, dr, dr["x"], xinB, x1d, x1B, cst, nblk=nblk)
            kb.barrier(); l0.close()
            la = emit_layer1a(nc, kb, dr, x1d, x1B, hnd, hnB, cst, nblk=nblk)
            kb.barrier(); la.close()
            lb = emit_layer1b(nc, kb, dr, x1d, x1B, hnd, hnB, out, outB, cst, nblk=nblk)
            kb.finish(); lb.close()
    return nc


_CACHE = {}


def make_inmaps(inputs, xs):
    cst = make_consts()
    shared = {k: np.ascontiguousarray(inputs[k][0], dtype=np.float32) for k in
              ("a_w_in", "a_w_uk", "a_w_uv", "a_w_out", "b_w_in", "b_conv_w", "b_w_out")}
    for k in ("a_kv_norm_g", "a_ln_g", "a_ln_b", "b_i_bias", "b_f_bias", "b_conv_b", "b_ln_g", "b_ln_b"):
        shared[k] = np.ascontiguousarray(inputs[k], dtype=np.float32).reshape(1, -1)
    shared["b_head_norm_g"] = np.ascontiguousarray(inputs["b_head_norm_g"], dtype=np.float32).reshape(1, 4, 512)
    shared.update(cst)
    maps = []
    for xx in xs:
        m = dict(shared)
        m["x"] = np.ascontiguousarray(xx, dtype=np.float32)
        maps.append(m)
    return maps


def kernel(**inputs):
    x = np.ascontiguousarray(inputs["x"], dtype=np.float32)
    if "nc" not in _CACHE:
        import os
        lay = os.environ.get("K_LAYERS")
        _CACHE["nc"] = build(layers=tuple(int(c) for c in lay)) if lay else build()
    nc = _CACHE["nc"]
    in_maps = make_inmaps(inputs, [x[c % 4] for c in range(8)])
    res = run_bass_kernel_spmd(nc, in_maps, core_ids=list(range(8)))
    return np.stack([res.results[c]["out"] for c in range(4)], axis=0)
```

```python
import numpy as np
from contextlib import ExitStack
import concourse.bass as bass
import concourse.mybir as mybir
from concourse.bass_utils import run_bass_kernel_spmd

F32 = mybir.dt.float32
BF16 = mybir.dt.bfloat16
AF = mybir.ActivationFunctionType
ALU = mybir.AluOpType
AX = mybir.AxisListType

T = 4096
D = 1024
NB = T // 128
A_IN = 2888
B_IN = 8200
ALPHA = float(4 ** 0.25)
EPS = 1e-5
NDMA = 12
BIS_STEPS = 28
BIS_LO = -2048.0
LAYERS = (0, 1)


class Buf:
    __slots__ = ("name", "w", "r")

    def __init__(self, name):
        self.name = name
        self.w = None
        self.r = {}


class KB:
    def __init__(self, nc, es):
        self.nc = nc
        self.es = es
        self.eng = {"pe": nc.tensor, "act": nc.scalar, "dve": nc.vector, "pool": nc.gpsimd, "sp": nc.sync}
        self.sem = {e: es.enter_context(nc.semaphore("s_" + e)) for e in ("pe", "act", "dve", "pool")}
        self.cnt = {e: 0 for e in self.sem}
        self.known = {e: {} for e in self.eng}
        self.dsem = [es.enter_context(nc.semaphore("d%d" % k)) for k in range(NDMA)]
        self.dtot = [0] * NDMA
        self.dnext = 0
        self.nbuf = 0
        self.root_es = es
        self.xsem = {}

    def buf(self, name=None):
        self.nbuf += 1
        return Buf(name or ("b%d" % self.nbuf))

    def sb(self, name, shape, dt):
        t = self.es.enter_context(self.nc.sbuf_tensor(name, list(shape), dt))
        return t, Buf(name)

    def ps(self, name, shape, dt):
        return self.es.enter_context(self.nc.psum_tensor(name, list(shape), dt))

    def _deps(self, reads, writes):
        toks = []
        for b in reads:
            if b.w is not None:
                toks.append(b.w)
        for b in writes:
            if b.w is not None:
                toks.append(b.w)
            toks.extend(b.r.items())
        return toks

    def _wait(self, e, toks):
        need = {}
        for k, v in toks:
            if k == e and e == "pe":
                continue
            if self.known[e].get(k, 0) >= v:
                continue
            if need.get(k, 0) < v:
                need[k] = v
        for k, v in need.items():
            sem = self.sem[k] if isinstance(k, str) else (self.dsem[k[1]] if k[0] == "d" else self.xsem[k])
            self.eng[e].wait_ge(sem, v)
            self.known[e][k] = v

    def _mark(self, tok, reads, writes):
        k, v = tok
        for b in reads:
            if b.r.get(k, 0) < v:
                b.r[k] = v
        for b in writes:
            b.w = tok
            b.r = {}

    def op(self, e, fn, reads=(), writes=(), inc=True):
        self._wait(e, self._deps(reads, writes))
        ins = fn(self.eng[e])
        if inc:
            self.cnt[e] += 1
            ins.then_inc(self.sem[e], 1)
            tok = (e, self.cnt[e])
        else:
            assert e == "pe"
            tok = (e, self.cnt[e] + 1)
        self._mark(tok, reads, writes)
        return tok

    def dma(self, out, in_, reads=(), writes=(), q="sp"):
        toks = self._deps(reads, writes)
        if q == "pool":
            key = ("x", len(self.xsem))
            self.xsem[key] = self.root_es.enter_context(self.nc.semaphore("x%d" % len(self.xsem)))
            self._wait(q, toks)
            self.eng[q].dma_start(out=out, in_=in_).then_inc(self.xsem[key], 16)
            tok = (key, 16)
            self._mark(tok, reads, writes)
            return tok
        k = self.dnext
        self.dnext = (k + 1) % NDMA
        if self.dtot[k] > 0:
            toks.append((("d", k), self.dtot[k]))
        self._wait(q, toks)
        self.dtot[k] += 16
        self.eng[q].dma_start(out=out, in_=in_).then_inc(self.dsem[k], 16)
        tok = (("d", k), self.dtot[k])
        self._mark(tok, reads, writes)
        return tok

    def barrier(self):
        toks = [(("d", k), self.dtot[k]) for k in range(NDMA) if self.dtot[k] > 0]
        toks += [(k, 16) for k in self.xsem]
        toks += [(e, c) for e, c in self.cnt.items() if c > 0]
        for e in self.eng:
            self._wait(e, toks)

    def finish(self):
        toks = [(("d", k), self.dtot[k]) for k in range(NDMA) if self.dtot[k] > 0]
        toks += [(k, 16) for k in self.xsem]
        toks += [(e, c) for e, c in self.cnt.items() if c > 0]
        self._wait("sp", toks)


def mm(kb, out, lhsT, rhs, reads, writes, start=True, stop=True, inc=None):
    if inc is None:
        inc = stop
    return kb.op("pe", lambda e: e.matmul(out, lhsT, rhs, start=start, stop=stop), reads, writes, inc=inc)


def tr(kb, out, in_, ident, reads, writes, inc=True):
    return kb.op("pe", lambda e: e.transpose(out, in_, ident), reads, writes, inc=inc)


def layernorm_store(kb, st, r_ap, rB, out_dram_ap, outB, G, Bt, GB):
    s1, s1B = st["s1"]
    kb.op("dve", lambda e: e.reduce_sum(s1[:], r_ap, AX.X), [rB], [s1B])
    kb.op("dve", lambda e: e.tensor_scalar(s1[:], s1[:], -1.0 / D, None, ALU.mult), [s1B], [s1B])
    kb.op("dve", lambda e: e.tensor_scalar(r_ap, r_ap, s1[:], None, ALU.add), [rB, s1B], [rB])
    junk, junkB = st["lnjunk"]
    ss, ssB = st["ss"]
    kb.op("act", lambda e: e.activation(junk[:], r_ap, AF.Square, accum_out=ss[:]), [rB], [junkB, ssB])
    kb.op("dve", lambda e: e.tensor_scalar(ss[:], ss[:], 1.0 / D, EPS, ALU.mult, ALU.add), [ssB], [ssB])
    kb.op("act", lambda e: e.activation(ss[:], ss[:], AF.Sqrt), [ssB], [ssB])
    kb.op("dve", lambda e: e.reciprocal(ss[:], ss[:]), [ssB], [ssB])
    kb.op("dve", lambda e: e.scalar_tensor_tensor(r_ap, r_ap, ss[:], G[:], ALU.mult, ALU.mult), [rB, ssB] + list(GB), [rB])
    kb.op("dve", lambda e: e.tensor_tensor(r_ap, r_ap, Bt[:], ALU.add), [rB] + list(GB), [rB])
    kb.dma(out_dram_ap, r_ap, [rB], [outB])


def emit_layer0(nc, kb, dr, x_in, xinB, x_out, xoutB, cst, nblk=NB):
    es = ExitStack()
    kb_es = kb.es
    kb.es = es
    sb = kb.sb
    identB, identF, CM, BIAS, ones, cB = cst["identB"], cst["identF"], cst["CM"], cst["BIAS"], cst["ones"], cst["buf"]

    W0, W0B = sb("W0", [128, 8, A_IN], BF16)
    Wk2, Wk2B = sb("Wk2", [128, 8, 128], BF16)
    Wuk, WukB = sb("Wuk", [128, 8, 256], BF16)
    Wuv, WuvB = sb("Wuv", [128, 8, 2, 128], BF16)
    Wout, WoutB = sb("Wout", [128, 8, 1024], BF16)
    Gkv, GkvB = sb("Gkv", [128, 256], F32)
    LG, LGB = sb("LG", [128, 1024], F32)
    LB, LBB = sb("LB", [128, 1024], F32)
    win = dr["a_w_in"].rearrange("(k p) n -> p k n", p=128)
    for k in range(0, 8, 4):
        kb.dma(W0[:, k:k + 4, :], win[:, k:k + 4, :], [], [W0B], q="pool")
    kb.dma(Wk2[:, :, 0:64], win[:, :, 1792:1856], [], [Wk2B], q="pool")
    kb.dma(Wk2[:, :, 64:128], win[:, :, 1792:1856], [], [Wk2B], q="pool")
    kb.dma(Wuk[:], dr["a_w_uk"].rearrange("h d c -> d h c"), [], [WukB], q="pool")
    kb.dma(Wuv[:], dr["a_w_uv"].rearrange("h (cc p) d -> p h cc d", p=128), [], [WuvB], q="pool")
    kb.dma(Wout[:], dr["a_w_out"].rearrange("(k p) n -> p k n", p=128), [], [WoutB], q="pool")
    kb.dma(Gkv[:], dr["a_kv_norm_g"].partition_broadcast(128), [], [GkvB])
    kb.dma(LG[:], dr["a_ln_g"].partition_broadcast(128), [], [LGB])
    kb.dma(LB[:], dr["a_ln_b"].partition_broadcast(128), [], [LBB])

    kT2, kT2B = sb("kT2", [128, T], BF16)
    cN, cNB = sb("cN", [128, NB, 256], BF16)
    cT, cTB = sb("cT", [128, 2, T], BF16)
    SC, SCB = sb("SC", [128, T], F32)
    M, MB = sb("M", [128, T], BF16)
    MT, MTB = sb("MT", [128, NB, 128], BF16)
    XF = [sb("XF%d" % s, [128, 1024], F32) for s in range(1)]
    xT, xTB = sb("xT", [128, 8, 128], BF16)
    qT, qTB = sb("qT", [128, 8, 128], BF16)
    qiT, qiTB = sb("qiT", [128, 4, 128], BF16)
    qlT, qlTB = sb("qlT", [128, 2, 1024], BF16)
    sz, szB = sb("sz", [128, 1024], BF16)
    cf, cfB = sb("cf", [128, 256], F32)
    wsb, wsbB = sb("wsb", [128, 8], F32)
    RR = [sb("R%d" % s, [128, 512], F32) for s in range(3)]
    PT = [sb("PT%d" % s, [128, 8, 128], BF16) for s in range(2)]
    olT, olTB = sb("olT", [128, 2, 1024], BF16)
    og, ogB = sb("og", [128, 1024], BF16)
    ogT, ogTB = sb("ogT", [128, 8, 128], BF16)
    rr, rrB = sb("rr", [128, 1024], F32)
    rden, rdenB = sb("rden", [128, 8], F32)
    dens, densB = sb("dens", [128, 8], F32)
    bh, bhB = sb("bh", [128, 32], F32)
    jm, jmB = sb("jm", [128, 1], F32)
    VH, VHB = sb("VH", [128, 8, 1], BF16)
    VHD, VHDB = sb("VHD", [128, 8, 8], BF16)
    SH, SHB = sb("SH", [8, 1024], BF16)
    CMISC = cst["CMISC"]
    OLs, OLsB = sb("OLs", [128, 2, 1024], F32)
    lo, loB = sb("lo", [128, 1], F32)
    mid, midB = sb("mid", [128, 1], F32)
    cnt, cntB = sb("cnt", [128, 1], F32)
    cntA, cntAB = sb("cntA", [128, 1], F32)
    MBa = kb.buf("MBa")
    ge, geB = sb("ge", [128, 1], F32)
    st = {"s1": sb("s1", [128, 1], F32), "ss": sb("ss", [128, 1], F32), "lnjunk": (og, ogB)}
    ssc, sscB = sb("ssc", [128, 1], F32)
    cjunk, cjunkB = sb("cjunk", [128, 256], BF16)

    P0 = kb.ps("P0", [128, 1024], F32)
    P1 = kb.ps("P1", [128, 1024], F32)
    P2 = kb.ps("P2", [128, 1024], F32)
    PX = kb.ps("PX", [128, 512], F32)
    P16 = kb.ps("P16", [128, 1024], BF16)
    B0a, B0b, B1a, B1b, B2a, B2b, BX, B16 = [kb.buf("ps%d" % i) for i in range(8)]
    OPB = [[B1a, B1b], [B2a, B2b]]
    BXh = [BX, BX]
    PTB = [[kb.buf("pt%d%d" % (s_, h_)) for h_ in range(2)] for s_ in range(2)]
    OLB = [[kb.buf("ol%d%d" % (c_, h_)) for h_ in range(2)] for c_ in range(2)]
    DNB = [kb.buf("dn0"), kb.buf("dn1")]

    scale_q = float(128 ** -0.5)

    for i in range(nblk):
        n = 128 * (i + 1)
        xf, xfB = XF[0]
        kb.dma(xf[:], x_in[i * 128:(i + 1) * 128, :], [xinB], [xfB])
        import os
        stage = float(os.environ.get("K_STAGE", "99")) if i == 0 else float(os.environ.get("K_STAGE1", os.environ.get("K_STAGE", "99")))
        if stage <= 0:
            kb.dma(x_out[i * 128:(i + 1) * 128, :], xf[:], [xfB], [xoutB])
            continue
        for k in range(8):
            tr(kb, P0[:, k * 128:(k + 1) * 128], xf[:, k * 128:(k + 1) * 128], identF[:], [xfB, cB], [B0a if k < 4 else B0b], inc=(k == 7))
        kb.op("act", lambda e: e.copy(xT[:].rearrange("p k t -> p (k t)"), P0[:]), [B0a, B0b], [xTB])
        if stage <= 0.1:
            kb.op("dve", lambda e: e.memset(rr[:], 0.0), [], [rrB])
            kb.op("dve", lambda e: e.tensor_copy(rr[:, 0:128], xT[:, 0, :]), [xTB], [rrB])
            kb.dma(x_out[i * 128:(i + 1) * 128, :], rr[:], [rrB], [xoutB])
            continue
        for h in range(8):
            for k in range(8):
                mm(kb, P1[:, h * 128:(h + 1) * 128], W0[:, k, h * 128:(h + 1) * 128], xT[:, k, :], [W0B, xTB],
                   [B1a if h < 4 else B1b], start=(k == 0), stop=(k == 7), inc=(k == 7 and h == 7))
        kb.op("dve", lambda e: e.tensor_copy(qT[:].rearrange("p h t -> p (h t)"), P1[:]), [B1a, B1b], [qTB])
        if stage <= 0.2:
            kb.op("dve", lambda e: e.memset(rr[:], 0.0), [], [rrB])
            kb.op("dve", lambda e: e.tensor_copy(rr[:, 0:128], xT[:, 0, :]), [xTB], [rrB])
            kb.dma(x_out[i * 128:(i + 1) * 128, :], rr[:], [rrB], [xoutB])
            continue
        for c in range(4):
            for k in range(8):
                mm(kb, P2[:, c * 128:(c + 1) * 128], W0[:, k, 1280 + c * 128:1280 + (c + 1) * 128], xT[:, k, :],
                   [W0B, xTB], [B2a], start=(k == 0), stop=(k == 7), inc=(k == 7 and c == 3))
        kb.op("act", lambda e: e.copy(qiT[:].rearrange("p c t -> p (c t)"), P2[:, 0:512]), [B2a], [qiTB])
        if stage <= 0.25:
            kb.op("dve", lambda e: e.memset(rr[:], 0.0), [], [rrB])
            kb.op("dve", lambda e: e.tensor_copy(rr[:, 0:128], xT[:, 0, :]), [xTB], [rrB])
            kb.dma(x_out[i * 128:(i + 1) * 128, :], rr[:], [rrB], [xoutB])
            continue
        for k in range(8):
            mm(kb, P2[:, 512:640], Wk2[:, k, :], xT[:, k, :], [Wk2B, xTB], [B2b], start=(k == 0), stop=(k == 7), inc=False)
        for k in range(8):
            mm(kb, P2[:, 640:896], xT[:, k, :], W0[:, k, 1024:1280], [W0B, xTB], [B2b], start=(k == 0), stop=(k == 7), inc=False)
        for k in range(8):
            mm(kb, P2[:, 896:960], xT[:, k, :], W0[:, k, 1856:1920], [W0B, xTB], [B2b], start=(k == 0), stop=(k == 7), inc=(k == 7))
        if stage <= 0.27:
            kb.op("dve", lambda e: e.memset(rr[:], 0.0), [], [rrB])
            kb.op("dve", lambda e: e.tensor_copy(rr[:, 0:128], xT[:, 0, :]), [xTB], [rrB])
            kb.dma(x_out[i * 128:(i + 1) * 128, :], rr[:], [rrB], [xoutB])
            continue
        kb.op("dve", lambda e: e.tensor_copy(kT2[:, i * 128:(i + 1) * 128], P2[:, 512:640]), [B2b], [kT2B])
        if stage <= 0.28:
            kb.op("dve", lambda e: e.memset(rr[:], 0.0), [], [rrB])
            kb.op("dve", lambda e: e.tensor_copy(rr[:, 0:128], xT[:, 0, :]), [xTB], [rrB])
            kb.dma(x_out[i * 128:(i + 1) * 128, :], rr[:], [rrB], [xoutB])
            continue
        kb.op("dve", lambda e: e.tensor_copy(wsb[:], P2[:, 896:904]), [B2b], [wsbB])
        if stage <= 0.29:
            kb.op("dve", lambda e: e.memset(rr[:], 0.0), [], [rrB])
            kb.op("dve", lambda e: e.tensor_copy(rr[:, 0:128], xT[:, 0, :]), [xTB], [rrB])
            kb.dma(x_out[i * 128:(i + 1) * 128, :], rr[:], [rrB], [xoutB])
            continue
        kb.op("dve", lambda e: e.tensor_copy(cf[:], P2[:, 640:896]), [B2b], [cfB])
        if stage <= 0.3:
            kb.op("dve", lambda e: e.memset(rr[:], 0.0), [], [rrB])
            kb.op("dve", lambda e: e.tensor_copy(rr[:, 0:128], xT[:, 0, :]), [xTB], [rrB])
            kb.dma(x_out[i * 128:(i + 1) * 128, :], rr[:], [rrB], [xoutB])
            continue
        for nn in range(2):
            for k in range(8):
                mm(kb, P0[:, nn * 512:(nn + 1) * 512], xT[:, k, :], W0[:, k, 1864 + nn * 512:1864 + (nn + 1) * 512],
                   [W0B, xTB], [B0a if nn == 0 else B0b], start=(k == 0), stop=(k == 7), inc=(k == 7))
        kb.op("act", lambda e: e.activation(sz[:], P0[:], AF.Silu), [B0a, B0b], [szB])
        if stage <= 0.4:
            kb.op("dve", lambda e: e.memset(rr[:], 0.0), [], [rrB])
            kb.op("dve", lambda e: e.tensor_copy(rr[:, 0:128], xT[:, 0, :]), [xTB], [rrB])
            kb.dma(x_out[i * 128:(i + 1) * 128, :], rr[:], [rrB], [xoutB])
            continue
        kb.op("act", lambda e: e.activation(cjunk[:], cf[:], AF.Square, accum_out=ssc[:]), [cfB], [cjunkB, sscB])
        kb.op("dve", lambda e: e.tensor_scalar(ssc[:], ssc[:], 1.0 / 256, EPS, ALU.mult, ALU.add), [sscB], [sscB])
        kb.op("act", lambda e: e.activation(ssc[:], ssc[:], AF.Sqrt), [sscB], [sscB])
        kb.op("dve", lambda e: e.reciprocal(ssc[:], ssc[:]), [sscB], [sscB])
        kb.op("dve", lambda e: e.scalar_tensor_tensor(cN[:, i, :], cf[:], ssc[:], Gkv[:], ALU.mult, ALU.mult),
              [cfB, sscB, GkvB], [cNB])
        for cc in range(2):
            tr(kb, P16[:, cc * 128:(cc + 1) * 128], cN[:, i, cc * 128:(cc + 1) * 128], identB[:], [cNB, cB], [B16], inc=(cc == 1))
        kb.op("dve", lambda e: e.tensor_copy(cT[:, :, i * 128:(i + 1) * 128], P16[:, 0:256].rearrange("p (c t) -> p c t", c=2)),
              [B16], [cTB])
        if stage <= 0.6:
            kb.op("dve", lambda e: e.memset(rr[:], 0.0), [], [rrB])
            kb.op("dve", lambda e: e.tensor_copy(rr[:, 0:128], xT[:, 0, :]), [xTB], [rrB])
            kb.dma(x_out[i * 128:(i + 1) * 128, :], rr[:], [rrB], [xoutB])
            continue
        for cc in range(2):
            Pq = P0 if cc == 0 else P1
            for h in range(8):
                bq = (B0a, B0b, B1a, B1b)[cc * 2 + (h // 4)]
                mm(kb, Pq[:, h * 128:(h + 1) * 128], Wuk[:, h, cc * 128:(cc + 1) * 128], qT[:, h, :], [WukB, qTB], [bq],
                   inc=(h == 7))
        kb.op("act", lambda e: e.activation(qlT[:, 0, :], P0[:], AF.Copy, scale=scale_q), [B0a, B0b], [qlTB])
        kb.op("dve", lambda e: e.tensor_scalar(qlT[:, 1, :], P1[:], scale_q, None, ALU.mult), [B1a, B1b], [qlTB])

        if stage <= 1:
            kb.op("dve", lambda e: e.tensor_copy(rr[:, 0:256], cN[:, i, :]), [cNB], [rrB])
            kb.op("dve", lambda e: e.tensor_copy(rr[:, 256:1024], qlT[:, 0, 0:768]), [qlTB], [rrB])
            kb.dma(x_out[i * 128:(i + 1) * 128, :], rr[:], [rrB], [xoutB])
            continue
        nkc = (n + 511) // 512
        slots = [(PX, BX, slice(0, 512)), (P2, B2a, slice(0, 512)), (P2, B2b, slice(512, 1024))]
        u = 0
        for kc in range(nkc):
            wd = min(512, n - 512 * kc)
            c0 = kc * 512
            for h in range(8):
                Pt, Bt, sl = slots[u % 3]
                R, RB = RR[u % 3]
                u += 1
                pr = slice((h % 2) * 64, (h % 2) * 64 + 64)
                mm(kb, Pt[:, sl.start:sl.start + wd], qiT[pr, h // 2, :], kT2[pr, c0:c0 + wd], [qiTB, kT2B], [Bt])
                kb.op("act", lambda e: e.activation(R[:, 0:wd], Pt[:, sl.start:sl.start + wd], AF.Relu), [Bt], [RB])
                if h == 0:
                    kb.op("dve", lambda e: e.tensor_scalar(SC[:, c0:c0 + wd], R[:, 0:wd], wsb[:, 0:1], None, ALU.mult),
                          [RB, wsbB], [SCB])
                else:
                    kb.op("dve", lambda e: e.scalar_tensor_tensor(SC[:, c0:c0 + wd], R[:, 0:wd], wsb[:, h:h + 1], SC[:, c0:c0 + wd],
                                                                   ALU.mult, ALU.add), [RB, wsbB, SCB], [SCB])
        kb.op("dve", lambda e: e.tensor_tensor(SC[:, i * 128:n], SC[:, i * 128:n], CM[:], ALU.add), [SCB, cB], [SCB])

        MBall = [MB, MBa]
        if i >= 2:
            nd = 128 * max(1, int(round(0.45 * (i + 1))))
            na = n - nd
            kb.op("dve", lambda e: e.memset(mid[:], 0.0), [], [midB])
            for s in range(BIS_STEPS):
                wk = float(-BIS_LO / (2 ** s))
                last = (s == BIS_STEPS - 1)
                kb.op("dve", lambda e: e.tensor_scalar(M[:, 0:nd], SC[:, 0:nd], mid[:], None, ALU.is_ge, ALU.add, accum_out=cnt[:]),
                      [SCB, midB], [MB, cntB])
                kb.op("act", lambda e: e.activation(M[:, nd:n], SC[:, nd:n], AF.Sign, bias=mid[:], scale=-1.0, accum_out=cntA[:]),
                      [SCB, midB], [MBa, cntAB])
                kb.op("dve", lambda e: e.scalar_tensor_tensor(ge[:], cnt[:], 2.0, cntA[:], ALU.mult, ALU.subtract), [cntB, cntAB], [geB])
                kb.op("dve", lambda e: e.tensor_scalar(ge[:], ge[:], float(511 - na), wk, ALU.is_ge, ALU.mult), [geB], [geB])
                if not last:
                    kb.op("dve", lambda e: e.scalar_tensor_tensor(mid[:], ge[:], -0.5 * wk, mid[:], ALU.add, ALU.add), [geB, midB], [midB])
                else:
                    kb.op("dve", lambda e: e.scalar_tensor_tensor(lo[:], ge[:], -wk, mid[:], ALU.add, ALU.add), [geB, midB], [loB])
            kb.op("dve", lambda e: e.tensor_scalar(M[:, 0:n], SC[:, 0:n], lo[:], -30000.0, ALU.is_lt, ALU.mult), [SCB, loB], MBall)
        else:
            kb.op("dve", lambda e: e.tensor_scalar(M[:, 0:n], SC[:, 0:n], -1e29, -30000.0, ALU.is_lt, ALU.mult), [SCB], MBall)
        nbk = i + 1
        kb.op("dve", lambda e: e.tensor_reduce(bh[:, 0:nbk], M[:, 0:n].rearrange("p (j s) -> p j s", s=128), AX.X, ALU.max), MBall, [bhB])
        kb.op("dve", lambda e: e.tensor_tensor(bh[:, 0:nbk], bh[:, 0:nbk], CMISC[:, 0:nbk], ALU.add), [bhB, cB], [bhB])
        kb.op("dve", lambda e: e.reduce_max(jm[:], bh[:, 0:nbk], AX.X), [bhB], [jmB])
        kb.op("dve", lambda e: e.tensor_scalar(jm[:], jm[:], -1.0, float(i + 1), ALU.mult, ALU.add), [jmB], [jmB])
        kb.op("dve", lambda e: e.tensor_scalar(VH[:].rearrange("p h o -> p (h o)"), CMISC[:, 32:40], jm[:], None, ALU.mult), [jmB, cB], [VHB])
        kb.op("dve", lambda e: e.tensor_tensor(VHD[:], CMISC[:, 40:104].rearrange("p (h g) -> p h g", g=8), VH[:].to_broadcast([128, 8, 8]), ALU.mult),
              [VHB, cB], [VHDB])
        for h in range(8):
            tr(kb, P16[0:8, h * 128:(h + 1) * 128], VHD[:, h, :], identB[:], [VHDB, cB], [B16], inc=(h == 7))
        kb.op("act", lambda e: e.copy(SH[:], P16[0:8, :]), [B16], [SHB])
        if stage <= 2:
            kb.op("dve", lambda e: e.tensor_copy(rr[:, 0:128], M[:, 0:128]), [MB], [rrB])
            kb.op("dve", lambda e: e.tensor_copy(rr[:, 128:256], SC[:, 0:128]), [SCB], [rrB])
            kb.dma(x_out[i * 128:(i + 1) * 128, :], rr[:], [rrB], [xoutB])
            continue
        for j0 in range(0, i + 1, 8):
            nj = min(8, i + 1 - j0)
            for jj in range(nj):
                j = j0 + jj
                tr(kb, P16[:, jj * 128:(jj + 1) * 128], M[:, j * 128:(j + 1) * 128], identB[:], [MB, MBa, cB], [B16], inc=(jj == nj - 1))
            kb.op("act", lambda e: e.copy(MT[:, j0:j0 + nj, :].rearrange("p j t -> p (j t)"), P16[:, 0:nj * 128]), [B16], [MTB])

        jlist = list(range(i + 1))
        if os.environ.get("K_JLAST"):
            jlist = [i]
        units = [(j, hb) for j in jlist for hb in range(2)]

        def emit_st(j, hb):
            bS = B0a if hb == 0 else B0b
            for cc in range(2):
                mm(kb, P0[:, hb * 512:(hb + 1) * 512], cT[:, cc, j * 128:(j + 1) * 128], qlT[:, cc, hb * 512:(hb + 1) * 512],
                   [cTB, qlTB], [bS], start=(cc == 0), stop=False, inc=False)
            mm(kb, P0[:, hb * 512:(hb + 1) * 512], ones[0:8, :], SH[:, hb * 512:(hb + 1) * 512], [SHB, cB], [bS],
               start=False, stop=False, inc=False)
            mm(kb, P0[:, hb * 512:(hb + 1) * 512], identB[:], MT[:, j:j + 1, :].to_broadcast([128, 4, 128]), [MTB, cB], [bS],
               start=False, stop=True, inc=True)

        def emit_rest(j, hb):
            d = i - j
            bS = B0a if hb == 0 else B0b
            Pt_ = PT[j % 2][0]
            PtB = PTB[j % 2][hb]
            for h in range(4 * hb, 4 * hb + 4):
                kb.op("act", lambda e: e.activation(Pt_[:, h, :], P0[:, h * 128:(h + 1) * 128], AF.Exp,
                                                     bias=BIAS[:, h * 32 + d:h * 32 + d + 1], scale=1.0), [bS, cB], [PtB])
            for cc in range(2):
                Po = P1 if cc == 0 else P2
                mm(kb, Po[:, hb * 512:(hb + 1) * 512], cN[:, j, cc * 128:(cc + 1) * 128],
                   Pt_[:, 4 * hb:4 * hb + 4, :].rearrange("p h t -> p (h t)"), [cNB, PtB], [OPB[cc][hb]], start=True, stop=True, inc=False)
            for h in range(4 * hb, 4 * hb + 4):
                mm(kb, PX[:, h:h + 1], Pt_[:, h, :], ones[:, 0:1], [PtB, cB], [BXh[hb]], start=(h % 4 == 0), stop=(h % 4 == 3), inc=(h % 4 == 3))
            first = (j == jlist[0])
            hs = slice(hb * 512, (hb + 1) * 512)
            ds = slice(4 * hb, 4 * hb + 4)
            if first:
                kb.op("dve", lambda e: e.tensor_copy(dens[:, ds], PX[:, ds]), [BXh[hb]], [DNB[hb]])
            else:
                kb.op("dve", lambda e: e.tensor_tensor(dens[:, ds], dens[:, ds], PX[:, ds], ALU.add), [BXh[hb], DNB[hb]], [DNB[hb]])
            for cc in range(2):
                Po = P1 if cc == 0 else P2
                if first:
                    kb.op("dve", lambda e: e.tensor_copy(OLs[:, cc, hs], Po[:, hs]), [OPB[cc][hb]], [OLB[cc][hb]])
                else:
                    kb.op("dve", lambda e: e.tensor_tensor(OLs[:, cc, hs], OLs[:, cc, hs], Po[:, hs], ALU.add), [OPB[cc][hb], OLB[cc][hb]], [OLB[cc][hb]])

        emit_st(*units[0])
        for ui, (j, hb) in enumerate(units):
            if ui + 1 < len(units):
                emit_st(*units[ui + 1])
            emit_rest(j, hb)

        kb.op("dve", lambda e: e.reciprocal(rden[:], dens[:]), DNB, [rdenB])
        kb.op("act", lambda e: e.copy(olT[:, 0, :], OLs[:, 0, :]), OLB[0], [olTB])
        kb.op("dve", lambda e: e.tensor_copy(olT[:, 1, :], OLs[:, 1, :]), OLB[1], [olTB])
        for h in range(8):
            for cc in range(2):
                mm(kb, P0[:, h * 128:(h + 1) * 128], olT[:, cc, h * 128:(h + 1) * 128], Wuv[:, h, cc, :], [olTB, WuvB],
                   [B0a if h < 4 else B0b], start=(cc == 0), stop=(cc == 1), inc=(cc == 1 and h == 7))
        for h in range(8):
            kb.op("dve", lambda e: e.scalar_tensor_tensor(og[:, h * 128:(h + 1) * 128], P0[:, h * 128:(h + 1) * 128], rden[:, h:h + 1],
                                                           sz[:, h * 128:(h + 1) * 128], ALU.mult, ALU.mult),
                  [B0a if h < 4 else B0b, rdenB, szB], [ogB])
        for k in range(8):
            tr(kb, P16[:, k * 128:(k + 1) * 128], og[:, k * 128:(k + 1) * 128], identB[:], [ogB, cB], [B16], inc=(k == 7))
        kb.op("act", lambda e: e.copy(ogT[:].rearrange("p k t -> p (k t)"), P16[:]), [B16], [ogTB])
        for nn in range(2):
            for k in range(8):
                mm(kb, P1[:, nn * 512:(nn + 1) * 512], ogT[:, k, :], Wout[:, k, nn * 512:(nn + 1) * 512], [ogTB, WoutB],
                   [B1a if nn == 0 else B1b], start=(k == 0), stop=(k == 7), inc=(k == 7))
        kb.op("dve", lambda e: e.scalar_tensor_tensor(rr[:], xf[:], ALPHA, P1[:], ALU.mult, ALU.add), [xfB, B1a, B1b], [rrB])
        layernorm_store(kb, st, rr[:], rrB, x_out[i * 128:(i + 1) * 128, :], xoutB, LG, LB, [LGB, LBB])

    kb.finish_layer = True
    kb.es = kb_es
    return es


def load_xT(kb, x_in, xinB, i, xf, xfB, xT, xTB, Ptr, Bs, identF, cB):
    kb.dma(xf[:], x_in[i * 128:(i + 1) * 128, :], [xinB], [xfB])
    for k in range(8):
        tr(kb, Ptr[:, k * 128:(k + 1) * 128], xf[:, k * 128:(k + 1) * 128], identF[:], [xfB, cB], Bs, inc=(k == 7))
    kb.op("act", lambda e: e.copy(xT[:].rearrange("p k t -> p (k t)"), Ptr[:, 0:1024]), Bs, [xTB])


def emit_layer1a(nc, kb, dr, x_in, xinB, hn, hnB, cst, nblk=NB):
    es = ExitStack()
    kb_es = kb.es
    kb.es = es
    sb = kb.sb
    identB, identF, ones, cB, TRIf = cst["identB"], cst["identF"], cst["ones"], cst["buf"], cst["TRIf"]
    win = dr["b_w_in"].rearrange("(k p) n -> p k n", p=128)
    Wqk, WqkB = sb("Wqk", [128, 8, 2048], BF16)
    Wv, WvB = sb("Wv", [128, 8, 2048], BF16)
    Wg, WgB = sb("Wg", [128, 8, 8], BF16)
    for k in range(0, 8, 4):
        kb.dma(Wqk[:, k:k + 4, :], win[:, k:k + 4, 0:2048], [], [WqkB], q="pool")
        kb.dma(Wv[:, k:k + 4, :], win[:, k:k + 4, 2048:4096], [], [WvB], q="pool")
    kb.dma(Wg[:], win[:, :, 4096:4104], [], [WgB], q="pool")
    CWr, CWrB = sb("CWr", [80, 128], F32)
    CWT, CWTB = sb("CWT", [128, 80], F32)
    kb.dma(CWr[0:64, :], dr["b_conv_w"].rearrange("j (c p) -> (j c) p", p=128), [], [CWrB])
    kb.dma(CWr[64:80, :], dr["b_conv_b"].rearrange("o (c p) -> (o c) p", p=128), [], [CWrB])
    GBt, GBB = sb("GBt", [128, 8], F32)
    kb.dma(GBt[:, 0:4], dr["b_i_bias"].partition_broadcast(128), [], [GBB])
    kb.dma(GBt[:, 4:8], dr["b_f_bias"].partition_broadcast(128), [], [GBB])
    HG, HGB = sb("HG", [128, 2048], F32)
    kb.dma(HG[:], dr["b_head_norm_g"].rearrange("o h v -> o (h v)").partition_broadcast(128), [], [HGB])

    PA = kb.ps("L1PA", [128, 2048], F32)
    PB = kb.ps("L1PB", [128, 1024], F32)
    PC = kb.ps("L1PC", [128, 512], F32)
    P16 = kb.ps("L1P16", [128, 1024], BF16)
    BA = [kb.buf("pa%d" % i) for i in range(4)]
    BBa, BBb, BC, B16 = kb.buf("pba"), kb.buf("pbb"), kb.buf("pc"), kb.buf("p16")

    tr(kb, PC[:, 0:80], CWr[:], identF[0:80, 0:80], [CWrB, cB], [BC])
    kb.op("dve", lambda e: e.tensor_copy(CWT[:], PC[:, 0:80]), [BC], [CWTB])

    XF2 = [sb("xf1_%d" % s_, [128, 1024], F32) for s_ in range(2)]
    XT2 = [sb("xT1_%d" % s_, [128, 8, 128], BF16) for s_ in range(2)]
    QKP, QKPB = sb("QKP", [128, 16, 131], F32)
    tmpc, tmpcB = sb("tmpc", [128, 128], F32)
    qkT, qkTB = sb("qkT", [128, 16, 128], BF16)
    VT2 = [sb("vt_%d" % s_, [128, 2048], BF16) for s_ in range(2)]
    gt, gtB = sb("gt", [128, 8], F32)
    lf, lfB = sb("lf", [128, 4], F32)
    ibm, ibmB = sb("ibm", [128, 4], F32)
    bcol, bcolB = sb("bcol", [128, 4], F32)
    ONESf, ONESfB = sb("ONESf", [128, 128], F32)
    LFB4 = [sb("LFB4_%d" % h_, [128, 128], F32) for h_ in range(4)]
    EB4 = [sb("EB4_%d" % h_, [128, 128], F32) for h_ in range(4)]
    DT4 = [sb("DT4_%d" % h_, [128, 128], F32) for h_ in range(4)]
    sT4 = [sb("sT4_%d" % h_, [128, 128], BF16) for h_ in range(4)]
    qh4 = [sb("qh4_%d" % h_, [128, 2, 128], BF16) for h_ in range(4)]
    vs4 = [sb("vs4_%d" % h_, [128, 512], BF16) for h_ in range(4)]
    hh4 = [sb("hh4_%d" % h_, [128, 512], F32) for h_ in range(4)]
    kTok4, kTok4B = sb("kTok4", [128, 1024], BF16)
    wcol4, wcol4B = sb("wcol4", [128, 4], BF16)
    rec4, rec4B = sb("rec4", [128, 4], F32)
    ssh4, ssh4B = sb("ssh4", [128, 4], F32)
    LFB, LFBB = sb("LFB", [128, 128], F32)
    EB, EBB = sb("EB", [128, 128], F32)
    DT, DTB = sb("DT", [128, 128], F32)
    sT, sTB = sb("sT", [128, 128], BF16)
    qh, qhB = sb("qh", [128, 2, 128], BF16)
    kTok, kTokB = sb("kTok", [128, 256], BF16)
    vs, vsB = sb("vs", [128, 512], BF16)
    wcol, wcolB = sb("wcol", [128, 1], BF16)
    hh, hhB = sb("hh", [128, 512], F32)
    hjunk, hjunkB = sb("hjunk", [128, 512], BF16)
    HN, HNB = sb("HN", [128, 2048], BF16)
    rec, recB = sb("rec", [128, 1], F32)
    ssh, sshB = sb("ssh", [128, 1], F32)
    Cs = [sb("C%d" % h, [128, 2, 512], F32) for h in range(4)]
    Cb = [sb("Cb%d" % h, [128, 2, 512], BF16) for h in range(4)]
    ns = [sb("n%d" % h, [128, 2], F32) for h in range(4)]
    nb = [sb("nb%d" % h, [128, 2], BF16) for h in range(4)]
    kb.op("dve", lambda e: e.memset(ONESf[:], 1.0), [], [ONESfB])
    kb.op("dve", lambda e: e.memset(QKP[:], 0.0), [], [QKPB])
    for h in range(4):
        kb.op("dve", lambda e: e.memset(Cs[h][0][:], 0.0), [], [Cs[h][1]])
        kb.op("dve", lambda e: e.memset(Cb[h][0][:], 0.0), [], [Cb[h][1]])
        kb.op("dve", lambda e: e.memset(ns[h][0][:], 0.0), [], [ns[h][1]])
        kb.op("dve", lambda e: e.memset(nb[h][0][:], 0.0), [], [nb[h][1]])

    def seg_A(i):
        xf, xfB = XF2[i % 2]; xT, xTB = XT2[i % 2]; vt, vtB = VT2[i % 2]
        load_xT(kb, x_in, xinB, i, xf, xfB, xT, xTB, PB, [BBa, BBb], identF, cB)

    def seg_Bq(i):
        xf, xfB = XF2[i % 2]; xT, xTB = XT2[i % 2]; vt, vtB = VT2[i % 2]
        for c in range(16):
            for k in range(8):
                mm(kb, PA[:, c * 128:(c + 1) * 128], Wqk[:, k, c * 128:(c + 1) * 128], xT[:, k, :], [WqkB, xTB], [BA[c // 4]],
                   start=(k == 0), stop=(k == 7), inc=(k == 7 and c % 4 == 3))

    def seg_Be(i):
        xf, xfB = XF2[i % 2]; xT, xTB = XT2[i % 2]; vt, vtB = VT2[i % 2]
        kb.op("dve", lambda e: e.tensor_copy(QKP[:, 0:8, 3:131], PA[:, 0:1024].rearrange("p (c t) -> p c t", c=8)), [BA[0], BA[1]], [QKPB])
        kb.op("dve", lambda e: e.tensor_copy(QKP[:, 8:16, 3:131], PA[:, 1024:2048].rearrange("p (c t) -> p c t", c=8)), [BA[2], BA[3]], [QKPB])

    def seg_mid(i):
        xf, xfB = XF2[i % 2]; xT, xTB = XT2[i % 2]; vt, vtB = VT2[i % 2]
        for c in range(16):
            kb.op("dve", lambda e: e.tensor_scalar(tmpc[:], QKP[:, c, 0:128], CWT[:, c:c + 1], CWT[:, 64 + c:65 + c], ALU.mult, ALU.add),
                  [QKPB, CWTB], [tmpcB])
            for j in range(1, 4):
                kb.op("dve", lambda e: e.scalar_tensor_tensor(tmpc[:], QKP[:, c, j:j + 128], CWT[:, j * 16 + c:j * 16 + c + 1], tmpc[:],
                                                               ALU.mult, ALU.add), [QKPB, CWTB, tmpcB], [tmpcB])
            kb.op("act", lambda e: e.activation(qkT[:, c, :], tmpc[:], AF.Silu), [tmpcB], [qkTB])
        kb.op("dve", lambda e: e.tensor_copy(tmpc[:, 0:48].rearrange("p (c t) -> p c t", c=16), QKP[:, :, 128:131]), [QKPB], [tmpcB])
        kb.op("dve", lambda e: e.tensor_copy(QKP[:, :, 0:3], tmpc[:, 0:48].rearrange("p (c t) -> p c t", c=16)), [tmpcB], [QKPB])

    def seg_V(i):
        xf, xfB = XF2[i % 2]; xT, xTB = XT2[i % 2]; vt, vtB = VT2[i % 2]
        for nn in range(4):
            for k in range(8):
                mm(kb, PA[:, nn * 512:(nn + 1) * 512], xT[:, k, :], Wv[:, k, nn * 512:(nn + 1) * 512], [WvB, xTB], [BA[nn]],
                   start=(k == 0), stop=(k == 7), inc=(k == 7))
        kb.op("act", lambda e: e.copy(vt[:, 0:1024], PA[:, 0:1024]), [BA[0], BA[1]], [vtB])
        kb.op("dve", lambda e: e.tensor_copy(vt[:, 1024:2048], PA[:, 1024:2048]), [BA[2], BA[3]], [vtB])
        for k in range(8):
            mm(kb, PC[:, 0:8], xT[:, k, :], Wg[:, k, :], [WgB, xTB], [BC], start=(k == 0), stop=(k == 7), inc=(k == 7))
        kb.op("dve", lambda e: e.tensor_tensor(gt[:], PC[:, 0:8], GBt[:], ALU.add), [BC, GBB], [gtB])
        kb.op("act", lambda e: e.activation(lf[:], gt[:, 4:8], AF.Exp, scale=-1.0), [gtB], [lfB])
        kb.op("act", lambda e: e.activation(lf[:], lf[:], AF.Ln, bias=1.0), [lfB], [lfB])
        kb.op("dve", lambda e: e.tensor_scalar(lf[:], lf[:], -1.0, None, ALU.mult), [lfB], [lfB])
        mm(kb, PC[:, 8:12], TRIf[:], lf[:], [lfB, cB], [BC])
        kb.op("dve", lambda e: e.tensor_copy(bcol[:], PC[:, 8:12]), [BC], [bcolB])
        kb.op("dve", lambda e: e.tensor_tensor(ibm[:], gt[:, 0:4], bcol[:], ALU.subtract), [gtB, bcolB], [ibmB])

    def seg_H(i):
        xf, xfB = XF2[i % 2]; xT, xTB = XT2[i % 2]; vt, vtB = VT2[i % 2]
        H4 = range(4)
        for h in H4:
            kb.op("dve", lambda e: e.tensor_scalar(LFB4[h][0][:], ONESf[:], lf[:, h:h + 1], None, ALU.mult), [ONESfB, lfB], [LFB4[h][1]])
        for h in H4:
            mm(kb, PB[:, h * 128:(h + 1) * 128], LFB4[h][0][:], TRIf[:], [LFB4[h][1], cB], [BBa], inc=(h == 3))
        for h in H4:
            kb.op("act", lambda e: e.activation(EB4[h][0][:], PB[:, h * 128:(h + 1) * 128], AF.Exp), [BBa], [EB4[h][1]])
            kb.op("act", lambda e: e.activation(DT4[h][0][:], PB[:, h * 128:(h + 1) * 128], AF.Exp, bias=ibm[:, h:h + 1]), [BBa, ibmB], [DT4[h][1]])
        for h in H4:
            DT, DTB = DT4[h]; EB, EBB = EB4[h]
            kb.op("dve", lambda e: e.tensor_copy(wcol4[:, h:h + 1], DT[:, 127:128]), [DTB], [wcol4B])
            kb.op("dve", lambda e: e.tensor_scalar(vs4[h][0][:], vt[:, h * 512:(h + 1) * 512], DT[:, 127:128], None, ALU.mult), [vtB, DTB], [vs4[h][1]])
            kb.op("dve", lambda e: e.tensor_tensor(DT[:], DT[:], TRIf[:], ALU.mult), [DTB, cB], [DTB])
            for cc in range(2):
                kb.op("dve", lambda e: e.scalar_tensor_tensor(qh4[h][0][:, cc, :], qkT[:, 2 * h + cc, :], 0.0625, EB[:], ALU.mult, ALU.mult),
                      [qkTB, EBB], [qh4[h][1]])
        for h in H4:
            for cc in range(2):
                mm(kb, PB[:, 512 + h * 128:512 + (h + 1) * 128], qkT[:, 8 + 2 * h + cc, :], qkT[:, 2 * h + cc, :], [qkTB], [BBb],
                   start=(cc == 0), stop=(cc == 1), inc=(cc == 1 and h == 3))
        for h in H4:
            kb.op("dve", lambda e: e.scalar_tensor_tensor(sT4[h][0][:], PB[:, 512 + h * 128:512 + (h + 1) * 128], 0.0625, DT4[h][0][:], ALU.mult, ALU.mult),
                  [BBb, DT4[h][1]], [sT4[h][1]])
        for h in H4:
            sT, sTB = sT4[h]; qh, qhB = qh4[h]
            mm(kb, PA[:, h * 512:(h + 1) * 512], sT[:], vt[:, h * 512:(h + 1) * 512], [sTB, vtB], [BA[h]], start=True, stop=False, inc=False)
            for cc in range(2):
                mm(kb, PA[:, h * 512:(h + 1) * 512], qh[:, cc, :], Cb[h][0][:, cc, :], [qhB, Cb[h][1]], [BA[h]], start=False, stop=(cc == 1), inc=(cc == 1))
        for h in H4:
            sT, sTB = sT4[h]; qh, qhB = qh4[h]
            mm(kb, PC[:, 16 + h:17 + h], sT[:], ones[:, 0:1], [sTB, cB], [BC], start=True, stop=False, inc=False)
            for cc in range(2):
                mm(kb, PC[:, 16 + h:17 + h], qh[:, cc, :], nb[h][0][:, cc:cc + 1], [qhB, nb[h][1]], [BC], start=False, stop=(cc == 1), inc=(cc == 1))
        kb.op("dve", lambda e: e.tensor_scalar(rec4[:], PC[:, 16:20], -1.0, None, ALU.mult), [BC], [rec4B])
        kb.op("dve", lambda e: e.tensor_tensor(rec4[:], rec4[:], PC[:, 16:20], ALU.max), [BC, rec4B], [rec4B])
        kb.op("dve", lambda e: e.tensor_scalar(rec4[:], rec4[:], 1.0, None, ALU.max), [rec4B], [rec4B])
        kb.op("dve", lambda e: e.reciprocal(rec4[:], rec4[:]), [rec4B], [rec4B])
        for h in H4:
            kb.op("dve", lambda e: e.tensor_scalar(hh4[h][0][:], PA[:, h * 512:(h + 1) * 512], rec4[:, h:h + 1], None, ALU.mult), [BA[h], rec4B], [hh4[h][1]])
        for h in H4:
            kb.op("act", lambda e: e.activation(hjunk[:], hh4[h][0][:], AF.Square, accum_out=ssh4[:, h:h + 1]), [hh4[h][1]], [hjunkB, ssh4B])
        kb.op("dve", lambda e: e.tensor_scalar(ssh4[:], ssh4[:], 1.0 / 512, EPS, ALU.mult, ALU.add), [ssh4B], [ssh4B])
        kb.op("act", lambda e: e.activation(ssh4[:], ssh4[:], AF.Sqrt), [ssh4B], [ssh4B])
        kb.op("dve", lambda e: e.reciprocal(ssh4[:], ssh4[:]), [ssh4B], [ssh4B])
        for h in H4:
            kb.op("dve", lambda e: e.scalar_tensor_tensor(HN[:, h * 512:(h + 1) * 512], hh4[h][0][:], ssh4[:, h:h + 1], HG[:, h * 512:(h + 1) * 512],
                                                           ALU.mult, ALU.mult), [hh4[h][1], ssh4B, HGB], [HNB])
        kb.dma(hn[i * 128:(i + 1) * 128, :], HN[:], [HNB], [hnB])
        for h in H4:
            for cc in range(2):
                tr(kb, P16[:, h * 256 + cc * 128:h * 256 + (cc + 1) * 128], qkT[:, 8 + 2 * h + cc, :], identB[:], [qkTB, cB], [B16], inc=(cc == 1 and h == 3))
        kb.op("act", lambda e: e.copy(kTok4[:], P16[:]), [B16], [kTok4B])
        u = 0
        for h in H4:
            C, CB_ = Cs[h]
            EB, EBB = EB4[h]
            for cc in range(2):
                bank = u % 2
                u += 1
                bb = BBa if bank == 0 else BBb
                mm(kb, PB[:, bank * 512:(bank + 1) * 512], kTok4[:, h * 256 + cc * 128:h * 256 + (cc + 1) * 128], vs4[h][0][:], [kTok4B, vs4[h][1]], [bb])
                kb.op("dve", lambda e: e.scalar_tensor_tensor(C[:, cc, :], C[:, cc, :], EB[:, 127:128], PB[:, bank * 512:(bank + 1) * 512], ALU.mult, ALU.add),
                      [CB_, EBB, bb], [CB_])
        for h in H4:
            for cc in range(2):
                mm(kb, PC[:, 32 + 2 * h + cc:33 + 2 * h + cc], kTok4[:, h * 256 + cc * 128:h * 256 + (cc + 1) * 128], wcol4[:, h:h + 1], [kTok4B, wcol4B], [BC],
                   inc=(cc == 1 and h == 3))
        for h in H4:
            nh, nhB = ns[h]
            kb.op("dve", lambda e: e.scalar_tensor_tensor(nh[:], nh[:], EB4[h][0][:, 127:128], PC[:, 32 + 2 * h:34 + 2 * h], ALU.mult, ALU.add),
                  [nhB, EB4[h][1], BC], [nhB])
        for h in H4:
            kb.op("act", lambda e: e.copy(Cb[h][0][:].rearrange("p c v -> p (c v)"), Cs[h][0][:].rearrange("p c v -> p (c v)")), [Cs[h][1]], [Cb[h][1]])
            kb.op("dve", lambda e: e.tensor_copy(nb[h][0][:], ns[h][0][:]), [ns[h][1]], [nb[h][1]])

    seg_A(0)
    seg_Bq(0)
    seg_Be(0)
    for i in range(nblk):
        seg_V(i)
        if i + 1 < nblk:
            seg_A(i + 1)
        seg_mid(i)
        if i + 1 < nblk:
            seg_Bq(i + 1)
            seg_Be(i + 1)
        seg_H(i)
    kb.es = kb_es
    return es


def emit_layer1b(nc, kb, dr, x_in, xinB, hn, hnB, x_out, xoutB, cst, nblk=NB):
    es = ExitStack()
    kb_es = kb.es
    kb.es = es
    sb = kb.sb
    identB, identF, cB = cst["identB"], cst["identF"], cst["buf"]
    win = dr["b_w_in"].rearrange("(k p) n -> p k n", p=128)
    Wo, WoB = sb("Wo", [128, 8, 2048], BF16)
    Wz, WzB = sb("Wz", [128, 8, 2048], BF16)
    Wout, WoutB = sb("Wout1", [128, 16, 1024], BF16)
    wout = dr["b_w_out"].rearrange("(k p) n -> p k n", p=128)
    for k in range(0, 8, 4):
        kb.dma(Wo[:, k:k + 4, :], win[:, k:k + 4, 4104:6152], [], [WoB], q="pool")
        kb.dma(Wz[:, k:k + 4, :], win[:, k:k + 4, 6152:8200], [], [WzB], q="pool")
    for k in range(0, 16, 8):
        kb.dma(Wout[:, k:k + 8, :], wout[:, k:k + 8, :], [], [WoutB], q="pool")
    LG, LGB = sb("LG1", [128, 1024], F32)
    LB, LBB = sb("LB1", [128, 1024], F32)
    kb.dma(LG[:], dr["b_ln_g"].partition_broadcast(128), [], [LGB])
    kb.dma(LB[:], dr["b_ln_b"].partition_broadcast(128), [], [LBB])
    PA = kb.ps("L2PA", [128, 2048], F32)
    PB = kb.ps("L2PB", [128, 1024], F32)
    P16 = kb.ps("L2P16", [128, 2048], BF16)
    BA = [kb.buf("qa%d" % i) for i in range(4)]
    BBa, BBb, B16 = kb.buf("qba"), kb.buf("qbb"), kb.buf("q16")
    xf, xfB = sb("xf2", [128, 1024], F32)
    xT, xTB = sb("xT2", [128, 8, 128], BF16)
    so, soB = sb("so", [128, 2048], BF16)
    hnb, hnbB = sb("hnb", [128, 2048], BF16)
    hg, hgB = sb("hg", [128, 2048], BF16)
    hgT, hgTB = sb("hgT", [128, 16, 128], BF16)
    rr, rrB = sb("rr2", [128, 1024], F32)
    st = {"s1": sb("s1b", [128, 1], F32), "ss": sb("ssb", [128, 1], F32), "lnjunk": sb("lnjunkb", [128, 1024], BF16)}
    for i in range(nblk):
        load_xT(kb, x_in, xinB, i, xf, xfB, xT, xTB, PB, [BBa, BBb], identF, cB)
        kb.dma(hnb[:], hn[i * 128:(i + 1) * 128, :], [hnB], [hnbB])
        for nn in range(4):
            for k in range(8):
                mm(kb, PA[:, nn * 512:(nn + 1) * 512], xT[:, k, :], Wo[:, k, nn * 512:(nn + 1) * 512], [WoB, xTB], [BA[nn]],
                   start=(k == 0), stop=(k == 7), inc=(k == 7))
        kb.op("act", lambda e: e.activation(so[:], PA[:], AF.Sigmoid), BA, [soB])
        kb.op("dve", lambda e: e.tensor_tensor(hg[:], hnb[:], so[:], ALU.mult), [hnbB, soB], [hgB])
        for nn in range(4):
            for k in range(8):
                mm(kb, PA[:, nn * 512:(nn + 1) * 512], xT[:, k, :], Wz[:, k, nn * 512:(nn + 1) * 512], [WzB, xTB], [BA[nn]],
                   start=(k == 0), stop=(k == 7), inc=(k == 7))
        kb.op("act", lambda e: e.activation(so[:], PA[:], AF.Silu), BA, [soB])
        kb.op("dve", lambda e: e.tensor_tensor(hg[:], hg[:], so[:], ALU.mult), [hgB, soB], [hgB])
        for k in range(16):
            tr(kb, P16[:, k * 128:(k + 1) * 128], hg[:, k * 128:(k + 1) * 128], identB[:], [hgB, cB], [B16], inc=(k == 15))
        kb.op("act", lambda e: e.copy(hgT[:].rearrange("p k t -> p (k t)"), P16[:]), [B16], [hgTB])
        for nn in range(2):
            for k in range(16):
                mm(kb, PB[:, nn * 512:(nn + 1) * 512], hgT[:, k, :], Wout[:, k, nn * 512:(nn + 1) * 512], [hgTB, WoutB],
                   [BBa if nn == 0 else BBb], start=(k == 0), stop=(k == 15), inc=(k == 15))
        kb.op("dve", lambda e: e.scalar_tensor_tensor(rr[:], xf[:], ALPHA, PB[:], ALU.mult, ALU.add), [xfB, BBa, BBb], [rrB])
        layernorm_store(kb, st, rr[:], rrB, x_out[i * 128:(i + 1) * 128, :], xoutB, LG, LB, [LGB, LBB])
    kb.es = kb_es
    return es


def make_consts():
    identF = np.eye(128, dtype=np.float32)
    q = np.arange(128)[:, None]
    s = np.arange(128)[None, :]
    CM = np.where(s <= q, 0.0, -1e30).astype(np.float32)
    slopes = 2.0 ** (-(np.arange(1, 9, dtype=np.float64)))
    sp = np.arange(128, dtype=np.float64)[:, None, None]
    dd = np.arange(32, dtype=np.float64)[None, None, :]
    BIAS = (slopes[None, :, None] * (sp - 127.0 - 128.0 * dd)).reshape(128, 256).astype(np.float32)
    misc = np.zeros((128, 104), np.float32)
    misc[:, 0:32] = np.arange(1, 33, dtype=np.float32)[None, :]
    misc[:, 32:40] = (128.0 * slopes)[None, :]
    misc[:, 40:104] = np.eye(8, dtype=np.float32).reshape(1, 64)
    tri = (np.arange(128)[:, None] <= np.arange(128)[None, :]).astype(np.float32)
    return {"c_ident": identF, "c_cm": CM, "c_bias": BIAS, "c_misc": misc, "c_tri": tri}


def build(layers=LAYERS, nblk=NB):
    nc = bass.Bass("TRN2", target_bir_lowering=False)
    dr = {}

    def din(name, shape):
        dr[name] = nc.dram_tensor(name, list(shape), F32, kind="ExternalInput").ap()

    din("x", [T, D])
    din("a_w_in", [D, A_IN]); din("a_kv_norm_g", [1, 256]); din("a_w_uk", [8, 128, 256]); din("a_w_uv", [8, 256, 128])
    din("a_w_out", [D, D]); din("a_ln_g", [1, D]); din("a_ln_b", [1, D])
    din("c_ident", [128, 128]); din("c_cm", [128, 128]); din("c_bias", [128, 256]); din("c_misc", [128, 104]); din("c_tri", [128, 128])
    din("b_w_in", [D, B_IN]); din("b_i_bias", [1, 4]); din("b_f_bias", [1, 4]); din("b_conv_w", [4, 2048]); din("b_conv_b", [1, 2048])
    din("b_head_norm_g", [1, 4, 512]); din("b_w_out", [2048, D]); din("b_ln_g", [1, D]); din("b_ln_b", [1, D])
    out = nc.dram_tensor("out", [T, D], F32, kind="ExternalOutput").ap()
    with ExitStack() as es:
        kb = KB(nc, es)
        identF, idB = kb.sb("identF", [128, 128], F32)
        identB, _ = kb.sb("identB", [128, 128], BF16)
        CM, _ = kb.sb("CM", [128, 128], F32)
        BIAS, _ = kb.sb("BIAS", [128, 256], F32)
        ones, _ = kb.sb("ones", [128, 128], BF16)
        CMISC, _ = kb.sb("CMISC", [128, 104], F32)
        TRIf, _ = kb.sb("TRIf", [128, 128], F32)
        cB = idB
        kb.dma(identF[:], dr["c_ident"], [], [cB])
        kb.dma(identB[:], dr["c_ident"], [], [cB], q="pool")
        kb.dma(CM[:], dr["c_cm"], [], [cB])
        kb.dma(BIAS[:], dr["c_bias"], [], [cB])
        kb.dma(CMISC[:], dr["c_misc"], [], [cB])
        kb.dma(TRIf[:], dr["c_tri"], [], [cB])
        kb.op("dve", lambda e: e.memset(ones[:], 1.0), [], [cB])
        cst = {"identB": identB, "identF": identF, "CM": CM, "BIAS": BIAS, "ones": ones, "buf": cB, "CMISC": CMISC, "TRIf": TRIf}
        xinB = kb.buf("xin")
        outB = kb.buf("out")
        x1d = nc.dram_tensor("x1d", [T, D], F32).ap()
        hnd = nc.dram_tensor("hnd", [T, 2048], BF16).ap()
        x1B = kb.buf("x1d")
        hnB = kb.buf("hnd")
        if layers == (0,):
            l0 = emit_layer0(nc, kb, dr, dr["x"], xinB, out, outB, cst, nblk=nblk)
            kb.finish(); l0.close()
        elif layers == (1,):
            la = emit_layer1a(nc, kb, dr, dr["x"], xinB, hnd, hnB, cst, nblk=nblk)
            kb.barrier(); la.close()
            lb = emit_layer1b(nc, kb, dr, dr["x"], xinB, hnd, hnB, out, outB, cst, nblk=nblk)
            kb.finish(); lb.close()
        else:
            l0 = emit_layer0(nc, kb, dr, dr["x"], xinB, x1d, x1B, cst, nblk=nblk)
            kb.barrier(); l0.close()
            la = emit_layer1a(nc, kb, dr, x1d, x1B, hnd, hnB, cst, nblk=nblk)
            kb.barrier(); la.close()
            lb = emit_layer1b(nc, kb, dr, x1d, x1B, hnd, hnB, out, outB, cst, nblk=nblk)
            kb.finish(); lb.close()
    return nc


_CACHE = {}


def make_inmaps(inputs, xs):
    cst = make_consts()
    shared = {k: np.ascontiguousarray(inputs[k][0], dtype=np.float32) for k in
              ("a_w_in", "a_w_uk", "a_w_uv", "a_w_out", "b_w_in", "b_conv_w", "b_w_out")}
    for k in ("a_kv_norm_g", "a_ln_g", "a_ln_b", "b_i_bias", "b_f_bias", "b_conv_b", "b_ln_g", "b_ln_b"):
        shared[k] = np.ascontiguousarray(inputs[k], dtype=np.float32).reshape(1, -1)
    shared["b_head_norm_g"] = np.ascontiguousarray(inputs["b_head_norm_g"], dtype=np.float32).reshape(1, 4, 512)
    shared.update(cst)
    maps = []
    for xx in xs:
        m = dict(shared)
        m["x"] = np.ascontiguousarray(xx, dtype=np.float32)
        maps.append(m)
    return maps


def kernel(**inputs):
    x = np.ascontiguousarray(inputs["x"], dtype=np.float32)
    if "nc" not in _CACHE:
        import os
        lay = os.environ.get("K_LAYERS")
        _CACHE["nc"] = build(layers=tuple(int(c) for c in lay)) if lay else build()
    nc = _CACHE["nc"]
    in_maps = make_inmaps(inputs, [x[c % 4] for c in range(8)])
    res = run_bass_kernel_spmd(nc, in_maps, core_ids=list(range(8)))
    return np.stack([res.results[c]["out"] for c in range(4)], axis=0)
```

```python
import numpy as np
from contextlib import ExitStack
import concourse.bass as bass
import concourse.mybir as mybir
from concourse.bass_utils import run_bass_kernel_spmd

F32 = mybir.dt.float32
BF16 = mybir.dt.bfloat16
AF = mybir.ActivationFunctionType
ALU = mybir.AluOpType
AX = mybir.AxisListType

T = 4096
D = 1024
NB = T // 128
A_IN = 2888
B_IN = 8200
ALPHA = float(4 ** 0.25)
EPS = 1e-5
NDMA = 12
BIS_STEPS = 26
BIS_LO = -512.0
LAYERS = (0, 1)


class Buf:
    __slots__ = ("name", "w", "r")

    def __init__(self, name):
        self.name = name
        self.w = None
        self.r = {}


class KB:
    def __init__(self, nc, es):
        self.nc = nc
        self.es = es
        self.eng = {"pe": nc.tensor, "act": nc.scalar, "dve": nc.vector, "pool": nc.gpsimd, "sp": nc.sync}
        self.sem = {e: es.enter_context(nc.semaphore("s_" + e)) for e in ("pe", "act", "dve", "pool")}
        self.cnt = {e: 0 for e in self.sem}
        self.known = {e: {} for e in self.eng}
        self.dsem = [es.enter_context(nc.semaphore("d%d" % k)) for k in range(NDMA)]
        self.dtot = [0] * NDMA
        self.dnext = 0
        self.nbuf = 0
        self.root_es = es
        self.xsem = {}

    def buf(self, name=None):
        self.nbuf += 1
        return Buf(name or ("b%d" % self.nbuf))

    def sb(self, name, shape, dt):
        t = self.es.enter_context(self.nc.sbuf_tensor(name, list(shape), dt))
        return t, Buf(name)

    def ps(self, name, shape, dt):
        return self.es.enter_context(self.nc.psum_tensor(name, list(shape), dt))

    def _deps(self, reads, writes):
        toks = []
        for b in reads:
            if b.w is not None:
                toks.append(b.w)
        for b in writes:
            if b.w is not None:
                toks.append(b.w)
            toks.extend(b.r.items())
        return toks

    def _wait(self, e, toks):
        need = {}
        for k, v in toks:
            if k == e and e == "pe":
                continue
            if self.known[e].get(k, 0) >= v:
                continue
            if need.get(k, 0) < v:
                need[k] = v
        for k, v in need.items():
            sem = self.sem[k] if isinstance(k, str) else (self.dsem[k[1]] if k[0] == "d" else self.xsem[k])
            self.eng[e].wait_ge(sem, v)
            self.known[e][k] = v

    def _mark(self, tok, reads, writes):
        k, v = tok
        for b in reads:
            if b.r.get(k, 0) < v:
                b.r[k] = v
        for b in writes:
            b.w = tok
            b.r = {}

    def op(self, e, fn, reads=(), writes=(), inc=True):
        self._wait(e, self._deps(reads, writes))
        ins = fn(self.eng[e])
        if inc:
            self.cnt[e] += 1
            ins.then_inc(self.sem[e], 1)
            tok = (e, self.cnt[e])
        else:
            assert e == "pe"
            tok = (e, self.cnt[e] + 1)
        self._mark(tok, reads, writes)
        return tok

    def dma(self, out, in_, reads=(), writes=(), q="sp"):
        toks = self._deps(reads, writes)
        if q == "pool":
            key = ("x", len(self.xsem))
            self.xsem[key] = self.root_es.enter_context(self.nc.semaphore("x%d" % len(self.xsem)))
            self._wait(q, toks)
            self.eng[q].dma_start(out=out, in_=in_).then_inc(self.xsem[key], 16)
            tok = (key, 16)
            self._mark(tok, reads, writes)
            return tok
        k = self.dnext
        self.dnext = (k + 1) % NDMA
        if self.dtot[k] > 0:
            toks.append((("d", k), self.dtot[k]))
        self._wait(q, toks)
        self.dtot[k] += 16
        self.eng[q].dma_start(out=out, in_=in_).then_inc(self.dsem[k], 16)
        tok = (("d", k), self.dtot[k])
        self._mark(tok, reads, writes)
        return tok

    def barrier(self):
        toks = [(("d", k), self.dtot[k]) for k in range(NDMA) if self.dtot[k] > 0]
        toks += [(k, 16) for k in self.xsem]
        toks += [(e, c) for e, c in self.cnt.items() if c > 0]
        for e in self.eng:
            self._wait(e, toks)

    def finish(self):
        toks = [(("d", k), self.dtot[k]) for k in range(NDMA) if self.dtot[k] > 0]
        toks += [(k, 16) for k in self.xsem]
        toks += [(e, c) for e, c in self.cnt.items() if c > 0]
        self._wait("sp", toks)


def mm(kb, out, lhsT, rhs, reads, writes, start=True, stop=True, inc=None):
    if inc is None:
        inc = stop
    return kb.op("pe", lambda e: e.matmul(out, lhsT, rhs, start=start, stop=stop), reads, writes, inc=inc)


def tr(kb, out, in_, ident, reads, writes, inc=True):
    return kb.op("pe", lambda e: e.transpose(out, in_, ident), reads, writes, inc=inc)


def layernorm_store(kb, st, r_ap, rB, out_dram_ap, outB, G, Bt, GB):
    s1, s1B = st["s1"]
    kb.op("dve", lambda e: e.reduce_sum(s1[:], r_ap, AX.X), [rB], [s1B])
    kb.op("dve", lambda e: e.tensor_scalar(s1[:], s1[:], -1.0 / D, None, ALU.mult), [s1B], [s1B])
    kb.op("dve", lambda e: e.tensor_scalar(r_ap, r_ap, s1[:], None, ALU.add), [rB, s1B], [rB])
    junk, junkB = st["lnjunk"]
    ss, ssB = st["ss"]
    kb.op("act", lambda e: e.activation(junk[:], r_ap, AF.Square, accum_out=ss[:]), [rB], [junkB, ssB])
    kb.op("dve", lambda e: e.tensor_scalar(ss[:], ss[:], 1.0 / D, EPS, ALU.mult, ALU.add), [ssB], [ssB])
    kb.op("act", lambda e: e.activation(ss[:], ss[:], AF.Sqrt), [ssB], [ssB])
    kb.op("dve", lambda e: e.reciprocal(ss[:], ss[:]), [ssB], [ssB])
    kb.op("dve", lambda e: e.scalar_tensor_tensor(r_ap, r_ap, ss[:], G[:], ALU.mult, ALU.mult), [rB, ssB] + list(GB), [rB])
    kb.op("dve", lambda e: e.tensor_tensor(r_ap, r_ap, Bt[:], ALU.add), [rB] + list(GB), [rB])
    kb.dma(out_dram_ap, r_ap, [rB], [outB])


def emit_layer0(nc, kb, dr, x_in, xinB, x_out, xoutB, cst, nblk=NB):
    es = ExitStack()
    kb_es = kb.es
    kb.es = es
    sb = kb.sb
    identB, identF, CM, BIAS, ones, cB = cst["identB"], cst["identF"], cst["CM"], cst["BIAS"], cst["ones"], cst["buf"]

    W0, W0B = sb("W0", [128, 8, A_IN], BF16)
    Wk2, Wk2B = sb("Wk2", [128, 8, 128], BF16)
    Wuk, WukB = sb("Wuk", [128, 8, 256], BF16)
    Wuv, WuvB = sb("Wuv", [128, 8, 2, 128], BF16)
    Wout, WoutB = sb("Wout", [128, 8, 1024], BF16)
    Gkv, GkvB = sb("Gkv", [128, 256], F32)
    LG, LGB = sb("LG", [128, 1024], F32)
    LB, LBB = sb("LB", [128, 1024], F32)
    win = dr["a_w_in"].rearrange("(k p) n -> p k n", p=128)
    for k in range(0, 8, 4):
        kb.dma(W0[:, k:k + 4, :], win[:, k:k + 4, :], [], [W0B], q="pool")
    kb.dma(Wk2[:, :, 0:64], win[:, :, 1792:1856], [], [Wk2B], q="pool")
    kb.dma(Wk2[:, :, 64:128], win[:, :, 1792:1856], [], [Wk2B], q="pool")
    kb.dma(Wuk[:], dr["a_w_uk"].rearrange("h d c -> d h c"), [], [WukB], q="pool")
    kb.dma(Wuv[:], dr["a_w_uv"].rearrange("h (cc p) d -> p h cc d", p=128), [], [WuvB], q="pool")
    kb.dma(Wout[:], dr["a_w_out"].rearrange("(k p) n -> p k n", p=128), [], [WoutB], q="pool")
    kb.dma(Gkv[:], dr["a_kv_norm_g"].partition_broadcast(128), [], [GkvB])
    kb.dma(LG[:], dr["a_ln_g"].partition_broadcast(128), [], [LGB])
    kb.dma(LB[:], dr["a_ln_b"].partition_broadcast(128), [], [LBB])

    kT2, kT2B = sb("kT2", [128, T], BF16)
    cN, cNB = sb("cN", [128, NB, 256], BF16)
    cT, cTB = sb("cT", [128, 2, T], BF16)
    SC, SCB = sb("SC", [128, T], F32)
    M, MB = sb("M", [128, T], BF16)
    MT, MTB = sb("MT", [128, NB, 128], BF16)
    XF = [sb("XF%d" % s, [128, 1024], F32) for s in range(1)]
    xT, xTB = sb("xT", [128, 8, 128], BF16)
    qT, qTB = sb("qT", [128, 8, 128], BF16)
    qiT, qiTB = sb("qiT", [128, 4, 128], BF16)
    qlT, qlTB = sb("qlT", [128, 2, 1024], BF16)
    sz, szB = sb("sz", [128, 1024], BF16)
    cf, cfB = sb("cf", [128, 256], F32)
    wsb, wsbB = sb("wsb", [128, 8], F32)
    RR = [sb("R%d" % s, [128, 512], F32) for s in range(3)]
    PT = [sb("PT%d" % s, [128, 8, 128], BF16) for s in range(2)]
    olT, olTB = sb("olT", [128, 2, 1024], BF16)
    og, ogB = sb("og", [128, 1024], BF16)
    ogT, ogTB = sb("ogT", [128, 8, 128], BF16)
    rr, rrB = sb("rr", [128, 1024], F32)
    rden, rdenB = sb("rden", [128, 8], F32)
    dens, densB = sb("dens", [128, 8], F32)
    bh, bhB = sb("bh", [128, 32], F32)
    jm, jmB = sb("jm", [128, 1], F32)
    VH, VHB = sb("VH", [128, 8, 1], BF16)
    VHD, VHDB = sb("VHD", [128, 8, 8], BF16)
    SH, SHB = sb("SH", [8, 1024], BF16)
    CMISC = cst["CMISC"]
    OLs, OLsB = sb("OLs", [128, 2, 1024], F32)
    lo, loB = sb("lo", [128, 1], F32)
    mid, midB = sb("mid", [128, 1], F32)
    cnt, cntB = sb("cnt", [128, 1], F32)
    cntA, cntAB = sb("cntA", [128, 1], F32)
    MBa = kb.buf("MBa")
    ge, geB = sb("ge", [128, 1], F32)
    st = {"s1": sb("s1", [128, 1], F32), "ss": sb("ss", [128, 1], F32), "lnjunk": (og, ogB)}
    ssc, sscB = sb("ssc", [128, 1], F32)
    cjunk, cjunkB = sb("cjunk", [128, 256], BF16)

    P0 = kb.ps("P0", [128, 1024], F32)
    P1 = kb.ps("P1", [128, 1024], F32)
    P2 = kb.ps("P2", [128, 1024], F32)
    PX = kb.ps("PX", [128, 512], F32)
    P16 = kb.ps("P16", [128, 1024], BF16)
    B0a, B0b, B1a, B1b, B2a, B2b, BX, B16 = [kb.buf("ps%d" % i) for i in range(8)]
    OPB = [[B1a, B1b], [B2a, B2b]]
    BXh = [BX, BX]
    PTB = [[kb.buf("pt%d%d" % (s_, h_)) for h_ in range(2)] for s_ in range(2)]
    OLB = [[kb.buf("ol%d%d" % (c_, h_)) for h_ in range(2)] for c_ in range(2)]
    DNB = [kb.buf("dn0"), kb.buf("dn1")]

    scale_q = float(128 ** -0.5)

    for i in range(nblk):
        n = 128 * (i + 1)
        xf, xfB = XF[0]
        kb.dma(xf[:], x_in[i * 128:(i + 1) * 128, :], [xinB], [xfB])
        import os
        stage = float(os.environ.get("K_STAGE", "99")) if i == 0 else float(os.environ.get("K_STAGE1", os.environ.get("K_STAGE", "99")))
        if stage <= 0:
            kb.dma(x_out[i * 128:(i + 1) * 128, :], xf[:], [xfB], [xoutB])
            continue
        for k in range(8):
            tr(kb, P0[:, k * 128:(k + 1) * 128], xf[:, k * 128:(k + 1) * 128], identF[:], [xfB, cB], [B0a if k < 4 else B0b], inc=(k == 7))
        kb.op("act", lambda e: e.copy(xT[:].rearrange("p k t -> p (k t)"), P0[:]), [B0a, B0b], [xTB])
        if stage <= 0.1:
            kb.op("dve", lambda e: e.memset(rr[:], 0.0), [], [rrB])
            kb.op("dve", lambda e: e.tensor_copy(rr[:, 0:128], xT[:, 0, :]), [xTB], [rrB])
            kb.dma(x_out[i * 128:(i + 1) * 128, :], rr[:], [rrB], [xoutB])
            continue
        for h in range(8):
            for k in range(8):
                mm(kb, P1[:, h * 128:(h + 1) * 128], W0[:, k, h * 128:(h + 1) * 128], xT[:, k, :], [W0B, xTB],
                   [B1a if h < 4 else B1b], start=(k == 0), stop=(k == 7), inc=(k == 7 and h == 7))
        kb.op("dve", lambda e: e.tensor_copy(qT[:].rearrange("p h t -> p (h t)"), P1[:]), [B1a, B1b], [qTB])
        if stage <= 0.2:
            kb.op("dve", lambda e: e.memset(rr[:], 0.0), [], [rrB])
            kb.op("dve", lambda e: e.tensor_copy(rr[:, 0:128], xT[:, 0, :]), [xTB], [rrB])
            kb.dma(x_out[i * 128:(i + 1) * 128, :], rr[:], [rrB], [xoutB])
            continue
        for c in range(4):
            for k in range(8):
                mm(kb, P2[:, c * 128:(c + 1) * 128], W0[:, k, 1280 + c * 128:1280 + (c + 1) * 128], xT[:, k, :],
                   [W0B, xTB], [B2a], start=(k == 0), stop=(k == 7), inc=(k == 7 and c == 3))
        kb.op("act", lambda e: e.copy(qiT[:].rearrange("p c t -> p (c t)"), P2[:, 0:512]), [B2a], [qiTB])
        if stage <= 0.25:
            kb.op("dve", lambda e: e.memset(rr[:], 0.0), [], [rrB])
            kb.op("dve", lambda e: e.tensor_copy(rr[:, 0:128], xT[:, 0, :]), [xTB], [rrB])
            kb.dma(x_out[i * 128:(i + 1) * 128, :], rr[:], [rrB], [xoutB])
            continue
        for k in range(8):
            mm(kb, P2[:, 512:640], Wk2[:, k, :], xT[:, k, :], [Wk2B, xTB], [B2b], start=(k == 0), stop=(k == 7), inc=False)
        for k in range(8):
            mm(kb, P2[:, 640:896], xT[:, k, :], W0[:, k, 1024:1280], [W0B, xTB], [B2b], start=(k == 0), stop=(k == 7), inc=False)
        for k in range(8):
            mm(kb, P2[:, 896:960], xT[:, k, :], W0[:, k, 1856:1920], [W0B, xTB], [B2b], start=(k == 0), stop=(k == 7), inc=(k == 7))
        if stage <= 0.27:
            kb.op("dve", lambda e: e.memset(rr[:], 0.0), [], [rrB])
            kb.op("dve", lambda e: e.tensor_copy(rr[:, 0:128], xT[:, 0, :]), [xTB], [rrB])
            kb.dma(x_out[i * 128:(i + 1) * 128, :], rr[:], [rrB], [xoutB])
            continue
        kb.op("dve", lambda e: e.tensor_copy(kT2[:, i * 128:(i + 1) * 128], P2[:, 512:640]), [B2b], [kT2B])
        if stage <= 0.28:
            kb.op("dve", lambda e: e.memset(rr[:], 0.0), [], [rrB])
            kb.op("dve", lambda e: e.tensor_copy(rr[:, 0:128], xT[:, 0, :]), [xTB], [rrB])
            kb.dma(x_out[i * 128:(i + 1) * 128, :], rr[:], [rrB], [xoutB])
            continue
        kb.op("dve", lambda e: e.tensor_copy(wsb[:], P2[:, 896:904]), [B2b], [wsbB])
        if stage <= 0.29:
            kb.op("dve", lambda e: e.memset(rr[:], 0.0), [], [rrB])
            kb.op("dve", lambda e: e.tensor_copy(rr[:, 0:128], xT[:, 0, :]), [xTB], [rrB])
            kb.dma(x_out[i * 128:(i + 1) * 128, :], rr[:], [rrB], [xoutB])
            continue
        kb.op("dve", lambda e: e.tensor_copy(cf[:], P2[:, 640:896]), [B2b], [cfB])
        if stage <= 0.3:
            kb.op("dve", lambda e: e.memset(rr[:], 0.0), [], [rrB])
            kb.op("dve", lambda e: e.tensor_copy(rr[:, 0:128], xT[:, 0, :]), [xTB], [rrB])
            kb.dma(x_out[i * 128:(i + 1) * 128, :], rr[:], [rrB], [xoutB])
            continue
        for nn in range(2):
            for k in range(8):
                mm(kb, P0[:, nn * 512:(nn + 1) * 512], xT[:, k, :], W0[:, k, 1864 + nn * 512:1864 + (nn + 1) * 512],
                   [W0B, xTB], [B0a if nn == 0 else B0b], start=(k == 0), stop=(k == 7), inc=(k == 7))
        kb.op("act", lambda e: e.activation(sz[:], P0[:], AF.Silu), [B0a, B0b], [szB])
        if stage <= 0.4:
            kb.op("dve", lambda e: e.memset(rr[:], 0.0), [], [rrB])
            kb.op("dve", lambda e: e.tensor_copy(rr[:, 0:128], xT[:, 0, :]), [xTB], [rrB])
            kb.dma(x_out[i * 128:(i + 1) * 128, :], rr[:], [rrB], [xoutB])
            continue
        kb.op("act", lambda e: e.activation(cjunk[:], cf[:], AF.Square, accum_out=ssc[:]), [cfB], [cjunkB, sscB])
        kb.op("dve", lambda e: e.tensor_scalar(ssc[:], ssc[:], 1.0 / 256, EPS, ALU.mult, ALU.add), [sscB], [sscB])
        kb.op("act", lambda e: e.activation(ssc[:], ssc[:], AF.Sqrt), [sscB], [sscB])
        kb.op("dve", lambda e: e.reciprocal(ssc[:], ssc[:]), [sscB], [sscB])
        kb.op("dve", lambda e: e.scalar_tensor_tensor(cN[:, i, :], cf[:], ssc[:], Gkv[:], ALU.mult, ALU.mult),
              [cfB, sscB, GkvB], [cNB])
        for cc in range(2):
            tr(kb, P16[:, cc * 128:(cc + 1) * 128], cN[:, i, cc * 128:(cc + 1) * 128], identB[:], [cNB, cB], [B16], inc=(cc == 1))
        kb.op("dve", lambda e: e.tensor_copy(cT[:, :, i * 128:(i + 1) * 128], P16[:, 0:256].rearrange("p (c t) -> p c t", c=2)),
              [B16], [cTB])
        if stage <= 0.6:
            kb.op("dve", lambda e: e.memset(rr[:], 0.0), [], [rrB])
            kb.op("dve", lambda e: e.tensor_copy(rr[:, 0:128], xT[:, 0, :]), [xTB], [rrB])
            kb.dma(x_out[i * 128:(i + 1) * 128, :], rr[:], [rrB], [xoutB])
            continue
        for cc in range(2):
            Pq = P0 if cc == 0 else P1
            for h in range(8):
                bq = (B0a, B0b, B1a, B1b)[cc * 2 + (h // 4)]
                mm(kb, Pq[:, h * 128:(h + 1) * 128], Wuk[:, h, cc * 128:(cc + 1) * 128], qT[:, h, :], [WukB, qTB], [bq],
                   inc=(h == 7))
        kb.op("act", lambda e: e.activation(qlT[:, 0, :], P0[:], AF.Copy, scale=scale_q), [B0a, B0b], [qlTB])
        kb.op("dve", lambda e: e.tensor_scalar(qlT[:, 1, :], P1[:], scale_q, None, ALU.mult), [B1a, B1b], [qlTB])

        if stage <= 1:
            kb.op("dve", lambda e: e.tensor_copy(rr[:, 0:256], cN[:, i, :]), [cNB], [rrB])
            kb.op("dve", lambda e: e.tensor_copy(rr[:, 256:1024], qlT[:, 0, 0:768]), [qlTB], [rrB])
            kb.dma(x_out[i * 128:(i + 1) * 128, :], rr[:], [rrB], [xoutB])
            continue
        nkc = (n + 511) // 512
        slots = [(PX, BX, slice(0, 512)), (P2, B2a, slice(0, 512)), (P2, B2b, slice(512, 1024))]
        u = 0
        for kc in range(nkc):
            wd = min(512, n - 512 * kc)
            c0 = kc * 512
            for h in range(8):
                Pt, Bt, sl = slots[u % 3]
                R, RB = RR[u % 3]
                u += 1
                pr = slice((h % 2) * 64, (h % 2) * 64 + 64)
                mm(kb, Pt[:, sl.start:sl.start + wd], qiT[pr, h // 2, :], kT2[pr, c0:c0 + wd], [qiTB, kT2B], [Bt])
                kb.op("act", lambda e: e.activation(R[:, 0:wd], Pt[:, sl.start:sl.start + wd], AF.Relu), [Bt], [RB])
                if h == 0:
                    kb.op("dve", lambda e: e.tensor_scalar(SC[:, c0:c0 + wd], R[:, 0:wd], wsb[:, 0:1], None, ALU.mult),
                          [RB, wsbB], [SCB])
                else:
                    kb.op("dve", lambda e: e.scalar_tensor_tensor(SC[:, c0:c0 + wd], R[:, 0:wd], wsb[:, h:h + 1], SC[:, c0:c0 + wd],
                                                                   ALU.mult, ALU.add), [RB, wsbB, SCB], [SCB])
        kb.op("dve", lambda e: e.tensor_tensor(SC[:, i * 128:n], SC[:, i * 128:n], CM[:], ALU.add), [SCB, cB], [SCB])

        MBall = [MB, MBa]
        if i >= 2:
            nd = 128 * max(1, int(round(0.45 * (i + 1))))
            na = n - nd
            kb.op("dve", lambda e: e.memset(mid[:], 0.0), [], [midB])
            for s in range(BIS_STEPS):
                wk = float(-BIS_LO / (2 ** s))
                last = (s == BIS_STEPS - 1)
                kb.op("dve", lambda e: e.tensor_scalar(M[:, 0:nd], SC[:, 0:nd], mid[:], None, ALU.is_ge, ALU.add, accum_out=cnt[:]),
                      [SCB, midB], [MB, cntB])
                kb.op("act", lambda e: e.activation(M[:, nd:n], SC[:, nd:n], AF.Sign, bias=mid[:], scale=-1.0, accum_out=cntA[:]),
                      [SCB, midB], [MBa, cntAB])
                kb.op("dve", lambda e: e.scalar_tensor_tensor(ge[:], cnt[:], 2.0, cntA[:], ALU.mult, ALU.subtract), [cntB, cntAB], [geB])
                kb.op("dve", lambda e: e.tensor_scalar(ge[:], ge[:], float(511 - na), wk, ALU.is_ge, ALU.mult), [geB], [geB])
                if not last:
                    kb.op("dve", lambda e: e.scalar_tensor_tensor(mid[:], ge[:], -0.5 * wk, mid[:], ALU.add, ALU.add), [geB, midB], [midB])
                else:
                    kb.op("dve", lambda e: e.scalar_tensor_tensor(lo[:], ge[:], -wk, mid[:], ALU.add, ALU.add), [geB, midB], [loB])
            kb.op("dve", lambda e: e.tensor_scalar(M[:, 0:n], SC[:, 0:n], lo[:], -30000.0, ALU.is_lt, ALU.mult), [SCB, loB], MBall)
        else:
            kb.op("dve", lambda e: e.tensor_scalar(M[:, 0:n], SC[:, 0:n], -1e29, -30000.0, ALU.is_lt, ALU.mult), [SCB], MBall)
        nbk = i + 1
        kb.op("dve", lambda e: e.tensor_reduce(bh[:, 0:nbk], M[:, 0:n].rearrange("p (j s) -> p j s", s=128), AX.X, ALU.max), MBall, [bhB])
        kb.op("dve", lambda e: e.tensor_tensor(bh[:, 0:nbk], bh[:, 0:nbk], CMISC[:, 0:nbk], ALU.add), [bhB, cB], [bhB])
        kb.op("dve", lambda e: e.reduce_max(jm[:], bh[:, 0:nbk], AX.X), [bhB], [jmB])
        kb.op("dve", lambda e: e.tensor_scalar(jm[:], jm[:], -1.0, float(i + 1), ALU.mult, ALU.add), [jmB], [jmB])
        kb.op("dve", lambda e: e.tensor_scalar(VH[:].rearrange("p h o -> p (h o)"), CMISC[:, 32:40], jm[:], None, ALU.mult), [jmB, cB], [VHB])
        kb.op("dve", lambda e: e.tensor_tensor(VHD[:], CMISC[:, 40:104].rearrange("p (h g) -> p h g", g=8), VH[:].to_broadcast([128, 8, 8]), ALU.mult),
              [VHB, cB], [VHDB])
        for h in range(8):
            tr(kb, P16[0:8, h * 128:(h + 1) * 128], VHD[:, h, :], identB[:], [VHDB, cB], [B16], inc=(h == 7))
        kb.op("act", lambda e: e.copy(SH[:], P16[0:8, :]), [B16], [SHB])
        if stage <= 2:
            kb.op("dve", lambda e: e.tensor_copy(rr[:, 0:128], M[:, 0:128]), [MB], [rrB])
            kb.op("dve", lambda e: e.tensor_copy(rr[:, 128:256], SC[:, 0:128]), [SCB], [rrB])
            kb.dma(x_out[i * 128:(i + 1) * 128, :], rr[:], [rrB], [xoutB])
            continue
        for j0 in range(0, i + 1, 8):
            nj = min(8, i + 1 - j0)
            for jj in range(nj):
                j = j0 + jj
                tr(kb, P16[:, jj * 128:(jj + 1) * 128], M[:, j * 128:(j + 1) * 128], identB[:], [MB, MBa, cB], [B16], inc=(jj == nj - 1))
            kb.op("act", lambda e: e.copy(MT[:, j0:j0 + nj, :].rearrange("p j t -> p (j t)"), P16[:, 0:nj * 128]), [B16], [MTB])

        jlist = list(range(i + 1))
        if os.environ.get("K_JLAST"):
            jlist = [i]
        units = [(j, hb) for j in jlist for hb in range(2)]

        def emit_st(j, hb):
            bS = B0a if hb == 0 else B0b
            for cc in range(2):
                mm(kb, P0[:, hb * 512:(hb + 1) * 512], cT[:, cc, j * 128:(j + 1) * 128], qlT[:, cc, hb * 512:(hb + 1) * 512],
                   [cTB, qlTB], [bS], start=(cc == 0), stop=False, inc=False)
            mm(kb, P0[:, hb * 512:(hb + 1) * 512], ones[0:8, :], SH[:, hb * 512:(hb + 1) * 512], [SHB, cB], [bS],
               start=False, stop=False, inc=False)
            mm(kb, P0[:, hb * 512:(hb + 1) * 512], identB[:], MT[:, j:j + 1, :].to_broadcast([128, 4, 128]), [MTB, cB], [bS],
               start=False, stop=True, inc=True)

        def emit_rest(j, hb):
            d = i - j
            bS = B0a if hb == 0 else B0b
            Pt_ = PT[j % 2][0]
            PtB = PTB[j % 2][hb]
            for h in range(4 * hb, 4 * hb + 4):
                kb.op("act", lambda e: e.activation(Pt_[:, h, :], P0[:, h * 128:(h + 1) * 128], AF.Exp,
                                                     bias=BIAS[:, h * 32 + d:h * 32 + d + 1], scale=1.0), [bS, cB], [PtB])
            for cc in range(2):
                Po = P1 if cc == 0 else P2
                mm(kb, Po[:, hb * 512:(hb + 1) * 512], cN[:, j, cc * 128:(cc + 1) * 128],
                   Pt_[:, 4 * hb:4 * hb + 4, :].rearrange("p h t -> p (h t)"), [cNB, PtB], [OPB[cc][hb]], start=True, stop=True, inc=False)
            for h in range(4 * hb, 4 * hb + 4):
                mm(kb, PX[:, h:h + 1], Pt_[:, h, :], ones[:, 0:1], [PtB, cB], [BXh[hb]], start=(h % 4 == 0), stop=(h % 4 == 3), inc=(h % 4 == 3))
            first = (j == jlist[0])
            hs = slice(hb * 512, (hb + 1) * 512)
            ds = slice(4 * hb, 4 * hb + 4)
            if first:
                kb.op("dve", lambda e: e.tensor_copy(dens[:, ds], PX[:, ds]), [BXh[hb]], [DNB[hb]])
            else:
                kb.op("dve", lambda e: e.tensor_tensor(dens[:, ds], dens[:, ds], PX[:, ds], ALU.add), [BXh[hb], DNB[hb]], [DNB[hb]])
            for cc in range(2):
                Po = P1 if cc == 0 else P2
                if first:
                    kb.op("dve", lambda e: e.tensor_copy(OLs[:, cc, hs], Po[:, hs]), [OPB[cc][hb]], [OLB[cc][hb]])
                else:
                    kb.op("dve", lambda e: e.tensor_tensor(OLs[:, cc, hs], OLs[:, cc, hs], Po[:, hs], ALU.add), [OPB[cc][hb], OLB[cc][hb]], [OLB[cc][hb]])

        emit_st(*units[0])
        for ui, (j, hb) in enumerate(units):
            if ui + 1 < len(units):
                emit_st(*units[ui + 1])
            emit_rest(j, hb)

        kb.op("dve", lambda e: e.reciprocal(rden[:], dens[:]), DNB, [rdenB])
        kb.op("act", lambda e: e.copy(olT[:, 0, :], OLs[:, 0, :]), OLB[0], [olTB])
        kb.op("dve", lambda e: e.tensor_copy(olT[:, 1, :], OLs[:, 1, :]), OLB[1], [olTB])
        for h in range(8):
            for cc in range(2):
                mm(kb, P0[:, h * 128:(h + 1) * 128], olT[:, cc, h * 128:(h + 1) * 128], Wuv[:, h, cc, :], [olTB, WuvB],
                   [B0a if h < 4 else B0b], start=(cc == 0), stop=(cc == 1), inc=(cc == 1 and h == 7))
        for h in range(8):
            kb.op("dve", lambda e: e.scalar_tensor_tensor(og[:, h * 128:(h + 1) * 128], P0[:, h * 128:(h + 1) * 128], rden[:, h:h + 1],
                                                           sz[:, h * 128:(h + 1) * 128], ALU.mult, ALU.mult),
                  [B0a if h < 4 else B0b, rdenB, szB], [ogB])
        for k in range(8):
            tr(kb, P16[:, k * 128:(k + 1) * 128], og[:, k * 128:(k + 1) * 128], identB[:], [ogB, cB], [B16], inc=(k == 7))
        kb.op("act", lambda e: e.copy(ogT[:].rearrange("p k t -> p (k t)"), P16[:]), [B16], [ogTB])
        for nn in range(2):
            for k in range(8):
                mm(kb, P1[:, nn * 512:(nn + 1) * 512], ogT[:, k, :], Wout[:, k, nn * 512:(nn + 1) * 512], [ogTB, WoutB],
                   [B1a if nn == 0 else B1b], start=(k == 0), stop=(k == 7), inc=(k == 7))
        kb.op("dve", lambda e: e.scalar_tensor_tensor(rr[:], xf[:], ALPHA, P1[:], ALU.mult, ALU.add), [xfB, B1a, B1b], [rrB])
        layernorm_store(kb, st, rr[:], rrB, x_out[i * 128:(i + 1) * 128, :], xoutB, LG, LB, [LGB, LBB])

    kb.finish_layer = True
    kb.es = kb_es
    return es


def load_xT(kb, x_in, xinB, i, xf, xfB, xT, xTB, Ptr, Bs, identF, cB):
    kb.dma(xf[:], x_in[i * 128:(i + 1) * 128, :], [xinB], [xfB])
    for k in range(8):
        tr(kb, Ptr[:, k * 128:(k + 1) * 128], xf[:, k * 128:(k + 1) * 128], identF[:], [xfB, cB], Bs, inc=(k == 7))
    kb.op("act", lambda e: e.copy(xT[:].rearrange("p k t -> p (k t)"), Ptr[:, 0:1024]), Bs, [xTB])


def emit_layer1a(nc, kb, dr, x_in, xinB, hn, hnB, cst, nblk=NB):
    es = ExitStack()
    kb_es = kb.es
    kb.es = es
    sb = kb.sb
    identB, identF, ones, cB, TRIf = cst["identB"], cst["identF"], cst["ones"], cst["buf"], cst["TRIf"]
    win = dr["b_w_in"].rearrange("(k p) n -> p k n", p=128)
    Wqk, WqkB = sb("Wqk", [128, 8, 2048], BF16)
    Wv, WvB = sb("Wv", [128, 8, 2048], BF16)
    Wg, WgB = sb("Wg", [128, 8, 8], BF16)
    for k in range(0, 8, 4):
        kb.dma(Wqk[:, k:k + 4, :], win[:, k:k + 4, 0:2048], [], [WqkB], q="pool")
        kb.dma(Wv[:, k:k + 4, :], win[:, k:k + 4, 2048:4096], [], [WvB], q="pool")
    kb.dma(Wg[:], win[:, :, 4096:4104], [], [WgB], q="pool")
    CWr, CWrB = sb("CWr", [80, 128], F32)
    CWT, CWTB = sb("CWT", [128, 80], F32)
    kb.dma(CWr[0:64, :], dr["b_conv_w"].rearrange("j (c p) -> (j c) p", p=128), [], [CWrB])
    kb.dma(CWr[64:80, :], dr["b_conv_b"].rearrange("o (c p) -> (o c) p", p=128), [], [CWrB])
    GBt, GBB = sb("GBt", [128, 8], F32)
    kb.dma(GBt[:, 0:4], dr["b_i_bias"].partition_broadcast(128), [], [GBB])
    kb.dma(GBt[:, 4:8], dr["b_f_bias"].partition_broadcast(128), [], [GBB])
    HG, HGB = sb("HG", [128, 2048], F32)
    kb.dma(HG[:], dr["b_head_norm_g"].rearrange("o h v -> o (h v)").partition_broadcast(128), [], [HGB])

    PA = kb.ps("L1PA", [128, 2048], F32)
    PB = kb.ps("L1PB", [128, 1024], F32)
    PC = kb.ps("L1PC", [128, 512], F32)
    P16 = kb.ps("L1P16", [128, 1024], BF16)
    BA = [kb.buf("pa%d" % i) for i in range(4)]
    BBa, BBb, BC, B16 = kb.buf("pba"), kb.buf("pbb"), kb.buf("pc"), kb.buf("p16")

    tr(kb, PC[:, 0:80], CWr[:], identF[0:80, 0:80], [CWrB, cB], [BC])
    kb.op("dve", lambda e: e.tensor_copy(CWT[:], PC[:, 0:80]), [BC], [CWTB])

    XF2 = [sb("xf1_%d" % s_, [128, 1024], F32) for s_ in range(2)]
    XT2 = [sb("xT1_%d" % s_, [128, 8, 128], BF16) for s_ in range(2)]
    QKP, QKPB = sb("QKP", [128, 16, 131], F32)
    tmpc, tmpcB = sb("tmpc", [128, 128], F32)
    qkT, qkTB = sb("qkT", [128, 16, 128], BF16)
    VT2 = [sb("vt_%d" % s_, [128, 2048], BF16) for s_ in range(2)]
    gt, gtB = sb("gt", [128, 8], F32)
    lf, lfB = sb("lf", [128, 4], F32)
    ibm, ibmB = sb("ibm", [128, 4], F32)
    bcol, bcolB = sb("bcol", [128, 4], F32)
    ONESf, ONESfB = sb("ONESf", [128, 128], F32)
    LFB4 = [sb("LFB4_%d" % h_, [128, 128], F32) for h_ in range(4)]
    EB4 = [sb("EB4_%d" % h_, [128, 128], F32) for h_ in range(4)]
    DT4 = [sb("DT4_%d" % h_, [128, 128], F32) for h_ in range(4)]
    sT4 = [sb("sT4_%d" % h_, [128, 128], BF16) for h_ in range(4)]
    qh4 = [sb("qh4_%d" % h_, [128, 2, 128], BF16) for h_ in range(4)]
    vs4 = [sb("vs4_%d" % h_, [128, 512], BF16) for h_ in range(4)]
    hh4 = [sb("hh4_%d" % h_, [128, 512], F32) for h_ in range(4)]
    kTok4, kTok4B = sb("kTok4", [128, 1024], BF16)
    wcol4, wcol4B = sb("wcol4", [128, 4], BF16)
    rec4, rec4B = sb("rec4", [128, 4], F32)
    ssh4, ssh4B = sb("ssh4", [128, 4], F32)
    LFB, LFBB = sb("LFB", [128, 128], F32)
    EB, EBB = sb("EB", [128, 128], F32)
    DT, DTB = sb("DT", [128, 128], F32)
    sT, sTB = sb("sT", [128, 128], BF16)
    qh, qhB = sb("qh", [128, 2, 128], BF16)
    kTok, kTokB = sb("kTok", [128, 256], BF16)
    vs, vsB = sb("vs", [128, 512], BF16)
    wcol, wcolB = sb("wcol", [128, 1], BF16)
    hh, hhB = sb("hh", [128, 512], F32)
    hjunk, hjunkB = sb("hjunk", [128, 512], BF16)
    HN, HNB = sb("HN", [128, 2048], BF16)
    rec, recB = sb("rec", [128, 1], F32)
    ssh, sshB = sb("ssh", [128, 1], F32)
    Cs = [sb("C%d" % h, [128, 2, 512], F32) for h in range(4)]
    Cb = [sb("Cb%d" % h, [128, 2, 512], BF16) for h in range(4)]
    ns = [sb("n%d" % h, [128, 2], F32) for h in range(4)]
    nb = [sb("nb%d" % h, [128, 2], BF16) for h in range(4)]
    kb.op("dve", lambda e: e.memset(ONESf[:], 1.0), [], [ONESfB])
    kb.op("dve", lambda e: e.memset(QKP[:], 0.0), [], [QKPB])
    for h in range(4):
        kb.op("dve", lambda e: e.memset(Cs[h][0][:], 0.0), [], [Cs[h][1]])
        kb.op("dve", lambda e: e.memset(Cb[h][0][:], 0.0), [], [Cb[h][1]])
        kb.op("dve", lambda e: e.memset(ns[h][0][:], 0.0), [], [ns[h][1]])
        kb.op("dve", lambda e: e.memset(nb[h][0][:], 0.0), [], [nb[h][1]])

    def seg_A(i):
        xf, xfB = XF2[i % 2]; xT, xTB = XT2[i % 2]; vt, vtB = VT2[i % 2]
        load_xT(kb, x_in, xinB, i, xf, xfB, xT, xTB, PB, [BBa, BBb], identF, cB)

    def seg_Bq(i):
        xf, xfB = XF2[i % 2]; xT, xTB = XT2[i % 2]; vt, vtB = VT2[i % 2]
        for c in range(16):
            for k in range(8):
                mm(kb, PA[:, c * 128:(c + 1) * 128], Wqk[:, k, c * 128:(c + 1) * 128], xT[:, k, :], [WqkB, xTB], [BA[c // 4]],
                   start=(k == 0), stop=(k == 7), inc=(k == 7 and c % 4 == 3))

    def seg_Be(i):
        xf, xfB = XF2[i % 2]; xT, xTB = XT2[i % 2]; vt, vtB = VT2[i % 2]
        kb.op("dve", lambda e: e.tensor_copy(QKP[:, 0:8, 3:131], PA[:, 0:1024].rearrange("p (c t) -> p c t", c=8)), [BA[0], BA[1]], [QKPB])
        kb.op("dve", lambda e: e.tensor_copy(QKP[:, 8:16, 3:131], PA[:, 1024:2048].rearrange("p (c t) -> p c t", c=8)), [BA[2], BA[3]], [QKPB])

    def seg_mid(i):
        xf, xfB = XF2[i % 2]; xT, xTB = XT2[i % 2]; vt, vtB = VT2[i % 2]
        for c in range(16):
            kb.op("dve", lambda e: e.tensor_scalar(tmpc[:], QKP[:, c, 0:128], CWT[:, c:c + 1], CWT[:, 64 + c:65 + c], ALU.mult, ALU.add),
                  [QKPB, CWTB], [tmpcB])
            for j in range(1, 4):
                kb.op("dve", lambda e: e.scalar_tensor_tensor(tmpc[:], QKP[:, c, j:j + 128], CWT[:, j * 16 + c:j * 16 + c + 1], tmpc[:],
                                                               ALU.mult, ALU.add), [QKPB, CWTB, tmpcB], [tmpcB])
            kb.op("act", lambda e: e.activation(qkT[:, c, :], tmpc[:], AF.Silu), [tmpcB], [qkTB])
        kb.op("dve", lambda e: e.tensor_copy(tmpc[:, 0:48].rearrange("p (c t) -> p c t", c=16), QKP[:, :, 128:131]), [QKPB], [tmpcB])
        kb.op("dve", lambda e: e.tensor_copy(QKP[:, :, 0:3], tmpc[:, 0:48].rearrange("p (c t) -> p c t", c=16)), [tmpcB], [QKPB])

    def seg_V(i):
        xf, xfB = XF2[i % 2]; xT, xTB = XT2[i % 2]; vt, vtB = VT2[i % 2]
        for nn in range(4):
            for k in range(8):
                mm(kb, PA[:, nn * 512:(nn + 1) * 512], xT[:, k, :], Wv[:, k, nn * 512:(nn + 1) * 512], [WvB, xTB], [BA[nn]],
                   start=(k == 0), stop=(k == 7), inc=(k == 7))
        kb.op("act", lambda e: e.copy(vt[:, 0:1024], PA[:, 0:1024]), [BA[0], BA[1]], [vtB])
        kb.op("dve", lambda e: e.tensor_copy(vt[:, 1024:2048], PA[:, 1024:2048]), [BA[2], BA[3]], [vtB])
        for k in range(8):
            mm(kb, PC[:, 0:8], xT[:, k, :], Wg[:, k, :], [WgB, xTB], [BC], start=(k == 0), stop=(k == 7), inc=(k == 7))
        kb.op("dve", lambda e: e.tensor_tensor(gt[:], PC[:, 0:8], GBt[:], ALU.add), [BC, GBB], [gtB])
        kb.op("act", lambda e: e.activation(lf[:], gt[:, 4:8], AF.Exp, scale=-1.0), [gtB], [lfB])
        kb.op("act", lambda e: e.activation(lf[:], lf[:], AF.Ln, bias=1.0), [lfB], [lfB])
        kb.op("dve", lambda e: e.tensor_scalar(lf[:], lf[:], -1.0, None, ALU.mult), [lfB], [lfB])
        mm(kb, PC[:, 8:12], TRIf[:], lf[:], [lfB, cB], [BC])
        kb.op("dve", lambda e: e.tensor_copy(bcol[:], PC[:, 8:12]), [BC], [bcolB])
        kb.op("dve", lambda e: e.tensor_tensor(ibm[:], gt[:, 0:4], bcol[:], ALU.subtract), [gtB, bcolB], [ibmB])

    def seg_H(i):
        xf, xfB = XF2[i % 2]; xT, xTB = XT2[i % 2]; vt, vtB = VT2[i % 2]
        H4 = range(4)
        for h in H4:
            kb.op("dve", lambda e: e.tensor_scalar(LFB4[h][0][:], ONESf[:], lf[:, h:h + 1], None, ALU.mult), [ONESfB, lfB], [LFB4[h][1]])
        for h in H4:
            mm(kb, PB[:, h * 128:(h + 1) * 128], LFB4[h][0][:], TRIf[:], [LFB4[h][1], cB], [BBa], inc=(h == 3))
        for h in H4:
            kb.op("act", lambda e: e.activation(EB4[h][0][:], PB[:, h * 128:(h + 1) * 128], AF.Exp), [BBa], [EB4[h][1]])
            kb.op("act", lambda e: e.activation(DT4[h][0][:], PB[:, h * 128:(h + 1) * 128], AF.Exp, bias=ibm[:, h:h + 1]), [BBa, ibmB], [DT4[h][1]])
        for h in H4:
            DT, DTB = DT4[h]; EB, EBB = EB4[h]
            kb.op("dve", lambda e: e.tensor_copy(wcol4[:, h:h + 1], DT[:, 127:128]), [DTB], [wcol4B])
            kb.op("dve", lambda e: e.tensor_scalar(vs4[h][0][:], vt[:, h * 512:(h + 1) * 512], DT[:, 127:128], None, ALU.mult), [vtB, DTB], [vs4[h][1]])
            kb.op("dve", lambda e: e.tensor_tensor(DT[:], DT[:], TRIf[:], ALU.mult), [DTB, cB], [DTB])
            for cc in range(2):
                kb.op("dve", lambda e: e.scalar_tensor_tensor(qh4[h][0][:, cc, :], qkT[:, 2 * h + cc, :], 0.0625, EB[:], ALU.mult, ALU.mult),
                      [qkTB, EBB], [qh4[h][1]])
        for h in H4:
            for cc in range(2):
                mm(kb, PB[:, 512 + h * 128:512 + (h + 1) * 128], qkT[:, 8 + 2 * h + cc, :], qkT[:, 2 * h + cc, :], [qkTB], [BBb],
                   start=(cc == 0), stop=(cc == 1), inc=(cc == 1 and h == 3))
        for h in H4:
            kb.op("dve", lambda e: e.scalar_tensor_tensor(sT4[h][0][:], PB[:, 512 + h * 128:512 + (h + 1) * 128], 0.0625, DT4[h][0][:], ALU.mult, ALU.mult),
                  [BBb, DT4[h][1]], [sT4[h][1]])
        for h in H4:
            sT, sTB = sT4[h]; qh, qhB = qh4[h]
            mm(kb, PA[:, h * 512:(h + 1) * 512], sT[:], vt[:, h * 512:(h + 1) * 512], [sTB, vtB], [BA[h]], start=True, stop=False, inc=False)
            for cc in range(2):
                mm(kb, PA[:, h * 512:(h + 1) * 512], qh[:, cc, :], Cb[h][0][:, cc, :], [qhB, Cb[h][1]], [BA[h]], start=False, stop=(cc == 1), inc=(cc == 1))
        for h in H4:
            sT, sTB = sT4[h]; qh, qhB = qh4[h]
            mm(kb, PC[:, 16 + h:17 + h], sT[:], ones[:, 0:1], [sTB, cB], [BC], start=True, stop=False, inc=False)
            for cc in range(2):
                mm(kb, PC[:, 16 + h:17 + h], qh[:, cc, :], nb[h][0][:, cc:cc + 1], [qhB, nb[h][1]], [BC], start=False, stop=(cc == 1), inc=(cc == 1))
        kb.op("dve", lambda e: e.tensor_scalar(rec4[:], PC[:, 16:20], -1.0, None, ALU.mult), [BC], [rec4B])
        kb.op("dve", lambda e: e.tensor_tensor(rec4[:], rec4[:], PC[:, 16:20], ALU.max), [BC, rec4B], [rec4B])
        kb.op("dve", lambda e: e.tensor_scalar(rec4[:], rec4[:], 1.0, None, ALU.max), [rec4B], [rec4B])
        kb.op("dve", lambda e: e.reciprocal(rec4[:], rec4[:]), [rec4B], [rec4B])
        for h in H4:
            kb.op("dve", lambda e: e.tensor_scalar(hh4[h][0][:], PA[:, h * 512:(h + 1) * 512], rec4[:, h:h + 1], None, ALU.mult), [BA[h], rec4B], [hh4[h][1]])
        for h in H4:
            kb.op("act", lambda e: e.activation(hjunk[:], hh4[h][0][:], AF.Square, accum_out=ssh4[:, h:h + 1]), [hh4[h][1]], [hjunkB, ssh4B])
        kb.op("dve", lambda e: e.tensor_scalar(ssh4[:], ssh4[:], 1.0 / 512, EPS, ALU.mult, ALU.add), [ssh4B], [ssh4B])
        kb.op("act", lambda e: e.activation(ssh4[:], ssh4[:], AF.Sqrt), [ssh4B], [ssh4B])
        kb.op("dve", lambda e: e.reciprocal(ssh4[:], ssh4[:]), [ssh4B], [ssh4B])
        for h in H4:
            kb.op("dve", lambda e: e.scalar_tensor_tensor(HN[:, h * 512:(h + 1) * 512], hh4[h][0][:], ssh4[:, h:h + 1], HG[:, h * 512:(h + 1) * 512],
                                                           ALU.mult, ALU.mult), [hh4[h][1], ssh4B, HGB], [HNB])
        kb.dma(hn[i * 128:(i + 1) * 128, :], HN[:], [HNB], [hnB])
        for h in H4:
            for cc in range(2):
                tr(kb, P16[:, h * 256 + cc * 128:h * 256 + (cc + 1) * 128], qkT[:, 8 + 2 * h + cc, :], identB[:], [qkTB, cB], [B16], inc=(cc == 1 and h == 3))
        kb.op("act", lambda e: e.copy(kTok4[:], P16[:]), [B16], [kTok4B])
        u = 0
        for h in H4:
            C, CB_ = Cs[h]
            EB, EBB = EB4[h]
            for cc in range(2):
                bank = u % 2
                u += 1
                bb = BBa if bank == 0 else BBb
                mm(kb, PB[:, bank * 512:(bank + 1) * 512], kTok4[:, h * 256 + cc * 128:h * 256 + (cc + 1) * 128], vs4[h][0][:], [kTok4B, vs4[h][1]], [bb])
                kb.op("dve", lambda e: e.scalar_tensor_tensor(C[:, cc, :], C[:, cc, :], EB[:, 127:128], PB[:, bank * 512:(bank + 1) * 512], ALU.mult, ALU.add),
                      [CB_, EBB, bb], [CB_])
        for h in H4:
            for cc in range(2):
                mm(kb, PC[:, 32 + 2 * h + cc:33 + 2 * h + cc], kTok4[:, h * 256 + cc * 128:h * 256 + (cc + 1) * 128], wcol4[:, h:h + 1], [kTok4B, wcol4B], [BC],
                   inc=(cc == 1 and h == 3))
        for h in H4:
            nh, nhB = ns[h]
            kb.op("dve", lambda e: e.scalar_tensor_tensor(nh[:], nh[:], EB4[h][0][:, 127:128], PC[:, 32 + 2 * h:34 + 2 * h], ALU.mult, ALU.add),
                  [nhB, EB4[h][1], BC], [nhB])
        for h in H4:
            kb.op("act", lambda e: e.copy(Cb[h][0][:].rearrange("p c v -> p (c v)"), Cs[h][0][:].rearrange("p c v -> p (c v)")), [Cs[h][1]], [Cb[h][1]])
            kb.op("dve", lambda e: e.tensor_copy(nb[h][0][:], ns[h][0][:]), [ns[h][1]], [nb[h][1]])

    seg_A(0)
    seg_Bq(0)
    seg_Be(0)
    for i in range(nblk):
        seg_V(i)
        if i + 1 < nblk:
            seg_A(i + 1)
        seg_mid(i)
        if i + 1 < nblk:
            seg_Bq(i + 1)
            seg_Be(i + 1)
        seg_H(i)
    kb.es = kb_es
    return es


def emit_layer1b(nc, kb, dr, x_in, xinB, hn, hnB, x_out, xoutB, cst, nblk=NB):
    es = ExitStack()
    kb_es = kb.es
    kb.es = es
    sb = kb.sb
    identB, identF, cB = cst["identB"], cst["identF"], cst["buf"]
    win = dr["b_w_in"].rearrange("(k p) n -> p k n", p=128)
    Wo, WoB = sb("Wo", [128, 8, 2048], BF16)
    Wz, WzB = sb("Wz", [128, 8, 2048], BF16)
    Wout, WoutB = sb("Wout1", [128, 16, 1024], BF16)
    wout = dr["b_w_out"].rearrange("(k p) n -> p k n", p=128)
    for k in range(0, 8, 4):
        kb.dma(Wo[:, k:k + 4, :], win[:, k:k + 4, 4104:6152], [], [WoB], q="pool")
        kb.dma(Wz[:, k:k + 4, :], win[:, k:k + 4, 6152:8200], [], [WzB], q="pool")
    for k in range(0, 16, 8):
        kb.dma(Wout[:, k:k + 8, :], wout[:, k:k + 8, :], [], [WoutB], q="pool")
    LG, LGB = sb("LG1", [128, 1024], F32)
    LB, LBB = sb("LB1", [128, 1024], F32)
    kb.dma(LG[:], dr["b_ln_g"].partition_broadcast(128), [], [LGB])
    kb.dma(LB[:], dr["b_ln_b"].partition_broadcast(128), [], [LBB])
    PA = kb.ps("L2PA", [128, 2048], F32)
    PB = kb.ps("L2PB", [128, 1024], F32)
    P16 = kb.ps("L2P16", [128, 2048], BF16)
    BA = [kb.buf("qa%d" % i) for i in range(4)]
    BBa, BBb, B16 = kb.buf("qba"), kb.buf("qbb"), kb.buf("q16")
    XF2 = [sb("xf2_%d" % s_, [128, 1024], F32) for s_ in range(2)]
    XT2 = [sb("xT2_%d" % s_, [128, 8, 128], BF16) for s_ in range(2)]
    so, soB = sb("so", [128, 2048], BF16)
    HNB2 = [sb("hnb_%d" % s_, [128, 2048], BF16) for s_ in range(2)]
    hg, hgB = sb("hg", [128, 2048], BF16)
    hgT, hgTB = sb("hgT", [128, 16, 128], BF16)
    rr, rrB = sb("rr2", [128, 1024], F32)
    st = {"s1": sb("s1b", [128, 1], F32), "ss": sb("ssb", [128, 1], F32), "lnjunk": sb("lnjunkb", [128, 1024], BF16)}
    def front(i):
        xf, xfB = XF2[i % 2]; xT, xTB = XT2[i % 2]; hnb, hnbB = HNB2[i % 2]
        load_xT(kb, x_in, xinB, i, xf, xfB, xT, xTB, PB, [BBa, BBb], identF, cB)
        kb.dma(hnb[:], hn[i * 128:(i + 1) * 128, :], [hnB], [hnbB])

    front(0)
    for i in range(nblk):
        xf, xfB = XF2[i % 2]; xT, xTB = XT2[i % 2]; hnb, hnbB = HNB2[i % 2]
        if i + 1 < nblk:
            front(i + 1)
        for nn in range(4):
            for k in range(8):
                mm(kb, PA[:, nn * 512:(nn + 1) * 512], xT[:, k, :], Wo[:, k, nn * 512:(nn + 1) * 512], [WoB, xTB], [BA[nn]],
                   start=(k == 0), stop=(k == 7), inc=(k == 7))
        kb.op("act", lambda e: e.activation(so[:], PA[:], AF.Sigmoid), BA, [soB])
        kb.op("dve", lambda e: e.tensor_tensor(hg[:], hnb[:], so[:], ALU.mult), [hnbB, soB], [hgB])
        for nn in range(4):
            for k in range(8):
                mm(kb, PA[:, nn * 512:(nn + 1) * 512], xT[:, k, :], Wz[:, k, nn * 512:(nn + 1) * 512], [WzB, xTB], [BA[nn]],
                   start=(k == 0), stop=(k == 7), inc=(k == 7))
        kb.op("act", lambda e: e.activation(so[:], PA[:], AF.Silu), BA, [soB])
        kb.op("dve", lambda e: e.tensor_tensor(hg[:], hg[:], so[:], ALU.mult), [hgB, soB], [hgB])
        for k in range(16):
            tr(kb, P16[:, k * 128:(k + 1) * 128], hg[:, k * 128:(k + 1) * 128], identB[:], [hgB, cB], [B16], inc=(k == 15))
        kb.op("act", lambda e: e.copy(hgT[:].rearrange("p k t -> p (k t)"), P16[:]), [B16], [hgTB])
        for nn in range(2):
            for k in range(16):
                mm(kb, PB[:, nn * 512:(nn + 1) * 512], hgT[:, k, :], Wout[:, k, nn * 512:(nn + 1) * 512], [hgTB, WoutB],
                   [BBa if nn == 0 else BBb], start=(k == 0), stop=(k == 15), inc=(k == 15))
        kb.op("dve", lambda e: e.scalar_tensor_tensor(rr[:], xf[:], ALPHA, PB[:], ALU.mult, ALU.add), [xfB, BBa, BBb], [rrB])
        layernorm_store(kb, st, rr[:], rrB, x_out[i * 128:(i + 1) * 128, :], xoutB, LG, LB, [LGB, LBB])
    kb.es = kb_es
    return es


def make_consts():
    identF = np.eye(128, dtype=np.float32)
    q = np.arange(128)[:, None]
    s = np.arange(128)[None, :]
    CM = np.where(s <= q, 0.0, -1e30).astype(np.float32)
    slopes = 2.0 ** (-(np.arange(1, 9, dtype=np.float64)))
    sp = np.arange(128, dtype=np.float64)[:, None, None]
    dd = np.arange(32, dtype=np.float64)[None, None, :]
    BIAS = (slopes[None, :, None] * (sp - 127.0 - 128.0 * dd)).reshape(128, 256).astype(np.float32)
    misc = np.zeros((128, 104), np.float32)
    misc[:, 0:32] = np.arange(1, 33, dtype=np.float32)[None, :]
    misc[:, 32:40] = (128.0 * slopes)[None, :]
    misc[:, 40:104] = np.eye(8, dtype=np.float32).reshape(1, 64)
    tri = (np.arange(128)[:, None] <= np.arange(128)[None, :]).astype(np.float32)
    return {"c_ident": identF, "c_cm": CM, "c_bias": BIAS, "c_misc": misc, "c_tri": tri}


def build(layers=LAYERS, nblk=NB):
    nc = bass.Bass("TRN2", target_bir_lowering=False)
    dr = {}

    def din(name, shape):
        dr[name] = nc.dram_tensor(name, list(shape), F32, kind="ExternalInput").ap()

    din("x", [T, D])
    din("a_w_in", [D, A_IN]); din("a_kv_norm_g", [1, 256]); din("a_w_uk", [8, 128, 256]); din("a_w_uv", [8, 256, 128])
    din("a_w_out", [D, D]); din("a_ln_g", [1, D]); din("a_ln_b", [1, D])
    din("c_ident", [128, 128]); din("c_cm", [128, 128]); din("c_bias", [128, 256]); din("c_misc", [128, 104]); din("c_tri", [128, 128])
    din("b_w_in", [D, B_IN]); din("b_i_bias", [1, 4]); din("b_f_bias", [1, 4]); din("b_conv_w", [4, 2048]); din("b_conv_b", [1, 2048])
    din("b_head_norm_g", [1, 4, 512]); din("b_w_out", [2048, D]); din("b_ln_g", [1, D]); din("b_ln_b", [1, D])
    out = nc.dram_tensor("out", [T, D], F32, kind="ExternalOutput").ap()
    with ExitStack() as es:
        kb = KB(nc, es)
        identF, idB = kb.sb("identF", [128, 128], F32)
        identB, _ = kb.sb("identB", [128, 128], BF16)
        CM, _ = kb.sb("CM", [128, 128], F32)
        BIAS, _ = kb.sb("BIAS", [128, 256], F32)
        ones, _ = kb.sb("ones", [128, 128], BF16)
        CMISC, _ = kb.sb("CMISC", [128, 104], F32)
        TRIf, _ = kb.sb("TRIf", [128, 128], F32)
        cB = idB
        kb.dma(identF[:], dr["c_ident"], [], [cB])
        kb.dma(identB[:], dr["c_ident"], [], [cB], q="pool")
        kb.dma(CM[:], dr["c_cm"], [], [cB])
        kb.dma(BIAS[:], dr["c_bias"], [], [cB])
        kb.dma(CMISC[:], dr["c_misc"], [], [cB])
        kb.dma(TRIf[:], dr["c_tri"], [], [cB])
        kb.op("dve", lambda e: e.memset(ones[:], 1.0), [], [cB])
        cst = {"identB": identB, "identF": identF, "CM": CM, "BIAS": BIAS, "ones": ones, "buf": cB, "CMISC": CMISC, "TRIf": TRIf}
        xinB = kb.buf("xin")
        outB = kb.buf("out")
        x1d = nc.dram_tensor("x1d", [T, D], F32).ap()
        hnd = nc.dram_tensor("hnd", [T, 2048], BF16).ap()
        x1B = kb.buf("x1d")
        hnB = kb.buf("hnd")
        if layers == (0,):
            l0 = emit_layer0(nc, kb, dr, dr["x"], xinB, out, outB, cst, nblk=nblk)
            kb.finish(); l0.close()
        elif layers == (1,):
            la = emit_layer1a(nc, kb, dr, dr["x"], xinB, hnd, hnB, cst, nblk=nblk)
            kb.barrier(); la.close()
            lb = emit_layer1b(nc, kb, dr, dr["x"], xinB, hnd, hnB, out, outB, cst, nblk=nblk)
            kb.finish(); lb.close()
        else:
            l0 = emit_layer0(nc, kb, dr, dr["x"], xinB, x1d, x1B, cst, nblk=nblk)
            kb.barrier(); l0.close()
            la = emit_layer1a(nc, kb, dr, x1d, x1B, hnd, hnB, cst, nblk=nblk)
            kb.barrier(); la.close()
            lb = emit_layer1b(nc, kb, dr, x1d, x1B, hnd, hnB, out, outB, cst, nblk=nblk)
            kb.finish(); lb.close()
    return nc


_CACHE = {}


def make_inmaps(inputs, xs):
    cst = make_consts()
    shared = {k: np.ascontiguousarray(inputs[k][0], dtype=np.float32) for k in
              ("a_w_in", "a_w_uk", "a_w_uv", "a_w_out", "b_w_in", "b_conv_w", "b_w_out")}
    for k in ("a_kv_norm_g", "a_ln_g", "a_ln_b", "b_i_bias", "b_f_bias", "b_conv_b", "b_ln_g", "b_ln_b"):
        shared[k] = np.ascontiguousarray(inputs[k], dtype=np.float32).reshape(1, -1)
    shared["b_head_norm_g"] = np.ascontiguousarray(inputs["b_head_norm_g"], dtype=np.float32).reshape(1, 4, 512)
    shared.update(cst)
    maps = []
    for xx in xs:
        m = dict(shared)
        m["x"] = np.ascontiguousarray(xx, dtype=np.float32)
        maps.append(m)
    return maps


def kernel(**inputs):
    x = np.ascontiguousarray(inputs["x"], dtype=np.float32)
    if "nc" not in _CACHE:
        import os
        lay = os.environ.get("K_LAYERS")
        _CACHE["nc"] = build(layers=tuple(int(c) for c in lay)) if lay else build()
    nc = _CACHE["nc"]
    in_maps = make_inmaps(inputs, [x[c % 4] for c in range(8)])
    res = run_bass_kernel_spmd(nc, in_maps, core_ids=list(range(8)))
    return np.stack([res.results[c]["out"] for c in range(4)], axis=0)
```

```python
import numpy as np
from contextlib import ExitStack
import concourse.bass as bass
import concourse.mybir as mybir
from concourse.bass_utils import run_bass_kernel_spmd

F32 = mybir.dt.float32
BF16 = mybir.dt.bfloat16
AF = mybir.ActivationFunctionType
ALU = mybir.AluOpType
AX = mybir.AxisListType

T = 4096
D = 1024
NB = T // 128
A_IN = 2888
B_IN = 8200
ALPHA = float(4 ** 0.25)
EPS = 1e-5
NDMA = 12
BIS_STEPS = 26
BIS_LO = -512.0
LAYERS = (0, 1)


class Buf:
    __slots__ = ("name", "w", "r")

    def __init__(self, name):
        self.name = name
        self.w = None
        self.r = {}


class KB:
    def __init__(self, nc, es):
        self.nc = nc
        self.es = es
        self.eng = {"pe": nc.tensor, "act": nc.scalar, "dve": nc.vector, "pool": nc.gpsimd, "sp": nc.sync}
        self.sem = {e: es.enter_context(nc.semaphore("s_" + e)) for e in ("pe", "act", "dve", "pool")}
        self.cnt = {e: 0 for e in self.sem}
        self.known = {e: {} for e in self.eng}
        self.dsem = [es.enter_context(nc.semaphore("d%d" % k)) for k in range(NDMA)]
        self.dtot = [0] * NDMA
        self.dnext = 0
        self.nbuf = 0
        self.root_es = es
        self.xsem = {}

    def buf(self, name=None):
        self.nbuf += 1
        return Buf(name or ("b%d" % self.nbuf))

    def sb(self, name, shape, dt):
        t = self.es.enter_context(self.nc.sbuf_tensor(name, list(shape), dt))
        return t, Buf(name)

    def ps(self, name, shape, dt):
        return self.es.enter_context(self.nc.psum_tensor(name, list(shape), dt))

    def _deps(self, reads, writes):
        toks = []
        for b in reads:
            if b.w is not None:
                toks.append(b.w)
        for b in writes:
            if b.w is not None:
                toks.append(b.w)
            toks.extend(b.r.items())
        return toks

    def _wait(self, e, toks):
        need = {}
        for k, v in toks:
            if k == e and e == "pe":
                continue
            if self.known[e].get(k, 0) >= v:
                continue
            if need.get(k, 0) < v:
                need[k] = v
        for k, v in need.items():
            sem = self.sem[k] if isinstance(k, str) else (self.dsem[k[1]] if k[0] == "d" else self.xsem[k])
            self.eng[e].wait_ge(sem, v)
            self.known[e][k] = v

    def _mark(self, tok, reads, writes):
        k, v = tok
        for b in reads:
            if b.r.get(k, 0) < v:
                b.r[k] = v
        for b in writes:
            b.w = tok
            b.r = {}

    def op(self, e, fn, reads=(), writes=(), inc=True):
        self._wait(e, self._deps(reads, writes))
        ins = fn(self.eng[e])
        if inc:
            self.cnt[e] += 1
            ins.then_inc(self.sem[e], 1)
            tok = (e, self.cnt[e])
        else:
            assert e == "pe"
            tok = (e, self.cnt[e] + 1)
        self._mark(tok, reads, writes)
        return tok

    def dma(self, out, in_, reads=(), writes=(), q="sp"):
        toks = self._deps(reads, writes)
        if q == "pool":
            key = ("x", len(self.xsem))
            self.xsem[key] = self.root_es.enter_context(self.nc.semaphore("x%d" % len(self.xsem)))
            self._wait(q, toks)
            self.eng[q].dma_start(out=out, in_=in_).then_inc(self.xsem[key], 16)
            tok = (key, 16)
            self._mark(tok, reads, writes)
            return tok
        k = self.dnext
        self.dnext = (k + 1) % NDMA
        if self.dtot[k] > 0:
            toks.append((("d", k), self.dtot[k]))
        self._wait(q, toks)
        self.dtot[k] += 16
        self.eng[q].dma_start(out=out, in_=in_).then_inc(self.dsem[k], 16)
        tok = (("d", k), self.dtot[k])
        self._mark(tok, reads, writes)
        return tok

    def barrier(self):
        toks = [(("d", k), self.dtot[k]) for k in range(NDMA) if self.dtot[k] > 0]
        toks += [(k, 16) for k in self.xsem]
        toks += [(e, c) for e, c in self.cnt.items() if c > 0]
        for e in self.eng:
            self._wait(e, toks)

    def finish(self):
        toks = [(("d", k), self.dtot[k]) for k in range(NDMA) if self.dtot[k] > 0]
        toks += [(k, 16) for k in self.xsem]
        toks += [(e, c) for e, c in self.cnt.items() if c > 0]
        self._wait("sp", toks)


def mm(kb, out, lhsT, rhs, reads, writes, start=True, stop=True, inc=None):
    if inc is None:
        inc = stop
    return kb.op("pe", lambda e: e.matmul(out, lhsT, rhs, start=start, stop=stop), reads, writes, inc=inc)


def tr(kb, out, in_, ident, reads, writes, inc=True):
    return kb.op("pe", lambda e: e.transpose(out, in_, ident), reads, writes, inc=inc)


def layernorm_store(kb, st, r_ap, rB, out_dram_ap, outB, G, Bt, GB):
    s1, s1B = st["s1"]
    kb.op("dve", lambda e: e.reduce_sum(s1[:], r_ap, AX.X), [rB], [s1B])
    kb.op("dve", lambda e: e.tensor_scalar(s1[:], s1[:], -1.0 / D, None, ALU.mult), [s1B], [s1B])
    kb.op("dve", lambda e: e.tensor_scalar(r_ap, r_ap, s1[:], None, ALU.add), [rB, s1B], [rB])
    junk, junkB = st["lnjunk"]
    ss, ssB = st["ss"]
    kb.op("act", lambda e: e.activation(junk[:], r_ap, AF.Square, accum_out=ss[:]), [rB], [junkB, ssB])
    kb.op("dve", lambda e: e.tensor_scalar(ss[:], ss[:], 1.0 / D, EPS, ALU.mult, ALU.add), [ssB], [ssB])
    kb.op("act", lambda e: e.activation(ss[:], ss[:], AF.Sqrt), [ssB], [ssB])
    kb.op("dve", lambda e: e.reciprocal(ss[:], ss[:]), [ssB], [ssB])
    kb.op("dve", lambda e: e.scalar_tensor_tensor(r_ap, r_ap, ss[:], G[:], ALU.mult, ALU.mult), [rB, ssB] + list(GB), [rB])
    kb.op("dve", lambda e: e.tensor_tensor(r_ap, r_ap, Bt[:], ALU.add), [rB] + list(GB), [rB])
    kb.dma(out_dram_ap, r_ap, [rB], [outB])


def emit_layer0(nc, kb, dr, x_in, xinB, x_out, xoutB, cst, nblk=NB):
    es = ExitStack()
    kb_es = kb.es
    kb.es = es
    sb = kb.sb
    identB, identF, CM, BIAS, ones, cB = cst["identB"], cst["identF"], cst["CM"], cst["BIAS"], cst["ones"], cst["buf"]

    W0, W0B = sb("W0", [128, 8, A_IN], BF16)
    Wk2, Wk2B = sb("Wk2", [128, 8, 128], BF16)
    Wuk, WukB = sb("Wuk", [128, 8, 256], BF16)
    Wuv, WuvB = sb("Wuv", [128, 8, 2, 128], BF16)
    Wout, WoutB = sb("Wout", [128, 8, 1024], BF16)
    Gkv, GkvB = sb("Gkv", [128, 256], F32)
    LG, LGB = sb("LG", [128, 1024], F32)
    LB, LBB = sb("LB", [128, 1024], F32)
    win = dr["a_w_in"].rearrange("(k p) n -> p k n", p=128)
    for k in range(0, 8, 4):
        kb.dma(W0[:, k:k + 4, :], win[:, k:k + 4, :], [], [W0B], q="pool")
    kb.dma(Wk2[:, :, 0:64], win[:, :, 1792:1856], [], [Wk2B], q="pool")
    kb.dma(Wk2[:, :, 64:128], win[:, :, 1792:1856], [], [Wk2B], q="pool")
    kb.dma(Wuk[:], dr["a_w_uk"].rearrange("h d c -> d h c"), [], [WukB], q="pool")
    kb.dma(Wuv[:], dr["a_w_uv"].rearrange("h (cc p) d -> p h cc d", p=128), [], [WuvB], q="pool")
    kb.dma(Wout[:], dr["a_w_out"].rearrange("(k p) n -> p k n", p=128), [], [WoutB], q="pool")
    kb.dma(Gkv[:], dr["a_kv_norm_g"].partition_broadcast(128), [], [GkvB])
    kb.dma(LG[:], dr["a_ln_g"].partition_broadcast(128), [], [LGB])
    kb.dma(LB[:], dr["a_ln_b"].partition_broadcast(128), [], [LBB])

    kT2, kT2B = sb("kT2", [128, T], BF16)
    cN, cNB = sb("cN", [128, NB, 256], BF16)
    cT, cTB = sb("cT", [128, 2, T], BF16)
    SC, SCB = sb("SC", [128, T], F32)
    M, MB = sb("M", [128, T], BF16)
    MT, MTB = sb("MT", [128, NB, 128], BF16)
    XF = [sb("XF%d" % s, [128, 1024], F32) for s in range(1)]
    xT, xTB = sb("xT", [128, 8, 128], BF16)
    qT, qTB = sb("qT", [128, 8, 128], BF16)
    qiT, qiTB = sb("qiT", [128, 4, 128], BF16)
    qlT, qlTB = sb("qlT", [128, 2, 1024], BF16)
    sz, szB = sb("sz", [128, 1024], BF16)
    cf, cfB = sb("cf", [128, 256], F32)
    wsb, wsbB = sb("wsb", [128, 8], F32)
    RR = [sb("R%d" % s, [128, 512], F32) for s in range(3)]
    PT = [sb("PT%d" % s, [128, 8, 128], BF16) for s in range(2)]
    olT, olTB = sb("olT", [128, 2, 1024], BF16)
    og, ogB = sb("og", [128, 1024], BF16)
    ogT, ogTB = sb("ogT", [128, 8, 128], BF16)
    rr, rrB = sb("rr", [128, 1024], F32)
    rden, rdenB = sb("rden", [128, 8], F32)
    dens, densB = sb("dens", [128, 8], F32)
    bh, bhB = sb("bh", [128, 32], F32)
    jm, jmB = sb("jm", [128, 1], F32)
    VH, VHB = sb("VH", [128, 8, 1], BF16)
    VHD, VHDB = sb("VHD", [128, 8, 8], BF16)
    SH, SHB = sb("SH", [8, 1024], BF16)
    CMISC = cst["CMISC"]
    OLs, OLsB = sb("OLs", [128, 2, 1024], F32)
    lo, loB = sb("lo", [128, 1], F32)
    mid, midB = sb("mid", [128, 1], F32)
    cnt, cntB = sb("cnt", [128, 1], F32)
    cntA, cntAB = sb("cntA", [128, 1], F32)
    MBa = kb.buf("MBa")
    ge, geB = sb("ge", [128, 1], F32)
    st = {"s1": sb("s1", [128, 1], F32), "ss": sb("ss", [128, 1], F32), "lnjunk": (og, ogB)}
    ssc, sscB = sb("ssc", [128, 1], F32)
    cjunk, cjunkB = sb("cjunk", [128, 256], BF16)

    P0 = kb.ps("P0", [128, 1024], F32)
    P1 = kb.ps("P1", [128, 1024], F32)
    P2 = kb.ps("P2", [128, 1024], F32)
    PX = kb.ps("PX", [128, 512], F32)
    P16 = kb.ps("P16", [128, 1024], BF16)
    B0a, B0b, B1a, B1b, B2a, B2b, BX, B16 = [kb.buf("ps%d" % i) for i in range(8)]
    OPB = [[B1a, B1b], [B2a, B2b]]
    BXh = [BX, BX]
    PTB = [[kb.buf("pt%d%d" % (s_, h_)) for h_ in range(2)] for s_ in range(2)]
    OLB = [[kb.buf("ol%d%d" % (c_, h_)) for h_ in range(2)] for c_ in range(2)]
    DNB = [kb.buf("dn0"), kb.buf("dn1")]

    scale_q = float(128 ** -0.5)

    for i in range(nblk):
        n = 128 * (i + 1)
        xf, xfB = XF[0]
        kb.dma(xf[:], x_in[i * 128:(i + 1) * 128, :], [xinB], [xfB])
        import os
        stage = float(os.environ.get("K_STAGE", "99")) if i == 0 else float(os.environ.get("K_STAGE1", os.environ.get("K_STAGE", "99")))
        if stage <= 0:
            kb.dma(x_out[i * 128:(i + 1) * 128, :], xf[:], [xfB], [xoutB])
            continue
        for k in range(8):
            tr(kb, P0[:, k * 128:(k + 1) * 128], xf[:, k * 128:(k + 1) * 128], identF[:], [xfB, cB], [B0a if k < 4 else B0b], inc=(k == 7))
        kb.op("act", lambda e: e.copy(xT[:].rearrange("p k t -> p (k t)"), P0[:]), [B0a, B0b], [xTB])
        if stage <= 0.1:
            kb.op("dve", lambda e: e.memset(rr[:], 0.0), [], [rrB])
            kb.op("dve", lambda e: e.tensor_copy(rr[:, 0:128], xT[:, 0, :]), [xTB], [rrB])
            kb.dma(x_out[i * 128:(i + 1) * 128, :], rr[:], [rrB], [xoutB])
            continue
        for h in range(8):
            for k in range(8):
                mm(kb, P1[:, h * 128:(h + 1) * 128], W0[:, k, h * 128:(h + 1) * 128], xT[:, k, :], [W0B, xTB],
                   [B1a if h < 4 else B1b], start=(k == 0), stop=(k == 7), inc=(k == 7 and h == 7))
        kb.op("dve", lambda e: e.tensor_copy(qT[:].rearrange("p h t -> p (h t)"), P1[:]), [B1a, B1b], [qTB])
        if stage <= 0.2:
            kb.op("dve", lambda e: e.memset(rr[:], 0.0), [], [rrB])
            kb.op("dve", lambda e: e.tensor_copy(rr[:, 0:128], xT[:, 0, :]), [xTB], [rrB])
            kb.dma(x_out[i * 128:(i + 1) * 128, :], rr[:], [rrB], [xoutB])
            continue
        for c in range(4):
            for k in range(8):
                mm(kb, P2[:, c * 128:(c + 1) * 128], W0[:, k, 1280 + c * 128:1280 + (c + 1) * 128], xT[:, k, :],
                   [W0B, xTB], [B2a], start=(k == 0), stop=(k == 7), inc=(k == 7 and c == 3))
        kb.op("act", lambda e: e.copy(qiT[:].rearrange("p c t -> p (c t)"), P2[:, 0:512]), [B2a], [qiTB])
        if stage <= 0.25:
            kb.op("dve", lambda e: e.memset(rr[:], 0.0), [], [rrB])
            kb.op("dve", lambda e: e.tensor_copy(rr[:, 0:128], xT[:, 0, :]), [xTB], [rrB])
            kb.dma(x_out[i * 128:(i + 1) * 128, :], rr[:], [rrB], [xoutB])
            continue
        for k in range(8):
            mm(kb, P2[:, 512:640], Wk2[:, k, :], xT[:, k, :], [Wk2B, xTB], [B2b], start=(k == 0), stop=(k == 7), inc=False)
        for k in range(8):
            mm(kb, P2[:, 640:896], xT[:, k, :], W0[:, k, 1024:1280], [W0B, xTB], [B2b], start=(k == 0), stop=(k == 7), inc=False)
        for k in range(8):
            mm(kb, P2[:, 896:960], xT[:, k, :], W0[:, k, 1856:1920], [W0B, xTB], [B2b], start=(k == 0), stop=(k == 7), inc=(k == 7))
        if stage <= 0.27:
            kb.op("dve", lambda e: e.memset(rr[:], 0.0), [], [rrB])
            kb.op("dve", lambda e: e.tensor_copy(rr[:, 0:128], xT[:, 0, :]), [xTB], [rrB])
            kb.dma(x_out[i * 128:(i + 1) * 128, :], rr[:], [rrB], [xoutB])
            continue
        kb.op("dve", lambda e: e.tensor_copy(kT2[:, i * 128:(i + 1) * 128], P2[:, 512:640]), [B2b], [kT2B])
        if stage <= 0.28:
            kb.op("dve", lambda e: e.memset(rr[:], 0.0), [], [rrB])
            kb.op("dve", lambda e: e.tensor_copy(rr[:, 0:128], xT[:, 0, :]), [xTB], [rrB])
            kb.dma(x_out[i * 128:(i + 1) * 128, :], rr[:], [rrB], [xoutB])
            continue
        kb.op("dve", lambda e: e.tensor_copy(wsb[:], P2[:, 896:904]), [B2b], [wsbB])
        if stage <= 0.29:
            kb.op("dve", lambda e: e.memset(rr[:], 0.0), [], [rrB])
            kb.op("dve", lambda e: e.tensor_copy(rr[:, 0:128], xT[:, 0, :]), [xTB], [rrB])
            kb.dma(x_out[i * 128:(i + 1) * 128, :], rr[:], [rrB], [xoutB])
            continue
        kb.op("dve", lambda e: e.tensor_copy(cf[:], P2[:, 640:896]), [B2b], [cfB])
        if stage <= 0.3:
            kb.op("dve", lambda e: e.memset(rr[:], 0.0), [], [rrB])
            kb.op("dve", lambda e: e.tensor_copy(rr[:, 0:128], xT[:, 0, :]), [xTB], [rrB])
            kb.dma(x_out[i * 128:(i + 1) * 128, :], rr[:], [rrB], [xoutB])
            continue
        for nn in range(2):
            for k in range(8):
                mm(kb, P0[:, nn * 512:(nn + 1) * 512], xT[:, k, :], W0[:, k, 1864 + nn * 512:1864 + (nn + 1) * 512],
                   [W0B, xTB], [B0a if nn == 0 else B0b], start=(k == 0), stop=(k == 7), inc=(k == 7))
        kb.op("act", lambda e: e.activation(sz[:], P0[:], AF.Silu), [B0a, B0b], [szB])
        if stage <= 0.4:
            kb.op("dve", lambda e: e.memset(rr[:], 0.0), [], [rrB])
            kb.op("dve", lambda e: e.tensor_copy(rr[:, 0:128], xT[:, 0, :]), [xTB], [rrB])
            kb.dma(x_out[i * 128:(i + 1) * 128, :], rr[:], [rrB], [xoutB])
            continue
        kb.op("act", lambda e: e.activation(cjunk[:], cf[:], AF.Square, accum_out=ssc[:]), [cfB], [cjunkB, sscB])
        kb.op("dve", lambda e: e.tensor_scalar(ssc[:], ssc[:], 1.0 / 256, EPS, ALU.mult, ALU.add), [sscB], [sscB])
        kb.op("act", lambda e: e.activation(ssc[:], ssc[:], AF.Sqrt), [sscB], [sscB])
        kb.op("dve", lambda e: e.reciprocal(ssc[:], ssc[:]), [sscB], [sscB])
        kb.op("dve", lambda e: e.scalar_tensor_tensor(cN[:, i, :], cf[:], ssc[:], Gkv[:], ALU.mult, ALU.mult),
              [cfB, sscB, GkvB], [cNB])
        for cc in range(2):
            tr(kb, P16[:, cc * 128:(cc + 1) * 128], cN[:, i, cc * 128:(cc + 1) * 128], identB[:], [cNB, cB], [B16], inc=(cc == 1))
        kb.op("dve", lambda e: e.tensor_copy(cT[:, :, i * 128:(i + 1) * 128], P16[:, 0:256].rearrange("p (c t) -> p c t", c=2)),
              [B16], [cTB])
        if stage <= 0.6:
            kb.op("dve", lambda e: e.memset(rr[:], 0.0), [], [rrB])
            kb.op("dve", lambda e: e.tensor_copy(rr[:, 0:128], xT[:, 0, :]), [xTB], [rrB])
            kb.dma(x_out[i * 128:(i + 1) * 128, :], rr[:], [rrB], [xoutB])
            continue
        for cc in range(2):
            Pq = P0 if cc == 0 else P1
            for h in range(8):
                bq = (B0a, B0b, B1a, B1b)[cc * 2 + (h // 4)]
                mm(kb, Pq[:, h * 128:(h + 1) * 128], Wuk[:, h, cc * 128:(cc + 1) * 128], qT[:, h, :], [WukB, qTB], [bq],
                   inc=(h == 7))
        kb.op("act", lambda e: e.activation(qlT[:, 0, :], P0[:], AF.Copy, scale=scale_q), [B0a, B0b], [qlTB])
        kb.op("dve", lambda e: e.tensor_scalar(qlT[:, 1, :], P1[:], scale_q, None, ALU.mult), [B1a, B1b], [qlTB])

        if stage <= 1:
            kb.op("dve", lambda e: e.tensor_copy(rr[:, 0:256], cN[:, i, :]), [cNB], [rrB])
            kb.op("dve", lambda e: e.tensor_copy(rr[:, 256:1024], qlT[:, 0, 0:768]), [qlTB], [rrB])
            kb.dma(x_out[i * 128:(i + 1) * 128, :], rr[:], [rrB], [xoutB])
            continue
        nkc = (n + 511) // 512
        slots = [(PX, BX, slice(0, 512)), (P2, B2a, slice(0, 512)), (P2, B2b, slice(512, 1024))]
        u = 0
        for kc in range(nkc):
            wd = min(512, n - 512 * kc)
            c0 = kc * 512
            for h in range(8):
                Pt, Bt, sl = slots[u % 3]
                R, RB = RR[u % 3]
                u += 1
                pr = slice((h % 2) * 64, (h % 2) * 64 + 64)
                mm(kb, Pt[:, sl.start:sl.start + wd], qiT[pr, h // 2, :], kT2[pr, c0:c0 + wd], [qiTB, kT2B], [Bt])
                kb.op("act", lambda e: e.activation(R[:, 0:wd], Pt[:, sl.start:sl.start + wd], AF.Relu), [Bt], [RB])
                if h == 0:
                    kb.op("dve", lambda e: e.tensor_scalar(SC[:, c0:c0 + wd], R[:, 0:wd], wsb[:, 0:1], None, ALU.mult),
                          [RB, wsbB], [SCB])
                else:
                    kb.op("dve", lambda e: e.scalar_tensor_tensor(SC[:, c0:c0 + wd], R[:, 0:wd], wsb[:, h:h + 1], SC[:, c0:c0 + wd],
                                                                   ALU.mult, ALU.add), [RB, wsbB, SCB], [SCB])
        kb.op("dve", lambda e: e.tensor_tensor(SC[:, i * 128:n], SC[:, i * 128:n], CM[:], ALU.add), [SCB, cB], [SCB])

        MBall = [MB, MBa]
        if i >= 2:
            nd = 128 * max(1, int(round(0.45 * (i + 1))))
            na = n - nd
            kb.op("dve", lambda e: e.memset(mid[:], 0.0), [], [midB])
            for s in range(BIS_STEPS):
                wk = float(-BIS_LO / (2 ** s))
                last = (s == BIS_STEPS - 1)
                kb.op("dve", lambda e: e.tensor_scalar(M[:, 0:nd], SC[:, 0:nd], mid[:], None, ALU.is_ge, ALU.add, accum_out=cnt[:]),
                      [SCB, midB], [MB, cntB])
                kb.op("act", lambda e: e.activation(M[:, nd:n], SC[:, nd:n], AF.Sign, bias=mid[:], scale=-1.0, accum_out=cntA[:]),
                      [SCB, midB], [MBa, cntAB])
                kb.op("dve", lambda e: e.scalar_tensor_tensor(ge[:], cnt[:], 2.0, cntA[:], ALU.mult, ALU.subtract), [cntB, cntAB], [geB])
                kb.op("dve", lambda e: e.tensor_scalar(ge[:], ge[:], float(511 - na), wk, ALU.is_ge, ALU.mult), [geB], [geB])
                if not last:
                    kb.op("dve", lambda e: e.scalar_tensor_tensor(mid[:], ge[:], -0.5 * wk, mid[:], ALU.add, ALU.add), [geB, midB], [midB])
                else:
                    kb.op("dve", lambda e: e.scalar_tensor_tensor(lo[:], ge[:], -wk, mid[:], ALU.add, ALU.add), [geB, midB], [loB])
            kb.op("dve", lambda e: e.tensor_scalar(M[:, 0:n], SC[:, 0:n], lo[:], -30000.0, ALU.is_lt, ALU.mult), [SCB, loB], MBall)
        else:
            kb.op("dve", lambda e: e.tensor_scalar(M[:, 0:n], SC[:, 0:n], -1e29, -30000.0, ALU.is_lt, ALU.mult), [SCB], MBall)
        nbk = i + 1
        kb.op("dve", lambda e: e.tensor_reduce(bh[:, 0:nbk], M[:, 0:n].rearrange("p (j s) -> p j s", s=128), AX.X, ALU.max), MBall, [bhB])
        kb.op("dve", lambda e: e.tensor_tensor(bh[:, 0:nbk], bh[:, 0:nbk], CMISC[:, 0:nbk], ALU.add), [bhB, cB], [bhB])
        kb.op("dve", lambda e: e.reduce_max(jm[:], bh[:, 0:nbk], AX.X), [bhB], [jmB])
        kb.op("dve", lambda e: e.tensor_scalar(jm[:], jm[:], -1.0, float(i + 1), ALU.mult, ALU.add), [jmB], [jmB])
        kb.op("dve", lambda e: e.tensor_scalar(VH[:].rearrange("p h o -> p (h o)"), CMISC[:, 32:40], jm[:], None, ALU.mult), [jmB, cB], [VHB])
        kb.op("dve", lambda e: e.tensor_tensor(VHD[:], CMISC[:, 40:104].rearrange("p (h g) -> p h g", g=8), VH[:].to_broadcast([128, 8, 8]), ALU.mult),
              [VHB, cB], [VHDB])
        for h in range(8):
            tr(kb, P16[0:8, h * 128:(h + 1) * 128], VHD[:, h, :], identB[:], [VHDB, cB], [B16], inc=(h == 7))
        kb.op("act", lambda e: e.copy(SH[:], P16[0:8, :]), [B16], [SHB])
        if stage <= 2:
            kb.op("dve", lambda e: e.tensor_copy(rr[:, 0:128], M[:, 0:128]), [MB], [rrB])
            kb.op("dve", lambda e: e.tensor_copy(rr[:, 128:256], SC[:, 0:128]), [SCB], [rrB])
            kb.dma(x_out[i * 128:(i + 1) * 128, :], rr[:], [rrB], [xoutB])
            continue
        for j0 in range(0, i + 1, 8):
            nj = min(8, i + 1 - j0)
            for jj in range(nj):
                j = j0 + jj
                tr(kb, P16[:, jj * 128:(jj + 1) * 128], M[:, j * 128:(j + 1) * 128], identB[:], [MB, MBa, cB], [B16], inc=(jj == nj - 1))
            kb.op("act", lambda e: e.copy(MT[:, j0:j0 + nj, :].rearrange("p j t -> p (j t)"), P16[:, 0:nj * 128]), [B16], [MTB])

        jlist = list(range(i + 1))
        if os.environ.get("K_JLAST"):
            jlist = [i]
        units = [(j, hb) for j in jlist for hb in range(2)]

        def emit_st(j, hb):
            bS = B0a if hb == 0 else B0b
            for cc in range(2):
                mm(kb, P0[:, hb * 512:(hb + 1) * 512], cT[:, cc, j * 128:(j + 1) * 128], qlT[:, cc, hb * 512:(hb + 1) * 512],
                   [cTB, qlTB], [bS], start=(cc == 0), stop=False, inc=False)
            mm(kb, P0[:, hb * 512:(hb + 1) * 512], ones[0:8, :], SH[:, hb * 512:(hb + 1) * 512], [SHB, cB], [bS],
               start=False, stop=False, inc=False)
            mm(kb, P0[:, hb * 512:(hb + 1) * 512], identB[:], MT[:, j:j + 1, :].to_broadcast([128, 4, 128]), [MTB, cB], [bS],
               start=False, stop=True, inc=True)

        def emit_rest(j, hb):
            d = i - j
            bS = B0a if hb == 0 else B0b
            Pt_ = PT[j % 2][0]
            PtB = PTB[j % 2][hb]
            for h in range(4 * hb, 4 * hb + 4):
                kb.op("act", lambda e: e.activation(Pt_[:, h, :], P0[:, h * 128:(h + 1) * 128], AF.Exp,
                                                     bias=BIAS[:, h * 32 + d:h * 32 + d + 1], scale=1.0), [bS, cB], [PtB])
            for cc in range(2):
                Po = P1 if cc == 0 else P2
                mm(kb, Po[:, hb * 512:(hb + 1) * 512], cN[:, j, cc * 128:(cc + 1) * 128],
                   Pt_[:, 4 * hb:4 * hb + 4, :].rearrange("p h t -> p (h t)"), [cNB, PtB], [OPB[cc][hb]], start=True, stop=True, inc=False)
            for h in range(4 * hb, 4 * hb + 4):
                mm(kb, PX[:, h:h + 1], Pt_[:, h, :], ones[:, 0:1], [PtB, cB], [BXh[hb]], start=(h % 4 == 0), stop=(h % 4 == 3), inc=(h % 4 == 3))
            first = (j == jlist[0])
            hs = slice(hb * 512, (hb + 1) * 512)
            ds = slice(4 * hb, 4 * hb + 4)
            if first:
                kb.op("dve", lambda e: e.tensor_copy(dens[:, ds], PX[:, ds]), [BXh[hb]], [DNB[hb]])
            else:
                kb.op("dve", lambda e: e.tensor_tensor(dens[:, ds], dens[:, ds], PX[:, ds], ALU.add), [BXh[hb], DNB[hb]], [DNB[hb]])
            for cc in range(2):
                Po = P1 if cc == 0 else P2
                if first:
                    kb.op("dve", lambda e: e.tensor_copy(OLs[:, cc, hs], Po[:, hs]), [OPB[cc][hb]], [OLB[cc][hb]])
                else:
                    kb.op("dve", lambda e: e.tensor_tensor(OLs[:, cc, hs], OLs[:, cc, hs], Po[:, hs], ALU.add), [OPB[cc][hb], OLB[cc][hb]], [OLB[cc][hb]])

        emit_st(*units[0])
        for ui, (j, hb) in enumerate(units):
            if ui + 1 < len(units):
                emit_st(*units[ui + 1])
            emit_rest(j, hb)

        kb.op("dve", lambda e: e.reciprocal(rden[:], dens[:]), DNB, [rdenB])
        kb.op("act", lambda e: e.copy(olT[:, 0, :], OLs[:, 0, :]), OLB[0], [olTB])
        kb.op("dve", lambda e: e.tensor_copy(olT[:, 1, :], OLs[:, 1, :]), OLB[1], [olTB])
        for h in range(8):
            for cc in range(2):
                mm(kb, P0[:, h * 128:(h + 1) * 128], olT[:, cc, h * 128:(h + 1) * 128], Wuv[:, h, cc, :], [olTB, WuvB],
                   [B0a if h < 4 else B0b], start=(cc == 0), stop=(cc == 1), inc=(cc == 1 and h == 7))
        for h in range(8):
            kb.op("dve", lambda e: e.scalar_tensor_tensor(og[:, h * 128:(h + 1) * 128], P0[:, h * 128:(h + 1) * 128], rden[:, h:h + 1],
                                                           sz[:, h * 128:(h + 1) * 128], ALU.mult, ALU.mult),
                  [B0a if h < 4 else B0b, rdenB, szB], [ogB])
        for k in range(8):
            tr(kb, P16[:, k * 128:(k + 1) * 128], og[:, k * 128:(k + 1) * 128], identB[:], [ogB, cB], [B16], inc=(k == 7))
        kb.op("act", lambda e: e.copy(ogT[:].rearrange("p k t -> p (k t)"), P16[:]), [B16], [ogTB])
        for nn in range(2):
            for k in range(8):
                mm(kb, P1[:, nn * 512:(nn + 1) * 512], ogT[:, k, :], Wout[:, k, nn * 512:(nn + 1) * 512], [ogTB, WoutB],
                   [B1a if nn == 0 else B1b], start=(k == 0), stop=(k == 7), inc=(k == 7))
        kb.op("dve", lambda e: e.scalar_tensor_tensor(rr[:], xf[:], ALPHA, P1[:], ALU.mult, ALU.add), [xfB, B1a, B1b], [rrB])
        layernorm_store(kb, st, rr[:], rrB, x_out[i * 128:(i + 1) * 128, :], xoutB, LG, LB, [LGB, LBB])

    kb.finish_layer = True
    kb.es = kb_es
    return es


def load_xT(kb, x_in, xinB, i, xf, xfB, xT, xTB, Ptr, Bs, identF, cB):
    kb.dma(xf[:], x_in[i * 128:(i + 1) * 128, :], [xinB], [xfB])
    for k in range(8):
        tr(kb, Ptr[:, k * 128:(k + 1) * 128], xf[:, k * 128:(k + 1) * 128], identF[:], [xfB, cB], Bs, inc=(k == 7))
    kb.op("act", lambda e: e.copy(xT[:].rearrange("p k t -> p (k t)"), Ptr[:, 0:1024]), Bs, [xTB])


def emit_layer1a(nc, kb, dr, x_in, xinB, hn, hnB, cst, nblk=NB):
    es = ExitStack()
    kb_es = kb.es
    kb.es = es
    sb = kb.sb
    identB, identF, ones, cB, TRIf = cst["identB"], cst["identF"], cst["ones"], cst["buf"], cst["TRIf"]
    win = dr["b_w_in"].rearrange("(k p) n -> p k n", p=128)
    Wqk, WqkB = sb("Wqk", [128, 8, 2048], BF16)
    Wv, WvB = sb("Wv", [128, 8, 2048], BF16)
    Wg, WgB = sb("Wg", [128, 8, 8], BF16)
    for k in range(0, 8, 4):
        kb.dma(Wqk[:, k:k + 4, :], win[:, k:k + 4, 0:2048], [], [WqkB], q="pool")
        kb.dma(Wv[:, k:k + 4, :], win[:, k:k + 4, 2048:4096], [], [WvB], q="pool")
    kb.dma(Wg[:], win[:, :, 4096:4104], [], [WgB], q="pool")
    CWr, CWrB = sb("CWr", [80, 128], F32)
    CWT, CWTB = sb("CWT", [128, 80], F32)
    kb.dma(CWr[0:64, :], dr["b_conv_w"].rearrange("j (c p) -> (j c) p", p=128), [], [CWrB])
    kb.dma(CWr[64:80, :], dr["b_conv_b"].rearrange("o (c p) -> (o c) p", p=128), [], [CWrB])
    GBt, GBB = sb("GBt", [128, 8], F32)
    kb.dma(GBt[:, 0:4], dr["b_i_bias"].partition_broadcast(128), [], [GBB])
    kb.dma(GBt[:, 4:8], dr["b_f_bias"].partition_broadcast(128), [], [GBB])
    HG, HGB = sb("HG", [128, 2048], F32)
    kb.dma(HG[:], dr["b_head_norm_g"].rearrange("o h v -> o (h v)").partition_broadcast(128), [], [HGB])

    PA = kb.ps("L1PA", [128, 2048], F32)
    PB = kb.ps("L1PB", [128, 1024], F32)
    PC = kb.ps("L1PC", [128, 512], F32)
    P16 = kb.ps("L1P16", [128, 1024], BF16)
    BA = [kb.buf("pa%d" % i) for i in range(4)]
    BBa, BBb, BC, B16 = kb.buf("pba"), kb.buf("pbb"), kb.buf("pc"), kb.buf("p16")

    tr(kb, PC[:, 0:80], CWr[:], identF[0:80, 0:80], [CWrB, cB], [BC])
    kb.op("dve", lambda e: e.tensor_copy(CWT[:], PC[:, 0:80]), [BC], [CWTB])

    XF2 = [sb("xf1_%d" % s_, [128, 1024], F32) for s_ in range(2)]
    XT2 = [sb("xT1_%d" % s_, [128, 8, 128], BF16) for s_ in range(2)]
    QKP, QKPB = sb("QKP", [128, 16, 131], F32)
    tmpc, tmpcB = sb("tmpc", [128, 128], F32)
    qkT, qkTB = sb("qkT", [128, 16, 128], BF16)
    VT2 = [sb("vt_%d" % s_, [128, 2048], BF16) for s_ in range(2)]
    gt, gtB = sb("gt", [128, 8], F32)
    lf, lfB = sb("lf", [128, 4], F32)
    ibm, ibmB = sb("ibm", [128, 4], F32)
    bcol, bcolB = sb("bcol", [128, 4], F32)
    ONESf, ONESfB = sb("ONESf", [128, 128], F32)
    LFB4 = [sb("LFB4_%d" % h_, [128, 128], F32) for h_ in range(4)]
    EB4 = [sb("EB4_%d" % h_, [128, 128], F32) for h_ in range(4)]
    DT4 = [sb("DT4_%d" % h_, [128, 128], F32) for h_ in range(4)]
    sT4 = [sb("sT4_%d" % h_, [128, 128], BF16) for h_ in range(4)]
    qh4 = [sb("qh4_%d" % h_, [128, 2, 128], BF16) for h_ in range(4)]
    vs4 = [sb("vs4_%d" % h_, [128, 512], BF16) for h_ in range(4)]
    hh4 = [sb("hh4_%d" % h_, [128, 512], F32) for h_ in range(4)]
    kTok4, kTok4B = sb("kTok4", [128, 1024], BF16)
    wcol4, wcol4B = sb("wcol4", [128, 4], BF16)
    rec4, rec4B = sb("rec4", [128, 4], F32)
    ssh4, ssh4B = sb("ssh4", [128, 4], F32)
    LFB, LFBB = sb("LFB", [128, 128], F32)
    EB, EBB = sb("EB", [128, 128], F32)
    DT, DTB = sb("DT", [128, 128], F32)
    sT, sTB = sb("sT", [128, 128], BF16)
    qh, qhB = sb("qh", [128, 2, 128], BF16)
    kTok, kTokB = sb("kTok", [128, 256], BF16)
    vs, vsB = sb("vs", [128, 512], BF16)
    wcol, wcolB = sb("wcol", [128, 1], BF16)
    hh, hhB = sb("hh", [128, 512], F32)
    hjunk, hjunkB = sb("hjunk", [128, 512], BF16)
    HN, HNB = sb("HN", [128, 2048], BF16)
    rec, recB = sb("rec", [128, 1], F32)
    ssh, sshB = sb("ssh", [128, 1], F32)
    Cs = [sb("C%d" % h, [128, 2, 512], F32) for h in range(4)]
    Cb = [sb("Cb%d" % h, [128, 2, 512], BF16) for h in range(4)]
    ns = [sb("n%d" % h, [128, 2], F32) for h in range(4)]
    nb = [sb("nb%d" % h, [128, 2], BF16) for h in range(4)]
    kb.op("dve", lambda e: e.memset(ONESf[:], 1.0), [], [ONESfB])
    kb.op("dve", lambda e: e.memset(QKP[:], 0.0), [], [QKPB])
    for h in range(4):
        kb.op("dve", lambda e: e.memset(Cs[h][0][:], 0.0), [], [Cs[h][1]])
        kb.op("dve", lambda e: e.memset(Cb[h][0][:], 0.0), [], [Cb[h][1]])
        kb.op("dve", lambda e: e.memset(ns[h][0][:], 0.0), [], [ns[h][1]])
        kb.op("dve", lambda e: e.memset(nb[h][0][:], 0.0), [], [nb[h][1]])

    def seg_A(i):
        xf, xfB = XF2[i % 2]; xT, xTB = XT2[i % 2]; vt, vtB = VT2[i % 2]
        load_xT(kb, x_in, xinB, i, xf, xfB, xT, xTB, PB, [BBa, BBb], identF, cB)

    def seg_Bq(i):
        xf, xfB = XF2[i % 2]; xT, xTB = XT2[i % 2]; vt, vtB = VT2[i % 2]
        for c in range(16):
            for k in range(8):
                mm(kb, PA[:, c * 128:(c + 1) * 128], Wqk[:, k, c * 128:(c + 1) * 128], xT[:, k, :], [WqkB, xTB], [BA[c // 4]],
                   start=(k == 0), stop=(k == 7), inc=(k == 7 and c % 4 == 3))

    def seg_Be(i):
        xf, xfB = XF2[i % 2]; xT, xTB = XT2[i % 2]; vt, vtB = VT2[i % 2]
        kb.op("dve", lambda e: e.tensor_copy(QKP[:, 0:8, 3:131], PA[:, 0:1024].rearrange("p (c t) -> p c t", c=8)), [BA[0], BA[1]], [QKPB])
        kb.op("dve", lambda e: e.tensor_copy(QKP[:, 8:16, 3:131], PA[:, 1024:2048].rearrange("p (c t) -> p c t", c=8)), [BA[2], BA[3]], [QKPB])

    def seg_mid(i):
        xf, xfB = XF2[i % 2]; xT, xTB = XT2[i % 2]; vt, vtB = VT2[i % 2]
        for c in range(16):
            kb.op("dve", lambda e: e.tensor_scalar(tmpc[:], QKP[:, c, 0:128], CWT[:, c:c + 1], CWT[:, 64 + c:65 + c], ALU.mult, ALU.add),
                  [QKPB, CWTB], [tmpcB])
            for j in range(1, 4):
                kb.op("dve", lambda e: e.scalar_tensor_tensor(tmpc[:], QKP[:, c, j:j + 128], CWT[:, j * 16 + c:j * 16 + c + 1], tmpc[:],
                                                               ALU.mult, ALU.add), [QKPB, CWTB, tmpcB], [tmpcB])
            kb.op("act", lambda e: e.activation(qkT[:, c, :], tmpc[:], AF.Silu), [tmpcB], [qkTB])
        kb.op("dve", lambda e: e.tensor_copy(tmpc[:, 0:48].rearrange("p (c t) -> p c t", c=16), QKP[:, :, 128:131]), [QKPB], [tmpcB])
        kb.op("dve", lambda e: e.tensor_copy(QKP[:, :, 0:3], tmpc[:, 0:48].rearrange("p (c t) -> p c t", c=16)), [tmpcB], [QKPB])

    def seg_V(i):
        xf, xfB = XF2[i % 2]; xT, xTB = XT2[i % 2]; vt, vtB = VT2[i % 2]
        for nn in range(4):
            for k in range(8):
                mm(kb, PA[:, nn * 512:(nn + 1) * 512], xT[:, k, :], Wv[:, k, nn * 512:(nn + 1) * 512], [WvB, xTB], [BA[nn]],
                   start=(k == 0), stop=(k == 7), inc=(k == 7))
        kb.op("act", lambda e: e.copy(vt[:, 0:1024], PA[:, 0:1024]), [BA[0], BA[1]], [vtB])
        kb.op("dve", lambda e: e.tensor_copy(vt[:, 1024:2048], PA[:, 1024:2048]), [BA[2], BA[3]], [vtB])
        for k in range(8):
            mm(kb, PC[:, 0:8], xT[:, k, :], Wg[:, k, :], [WgB, xTB], [BC], start=(k == 0), stop=(k == 7), inc=(k == 7))
        kb.op("dve", lambda e: e.tensor_tensor(gt[:], PC[:, 0:8], GBt[:], ALU.add), [BC, GBB], [gtB])
        kb.op("act", lambda e: e.activation(lf[:], gt[:, 4:8], AF.Exp, scale=-1.0), [gtB], [lfB])
        kb.op("act", lambda e: e.activation(lf[:], lf[:], AF.Ln, bias=1.0), [lfB], [lfB])
        kb.op("dve", lambda e: e.tensor_scalar(lf[:], lf[:], -1.0, None, ALU.mult), [lfB], [lfB])
        mm(kb, PC[:, 8:12], TRIf[:], lf[:], [lfB, cB], [BC])
        kb.op("dve", lambda e: e.tensor_copy(bcol[:], PC[:, 8:12]), [BC], [bcolB])
        kb.op("dve", lambda e: e.tensor_tensor(ibm[:], gt[:, 0:4], bcol[:], ALU.subtract), [gtB, bcolB], [ibmB])

    def seg_H(i):
        xf, xfB = XF2[i % 2]; xT, xTB = XT2[i % 2]; vt, vtB = VT2[i % 2]
        H4 = range(4)
        for h in H4:
            kb.op("dve", lambda e: e.tensor_scalar(LFB4[h][0][:], ONESf[:], lf[:, h:h + 1], None, ALU.mult), [ONESfB, lfB], [LFB4[h][1]])
        for h in H4:
            mm(kb, PB[:, h * 128:(h + 1) * 128], LFB4[h][0][:], TRIf[:], [LFB4[h][1], cB], [BBa], inc=(h == 3))
        for h in H4:
            kb.op("act", lambda e: e.activation(EB4[h][0][:], PB[:, h * 128:(h + 1) * 128], AF.Exp), [BBa], [EB4[h][1]])
            kb.op("act", lambda e: e.activation(DT4[h][0][:], PB[:, h * 128:(h + 1) * 128], AF.Exp, bias=ibm[:, h:h + 1]), [BBa, ibmB], [DT4[h][1]])
        for h in H4:
            DT, DTB = DT4[h]; EB, EBB = EB4[h]
            kb.op("dve", lambda e: e.tensor_copy(wcol4[:, h:h + 1], DT[:, 127:128]), [DTB], [wcol4B])
            kb.op("dve", lambda e: e.tensor_scalar(vs4[h][0][:], vt[:, h * 512:(h + 1) * 512], DT[:, 127:128], None, ALU.mult), [vtB, DTB], [vs4[h][1]])
            kb.op("dve", lambda e: e.tensor_tensor(DT[:], DT[:], TRIf[:], ALU.mult), [DTB, cB], [DTB])
            for cc in range(2):
                kb.op("dve", lambda e: e.scalar_tensor_tensor(qh4[h][0][:, cc, :], qkT[:, 2 * h + cc, :], 0.0625, EB[:], ALU.mult, ALU.mult),
                      [qkTB, EBB], [qh4[h][1]])
        for h in H4:
            for cc in range(2):
                mm(kb, PB[:, 512 + h * 128:512 + (h + 1) * 128], qkT[:, 8 + 2 * h + cc, :], qkT[:, 2 * h + cc, :], [qkTB], [BBb],
                   start=(cc == 0), stop=(cc == 1), inc=(cc == 1 and h == 3))
        for h in H4:
            kb.op("dve", lambda e: e.scalar_tensor_tensor(sT4[h][0][:], PB[:, 512 + h * 128:512 + (h + 1) * 128], 0.0625, DT4[h][0][:], ALU.mult, ALU.mult),
                  [BBb, DT4[h][1]], [sT4[h][1]])
        for h in H4:
            sT, sTB = sT4[h]; qh, qhB = qh4[h]
            mm(kb, PA[:, h * 512:(h + 1) * 512], sT[:], vt[:, h * 512:(h + 1) * 512], [sTB, vtB], [BA[h]], start=True, stop=False, inc=False)
            for cc in range(2):
                mm(kb, PA[:, h * 512:(h + 1) * 512], qh[:, cc, :], Cb[h][0][:, cc, :], [qhB, Cb[h][1]], [BA[h]], start=False, stop=(cc == 1), inc=(cc == 1))
        for h in H4:
            sT, sTB = sT4[h]; qh, qhB = qh4[h]
            mm(kb, PC[:, 16 + h:17 + h], sT[:], ones[:, 0:1], [sTB, cB], [BC], start=True, stop=False, inc=False)
            for cc in range(2):
                mm(kb, PC[:, 16 + h:17 + h], qh[:, cc, :], nb[h][0][:, cc:cc + 1], [qhB, nb[h][1]], [BC], start=False, stop=(cc == 1), inc=(cc == 1))
        kb.op("dve", lambda e: e.tensor_scalar(rec4[:], PC[:, 16:20], -1.0, None, ALU.mult), [BC], [rec4B])
        kb.op("dve", lambda e: e.tensor_tensor(rec4[:], rec4[:], PC[:, 16:20], ALU.max), [BC, rec4B], [rec4B])
        kb.op("dve", lambda e: e.tensor_scalar(rec4[:], rec4[:], 1.0, None, ALU.max), [rec4B], [rec4B])
        kb.op("dve", lambda e: e.reciprocal(rec4[:], rec4[:]), [rec4B], [rec4B])
        for h in H4:
            kb.op("dve", lambda e: e.tensor_scalar(hh4[h][0][:], PA[:, h * 512:(h + 1) * 512], rec4[:, h:h + 1], None, ALU.mult), [BA[h], rec4B], [hh4[h][1]])
        for h in H4:
            kb.op("act", lambda e: e.activation(hjunk[:], hh4[h][0][:], AF.Square, accum_out=ssh4[:, h:h + 1]), [hh4[h][1]], [hjunkB, ssh4B])
        kb.op("dve", lambda e: e.tensor_scalar(ssh4[:], ssh4[:], 1.0 / 512, EPS, ALU.mult, ALU.add), [ssh4B], [ssh4B])
        kb.op("act", lambda e: e.activation(ssh4[:], ssh4[:], AF.Sqrt), [ssh4B], [ssh4B])
        kb.op("dve", lambda e: e.reciprocal(ssh4[:], ssh4[:]), [ssh4B], [ssh4B])
        for h in H4:
            kb.op("dve", lambda e: e.scalar_tensor_tensor(HN[:, h * 512:(h + 1) * 512], hh4[h][0][:], ssh4[:, h:h + 1], HG[:, h * 512:(h + 1) * 512],
                                                           ALU.mult, ALU.mult), [hh4[h][1], ssh4B, HGB], [HNB])
        kb.dma(hn[i * 128:(i + 1) * 128, :], HN[:], [HNB], [hnB])
        for h in H4:
            for cc in range(2):
                tr(kb, P16[:, h * 256 + cc * 128:h * 256 + (cc + 1) * 128], qkT[:, 8 + 2 * h + cc, :], identB[:], [qkTB, cB], [B16], inc=(cc == 1 and h == 3))
        kb.op("act", lambda e: e.copy(kTok4[:], P16[:]), [B16], [kTok4B])
        u = 0
        for h in H4:
            C, CB_ = Cs[h]
            EB, EBB = EB4[h]
            for cc in range(2):
                bank = u % 2
                u += 1
                bb = BBa if bank == 0 else BBb
                mm(kb, PB[:, bank * 512:(bank + 1) * 512], kTok4[:, h * 256 + cc * 128:h * 256 + (cc + 1) * 128], vs4[h][0][:], [kTok4B, vs4[h][1]], [bb])
                kb.op("dve", lambda e: e.scalar_tensor_tensor(C[:, cc, :], C[:, cc, :], EB[:, 127:128], PB[:, bank * 512:(bank + 1) * 512], ALU.mult, ALU.add),
                      [CB_, EBB, bb], [CB_])
        for h in H4:
            for cc in range(2):
                mm(kb, PC[:, 32 + 2 * h + cc:33 + 2 * h + cc], kTok4[:, h * 256 + cc * 128:h * 256 + (cc + 1) * 128], wcol4[:, h:h + 1], [kTok4B, wcol4B], [BC],
                   inc=(cc == 1 and h == 3))
        for h in H4:
            nh, nhB = ns[h]
            kb.op("dve", lambda e: e.scalar_tensor_tensor(nh[:], nh[:], EB4[h][0][:, 127:128], PC[:, 32 + 2 * h:34 + 2 * h], ALU.mult, ALU.add),
                  [nhB, EB4[h][1], BC], [nhB])
        for h in H4:
            kb.op("act", lambda e: e.copy(Cb[h][0][:].rearrange("p c v -> p (c v)"), Cs[h][0][:].rearrange("p c v -> p (c v)")), [Cs[h][1]], [Cb[h][1]])
            kb.op("dve", lambda e: e.tensor_copy(nb[h][0][:], ns[h][0][:]), [ns[h][1]], [nb[h][1]])

    seg_A(0)
    seg_Bq(0)
    seg_Be(0)
    for i in range(nblk):
        seg_V(i)
        if i + 1 < nblk:
            seg_A(i + 1)
        seg_mid(i)
        if i + 1 < nblk:
            seg_Bq(i + 1)
            seg_Be(i + 1)
        seg_H(i)
    kb.es = kb_es
    return es


def emit_layer1b(nc, kb, dr, x_in, xinB, hn, hnB, x_out, xoutB, cst, nblk=NB):
    es = ExitStack()
    kb_es = kb.es
    kb.es = es
    sb = kb.sb
    identB, identF, cB = cst["identB"], cst["identF"], cst["buf"]
    win = dr["b_w_in"].rearrange("(k p) n -> p k n", p=128)
    Wo, WoB = sb("Wo", [128, 8, 2048], BF16)
    Wz, WzB = sb("Wz", [128, 8, 2048], BF16)
    Wout, WoutB = sb("Wout1", [128, 16, 1024], BF16)
    wout = dr["b_w_out"].rearrange("(k p) n -> p k n", p=128)
    for k in range(0, 8, 4):
        kb.dma(Wo[:, k:k + 4, :], win[:, k:k + 4, 4104:6152], [], [WoB], q="pool")
        kb.dma(Wz[:, k:k + 4, :], win[:, k:k + 4, 6152:8200], [], [WzB], q="pool")
    for k in range(0, 16, 8):
        kb.dma(Wout[:, k:k + 8, :], wout[:, k:k + 8, :], [], [WoutB], q="pool")
    LG, LGB = sb("LG1", [128, 1024], F32)
    LB, LBB = sb("LB1", [128, 1024], F32)
    kb.dma(LG[:], dr["b_ln_g"].partition_broadcast(128), [], [LGB])
    kb.dma(LB[:], dr["b_ln_b"].partition_broadcast(128), [], [LBB])
    PA = kb.ps("L2PA", [128, 2048], F32)
    PB = kb.ps("L2PB", [128, 1024], F32)
    P16 = kb.ps("L2P16", [128, 2048], BF16)
    BA = [kb.buf("qa%d" % i) for i in range(4)]
    BBa, BBb, B16 = kb.buf("qba"), kb.buf("qbb"), kb.buf("q16")
    XF2 = [sb("xf2_%d" % s_, [128, 1024], F32) for s_ in range(2)]
    XT2 = [sb("xT2_%d" % s_, [128, 8, 128], BF16) for s_ in range(2)]
    SO2 = [sb("so_%d" % s_, [128, 1024], BF16) for s_ in range(2)]
    SZ2 = [sb("sz_%d" % s_, [128, 1024], BF16) for s_ in range(2)]
    HNB2 = [sb("hnb_%d" % s_, [128, 2048], BF16) for s_ in range(2)]
    hg, hgB = sb("hg", [128, 2048], BF16)
    hgT, hgTB = sb("hgT", [128, 16, 128], BF16)
    rr, rrB = sb("rr2", [128, 1024], F32)
    st = {"s1": sb("s1b", [128, 1], F32), "ss": sb("ssb", [128, 1], F32), "lnjunk": sb("lnjunkb", [128, 1024], BF16)}
    def front(i):
        xf, xfB = XF2[i % 2]; xT, xTB = XT2[i % 2]; hnb, hnbB = HNB2[i % 2]
        load_xT(kb, x_in, xinB, i, xf, xfB, xT, xTB, PB, [BBa, BBb], identF, cB)
        kb.dma(hnb[:], hn[i * 128:(i + 1) * 128, :], [hnB], [hnbB])

    front(0)
    for i in range(nblk):
        xf, xfB = XF2[i % 2]; xT, xTB = XT2[i % 2]; hnb, hnbB = HNB2[i % 2]
        if i + 1 < nblk:
            front(i + 1)
        for half in range(2):
            cs = slice(half * 1024, (half + 1) * 1024)
            so_, soB_ = SO2[half]
            sz_, szB_ = SZ2[half]
            for nn in range(2):
                col = half * 1024 + nn * 512
                for k in range(8):
                    mm(kb, PA[:, nn * 512:(nn + 1) * 512], xT[:, k, :], Wo[:, k, col:col + 512], [WoB, xTB], [BA[nn]],
                       start=(k == 0), stop=(k == 7), inc=(k == 7))
            kb.op("act", lambda e: e.activation(so_[:], PA[:, 0:1024], AF.Sigmoid), [BA[0], BA[1]], [soB_])
            for nn in range(2):
                col = half * 1024 + nn * 512
                for k in range(8):
                    mm(kb, PA[:, 1024 + nn * 512:1024 + (nn + 1) * 512], xT[:, k, :], Wz[:, k, col:col + 512], [WzB, xTB], [BA[2 + nn]],
                       start=(k == 0), stop=(k == 7), inc=(k == 7))
            kb.op("act", lambda e: e.activation(sz_[:], PA[:, 1024:2048], AF.Silu), [BA[2], BA[3]], [szB_])
            kb.op("dve", lambda e: e.tensor_tensor(hg[:, cs], hnb[:, cs], so_[:], ALU.mult), [hnbB, soB_], [hgB])
            kb.op("dve", lambda e: e.tensor_tensor(hg[:, cs], hg[:, cs], sz_[:], ALU.mult), [hgB, szB_], [hgB])
        for k in range(16):
            tr(kb, P16[:, k * 128:(k + 1) * 128], hg[:, k * 128:(k + 1) * 128], identB[:], [hgB, cB], [B16], inc=(k == 15))
        kb.op("act", lambda e: e.copy(hgT[:].rearrange("p k t -> p (k t)"), P16[:]), [B16], [hgTB])
        for nn in range(2):
            for k in range(16):
                mm(kb, PB[:, nn * 512:(nn + 1) * 512], hgT[:, k, :], Wout[:, k, nn * 512:(nn + 1) * 512], [hgTB, WoutB],
                   [BBa if nn == 0 else BBb], start=(k == 0), stop=(k == 15), inc=(k == 15))
        kb.op("dve", lambda e: e.scalar_tensor_tensor(rr[:], xf[:], ALPHA, PB[:], ALU.mult, ALU.add), [xfB, BBa, BBb], [rrB])
        layernorm_store(kb, st, rr[:], rrB, x_out[i * 128:(i + 1) * 128, :], xoutB, LG, LB, [LGB, LBB])
    kb.es = kb_es
    return es


def make_consts():
    identF = np.eye(128, dtype=np.float32)
    q = np.arange(128)[:, None]
    s = np.arange(128)[None, :]
    CM = np.where(s <= q, 0.0, -1e30).astype(np.float32)
    slopes = 2.0 ** (-(np.arange(1, 9, dtype=np.float64)))
    sp = np.arange(128, dtype=np.float64)[:, None, None]
    dd = np.arange(32, dtype=np.float64)[None, None, :]
    BIAS = (slopes[None, :, None] * (sp - 127.0 - 128.0 * dd)).reshape(128, 256).astype(np.float32)
    misc = np.zeros((128, 104), np.float32)
    misc[:, 0:32] = np.arange(1, 33, dtype=np.float32)[None, :]
    misc[:, 32:40] = (128.0 * slopes)[None, :]
    misc[:, 40:104] = np.eye(8, dtype=np.float32).reshape(1, 64)
    tri = (np.arange(128)[:, None] <= np.arange(128)[None, :]).astype(np.float32)
    return {"c_ident": identF, "c_cm": CM, "c_bias": BIAS, "c_misc": misc, "c_tri": tri}


def build(layers=LAYERS, nblk=NB):
    nc = bass.Bass("TRN2", target_bir_lowering=False)
    dr = {}

    def din(name, shape):
        dr[name] = nc.dram_tensor(name, list(shape), F32, kind="ExternalInput").ap()

    din("x", [T, D])
    din("a_w_in", [D, A_IN]); din("a_kv_norm_g", [1, 256]); din("a_w_uk", [8, 128, 256]); din("a_w_uv", [8, 256, 128])
    din("a_w_out", [D, D]); din("a_ln_g", [1, D]); din("a_ln_b", [1, D])
    din("c_ident", [128, 128]); din("c_cm", [128, 128]); din("c_bias", [128, 256]); din("c_misc", [128, 104]); din("c_tri", [128, 128])
    din("b_w_in", [D, B_IN]); din("b_i_bias", [1, 4]); din("b_f_bias", [1, 4]); din("b_conv_w", [4, 2048]); din("b_conv_b", [1, 2048])
    din("b_head_norm_g", [1, 4, 512]); din("b_w_out", [2048, D]); din("b_ln_g", [1, D]); din("b_ln_b", [1, D])
    out = nc.dram_tensor("out", [T, D], F32, kind="ExternalOutput").ap()
    with ExitStack() as es:
        kb = KB(nc, es)
        identF, idB = kb.sb("identF", [128, 128], F32)
        identB, _ = kb.sb("identB", [128, 128], BF16)
        CM, _ = kb.sb("CM", [128, 128], F32)
        BIAS, _ = kb.sb("BIAS", [128, 256], F32)
        ones, _ = kb.sb("ones", [128, 128], BF16)
        CMISC, _ = kb.sb("CMISC", [128, 104], F32)
        TRIf, _ = kb.sb("TRIf", [128, 128], F32)
        cB = idB
        kb.dma(identF[:], dr["c_ident"], [], [cB])
        kb.dma(identB[:], dr["c_ident"], [], [cB], q="pool")
        kb.dma(CM[:], dr["c_cm"], [], [cB])
        kb.dma(BIAS[:], dr["c_bias"], [], [cB])
        kb.dma(CMISC[:], dr["c_misc"], [], [cB])
        kb.dma(TRIf[:], dr["c_tri"], [], [cB])
        kb.op("dve", lambda e: e.memset(ones[:], 1.0), [], [cB])
        cst = {"identB": identB, "identF": identF, "CM": CM, "BIAS": BIAS, "ones": ones, "buf": cB, "CMISC": CMISC, "TRIf": TRIf}
        xinB = kb.buf("xin")
        outB = kb.buf("out")
        x1d = nc.dram_tensor("x1d", [T, D], F32).ap()
        hnd = nc.dram_tensor("hnd", [T, 2048], BF16).ap()
        x1B = kb.buf("x1d")
        hnB = kb.buf("hnd")
        if layers == (0,):
            l0 = emit_layer0(nc, kb, dr, dr["x"], xinB, out, outB, cst, nblk=nblk)
            kb.finish(); l0.close()
        elif layers == (1,):
            la = emit_layer1a(nc, kb, dr, dr["x"], xinB, hnd, hnB, cst, nblk=nblk)
            kb.barrier(); la.close()
            lb = emit_layer1b(nc, kb, dr, dr["x"], xinB, hnd, hnB, out, outB, cst, nblk=nblk)
            kb.finish(); lb.close()
        else:
            l0 = emit_layer0(nc, kb, dr, dr["x"], xinB, x1d, x1B, cst, nblk=nblk)
            kb.barrier(); l0.close()
            la = emit_layer1a(nc, kb, dr, x1d, x1B, hnd, hnB, cst, nblk=nblk)
            kb.barrier(); la.close()
            lb = emit_layer1b(nc, kb, dr, x1d, x1B, hnd, hnB, out, outB, cst, nblk=nblk)
            kb.finish(); lb.close()
    return nc


_CACHE = {}


def make_inmaps(inputs, xs):
    cst = make_consts()
    shared = {k: np.ascontiguousarray(inputs[k][0], dtype=np.float32) for k in
              ("a_w_in", "a_w_uk", "a_w_uv", "a_w_out", "b_w_in", "b_conv_w", "b_w_out")}
    for k in ("a_kv_norm_g", "a_ln_g", "a_ln_b", "b_i_bias", "b_f_bias", "b_conv_b", "b_ln_g", "b_ln_b"):
        shared[k] = np.ascontiguousarray(inputs[k], dtype=np.float32).reshape(1, -1)
    shared["b_head_norm_g"] = np.ascontiguousarray(inputs["b_head_norm_g"], dtype=np.float32).reshape(1, 4, 512)
    shared.update(cst)
    maps = []
    for xx in xs:
        m = dict(shared)
        m["x"] = np.ascontiguousarray(xx, dtype=np.float32)
        maps.append(m)
    return maps


def kernel(**inputs):
    x = np.ascontiguousarray(inputs["x"], dtype=np.float32)
    if "nc" not in _CACHE:
        import os
        lay = os.environ.get("K_LAYERS")
        _CACHE["nc"] = build(layers=tuple(int(c) for c in lay)) if lay else build()
    nc = _CACHE["nc"]
    in_maps = make_inmaps(inputs, [x[c % 4] for c in range(8)])
    res = run_bass_kernel_spmd(nc, in_maps, core_ids=list(range(8)))
    return np.stack([res.results[c]["out"] for c in range(4)], axis=0)
```
